# Optimizing a Trainium2 kernel written in Bass

```python
import math
import jax
import jax.numpy as jnp
from jax import lax
import numpy as np


D_MODEL = 1024
BATCH = 8
SEQ = 4096
DEPTH = 4

GRID_W = 64
CTX_LEN = 256
CHUNK = 64
EPS = 1e-6
N_MOD = 6
S5_WIDTH = D_MODEL // 4
S5_GROUP = 16
S5_GROUPS = S5_WIDTH // S5_GROUP
S5_STATE = 64
HG_HEADS = 6
HG_DK = 64
HG_DV = 64
HG_QK = HG_HEADS * HG_DK
HG_WIDTH = HG_HEADS * HG_DV
RET_HEADS = 6
RET_DK = 64
RET_DV = 64
RET_QK = RET_HEADS * RET_DK
RET_WIDTH = RET_HEADS * RET_DV
ROPE_BASE = 10000.0
N_BRANCH = 3
IN_SIZES = (S5_WIDTH, HG_QK, HG_QK, HG_QK, HG_WIDTH, HG_WIDTH, RET_QK, RET_QK, RET_WIDTH, RET_WIDTH, N_BRANCH * D_MODEL)
IN_WIDTH = sum(IN_SIZES)
N_GROUPS = 4
EXPERTS_PER_GROUP = 8
N_EXPERTS = N_GROUPS * EXPERTS_PER_GROUP
TOP_K = 2
EXPERT_HIDDEN = D_MODEL // 2
MOE_BLOCK = 128
F32 = jnp.float32

kernel_name = 'hybrid_s5_hgrn2_retention_hmoe_prefix_dit'


def rmsnorm(x, g):
    xf = x.astype(F32)
    y = xf * lax.rsqrt(jnp.mean(xf * xf, axis=-1, keepdims=True) + EPS)
    return (y * g.astype(F32)).astype(x.dtype)


def head_rmsnorm(o):
    return o * lax.rsqrt(jnp.mean(o * o, axis=-1, keepdims=True) + EPS)


def modulate(h, shift, scale):
    return h * (1.0 + scale) + shift


def axial_rope(rows):
    row = jnp.repeat(jnp.arange(rows, dtype=F32), GRID_W)
    col = jnp.broadcast_to(jnp.arange(GRID_W, dtype=F32)[None, :], (rows, GRID_W)).reshape(-1)
    n_freq = RET_DK // 4
    inv_freq = ROPE_BASE ** (-jnp.arange(n_freq, dtype=F32) / n_freq)
    ang = jnp.concatenate([row[:, None] * inv_freq, col[:, None] * inv_freq], axis=-1)
    return jnp.cos(ang), jnp.sin(ang)


def apply_rope(x, cos, sin):
    half = x.shape[-1] // 2
    x1, x2 = x[..., :half], x[..., half:]
    cs, sn = cos[None, :, None, :], sin[None, :, None, :]
    return jnp.concatenate([x1 * cs - x2 * sn, x1 * sn + x2 * cs], axis=-1)


def chunk_gated_recurrence(q, k, v, logf, h0, strict):
    B, L, H, K = q.shape
    V = v.shape[-1]
    n = L // CHUNK

    def blocks(a):
        return a.reshape(B, n, CHUNK, H, a.shape[-1]).transpose(1, 0, 3, 2, 4)

    idx = jnp.arange(CHUNK)
    mask = (idx[:, None] > idx[None, :]) if strict else (idx[:, None] >= idx[None, :])

    def step(h, inp):
        qi, ki, vi, fi = inp
        b = jnp.cumsum(fi, axis=2)
        o = jnp.einsum('bhtk,bhkv->bhtv', qi * jnp.exp(b), h)
        diff = b[:, :, :, None, :] - b[:, :, None, :, :]
        dec = jnp.exp(jnp.where(mask[:, :, None], diff, -jnp.inf))
        scores = jnp.einsum('bhtk,bhsk,bhtsk->bhts', qi, ki, dec)
        o = o + jnp.einsum('bhts,bhsv->bhtv', scores, vi)
        b_end = b[:, :, -1:, :]
        h = jnp.exp(b_end[:, :, 0, :])[..., None] * h + jnp.einsum('bhsk,bhsv->bhkv', ki * jnp.exp(b_end - b), vi)
        return h, o

    h_final, o = lax.scan(step, h0, (blocks(q), blocks(k), blocks(v), blocks(logf)))
    return o.transpose(1, 0, 3, 2, 4).reshape(B, L, H, V), h_final


def bidir_prefix_recurrence(q, k_fwd, k_bwd, v, logf_fwd, logf_bwd, n_ctx, strict_bwd):
    B, T, H, K = q.shape
    h0 = jnp.zeros((B, H, K, v.shape[-1]), F32)

    def run(k, logf, reverse, strict):
        h = h0
        outs = []
        for lo, hi in ((0, n_ctx), (n_ctx, T)):
            seg = [a[:, lo:hi] for a in (q, k, v, logf)]
            if reverse:
                seg = [jnp.flip(a, axis=1) for a in seg]
            o, h = chunk_gated_recurrence(seg[0], seg[1], seg[2], seg[3], h, strict)
            outs.append(jnp.flip(o, axis=1) if reverse else o)
        return jnp.concatenate(outs, axis=1)

    return run(k_fwd, logf_fwd, False, False) + run(k_bwd, logf_bwd, True, strict_bwd)


def s5_scan(a_re, a_im, b_re, b_im, h0_re, h0_im, reverse):
    end = -1 if reverse else 0
    b_re = b_re.at[:, end].add(a_re * h0_re - a_im * h0_im)
    b_im = b_im.at[:, end].add(a_re * h0_im + a_im * h0_re)
    shape = (1,) + b_re.shape[1:]
    A_re = jnp.broadcast_to(a_re, shape)
    A_im = jnp.broadcast_to(a_im, shape)

    def combine(e1, e2):
        a1r, a1i, b1r, b1i = e1
        a2r, a2i, b2r, b2i = e2
        return (a2r * a1r - a2i * a1i, a2r * a1i + a2i * a1r,
                a2r * b1r - a2i * b1i + b2r, a2r * b1i + a2i * b1r + b2i)

    _, _, h_re, h_im = lax.associative_scan(combine, (A_re, A_im, b_re, b_im), reverse=reverse, axis=1)
    return h_re, h_im


def s5_mixer(u, n_ctx, lam_re, lam_im, log_dt, b_re, b_im, c_re, c_im, d_skip, w_glu):
    B, T, _ = u.shape
    uf = u.astype(F32).reshape(B, T, S5_GROUPS, S5_GROUP)
    y = d_skip.astype(F32).reshape(S5_GROUPS, S5_GROUP) * uf
    for d in range(2):
        reverse = d == 1
        lr = lam_re[d].astype(F32)
        li = lam_im[d].astype(F32)
        dt = jnp.exp(log_dt[d].astype(F32))[:, None]
        mag = jnp.exp(lr * dt)
        ar = mag * jnp.cos(li * dt)
        ai = mag * jnp.sin(li * dt)
        den = lr * lr + li * li
        zr = ((ar - 1.0) * lr + ai * li) / den
        zi = (ai * lr - (ar - 1.0) * li) / den
        br = b_re[d].astype(F32)
        bi = b_im[d].astype(F32)
        bbar_re = zr[..., None] * br - zi[..., None] * bi
        bbar_im = zr[..., None] * bi + zi[..., None] * br
        bu_re = jnp.einsum('btgh,gph->btgp', uf, bbar_re)
        bu_im = jnp.einsum('btgh,gph->btgp', uf, bbar_im)
        h_re = jnp.zeros((B, S5_GROUPS, S5_STATE), F32)
        h_im = jnp.zeros((B, S5_GROUPS, S5_STATE), F32)
        seg_re, seg_im = [], []
        for lo, hi in ((0, n_ctx), (n_ctx, T)):
            s_re, s_im = s5_scan(ar, ai, bu_re[:, lo:hi], bu_im[:, lo:hi], h_re, h_im, reverse)
            last = 0 if reverse else -1
            h_re, h_im = s_re[:, last], s_im[:, last]
            seg_re.append(s_re)
            seg_im.append(s_im)
        s_re = jnp.concatenate(seg_re, axis=1)
        s_im = jnp.concatenate(seg_im, axis=1)
        y = y + jnp.einsum('ghp,btgp->btgh', c_re[d].astype(F32), s_re) - jnp.einsum('ghp,btgp->btgh', c_im[d].astype(F32), s_im)
    y = jax.nn.gelu(y.reshape(B, T, S5_WIDTH))
    return y * jax.nn.sigmoid(y @ w_glu.astype(F32))


def hgrn2_mixer(q, zf_fwd, zf_bwd, v, g, lb, n_ctx):
    B, T, _ = q.shape

    def forget(z, lb_d):
        f = lb_d + (1.0 - lb_d) * jax.nn.sigmoid(z.astype(F32))
        f = f.reshape(B, T, HG_HEADS, HG_DK)
        return jnp.log(f), 1.0 - f

    logf_f, k_f = forget(zf_fwd, lb[0])
    logf_b, k_b = forget(zf_bwd, lb[1])
    qh = q.astype(F32).reshape(B, T, HG_HEADS, HG_DK)
    vh = v.astype(F32).reshape(B, T, HG_HEADS, HG_DV)
    o = bidir_prefix_recurrence(qh, k_f, k_b, vh, logf_f, logf_b, n_ctx, False)
    return head_rmsnorm(o).reshape(B, T, HG_WIDTH) * jax.nn.silu(g.astype(F32))


def retention_mixer(q, k, v, g, n_ctx, rope_cos, rope_sin):
    B, T, _ = q.shape
    qh = q.astype(F32).reshape(B, T, RET_HEADS, RET_DK)
    kh = k.astype(F32).reshape(B, T, RET_HEADS, RET_DK) * (RET_DK ** -0.5)
    qh = jnp.concatenate([qh[:, :n_ctx], apply_rope(qh[:, n_ctx:], rope_cos, rope_sin)], axis=1)
    kh = jnp.concatenate([kh[:, :n_ctx], apply_rope(kh[:, n_ctx:], rope_cos, rope_sin)], axis=1)
    vh = v.astype(F32).reshape(B, T, RET_HEADS, RET_DV)
    log_gamma = jnp.log(1.0 - 2.0 ** (-5.0 - jnp.arange(RET_HEADS, dtype=F32)))
    logf = jnp.broadcast_to(log_gamma[:, None], qh.shape)
    o = bidir_prefix_recurrence(qh, kh, kh, vh, logf, logf, n_ctx, True)
    return head_rmsnorm(o).reshape(B, T, RET_WIDTH) * jax.nn.silu(g.astype(F32))


def token_mixer(h, n_ctx, rope_cos, rope_sin, lb, w_in, lam_re, lam_im, log_dt, b_re, b_im, c_re, c_im,
                d_skip, w_glu, w_br_s5, w_br_hg, w_br_ret, w_out):
    B, T, D = h.shape
    proj = h @ w_in
    parts = jnp.split(proj, np.cumsum(IN_SIZES)[:-1].tolist(), axis=-1)
    u, hq, hf_f, hf_b, hv, hg, rq, rk, rv, rg, gz = parts
    y_s5 = s5_mixer(u, n_ctx, lam_re, lam_im, log_dt, b_re, b_im, c_re, c_im, d_skip, w_glu)
    y_hg = hgrn2_mixer(hq, hf_f, hf_b, hv, hg, lb, n_ctx)
    y_ret = retention_mixer(rq, rk, rv, rg, n_ctx, rope_cos, rope_sin)
    gates = jax.nn.sigmoid(gz.astype(F32)).reshape(B, T, N_BRANCH, D)
    merged = (gates[:, :, 0] * (y_s5 @ w_br_s5.astype(F32))
              + gates[:, :, 1] * (y_hg @ w_br_hg.astype(F32))
              + gates[:, :, 2] * (y_ret @ w_br_ret.astype(F32)))
    return (merged @ w_out.astype(F32)).astype(h.dtype)


def hier_moe(t, w_grp, b_grp, w_exp, b_exp, w_gate, w_up, w_down):
    N, D = t.shape
    tf = t.astype(F32)
    grp_prob = jax.nn.softmax(tf @ w_grp.astype(F32) + b_grp.astype(F32), axis=-1)
    gp, gi = lax.top_k(grp_prob, 1)
    exp_logits = (tf @ w_exp.astype(F32) + b_exp.astype(F32)).reshape(N, N_GROUPS, EXPERTS_PER_GROUP)
    exp_logits = exp_logits[jnp.arange(N), gi[:, 0]]
    ep, ei = lax.top_k(jax.nn.softmax(exp_logits, axis=-1), TOP_K)
    wts = gp * ep / jnp.sum(ep, axis=-1, keepdims=True)
    eid = gi * EXPERTS_PER_GROUP + ei
    A = N * TOP_K
    flat_e = eid.reshape(A)
    flat_w = wts.reshape(A)
    flat_tok = jnp.arange(A, dtype=jnp.int32) // TOP_K
    order = jnp.argsort(flat_e)
    se = flat_e[order]
    counts = jnp.zeros((N_EXPERTS,), jnp.int32).at[flat_e].add(1)
    padded = (counts + MOE_BLOCK - 1) // MOE_BLOCK * MOE_BLOCK
    pad_end = jnp.cumsum(padded)
    pad_start = pad_end - padded
    start = jnp.cumsum(counts) - counts
    dest = pad_start[se] + jnp.arange(A, dtype=jnp.int32) - start[se]
    n_blocks = -(-A // MOE_BLOCK) + N_EXPERTS
    slot_tok = jnp.full((n_blocks * MOE_BLOCK,), N, jnp.int32).at[dest].set(flat_tok[order])
    slot_w = jnp.zeros((n_blocks * MOE_BLOCK,), F32).at[dest].set(flat_w[order])
    blk_e = jnp.minimum(jnp.searchsorted(pad_end, jnp.arange(n_blocks) * MOE_BLOCK, side='right'), N_EXPERTS - 1)
    t_pad = jnp.concatenate([t, jnp.zeros((1, D), t.dtype)], axis=0)

    def block_fn(args):
        e, tok = args
        xb = t_pad[tok]
        return (jax.nn.silu(xb @ w_gate[e]) * (xb @ w_up[e])) @ w_down[e]

    yb = lax.map(block_fn, (blk_e, slot_tok.reshape(n_blocks, MOE_BLOCK)))
    y = jnp.zeros((N + 1, D), F32).at[slot_tok].add(yb.reshape(-1, D).astype(F32) * slot_w[:, None])
    return y[:N].astype(t.dtype)


def setup_inputs(seed: int = 0) -> dict:
    key = jax.random.key(seed)
    ks = iter(jax.random.split(key, 40))

    def nrm(shape, scale):
        return scale * jax.random.normal(next(ks), shape, F32)

    G, P, Hg = S5_GROUPS, S5_STATE, S5_GROUP
    E, F = N_EXPERTS, EXPERT_HIDDEN
    inputs = {}
    inputs['x'] = nrm((BATCH, SEQ, D_MODEL), 1.0)
    inputs['c'] = nrm((BATCH, D_MODEL), 1.0)
    inputs['ctx'] = nrm((BATCH, CTX_LEN, D_MODEL), 1.0)
    inputs['c_ctx'] = nrm((D_MODEL,), 1.0)
    inputs['w_ada'] = nrm((DEPTH, D_MODEL, N_MOD * D_MODEL), 0.5 * D_MODEL ** -0.5)
    inputs['b_ada'] = nrm((DEPTH, N_MOD * D_MODEL), 0.01)
    inputs['norm1_g'] = 1.0 + nrm((DEPTH, D_MODEL), 0.02)
    inputs['norm2_g'] = 1.0 + nrm((DEPTH, D_MODEL), 0.02)
    inputs['w_in'] = nrm((DEPTH, D_MODEL, IN_WIDTH), D_MODEL ** -0.5)
    inputs['s5_lam_re'] = -0.5 + nrm((DEPTH, 2, G, P), 0.01)
    inputs['s5_lam_im'] = jnp.pi * jnp.arange(P, dtype=F32) + nrm((DEPTH, 2, G, P), 0.01)
    inputs['s5_log_dt'] = jax.random.uniform(next(ks), (DEPTH, 2, G), F32, math.log(1e-3), math.log(1e-1))
    inputs['s5_b_re'] = nrm((DEPTH, 2, G, P, Hg), (2.0 * Hg) ** -0.5)
    inputs['s5_b_im'] = nrm((DEPTH, 2, G, P, Hg), (2.0 * Hg) ** -0.5)
    inputs['s5_c_re'] = nrm((DEPTH, 2, G, Hg, P), P ** -0.5)
    inputs['s5_c_im'] = nrm((DEPTH, 2, G, Hg, P), P ** -0.5)
    inputs['s5_d'] = nrm((DEPTH, S5_WIDTH), 1.0)
    inputs['s5_w_glu'] = nrm((DEPTH, S5_WIDTH, S5_WIDTH), S5_WIDTH ** -0.5)
    inputs['hgrn_lb_raw'] = nrm((DEPTH, 2, HG_QK), 0.5)
    inputs['w_branch_s5'] = nrm((DEPTH, S5_WIDTH, D_MODEL), S5_WIDTH ** -0.5)
    inputs['w_branch_hgrn'] = nrm((DEPTH, HG_WIDTH, D_MODEL), HG_WIDTH ** -0.5)
    inputs['w_branch_ret'] = nrm((DEPTH, RET_WIDTH, D_MODEL), RET_WIDTH ** -0.5)
    inputs['w_out'] = nrm((DEPTH, D_MODEL, D_MODEL), D_MODEL ** -0.5)
    inputs['moe_w_group'] = nrm((DEPTH, D_MODEL, N_GROUPS), D_MODEL ** -0.5)
    inputs['moe_b_group'] = nrm((DEPTH, N_GROUPS), 0.01)
    inputs['moe_w_expert'] = nrm((DEPTH, D_MODEL, E), D_MODEL ** -0.5)
    inputs['moe_b_expert'] = nrm((DEPTH, E), 0.01)
    inputs['moe_w_gate'] = nrm((DEPTH, E, D_MODEL, F), D_MODEL ** -0.5)
    inputs['moe_w_up'] = nrm((DEPTH, E, D_MODEL, F), D_MODEL ** -0.5)
    inputs['moe_w_down'] = nrm((DEPTH, E, F, D_MODEL), F ** -0.5)
    inputs['final_norm_g'] = 1.0 + nrm((D_MODEL,), 0.02)
    return inputs


def reference(x, c, ctx, c_ctx, w_ada, b_ada, norm1_g, norm2_g, w_in, s5_lam_re, s5_lam_im, s5_log_dt,
              s5_b_re, s5_b_im, s5_c_re, s5_c_im, s5_d, s5_w_glu, hgrn_lb_raw, w_branch_s5, w_branch_hgrn,
              w_branch_ret, w_out, moe_w_group, moe_b_group, moe_w_expert, moe_b_expert, moe_w_gate,
              moe_w_up, moe_w_down, final_norm_g):
    B, L, D = x.shape
    n_ctx = ctx.shape[1]
    rows = L // GRID_W
    rope_cos, rope_sin = axial_rope(rows)
    p = jax.nn.softmax(hgrn_lb_raw.astype(F32), axis=0)
    lower_bounds = jnp.cumsum(p, axis=0) - p[0:1]
    xl, xc = x, ctx
    for l in range(DEPTH):
        last = l == DEPTH - 1
        mod_l = (jax.nn.silu(c) @ w_ada[l] + b_ada[l]).reshape(B, N_MOD, 1, D)
        mod_c = (jax.nn.silu(c_ctx) @ w_ada[l] + b_ada[l]).reshape(N_MOD, D)
        h = jnp.concatenate([modulate(rmsnorm(xc, norm1_g[l]), mod_c[0], mod_c[1]),
                             modulate(rmsnorm(xl, norm1_g[l]), mod_l[:, 0], mod_l[:, 1])], axis=1)
        mix = token_mixer(h, n_ctx, rope_cos, rope_sin, lower_bounds[l], w_in[l], s5_lam_re[l], s5_lam_im[l],
                          s5_log_dt[l], s5_b_re[l], s5_b_im[l], s5_c_re[l], s5_c_im[l], s5_d[l], s5_w_glu[l],
                          w_branch_s5[l], w_branch_hgrn[l], w_branch_ret[l], w_out[l])
        xl = xl + mod_l[:, 2] * mix[:, n_ctx:]
        h2_l = modulate(rmsnorm(xl, norm2_g[l]), mod_l[:, 3], mod_l[:, 4]).reshape(B * L, D)
        if last:
            y = hier_moe(h2_l, moe_w_group[l], moe_b_group[l], moe_w_expert[l], moe_b_expert[l],
                         moe_w_gate[l], moe_w_up[l], moe_w_down[l])
            xl = xl + mod_l[:, 5] * y.reshape(B, L, D)
        else:
            xc = xc + mod_c[2] * mix[:, :n_ctx]
            h2_c = modulate(rmsnorm(xc, norm2_g[l]), mod_c[3], mod_c[4]).reshape(B * n_ctx, D)
            y = hier_moe(jnp.concatenate([h2_c, h2_l], axis=0), moe_w_group[l], moe_b_group[l],
                         moe_w_expert[l], moe_b_expert[l], moe_w_gate[l], moe_w_up[l], moe_w_down[l])
            xc = xc + mod_c[5] * y[:B * n_ctx].reshape(B, n_ctx, D)
            xl = xl + mod_l[:, 5] * y[B * n_ctx:].reshape(B, L, D)
    return rmsnorm(xl, final_norm_g)
```

```python
import math
import numpy as np
from contextlib import ExitStack
import concourse.bass as bass
import concourse.mybir as mybir
from concourse.bass_utils import run_bass_kernel_spmd

F32 = mybir.dt.float32
BF16 = mybir.dt.bfloat16
I32 = mybir.dt.int32
AF = mybir.ActivationFunctionType
ALU = mybir.AluOpType
AX = mybir.AxisListType

COMPUTE = ("tensor", "vector", "scalar", "gpsimd")
ALLENG = ("tensor", "vector", "scalar", "gpsimd", "sync")
NDMASEM = 6

D = 1024
NCTX = 256
IN_SIZES = (256, 384, 384, 384, 384, 384, 384, 384, 384, 384, 3072)
IN_OFF = [0]
for _s in IN_SIZES:
    IN_OFF.append(IN_OFF[-1] + _s)
(O_U, O_HQ, O_HFF, O_HFB, O_HV, O_HG, O_RQ, O_RK, O_RV, O_RG, O_GZ) = IN_OFF[:11]
IN_WIDTH = IN_OFF[-1]
NE = 32
EH = 512
EPS = 1e-6


class Prog:
    def __init__(self, nc, stack):
        self.nc = nc
        self.stack = stack
        self.ops = {e: [] for e in ALLENG}
        self.sem = {}
        self.cnt = {}
        for e in COMPUTE:
            self.sem[e] = stack.enter_context(nc.semaphore("s_" + e))
            self.cnt[e] = 0
        self.dsem = {}
        self.dcnt = {}
        self.dnext = {}
        for q in ("sync", "gpsimd"):
            self.dsem[q] = [stack.enter_context(nc.semaphore("d_%s%d" % (q, i))) for i in range(NDMASEM)]
            self.dcnt[q] = [0] * NDMASEM
            self.dnext[q] = 0
        self.semid = {}
        self.last_write = {}
        self.readers = {}
        self.seen = {e: {} for e in ALLENG}
        self.nalloc = 0
        self.out_tokens = []

    def sb(self, stack, shape, dtype=F32, name=None):
        self.nalloc += 1
        name = (name or "t") + "_%d" % self.nalloc
        return stack.enter_context(self.nc.sbuf_tensor(name, list(shape), dtype))

    def ps(self, stack, shape, dtype=F32, name=None):
        self.nalloc += 1
        name = (name or "p") + "_%d" % self.nalloc
        return stack.enter_context(self.nc.psum_tensor(name, list(shape), dtype))

    def _deps(self, eng, reads, writes):
        need = {}

        def add(tok):
            s, v = tok
            k = id(s)
            self.semid[k] = s
            if need.get(k, 0) < v:
                need[k] = v

        for k in reads:
            if k in self.last_write:
                add(self.last_write[k])
        for k in writes:
            if k in self.last_write:
                add(self.last_write[k])
            for t in self.readers.get(k, ()):
                add(t)
        waits = []
        for k, v in need.items():
            s = self.semid[k]
            if eng == "tensor" and s is self.sem["tensor"]:
                continue
            if self.seen[eng].get(k, 0) >= v:
                continue
            self.seen[eng][k] = v
            waits.append((s, v))
        return waits

    def _commit(self, tok, reads, writes):
        for k in reads:
            self.readers.setdefault(k, []).append(tok)
        for k in writes:
            self.last_write[k] = tok
            self.readers[k] = []

    def op(self, eng, fn, reads=(), writes=()):
        waits = self._deps(eng, reads, writes)
        self.cnt[eng] += 1
        tok = (self.sem[eng], self.cnt[eng])
        self.ops[eng].append((fn, waits, self.sem[eng], 1))
        self._commit(tok, reads, writes)
        return tok

    def dma(self, q, out, in_, reads=(), writes=(), is_output=False):
        i = self.dnext[q]
        self.dnext[q] = (i + 1) % NDMASEM
        s = self.dsem[q][i]
        waits = self._deps(q, reads, writes)
        prev = self.dcnt[q][i]
        k = id(s)
        self.semid[k] = s
        if prev > 0 and self.seen[q].get(k, 0) < prev:
            self.seen[q][k] = prev
            waits.append((s, prev))
        self.dcnt[q][i] = prev + 16
        tok = (s, prev + 16)

        def fn(e, out=out, in_=in_):
            return e.dma_start(out=out, in_=in_)

        self.ops[q].append((fn, waits, s, 16))
        self._commit(tok, reads, writes)
        if is_output:
            self.out_tokens.append(tok)
        return tok

    def barrier(self):
        toks = []
        for e in COMPUTE:
            if self.cnt[e] > 0:
                toks.append((self.sem[e], self.cnt[e]))
        for q in self.dsem:
            for i, s in enumerate(self.dsem[q]):
                if self.dcnt[q][i] > 0:
                    toks.append((s, self.dcnt[q][i]))
        for eng in ALLENG:
            waits = []
            for s, v in toks:
                k = id(s)
                self.semid[k] = s
                if eng in COMPUTE and s is self.sem[eng]:
                    if eng == "tensor":
                        continue
                if self.seen[eng].get(k, 0) >= v:
                    continue
                self.seen[eng][k] = v
                waits.append((s, v))
            if waits:
                self.ops[eng].append((None, waits, None, 0))
        self.last_write = {}
        self.readers = {}

    def emit(self):
        nc = self.nc
        fin = list(self.out_tokens)
        with nc.Block() as block:
            def mk(eng):
                def body(e):
                    for fn, waits, s, inc in self.ops[eng]:
                        for ws, wv in waits:
                            e.wait_ge(ws, wv)
                        if fn is not None:
                            ins = fn(e)
                            ins.then_inc(s, inc)
                    if eng == "sync":
                        for ws, wv in fin:
                            e.wait_ge(ws, wv)
                return body
            block.sync(mk("sync"))
            block.tensor(mk("tensor"))
            block.vector(mk("vector"))
            block.scalar(mk("scalar"))
            block.gpsimd(mk("gpsimd"))

    def mm(self, out, lhsT, rhs, start, stop, reads, writes):
        return self.op("tensor", lambda e: e.matmul(out, lhsT, rhs, start=start, stop=stop), reads, writes)

    def act(self, out, in_, func, reads, writes, bias=None, scale=None):
        kw = {}
        if bias is not None:
            kw["bias"] = bias
        if scale is not None:
            kw["scale"] = scale
        return self.op("scalar", lambda e: e.activation(out, in_, func, **kw), reads, writes)

    def tt(self, out, in0, in1, op, reads, writes, eng="vector"):
        return self.op(eng, lambda e: e.tensor_tensor(out, in0, in1, op), reads, writes)

    def ts(self, out, in0, s1, s2, op0, op1, reads, writes, eng="vector"):
        if s2 is None:
            return self.op(eng, lambda e: e.tensor_scalar(out, in0, s1, None, op0), reads, writes)
        return self.op(eng, lambda e: e.tensor_scalar(out, in0, s1, s2, op0, op1), reads, writes)

    def stt(self, out, in0, scalar, in1, op0, op1, reads, writes):
        return self.op("vector", lambda e: e.scalar_tensor_tensor(out, in0, scalar, in1, op0, op1), reads, writes)

    def copy(self, out, in_, reads, writes, eng="vector"):
        if eng == "scalar":
            return self.op("scalar", lambda e: e.copy(out, in_), reads, writes)
        return self.op(eng, lambda e: e.tensor_copy(out, in_), reads, writes)

    def memset(self, ap, val, writes, eng="vector"):
        return self.op(eng, lambda e: e.memset(ap, val), (), writes)

    def recip(self, out, in_, reads, writes):
        return self.op("vector", lambda e: e.reciprocal(out, in_), reads, writes)


def rev_ap(a):
    ap = [list(d) for d in a.ap]
    n = ap[-1][1]
    st = ap[-1][0]
    ap[-1] = [-st, n]
    return bass.AP(a.tensor, a.offset + st * (n - 1), ap)


C_IDENT = 0
C_RESET = 128
C_IOTA = 640
C_MR = 896
C_MC = 897
C_FIDX = 898
C_SIGN = 899
C_PIDX = 900
C_RESET32 = 904
C_W = 904 + 512

M_FWD = 0
M_BWD = 128
M_BWDS = 256
M32_FWD = 384
M32_BWD = 448
M_W = 512


def host_consts(L):
    c = np.zeros((128, C_W), np.float32)
    c[:, C_IDENT:C_IDENT + 128] = np.eye(128, dtype=np.float32)
    t = np.arange(512)
    c[:, C_RESET:C_RESET + 512] = (t % 64 != 0).astype(np.float32)[None, :]
    c[:, C_IOTA:C_IOTA + 256] = np.arange(256, dtype=np.float32)[None, :]
    c[:, C_RESET32:C_RESET32 + 512] = (t % 32 != 0).astype(np.float32)[None, :]
    p = np.arange(128)
    j = p % 32
    c[:, C_MR] = (j < 16)
    c[:, C_MC] = (j >= 16)
    c[:, C_FIDX] = p % 16
    c[:, C_SIGN] = np.where((p % 64) < 32, -1.0, 1.0)
    c[:, C_PIDX] = p
    m = np.zeros((128, M_W), np.int32)
    s = (p % 64)[:, None]
    tt = np.arange(64)[None, :]
    m[:, M_FWD:M_FWD + 128] = np.tile((s <= tt).astype(np.int32), (1, 2))
    m[:, M_BWD:M_BWD + 128] = np.tile((s >= tt).astype(np.int32), (1, 2))
    m[:, M_BWDS:M_BWDS + 128] = np.tile((s > tt).astype(np.int32), (1, 2))
    s32 = (p % 32)[:, None]
    t32 = np.arange(32)[None, :]
    m[:, M32_FWD:M32_FWD + 64] = np.tile((s32 <= t32).astype(np.int32), (1, 2))
    m[:, M32_BWD:M32_BWD + 64] = np.tile((s32 >= t32).astype(np.int32), (1, 2))
    tl = np.arange(L)
    pos = np.stack([tl // 64, tl % 64]).astype(np.float32)
    return c, m, pos


PARAM_NAMES = ["c", "c_ctx", "w_ada", "b_ada", "norm1_g", "norm2_g", "w_in", "s5_lam_re", "s5_lam_im",
               "s5_log_dt", "s5_b_re", "s5_b_im", "s5_c_re", "s5_c_im", "s5_d", "s5_w_glu", "hgrn_lb_raw",
               "w_branch_s5", "w_branch_hgrn", "w_branch_ret", "w_out", "moe_w_group", "moe_b_group",
               "moe_w_expert", "moe_b_expert", "moe_w_gate", "moe_w_up", "moe_w_down", "final_norm_g"]


class Builder:
    def __init__(self, L, depth, shapes, debug=()):
        self.L = L
        self.T = NCTX + L
        self.depth = depth
        self.debug = set(debug)
        nc = bass.Bass("TRN2", target_bir_lowering=False)
        self.nc = nc
        T = self.T
        self.din = {}
        self.din["x"] = nc.dram_tensor("x", [L, D], F32, kind="ExternalInput").ap()
        self.din["ctx"] = nc.dram_tensor("ctx", [NCTX, D], F32, kind="ExternalInput").ap()
        for n in PARAM_NAMES:
            self.din[n] = nc.dram_tensor(n, list(shapes[n]), F32, kind="ExternalInput").ap()
        self.din["cst"] = nc.dram_tensor("cst", [128, C_W], F32, kind="ExternalInput").ap()
        self.din["cmask"] = nc.dram_tensor("cmask", [128, M_W], I32, kind="ExternalInput").ap()
        self.din["pos"] = nc.dram_tensor("pos", [2, L], F32, kind="ExternalInput").ap()
        self.out = nc.dram_tensor("out", [L, D], F32, kind="ExternalOutput").ap()
        self.scr = {}
        self.blocks = [(0, NCTX)] + [(NCTX + 512 * i, 512) for i in range(L // 512)]
        assert L % 512 == 0

    def dump(self, name, ap, shape, dtype, reads):
        if "dumps" not in self.debug:
            return
        t = self.nc.dram_tensor("dbg_" + name, list(shape), dtype, kind="ExternalOutput").ap()
        self.P.dma("sync", t, ap, reads=reads, writes=["dbg_" + name])

    def scratch(self, name, shape, dtype):
        kind = "ExternalOutput" if name in self.debug else "Internal"
        t = self.nc.dram_tensor("scr_" + name, list(shape), dtype, kind=kind).ap()
        self.scr[name] = t
        return t

    def build(self):
        nc = self.nc
        T, L = self.T, self.L
        with ExitStack() as top, nc.allow_non_contiguous_dma(reason="small strided parameter loads"), \
                nc.allow_low_precision(reason="bf16 matmul operands"):
            P = Prog(nc, top)
            self.P = P
            self.cst = P.sb(top, [128, C_W], F32, "cst")
            self.cmask = P.sb(top, [128, M_W], I32, "cmask")
            P.dma("sync", self.cst[:], self.din["cst"], writes=["cst"])
            P.dma("sync", self.cmask[:], self.din["cmask"], writes=["cmask"])
            self.identb = P.sb(top, [128, 128], BF16, "identb")
            P.copy(self.identb[:], self.cst[:, C_IDENT:C_IDENT + 128], ["cst"], ["identb"])
            self.onesb = P.sb(top, [128, 128], BF16, "onesb")
            P.memset(self.onesb[:], 1.0, ["onesb"])
            self.onesf = P.sb(top, [128, 128], F32, "onesf")
            P.memset(self.onesf[:], 1.0, ["onesf"])
            self.epsc = P.sb(top, [128, 1], F32, "epsc")
            P.memset(self.epsc[:], EPS, ["epsc"])
            self.mod = P.sb(top, [128, 6, 8, 2], F32, "mod")
            self.g1s = P.sb(top, [128, 8, 2], F32, "g1s")
            self.g2s = P.sb(top, [128, 8, 2], F32, "g2s")
            self.XT = self.scratch("XT", [D, T], F32)
            self.PJ = self.scratch("PJ", [O_GZ, T], BF16)
            self.LF = self.scratch("LF", [2, 384, T], F32)
            self.VT = self.scratch("VT", [2, T, 384], BF16)
            self.OA = self.scratch("OA", [2, 384, T], F32)
            self.YB = self.scratch("YB", [D, T], BF16)
            self.GT = self.scratch("GT", [T, NE], F32)
            self.phase_input()
            P.barrier()
            self.phase_rope()
            P.barrier()
            for l in range(self.depth):
                self.l = l
                self.phase_mod(l)
                P.barrier()
                self.phase_proj(l)
                P.barrier()
                if "stop_proj" in self.debug:
                    break
                self.phase_s5(l)
                P.barrier()
                if "stop_s5" in self.debug:
                    break
                if "skip_hg" not in self.debug:
                    self.phase_gla(l, 0)
                    P.barrier()
                if "skip_ret" not in self.debug:
                    self.phase_gla(l, 1)
                    P.barrier()
                if "stop_mix" in self.debug:
                    break
                self.phase_merge(l)
                P.barrier()
                if "stop_merge" in self.debug:
                    break
                self.phase_moe(l)
                P.barrier()
            self.phase_output()
            P.emit()
        return nc

    def phase_input(self):
        P, nc = self.P, self.nc
        with ExitStack() as st:
            ident = self.cst[:, C_IDENT:C_IDENT + 128]
            xin = [P.sb(st, [128, D], F32, "xin") for _ in range(2)]
            xo = [P.sb(st, [128, 8, 128], F32, "xo") for _ in range(2)]
            pt = [P.ps(st, [128, 4, 128], F32, "pt") for _ in range(2)]
            ntile = self.T // 128
            for i in range(ntile):
                b = i % 2
                if i < NCTX // 128:
                    src = self.din["ctx"][i * 128:(i + 1) * 128, :]
                else:
                    j = i - NCTX // 128
                    src = self.din["x"][j * 128:(j + 1) * 128, :]
                P.dma("sync", xin[b][:], src, writes=[("xin", b)])
                for hf in range(2):
                    for q in range(4):
                        ft = hf * 4 + q
                        P.mm(pt[hf][:, q, :], xin[b][:, ft * 128:(ft + 1) * 128], ident, True, True,
                             [("xin", b), "cst"], [("pt", hf, q)])
                    P.copy(xo[b][:, hf * 4:(hf + 1) * 4, :], pt[hf][:], [("pt", hf, q) for q in range(4)],
                           [("xo", b, hf)], eng=("vector" if hf == 0 else "scalar"))
                dst = self.XT.rearrange("(ft p) t -> p ft t", p=128)[:, :, i * 128:(i + 1) * 128]
                P.dma("sync", dst, xo[b][:], reads=[("xo", b, 0), ("xo", b, 1)], writes=[("XT", i)])

    def phase_rope(self):
        P = self.P
        L = self.L
        self.ROPE = self.scratch("ROPE", [2, 128, L], F32)
        with ExitStack() as st:
            ropc = P.sb(st, [128, L], F32, "ropc")
            rops = P.sb(st, [128, L], F32, "rops")
            posr = P.sb(st, [128, L], F32, "posr")
            posc = P.sb(st, [128, L], F32, "posc")
            P.dma("sync", posr[:], self.din["pos"][0:1, :].partition_broadcast(128), writes=["posr"])
            P.dma("sync", posc[:], self.din["pos"][1:2, :].partition_broadcast(128), writes=["posc"])
            invf = P.sb(st, [128, 1], F32, "invf")
            P.act(invf[:], self.cst[:, C_FIDX:C_FIDX + 1], AF.Exp, ["cst"], ["invf"], scale=-math.log(10000.0) / 16.0)
            P.ts(posr[:], posr[:], self.cst[:, C_MR:C_MR + 1], None, ALU.mult, None, ["posr", "cst"], ["posr"])
            P.stt(posr[:], posc[:], self.cst[:, C_MC:C_MC + 1], posr[:], ALU.mult, ALU.add, ["posc", "posr", "cst"], ["posr"])
            P.ts(posr[:], posr[:], invf[:, 0:1], None, ALU.mult, None, ["posr", "invf"], ["posr"])
            self.sincos(st, posr, ropc, rops, L, "posr", "ropc", "rops")
            P.ts(rops[:], rops[:], self.cst[:, C_SIGN:C_SIGN + 1], None, ALU.mult, None, ["rops", "cst"], ["rops"])
            P.dma("sync", self.ROPE[0], ropc[:], reads=["ropc"], writes=["ROPE0"])
            P.dma("sync", self.ROPE[1], rops[:], reads=["rops"], writes=["ROPE1"])

    def phase_mod(self, l):
        P, nc = self.P, self.nc
        with ExitStack() as st:
            cc = P.sb(st, [128, 8, 2], F32, "cc")
            P.dma("sync", cc[:, :, 0], self.din["c"].rearrange("(kt p) -> p kt", p=128), writes=["cc0"])
            P.dma("sync", cc[:, :, 1], self.din["c_ctx"].rearrange("(kt p) -> p kt", p=128), writes=["cc1"])
            sc = P.sb(st, [128, 8, 2], F32, "sc")
            P.act(sc[:], cc[:], AF.Silu, ["cc0", "cc1"], ["sc"])
            bada = P.sb(st, [128, 6, 8], F32, "bada")
            P.dma("sync", bada[:], self.din["b_ada"][l].rearrange("(j ft p) -> p j ft", p=128, ft=8), writes=["bada"])
            wa = [P.sb(st, [128, 8, 1024], F32, "wa") for _ in range(2)]
            pm = P.ps(st, [128, 8, 2], F32, "pm")
            for j in range(6):
                b = j % 2
                src = self.din["w_ada"][l].rearrange("(kt p) f -> p kt f", p=128)[:, :, j * 1024:(j + 1) * 1024]
                P.dma("sync", wa[b][:], src, writes=[("wa", b)])
                for ft in range(8):
                    for kt in range(8):
                        P.mm(pm[:, ft, :], wa[b][:, kt, ft * 128:(ft + 1) * 128], sc[:, kt, :], kt == 0, kt == 7,
                             [("wa", b), "sc"], [("pm", ft)])
                for w in range(2):
                    P.tt(self.mod[:, j, :, w], pm[:, :, w], bada[:, j, :], ALU.add,
                         [("pm", ft) for ft in range(8)] + ["bada"], [("mod", j, w)])
            for (gname, gdst, jsc) in (("norm1_g", self.g1s, 1), ("norm2_g", self.g2s, 4)):
                g = P.sb(st, [128, 8], F32, "g")
                P.dma("sync", g[:], self.din[gname][l].rearrange("(ft p) -> p ft", p=128), writes=[gname])
                for w in range(2):
                    P.stt(gdst[:, :, w], self.mod[:, jsc, :, w], 1.0, g[:], ALU.add, ALU.mult,
                          [("mod", jsc, w), gname], [(gname + "s", w)])

    def norm_block(self, st_bufs, xt, gs, jshift, who, hout, keys_in, key_out, n):
        P = self.P
        sq, pss, rstd, tmp = st_bufs
        P.act(sq[:, :, :n], xt[:, :, :n], AF.Square, keys_in, ["nb_sq"])
        for kt in range(8):
            P.mm(pss[:, :n], self.onesb[:], sq[:, kt, :n], kt == 0, kt == 7, ["nb_sq", "onesb"], ["nb_ps"])
        P.act(rstd[:, :n], pss[:, :n], AF.Sqrt, ["nb_ps"], ["nb_rstd"], bias=self.epsc[:, 0:1], scale=1.0 / D)
        P.recip(rstd[:, :n], rstd[:, :n], ["nb_rstd"], ["nb_rstd"])
        for ft in range(8):
            P.tt(tmp[:, ft, :n], xt[:, ft, :n], rstd[:, :n], ALU.mult, keys_in + ["nb_rstd"], [("nb_tmp", ft)])
            P.act(hout[:, ft, :n], tmp[:, ft, :n], AF.Identity, [("nb_tmp", ft), (gs, who), ("mod", jshift, who)],
                  [key_out], bias=self.mod[:, jshift, ft, who:who + 1],
                  scale=(self.g1s if gs == "norm1_gs" else self.g2s)[:, ft, who:who + 1])

    def alloc_norm_bufs(self, st):
        P = self.P
        sq = P.sb(st, [128, 8, 512], BF16, "nsq")
        pss = P.ps(st, [128, 512], F32, "npss")
        rstd = P.sb(st, [128, 512], F32, "nrstd")
        tmp = P.sb(st, [128, 8, 512], F32, "ntmp")
        return (sq, pss, rstd, tmp)

    def phase_proj(self, l):
        P, nc = self.P, self.nc
        T, L = self.T, self.L
        w_in = self.din["w_in"][l].rearrange("(kt p) f -> p kt f", p=128)
        XTv = self.XT.rearrange("(ft p) t -> p ft t", p=128)
        PJv = self.PJ.rearrange("(ft p) t -> p ft t", p=128)
        self.hT_scr = self.scr.get("hT") or self.scratch("hT", [D, T], BF16)
        hTv = self.hT_scr.rearrange("(ft p) t -> p ft t", p=128)
        with ExitStack() as st:
            nb = self.alloc_norm_bufs(st)
            hT = P.sb(st, [128, 8, T], BF16, "hT")
            xt = [P.sb(st, [128, 8, 512], F32, "xt") for _ in range(2)]
            for bi, (t0, n) in enumerate(self.blocks):
                b = bi % 2
                P.dma("sync", xt[b][:, :, :n], XTv[:, :, t0:t0 + n], reads=["XTall"], writes=[("xt", b)])
                self.norm_block(nb, xt[b], "norm1_gs", 0, 1 if bi == 0 else 0, hT[:, :, t0:t0 + n],
                                [("xt", b)], ("hT", bi), n)
                P.dma("sync", hTv[:, :, t0:t0 + n], hT[:, :, t0:t0 + n], reads=[("hT", bi)], writes=[("hTd", bi)])
            hkeys = [("hT", bi) for bi in range(len(self.blocks))]
            lbr = P.sb(st, [128, 3, 2, 4], F32, "lbr")
            for li in range(4):
                for d in range(2):
                    P.dma("sync", lbr[:, :, d, li], self.din["hgrn_lb_raw"][li, d].rearrange("(ft p) -> p ft", p=128),
                          writes=[("lbr", li, d)])
            lbk = [("lbr", li, d) for li in range(4) for d in range(2)]
            lbe = P.sb(st, [128, 3, 2, 4], F32, "lbe")
            P.act(lbe[:], lbr[:], AF.Exp, lbk, ["lbe"])
            lsum = P.sb(st, [128, 3, 2], F32, "lsum")
            P.op("vector", lambda e: e.tensor_reduce(lsum[:], lbe[:], AX.X, ALU.add), ["lbe"], ["lsum"])
            P.recip(lsum[:], lsum[:], ["lsum"], ["lsum"])
            lb = P.sb(st, [128, 3, 2], F32, "lb")
            oml = P.sb(st, [128, 3, 2], F32, "oml")
            P.memset(lb[:], 0.0, ["lb"])
            for li in range(1, l + 1):
                P.tt(lb[:], lb[:], lbe[:, :, :, li], ALU.add, ["lb", "lbe"], ["lb"])
            P.tt(lb[:], lb[:], lsum[:], ALU.mult, ["lb", "lsum"], ["lb"])
            P.ts(oml[:], lb[:], -1.0, 1.0, ALU.mult, ALU.add, ["lb"], ["oml"])
            rcb = [P.sb(st, [128, 512], F32, "rcb") for _ in range(2)]
            rsb = [P.sb(st, [128, 512], F32, "rsb") for _ in range(2)]
            wb = [P.sb(st, [128, 8, 384], BF16, "wb") for _ in range(3)]
            pp = [P.ps(st, [128, 512], F32, "pp") for _ in range(4)]
            ob = [P.sb(st, [128, 3, 512], BF16, "ob") for _ in range(2)]
            of = [P.sb(st, [128, 3, 512], F32, "of") for _ in range(2)]
            t1 = P.sb(st, [128, 512], F32, "pt1")
            t2 = P.sb(st, [128, 512], F32, "pt2")
            cnt = {"pp": 0, "ob": 0}

            def load_w(slot, c0, ncol, swap=False):
                if not swap:
                    P.dma("gpsimd", wb[slot][:, :, :ncol], w_in[:, :, c0:c0 + ncol], reads=[], writes=[("wb", slot)])
                else:
                    src = w_in[:, :, c0:c0 + ncol].rearrange("p kt (h two j) -> p kt h two j", two=2, j=32)
                    dst = wb[slot][:, :, :ncol].rearrange("p kt (h two j) -> p kt h two j", two=2, j=32)
                    for kt in range(8):
                        P.dma("gpsimd", dst[:, kt, :, 0, :], src[:, kt, :, 1, :], reads=[], writes=[("wb", slot, kt, 0)])
                        P.dma("gpsimd", dst[:, kt, :, 1, :], src[:, kt, :, 0, :], reads=[], writes=[("wb", slot, kt, 1)])

            def wkeys(slot, swap=False):
                if not swap:
                    return [("wb", slot)]
                return [("wb", slot, kt, x) for kt in range(8) for x in range(2)]

            def fm_proj(slot, ft, t0, n, wk):
                i = cnt["pp"] % 4
                cnt["pp"] += 1
                for kt in range(8):
                    P.mm(pp[i][:, :n], wb[slot][:, kt, ft * 128:(ft + 1) * 128], hT[:, kt, t0:t0 + n], kt == 0, kt == 7,
                         wk + hkeys, [("pp", i)])
                return i

            def feature_group(c0, nft, row0, post, extra_w=None):
                load_w(0, c0, nft * 128)
                for bi, (t0, n) in enumerate(self.blocks):
                    o = cnt["ob"] % 2
                    cnt["ob"] += 1
                    for ft in range(nft):
                        i = fm_proj(0, ft, t0, n, wkeys(0))
                        post(i, ft, n, ob[o], o, bi, t0)
                    P.dma("sync", PJv[:, row0 // 128:row0 // 128 + nft, t0:t0 + n], ob[o][:, :nft, :n],
                          reads=[("ob", o, ft) for ft in range(nft)], writes=[("PJ", row0, bi)])

            def post_copy(i, ft, n, obt, o, bi, t0):
                P.copy(obt[:, ft, :n], pp[i][:, :n], [("pp", i)], [("ob", o, ft)], eng="scalar")

            def post_silu(i, ft, n, obt, o, bi, t0):
                P.act(obt[:, ft, :n], pp[i][:, :n], AF.Silu, [("pp", i)], [("ob", o, ft)])

            feature_group(O_U, 2, O_U, post_copy)
            feature_group(O_HQ, 3, O_HQ, post_copy)
            feature_group(O_HG, 3, O_HG, post_silu)
            feature_group(O_RG, 3, O_RG, post_silu)
            LFv = self.LF.rearrange("d (ft p) t -> d p ft t", p=128)
            for d, c0 in ((0, O_HFF), (1, O_HFB)):
                load_w(0, c0, 384)
                for bi, (t0, n) in enumerate(self.blocks):
                    o = cnt["ob"] % 2
                    cnt["ob"] += 1
                    for ft in range(3):
                        i = fm_proj(0, ft, t0, n, wkeys(0))
                        P.act(t1[:, :n], pp[i][:, :n], AF.Sigmoid, [("pp", i)], ["pt1"])
                        P.ts(t1[:, :n], t1[:, :n], oml[:, ft, d:d + 1], lb[:, ft, d:d + 1], ALU.mult, ALU.add,
                             ["pt1", "oml", "lb"], ["pt1"])
                        P.act(of[o][:, ft, :n], t1[:, :n], AF.Ln, ["pt1"], [("of", o, ft)])
                        P.ts(ob[o][:, ft, :n], t1[:, :n], -1.0, 1.0, ALU.mult, ALU.add, ["pt1"], [("ob", o, ft)])
                    P.dma("sync", PJv[:, c0 // 128:c0 // 128 + 3, t0:t0 + n], ob[o][:, :3, :n],
                          reads=[("ob", o, ft) for ft in range(3)], writes=[("PJ", c0, bi)])
                    P.dma("sync", LFv[d][:, :, t0:t0 + n], of[o][:, :3, :n],
                          reads=[("of", o, ft) for ft in range(3)], writes=[("LF", d, bi)])
            for c0, scl in ((O_RQ, 1.0), (O_RK, 0.125)):
                load_w(0, c0, 384)
                load_w(1, c0, 384, swap=True)
                for bi, (t0, n) in enumerate(self.blocks):
                    o = cnt["ob"] % 2
                    cnt["ob"] += 1
                    if bi > 0:
                        l0 = t0 - NCTX
                        P.dma("sync", rcb[bi % 2][:, :n], self.ROPE[0, :, l0:l0 + n], writes=[("rcb", bi % 2)])
                        P.dma("sync", rsb[bi % 2][:, :n], self.ROPE[1, :, l0:l0 + n], writes=[("rsb", bi % 2)])
                    for ft in range(3):
                        i = fm_proj(0, ft, t0, n, wkeys(0))
                        if bi == 0:
                            P.act(ob[o][:, ft, :n], pp[i][:, :n], AF.Identity, [("pp", i)], [("ob", o, ft)], scale=scl)
                        else:
                            i2 = fm_proj(1, ft, t0, n, wkeys(1, True))
                            rb_ = bi % 2
                            P.tt(t1[:, :n], pp[i][:, :n], rcb[rb_][:, :n], ALU.mult, [("pp", i), ("rcb", rb_)], ["pt1"])
                            P.tt(t2[:, :n], pp[i2][:, :n], rsb[rb_][:, :n], ALU.mult, [("pp", i2), ("rsb", rb_)], ["pt2"])
                            P.tt(t1[:, :n], t1[:, :n], t2[:, :n], ALU.add, ["pt1", "pt2"], ["pt1"])
                            P.act(ob[o][:, ft, :n], t1[:, :n], AF.Identity, ["pt1"], [("ob", o, ft)], scale=scl)
                    P.dma("sync", PJv[:, c0 // 128:c0 // 128 + 3, t0:t0 + n], ob[o][:, :3, :n],
                          reads=[("ob", o, ft) for ft in range(3)], writes=[("PJ", c0, bi)])
            vb = [P.sb(st, [128, 384], BF16, "vb") for _ in range(2)]
            for vi, c0 in ((0, O_HV), (1, O_RV)):
                load_w(2, c0, 384)
                for tt_ in range(T // 128):
                    i = cnt["pp"] % 4
                    cnt["pp"] += 1
                    for kt in range(8):
                        P.mm(pp[i][:, :384], hT[:, kt, tt_ * 128:(tt_ + 1) * 128], wb[2][:, kt, :384], kt == 0, kt == 7,
                             [("wb", 2)] + hkeys, [("pp", i)])
                    o = tt_ % 2
                    P.copy(vb[o][:], pp[i][:, :384], [("pp", i)], [("vb", o)], eng=("vector" if o == 0 else "scalar"))
                    P.dma("sync", self.VT[vi, tt_ * 128:(tt_ + 1) * 128, :], vb[o][:], reads=[("vb", o)], writes=[("VT", vi, tt_)])

    def sincos(self, st, ang, cosd, sind, n, akey, kc, ks, bufs=None):
        P = self.P
        if bufs is None:
            ki = P.sb(st, [128, n], I32, "sc_ki")
            kf = P.sb(st, [128, n], F32, "sc_kf")
        else:
            ki, kf = bufs[0][:, :n], bufs[1][:, :n]
        P.ts(kf[:], ang[:, :n], 1.0 / (2 * math.pi), None, ALU.mult, None, [akey], ["sc_kf"])
        P.copy(ki[:], kf[:], ["sc_kf"], ["sc_ki"])
        P.copy(kf[:], ki[:], ["sc_ki"], ["sc_kf"])
        P.stt(ang[:, :n], kf[:], -2 * math.pi, ang[:, :n], ALU.mult, ALU.add, ["sc_kf", akey], [akey])
        P.ts(kf[:], ang[:, :n], math.pi, -2 * math.pi, ALU.is_gt, ALU.mult, [akey], ["sc_kf"])
        P.tt(ang[:, :n], ang[:, :n], kf[:], ALU.add, [akey, "sc_kf"], [akey])
        P.ts(kf[:], ang[:, :n], -math.pi, 2 * math.pi, ALU.is_lt, ALU.mult, [akey], ["sc_kf"])
        P.tt(ang[:, :n], ang[:, :n], kf[:], ALU.add, [akey, "sc_kf"], [akey])
        P.ts(ang[:, :n], ang[:, :n], math.pi, -math.pi, ALU.min, ALU.max, [akey], [akey])
        P.act(sind[:, :n], ang[:, :n], AF.Sin, [akey], [ks])
        P.act(kf[:], ang[:, :n], AF.Abs, [akey], ["sc_kf"])
        P.ts(kf[:], kf[:], -1.0, math.pi / 2, ALU.mult, ALU.add, ["sc_kf"], ["sc_kf"])
        P.act(cosd[:, :n], kf[:], AF.Sin, ["sc_kf"], [kc])

    def phase_output(self):
        P, nc = self.P, self.nc
        T, L = self.T, self.L
        XTv = self.XT.rearrange("(ft p) t -> p ft t", p=128)
        with ExitStack() as st:
            nb = self.alloc_norm_bufs(st)
            sq, pss, rstd, tmp = nb
            ident = self.cst[:, C_IDENT:C_IDENT + 128]
            fg = P.sb(st, [128, 8], F32, "fg")
            P.dma("sync", fg[:], self.din["final_norm_g"].rearrange("(ft p) -> p ft", p=128), writes=["fg"])
            xt = [P.sb(st, [128, 8, 512], F32, "oxt") for _ in range(2)]
            xn = [P.sb(st, [128, 8, 512], F32, "oxn") for _ in range(2)]
            po = [P.ps(st, [128, 4, 128], F32, "opo") for _ in range(2)]
            ot = [P.sb(st, [128, D], F32, "oot") for _ in range(2)]
            k = 0
            for bi, (t0, n) in enumerate(self.blocks):
                if bi == 0:
                    continue
                b = bi % 2
                P.dma("sync", xt[b][:, :, :n], XTv[:, :, t0:t0 + n], reads=["XTall"], writes=[("oxt", b)])
                P.act(sq[:, :, :n], xt[b][:, :, :n], AF.Square, [("oxt", b)], ["nb_sq"])
                for kt in range(8):
                    P.mm(pss[:, :n], self.onesb[:], sq[:, kt, :n], kt == 0, kt == 7, ["nb_sq", "onesb"], ["nb_ps"])
                P.act(rstd[:, :n], pss[:, :n], AF.Sqrt, ["nb_ps"], ["nb_rstd"], bias=self.epsc[:, 0:1], scale=1.0 / D)
                P.recip(rstd[:, :n], rstd[:, :n], ["nb_rstd"], ["nb_rstd"])
                for ft in range(8):
                    P.stt(xn[b][:, ft, :n], xt[b][:, ft, :n], fg[:, ft:ft + 1], rstd[:, :n], ALU.mult, ALU.mult,
                          [("oxt", b), "fg", "nb_rstd"], [("oxn", b, ft)])
                for s in range(n // 128):
                    o = k % 2
                    k += 1
                    for hf in range(2):
                        for q in range(4):
                            ft = hf * 4 + q
                            P.mm(po[hf][:, q, :], xn[b][:, ft, s * 128:(s + 1) * 128], ident, True, True,
                                 [("oxn", b, ft), "cst"], [("opo", hf, q)])
                        P.copy(ot[o][:, hf * 512:(hf + 1) * 512], po[hf][:].rearrange("p a b -> p (a b)"),
                               [("opo", hf, q) for q in range(4)], [("oot", o, hf)], eng=("vector" if hf == 0 else "scalar"))
                    r0 = t0 - NCTX + s * 128
                    P.dma("sync", self.out[r0:r0 + 128, :], ot[o][:], reads=[("oot", o, 0), ("oot", o, 1)],
                          writes=[("out", r0)], is_output=True)


    def phase_s5(self, l):
        P, nc = self.P, self.nc
        T, L = self.T, self.L
        PJv = self.PJ.rearrange("(ft p) t -> p ft t", p=128)
        YBv = self.YB.rearrange("(ft p) t -> p ft t", p=128)
        nblk = T // 256
        with ExitStack() as st:
            uT = P.sb(st, [128, 2, T], BF16, "uT")
            P.dma("sync", uT[:], PJv[:, 0:2, :], writes=["uT"])
            yacc = P.sb(st, [128, 2, T], F32, "yacc")
            Ec = P.sb(st, [128, 8, 256], F32, "Ec")
            Es = P.sb(st, [128, 8, 256], F32, "Es")
            Fc = P.sb(st, [128, 8, 256], F32, "Fc")
            Fs = P.sb(st, [128, 8, 256], F32, "Fs")
            ang = P.sb(st, [128, 8, 256], F32, "ang")
            tA = P.sb(st, [128, 4, 256], F32, "tA")
            tB = P.sb(st, [128, 4, 256], F32, "tB")
            xt_ = P.sb(st, [128, 4, 2, 256], F32, "xtl")
            M = P.sb(st, [128, 4, 2, 256], F32, "M")
            hb = P.sb(st, [128, 4, 2, 256], BF16, "hb")
            BD = [P.sb(st, [128, 2, 512], BF16, "BD%d" % c) for c in range(2)]
            CT = [P.sb(st, [128, 8, 128], BF16, "CT%d" % c) for c in range(2)]
            sm = {n: P.sb(st, [128, 8], F32, "s5" + n) for n in
                  ("lr", "li", "dt", "th", "mag", "c", "s", "ar", "ai", "den", "am1", "zr", "zi", "t1", "t2", "a256", "Rc", "Rs")}
            init = [P.sb(st, [128, 8, 2], F32, "init%d" % i) for i in range(2)]
            pbu = P.ps(st, [128, 4, 2, 256], F32, "pbu")
            py = P.ps(st, [128, 256], F32, "py")
            iota = self.cst[:, C_IOTA:C_IOTA + 256]
            scb = (P.sb(st, [128, 2048], I32, "sc_ki"), P.sb(st, [128, 2048], F32, "sc_kf"))
            magf = P.sb(st, [128, 8, 256], F32, "magf")
            for d in range(2):
                P.dma("sync", sm["lr"][:], self.din["s5_lam_re"][l, d].rearrange("g p -> (g p)").rearrange("(nt p) -> p nt", p=128), writes=["lr"])
                P.dma("sync", sm["li"][:], self.din["s5_lam_im"][l, d].rearrange("g p -> (g p)").rearrange("(nt p) -> p nt", p=128), writes=["li"])
                ld = self.din["s5_log_dt"][l, d]
                for half in range(2):
                    src = bass.AP(ld.tensor, ld.offset + half, [[0, 64], [2, 8]])
                    P.dma("sync", sm["dt"][half * 64:(half + 1) * 64, :], src, writes=[("dt", half)])
                P.act(sm["dt"][:], sm["dt"][:], AF.Exp, [("dt", 0), ("dt", 1)], ["dt"])
                P.tt(sm["th"][:], sm["li"][:], sm["dt"][:], ALU.mult, ["li", "dt"], ["th"])
                P.tt(sm["mag"][:], sm["lr"][:], sm["dt"][:], ALU.mult, ["lr", "dt"], ["mag"])
                P.act(sm["mag"][:], sm["mag"][:], AF.Exp, ["mag"], ["mag"])
                P.ts(sm["a256"][:], sm["th"][:], 256.0, None, ALU.mult, None, ["th"], ["a256"])
                P.copy(sm["t1"][:], sm["th"][:], ["th"], ["t1"])
                with ExitStack() as st2:
                    self.sincos(st2, sm["t1"], sm["c"], sm["s"], 8, "t1", "c", "s", scb)
                    self.sincos(st2, sm["a256"], sm["Rc"], sm["Rs"], 8, "a256", "Rc", "Rs", scb)
                    P.tt(sm["ar"][:], sm["mag"][:], sm["c"][:], ALU.mult, ["mag", "c"], ["ar"])
                    P.tt(sm["ai"][:], sm["mag"][:], sm["s"][:], ALU.mult, ["mag", "s"], ["ai"])
                    P.tt(sm["den"][:], sm["lr"][:], sm["lr"][:], ALU.mult, ["lr"], ["den"])
                    P.tt(sm["t2"][:], sm["li"][:], sm["li"][:], ALU.mult, ["li"], ["t2"])
                    P.tt(sm["den"][:], sm["den"][:], sm["t2"][:], ALU.add, ["den", "t2"], ["den"])
                    P.recip(sm["den"][:], sm["den"][:], ["den"], ["den"])
                    P.ts(sm["am1"][:], sm["ar"][:], -1.0, None, ALU.add, None, ["ar"], ["am1"])
                    P.tt(sm["zr"][:], sm["am1"][:], sm["lr"][:], ALU.mult, ["am1", "lr"], ["zr"])
                    P.tt(sm["t2"][:], sm["ai"][:], sm["li"][:], ALU.mult, ["ai", "li"], ["t2"])
                    P.tt(sm["zr"][:], sm["zr"][:], sm["t2"][:], ALU.add, ["zr", "t2"], ["zr"])
                    P.tt(sm["zr"][:], sm["zr"][:], sm["den"][:], ALU.mult, ["zr", "den"], ["zr"])
                    P.tt(sm["zi"][:], sm["ai"][:], sm["lr"][:], ALU.mult, ["ai", "lr"], ["zi"])
                    P.tt(sm["t2"][:], sm["am1"][:], sm["li"][:], ALU.mult, ["am1", "li"], ["t2"])
                    P.tt(sm["zi"][:], sm["zi"][:], sm["t2"][:], ALU.subtract, ["zi", "t2"], ["zi"])
                    P.tt(sm["zi"][:], sm["zi"][:], sm["den"][:], ALU.mult, ["zi", "den"], ["zi"])
                    P.tt(ang[:], iota.unsqueeze(1).to_broadcast([128, 8, 256]), sm["th"][:].unsqueeze(2).to_broadcast([128, 8, 256]),
                         ALU.mult, ["cst", "th"], ["ang"])
                    a2 = ang[:].rearrange("p a b -> p (a b)")
                    self.sincos(st2, ang[:].rearrange("p a b -> p (a b)"), Ec[:].rearrange("p a b -> p (a b)"),
                                Es[:].rearrange("p a b -> p (a b)"), 2048, "ang", "Ec", "Es", scb)
                for nt in range(8):
                    P.ts(magf[:, nt, :], self.onesf[:, 0:128].unsqueeze(1).to_broadcast([128, 2, 128]).rearrange("p a b -> p (a b)") if False else Ec[:, nt, :],
                         0.0, sm["mag"][:, nt:nt + 1], ALU.mult, ALU.add, ["Ec", "mag"], ["magf"])
                zrb = sm["zr"][:].unsqueeze(2).to_broadcast([128, 8, 256])
                zib = sm["zi"][:].unsqueeze(2).to_broadcast([128, 8, 256])
                P.tt(Fc[:], Ec[:], zrb, ALU.mult, ["Ec", "zr"], ["Fc"])
                P.tt(ang[:], Es[:], zib, ALU.mult, ["Es", "zi"], ["ang"])
                P.tt(Fc[:], Fc[:], ang[:], ALU.add, ["Fc", "ang"], ["Fc"])
                P.tt(Fs[:], Ec[:], zib, ALU.mult, ["Ec", "zi"], ["Fs"])
                P.tt(ang[:], Es[:], zrb, ALU.mult, ["Es", "zr"], ["ang"])
                P.tt(Fs[:], Fs[:], ang[:], ALU.subtract, ["Fs", "ang"], ["Fs"])
                for c, nm in ((0, "s5_b_re"), (1, "s5_b_im")):
                    P.memset(BD[c][:], 0.0, [("BD", c)])
                    for g in range(16):
                        kt, gl = g // 8, g % 8
                        P.dma("gpsimd", BD[c][gl * 16:(gl + 1) * 16, kt, gl * 64:(gl + 1) * 64],
                              self.din[nm][l, d, g].rearrange("p h -> h p"), reads=[], writes=[("BD", c)])
                for c, nm in ((0, "s5_c_re"), (1, "s5_c_im")):
                    P.memset(CT[c][:], 0.0, [("CT", c)])
                    for g in range(16):
                        nt, g2, gl = g // 2, g % 2, g % 8
                        P.dma("gpsimd", CT[c][g2 * 64:(g2 + 1) * 64, nt, gl * 16:(gl + 1) * 16],
                              self.din[nm][l, d, g].rearrange("h p -> p h"), reads=[], writes=[("CT", c)])
                P.ts(CT[1][:], CT[1][:], -1.0, None, ALU.mult, None, [("CT", 1)], [("CT", 1)])
                if d == 0:
                    for nm_ in ("th", "mag", "zr", "zi", "Rc", "Rs", "dt", "lr", "li"):
                        self.dump(nm_, sm[nm_][:], [128, 8], F32, [nm_])
                    self.dump("Ec", Ec[:], [128, 8, 256], F32, ["Ec"])
                    self.dump("Es", Es[:], [128, 8, 256], F32, ["Es"])
                    self.dump("Fc", Fc[:], [128, 8, 256], F32, ["Fc"])
                    self.dump("BD0", BD[0][:], [128, 2, 512], BF16, [("BD", 0)])
                    self.dump("CT0", CT[0][:], [128, 8, 128], BF16, [("CT", 0)])
                    self.dump("CT1", CT[1][:], [128, 8, 128], BF16, [("CT", 1)])
                if d == 1 and "YD" in self.debug:
                    YD = self.scratch("YD", [256, T], F32)
                    P.dma("sync", YD.rearrange("(q p) t -> p q t", p=128), yacc[:], reads=[("yacc", q, b2) for q in range(2) for b2 in range(nblk)], writes=["YD"])
                order = list(range(nblk)) if d == 0 else [0] + list(range(nblk - 1, 0, -1))
                P.memset(init[0][:], 0.0, [("init", 0)])
                last = 255 if d == 0 else 0
                R = (lambda a: a) if d == 0 else rev_ap
                for bi, blk in enumerate(order):
                    t0 = blk * 256
                    ii, io = bi % 2, (bi + 1) % 2
                    for q in range(2):
                        for j in range(4):
                            for c in range(2):
                                P.mm(pbu[:, j, c, :], BD[c][:, q, j * 128:(j + 1) * 128], uT[:, q, t0:t0 + 256], True, True,
                                     [("BD", c), "uT"], [("pbu", j, c)])
                        pk = [("pbu", j, c) for j in range(4) for c in range(2)]
                        fc = R(Fc[:, 4 * q:4 * q + 4, :])
                        fs = R(Fs[:, 4 * q:4 * q + 4, :])
                        ec = R(Ec[:, 4 * q:4 * q + 4, :])
                        es = R(Es[:, 4 * q:4 * q + 4, :])
                        P.tt(tA[:], pbu[:, :, 0, :], fc, ALU.mult, pk + ["Fc"], ["tA"])
                        P.tt(tB[:], pbu[:, :, 1, :], fs, ALU.mult, pk + ["Fs"], ["tB"])
                        P.tt(xt_[:, :, 0, :], tA[:], tB[:], ALU.subtract, ["tA", "tB"], [("xtl", 0)])
                        P.tt(tA[:], pbu[:, :, 1, :], fc, ALU.mult, pk + ["Fc"], ["tA"])
                        P.tt(tB[:], pbu[:, :, 0, :], fs, ALU.mult, pk + ["Fs"], ["tB"])
                        P.tt(xt_[:, :, 1, :], tA[:], tB[:], ALU.add, ["tA", "tB"], [("xtl", 1)])
                        for j in range(4):
                            nt = 4 * q + j
                            for c in range(2):
                                def f(e, j=j, c=c, nt=nt, ii=ii, R=R):
                                    return e.tensor_tensor_scan(R(M[:, j, c, :]), magf[:, nt, :],
                                                                R(xt_[:, j, c, :]), init[ii][:, nt, c:c + 1], ALU.mult, ALU.add)
                                P.op("vector", f, [("xtl", c), "magf", ("init", ii)], [("M", c)])
                        rc = sm["Rc"][:, 4 * q:4 * q + 4]
                        rs = sm["Rs"][:, 4 * q:4 * q + 4]
                        mre = M[:, :, 0, last]
                        mim = M[:, :, 1, last]
                        t1 = sm["t1"][:, 0:4]
                        t2 = sm["t2"][:, 0:4]
                        P.tt(t1, mre, rc, ALU.mult, [("M", 0), "Rc"], ["t1"])
                        P.tt(t2, mim, rs, ALU.mult, [("M", 1), "Rs"], ["t2"])
                        P.tt(init[io][:, 4 * q:4 * q + 4, 0], t1, t2, ALU.subtract, ["t1", "t2"], [("init", io)])
                        P.tt(t1, mim, rc, ALU.mult, [("M", 1), "Rc"], ["t1"])
                        P.tt(t2, mre, rs, ALU.mult, [("M", 0), "Rs"], ["t2"])
                        P.tt(init[io][:, 4 * q:4 * q + 4, 1], t1, t2, ALU.add, ["t1", "t2"], [("init", io)])
                        P.tt(tA[:], M[:, :, 0, :], ec, ALU.mult, [("M", 0), "Ec"], ["tA"])
                        P.tt(tB[:], M[:, :, 1, :], es, ALU.mult, [("M", 1), "Es"], ["tB"])
                        P.tt(hb[:, :, 0, :], tA[:], tB[:], ALU.subtract, ["tA", "tB"], [("hb", 0)])
                        P.tt(tA[:], M[:, :, 1, :], ec, ALU.mult, [("M", 1), "Ec"], ["tA"])
                        P.tt(tB[:], M[:, :, 0, :], es, ALU.mult, [("M", 0), "Es"], ["tB"])
                        P.tt(hb[:, :, 1, :], tA[:], tB[:], ALU.add, ["tA", "tB"], [("hb", 1)])
                        k = 0
                        for j in range(4):
                            for c in range(2):
                                P.mm(py[:], CT[c][:, 4 * q + j, :], hb[:, j, c, :], k == 0, k == 7,
                                     [("CT", c), ("hb", c)], ["py"])
                                k += 1
                        if d == 0:
                            P.copy(yacc[:, q, t0:t0 + 256], py[:], ["py"], [("yacc", q, blk)], eng="scalar")
                            if bi == 0 and q == 0:
                                self.dump("pbu", xt_[:], [128, 4, 2, 256], F32, [("xtl", 0), ("xtl", 1)])
                                self.dump("M", M[:], [128, 4, 2, 256], F32, [("M", 0), ("M", 1)])
                                self.dump("hb", hb[:], [128, 4, 2, 256], BF16, [("hb", 0), ("hb", 1)])
                        else:
                            P.tt(yacc[:, q, t0:t0 + 256], yacc[:, q, t0:t0 + 256], py[:], ALU.add, ["py", ("yacc", q, blk)], [("yacc", q, blk)])
            if False:
                YD = self.scratch("YD", [256, T], F32)
                P.dma("sync", YD.rearrange("(q p) t -> p q t", p=128), yacc[:], reads=[("yacc", q, b2) for q in range(2) for b2 in range(nblk)] + [("yg", 0), ("yg", 1)], writes=["YD"])
            dsk = P.sb(st, [128, 2], F32, "dsk")
            P.dma("sync", dsk[:], self.din["s5_d"][l].rearrange("(kt p) -> p kt", p=128), writes=["dsk"])
            wg = P.sb(st, [128, 2, 256], BF16, "wglu")
            P.dma("gpsimd", wg[:], self.din["s5_w_glu"][l].rearrange("(kt p) f -> p kt f", p=128), reads=[], writes=["wglu"])
            yb = P.sb(st, [128, 2, 512], BF16, "ybf")
            yo = [P.sb(st, [128, 2, 512], BF16, "yo") for _ in range(2)]
            sg = P.sb(st, [128, 512], F32, "sg")
            pg = P.ps(st, [128, 512], F32, "pg")
            for bi, (t0, n) in enumerate(self.blocks):
                o = bi % 2
                yk = [("yacc", q, b2) for q in range(2) for b2 in range(nblk)]
                for q in range(2):
                    P.stt(yacc[:, q, t0:t0 + n], uT[:, q, t0:t0 + n], dsk[:, q:q + 1], yacc[:, q, t0:t0 + n], ALU.mult, ALU.add,
                          ["uT", "dsk"] + yk, [("yg", q)])
                    P.act(yacc[:, q, t0:t0 + n], yacc[:, q, t0:t0 + n], AF.Gelu_apprx_tanh, [("yg", q)], [("yg", q)])
                    P.copy(yb[:, q, :n], yacc[:, q, t0:t0 + n], [("yg", q)], [("ybf", q)])
                for ft in range(2):
                    for kt in range(2):
                        P.mm(pg[:, :n], wg[:, kt, ft * 128:(ft + 1) * 128], yb[:, kt, :n], kt == 0, kt == 1,
                             ["wglu", ("ybf", 0), ("ybf", 1)], ["pg"])
                    P.act(sg[:, :n], pg[:, :n], AF.Sigmoid, ["pg"], ["sg"])
                    P.tt(yo[o][:, ft, :n], yacc[:, ft, t0:t0 + n], sg[:, :n], ALU.mult, ["sg", ("yg", ft)], [("yo", o, ft)])
                P.dma("sync", YBv[:, 0:2, t0:t0 + n], yo[o][:, :, :n], reads=[("yo", o, 0), ("yo", o, 1)], writes=[("YB", 0, bi)])

    def phase_gla(self, l, which):
        P, nc = self.P, self.nc
        T, L = self.T, self.L
        PJ = self.PJ
        qoff = O_HQ if which == 0 else O_RQ
        goff = O_HG if which == 0 else O_RG
        yrow0 = 256 + which * 384
        CS = 32 if which == 0 else 64
        NH = 6
        with ExitStack() as st:
            def mk(shape, dt, nm):
                return [P.sb(st, shape, dt, nm) for _ in range(NH)]
            qf = mk([64, 512], BF16, "qf")
            kf = mk([64, 512], BF16, "kf")
            qt = mk([64, 512], BF16, "qt")
            ktl = mk([64, 512 + 64], BF16, "ktl")
            qh = mk([64, 512], BF16, "qh")
            kd = mk([64, 512 + 64], BF16, "kd")
            kdT = mk([64, 512 // CS, 64], BF16, "kdT")
            vv = mk([64, 512 // CS, 64], BF16, "vv")
            S = mk([64, 64], F32, "S")
            Sb = mk([64, 64], BF16, "Sb")
            ob = mk([64, 512], F32, "obk")
            oa = mk([64, 512], F32, "oak")
            ebend = mk([64, 16], F32, "ebend")
            Asb = [[[P.sb(st, [64, CS], BF16, "Asb") for _ in range(2)] for _ in range(2)] for _ in range(NH)]
            for hd in range(NH):
                P.memset(ktl[hd][:], 0.0, [("ktl", hd)])
                P.memset(kd[hd][:], 0.0, [("kd", hd)])
                for dd in range(2):
                    for sl in range(2):
                        P.memset(Asb[hd][dd][sl][:], 0.0, [("Asb", hd, dd, sl)])
            lf = [P.sb(st, [64, 512], F32, "lf") for _ in range(2)]
            bb = [P.sb(st, [64, 512], F32, "bb") for _ in range(2)]
            d1 = [P.sb(st, [64, 512], F32, "d1") for _ in range(2)]
            ex = [P.sb(st, [64, 512], F32, "ex") for _ in range(2)]
            gsb = [P.sb(st, [64, 512], BF16, "gsb") for _ in range(2)]
            osq = [P.sb(st, [64, 512], BF16, "osq") for _ in range(2)]
            rs_ = [P.sb(st, [64, 512], F32, "rs_") for _ in range(2)]
            yo = [P.sb(st, [64, 512], BF16, "yo") for _ in range(2)]
            LB = [P.ps(st, [128, 512], F32, "LB") for _ in range(NH)]
            PT = P.ps(st, [128, 512], F32, "PT")
            PN = P.ps(st, [128, 512], F32, "PN")
            if which == 1:
                tb = [{n: P.sb(st, [64, 64], F32, "rt" + n) for n in ("b", "q", "k", "e", "d")} for _ in range(NH)]
                ebr = mk([64, 1], F32, "ebr")
                for hd in range(NH):
                    lgc = math.log(1.0 - 2.0 ** (-5.0 - hd))
                    t_ = tb[hd]
                    P.ts(t_["b"][:], self.cst[0:64, C_IOTA:C_IOTA + 64], 1.0, lgc, ALU.add, ALU.mult, ["cst"], [("rtb", hd)])
                    P.act(t_["e"][:], t_["b"][:], AF.Exp, [("rtb", hd)], [("rte", hd)])
                    P.ts(t_["q"][:], t_["b"][:], t_["b"][:, 31:32], None, ALU.subtract, None, [("rtb", hd)], [("rtq", hd)])
                    P.act(t_["k"][:], t_["q"][:], AF.Exp, [("rtq", hd)], [("rtk", hd)], scale=-1.0)
                    P.act(t_["q"][:], t_["q"][:], AF.Exp, [("rtq", hd)], [("rtq", hd)])
                    P.ts(t_["d"][:], t_["b"][:], t_["b"][:, 63:64], None, ALU.subtract, None, [("rtb", hd)], [("rtd", hd)])
                    P.act(t_["d"][:], t_["d"][:], AF.Exp, [("rtd", hd)], [("rtd", hd)], scale=-1.0)
                    P.copy(ebr[hd][:], t_["e"][:, 63:64], [("rte", hd)], [("ebr", hd)])
            if which == 0:
                mask_f = self.cmask[0:CS, M32_FWD:M32_FWD + 32]
                mask_b = self.cmask[0:CS, M32_BWD:M32_BWD + 32]
            else:
                mask_f = self.cmask[0:CS, M_FWD:M_FWD + 64]
                mask_b = self.cmask[0:CS, M_BWDS:M_BWDS + 64]
            reset = self.cst[0:64, C_RESET32:C_RESET32 + 512]
            nstep = 0
            npre = 0
            for d in range(2):
                order = list(range(len(self.blocks))) if d == 0 else [0] + list(range(len(self.blocks) - 1, 0, -1))
                R = (lambda a: a) if d == 0 else rev_ap
                mask = mask_f if d == 0 else mask_b
                for hd in range(NH):
                    P.memset(S[hd][:], 0.0, [("S", hd)])
                    P.memset(Sb[hd][:], 0.0, [("Sb", hd)])
                for blk in order:
                    t0, n = self.blocks[blk]
                    nch = n // CS
                    pm = (CS // 2 - 1) if d == 0 else CS // 2
                    pe = (CS - 1) if d == 0 else 0
                    for hd in range(NH):
                        u = npre % 2
                        npre += 1
                        koff = (O_HFF if d == 0 else O_HFB) if which == 0 else O_RK
                        r0 = qoff + hd * 64
                        P.dma("sync", qf[hd][:, :n], PJ[r0:r0 + 64, t0:t0 + n], writes=[("qf", hd)])
                        r1 = koff + hd * 64
                        P.dma("sync", kf[hd][:, :n], PJ[r1:r1 + 64, t0:t0 + n], writes=[("kf", hd)])
                        P.dma("sync", vv[hd][0:CS, :n // CS, :],
                              self.VT[which, t0:t0 + n, hd * 64:(hd + 1) * 64].rearrange("(a p) c -> p a c", p=CS), writes=[("vv", hd)])
                        if d == 1:
                            P.dma("sync", oa[hd][:, :n], self.OA[which, hd * 64:(hd + 1) * 64, t0:t0 + n], writes=[("oak", hd)])
                        if which == 0:
                            P.dma("sync", lf[u][:, :n], self.LF[d, hd * 64:(hd + 1) * 64, t0:t0 + n], writes=[("lf", u)])
                            rr, rb, rl = reset[:, :n], R(bb[u][:, :n]), R(lf[u][:, :n])
                            P.op("vector", (lambda e, rr=rr, rb=rb, rl=rl: e.tensor_tensor_scan(rb, rr, rl, 0.0, ALU.mult, ALU.add)),
                                 [("lf", u), "cst"], [("bb", u)])
                            b3 = bb[u][:, :n].rearrange("p (c i) -> p c i", i=CS)
                            d3 = d1[u][:, :n].rearrange("p (c i) -> p c i", i=CS)
                            kb, kd1, kex = ("bb", u), ("d1", u), ("ex", u)
                            P.tt(d3, b3, b3[:, :, pm:pm + 1].to_broadcast([64, nch, CS]), ALU.subtract, [kb], [kd1])
                            P.act(ex[u][:, :n], d1[u][:, :n], AF.Exp, [kd1], [kex])
                            P.tt(qt[hd][:, :n], qf[hd][:, :n], ex[u][:, :n], ALU.mult, [("qf", hd), kex], [("qt", hd)])
                            P.act(ex[u][:, :n], d1[u][:, :n], AF.Exp, [kd1], [kex], scale=-1.0)
                            P.tt(ktl[hd][:, :n], kf[hd][:, :n], ex[u][:, :n], ALU.mult, [("kf", hd), kex], [("ktl", hd)])
                            P.act(ex[u][:, :n], bb[u][:, :n], AF.Exp, [kb], [kex])
                            P.tt(qh[hd][:, :n], qf[hd][:, :n], ex[u][:, :n], ALU.mult, [("qf", hd), kex], [("qh", hd)])
                            P.tt(d3, b3, b3[:, :, pe:pe + 1].to_broadcast([64, nch, CS]), ALU.subtract, [kb], [kd1])
                            P.act(ex[u][:, :n], d1[u][:, :n], AF.Exp, [kd1], [kex], scale=-1.0)
                            P.tt(kd[hd][:, :n], kf[hd][:, :n], ex[u][:, :n], ALU.mult, [("kf", hd), kex], [("kd", hd)])
                            P.act(ebend[hd][:, :nch], b3[:, :, pe], AF.Exp, [kb], [("ebend", hd)])
                        else:
                            q3 = qf[hd][:, :n].rearrange("p (c i) -> p c i", i=CS)
                            k3 = kf[hd][:, :n].rearrange("p (c i) -> p c i", i=CS)

                            def tbc(nm, R=R, nch=nch, hd=hd):
                                return R(tb[hd][nm][:]).unsqueeze(1).to_broadcast([64, nch, CS])
                            P.tt(qt[hd][:, :n].rearrange("p (c i) -> p c i", i=CS), q3, tbc("q"), ALU.mult, [("qf", hd), ("rtq", hd)], [("qt", hd)])
                            P.tt(ktl[hd][:, :n].rearrange("p (c i) -> p c i", i=CS), k3, tbc("k"), ALU.mult, [("kf", hd), ("rtk", hd)], [("ktl", hd)])
                            P.tt(qh[hd][:, :n].rearrange("p (c i) -> p c i", i=CS), q3, tbc("e"), ALU.mult, [("qf", hd), ("rte", hd)], [("qh", hd)])
                            P.tt(kd[hd][:, :n].rearrange("p (c i) -> p c i", i=CS), k3, tbc("d"), ALU.mult, [("kf", hd), ("rtd", hd)], [("kd", hd)])
                        for a in range(n // CS):
                            pc = (a % 4) * 64
                            P.mm(PT[0:64, pc:pc + 64], kd[hd][:, a * CS:a * CS + 64], self.identb[0:64, 0:64], True, True,
                                 [("kd", hd), "identb"], ["PT"])
                            if a % 4 == 3 or a == n // CS - 1:
                                a0 = a - (a % 4)
                                na = a - a0 + 1
                                P.copy(kdT[hd][0:CS, a0:a0 + na, :], PT[0:CS, 0:na * 64].rearrange("p (a k) -> p a k", k=64), ["PT"],
                                       [("kdT", hd)], eng="scalar")
                    corder = list(range(nch)) if d == 0 else list(range(nch - 1, -1, -1))
                    for c in corder:
                        sl = nstep % 2
                        nstep += 1
                        cs = slice(c * CS, (c + 1) * CS)
                        for hd in range(NH):
                            lk = ("LB", hd)
                            PAr = LB[hd][0:64, sl * 192:sl * 192 + CS]
                            POr = LB[hd][0:64, sl * 192 + 64:sl * 192 + 64 + CS]
                            PSr = LB[hd][0:64, 384:448]
                            A = Asb[hd][d][sl]
                            ak = ("Asb", hd, d, sl)
                            P.mm(PAr, ktl[hd][:, c * CS:c * CS + 64], qt[hd][:, cs], True, True, [("ktl", hd), ("qt", hd)], [lk])
                            P.op("vector", (lambda e, A=A, PAr=PAr, mask=mask: e.copy_predicated(A[0:CS, :], mask, PAr[0:CS, :])),
                                 [lk, "cmask", ak], [ak])
                            P.mm(POr, vv[hd][0:CS, c, :], A[0:CS, :], True, False, [("vv", hd), ak], [lk])
                            P.mm(POr, Sb[hd][:, :], qh[hd][:, cs], False, True, [("Sb", hd), ("qh", hd)], [lk])
                            if d == 0:
                                P.copy(ob[hd][:, cs], POr, [lk], [("obk", hd)], eng="scalar")
                            else:
                                P.tt(ob[hd][:, cs], POr, oa[hd][:, cs], ALU.add, [lk, ("oak", hd)], [("obk", hd)])
                            P.mm(PSr, kdT[hd][0:CS, c, :], vv[hd][0:CS, c, :], True, True, [("kdT", hd), ("vv", hd)], [lk])
                            esc = ebend[hd][:, c:c + 1] if which == 0 else ebr[hd][:, 0:1]
                            P.stt(S[hd][:], S[hd][:], esc, PSr, ALU.mult, ALU.add,
                                  [("S", hd), ("ebend", hd) if which == 0 else ("ebr", hd), lk], [("S", hd)])
                            P.copy(Sb[hd][:], S[hd][:], [("S", hd)], [("Sb", hd)], eng="scalar")
                    for hd in range(NH):
                        u = hd % 2
                        if d == 0:
                            P.dma("sync", self.OA[which, hd * 64:(hd + 1) * 64, t0:t0 + n], ob[hd][:, :n], reads=[("obk", hd)],
                                  writes=[("OA", blk, hd)])
                        else:
                            gr = goff + hd * 64
                            P.dma("sync", gsb[u][:, :n], PJ[gr:gr + 64, t0:t0 + n], writes=[("gsb", u)])
                            P.act(osq[u][:, :n], ob[hd][:, :n], AF.Square, [("obk", hd)], [("osq", u)])
                            P.mm(PN[0:64, :n], self.onesb[0:64, 0:64], osq[u][:, :n], True, True, [("osq", u), "onesb"], ["PN"])
                            P.act(rs_[u][:, :n], PN[0:64, :n], AF.Sqrt, ["PN"], [("rs_", u)], bias=self.epsc[0:64, 0:1], scale=1.0 / 64)
                            P.recip(rs_[u][:, :n], rs_[u][:, :n], [("rs_", u)], [("rs_", u)])
                            P.tt(rs_[u][:, :n], rs_[u][:, :n], ob[hd][:, :n], ALU.mult, [("rs_", u), ("obk", hd)], [("rs_", u)])
                            P.tt(yo[u][:, :n], rs_[u][:, :n], gsb[u][:, :n], ALU.mult, [("rs_", u), ("gsb", u)], [("yo", u)])
                            yr = yrow0 + hd * 64
                            P.dma("sync", self.YB[yr:yr + 64, t0:t0 + n], yo[u][:, :n], reads=[("yo", u)], writes=[("YBg", blk, hd)])

    def phase_merge(self, l):
        P, nc = self.P, self.nc
        T, L = self.T, self.L
        XTv = self.XT.rearrange("(ft p) t -> p ft t", p=128)
        YBv = self.YB.rearrange("(ft p) t -> p ft t", p=128)
        hTv = self.hT_scr.rearrange("(ft p) t -> p ft t", p=128)
        self.h2T_scr = self.scr.get("h2T") or self.scratch("h2T", [D, T], BF16)
        self.GTT = self.scr.get("GTT") or self.scratch("GTT", [NE, T], F32)
        h2v = self.h2T_scr.rearrange("(ft p) t -> p ft t", p=128)
        w_in = self.din["w_in"][l].rearrange("(kt p) f -> p kt f", p=128)
        with ExitStack() as st:
            nb = self.alloc_norm_bufs(st)
            gz = P.sb(st, [128, 8, 3072], BF16, "gz")
            for j in range(3):
                P.dma("gpsimd", gz[:, :, j * 1024:(j + 1) * 1024], w_in[:, :, O_GZ + j * 1024:O_GZ + (j + 1) * 1024], writes=[("gz", j)])
            wbr = P.sb(st, [128, 8, 1024], BF16, "wbr")
            P.dma("gpsimd", wbr[:, 0:2, :], self.din["w_branch_s5"][l].rearrange("(kt p) f -> p kt f", p=128), writes=[("wbr", 0)])
            P.dma("gpsimd", wbr[:, 2:5, :], self.din["w_branch_hgrn"][l].rearrange("(kt p) f -> p kt f", p=128), writes=[("wbr", 1)])
            P.dma("gpsimd", wbr[:, 5:8, :], self.din["w_branch_ret"][l].rearrange("(kt p) f -> p kt f", p=128), writes=[("wbr", 2)])
            wout = P.sb(st, [128, 8, 1024], BF16, "wout")
            P.dma("gpsimd", wout[:], self.din["w_out"][l].rearrange("(kt p) f -> p kt f", p=128), writes=["wout"])
            wr = P.sb(st, [128, 8, 36], F32, "wr")
            P.dma("sync", wr[:, :, 0:4], self.din["moe_w_group"][l].rearrange("(kt p) e -> p kt e", p=128), writes=[("wr", 0)])
            P.dma("sync", wr[:, :, 4:36], self.din["moe_w_expert"][l].rearrange("(kt p) e -> p kt e", p=128), writes=[("wr", 1)])
            brow = P.sb(st, [128, 36], F32, "brow")
            P.dma("sync", brow[:, 0:4], self.din["moe_b_group"][l:l + 1, :].partition_broadcast(128), writes=[("brow", 0)])
            P.dma("sync", brow[:, 4:36], self.din["moe_b_expert"][l:l + 1, :].partition_broadcast(128), writes=[("brow", 1)])
            hT = P.sb(st, [128, 8, 512], BF16, "mhT")
            yb = P.sb(st, [128, 8, 512], BF16, "myb")
            xt = P.sb(st, [128, 8, 512], F32, "mxt")
            sig = P.sb(st, [128, 3, 512], F32, "msig")
            mt = P.sb(st, [128, 512], F32, "mmt")
            mt2 = P.sb(st, [128, 512], F32, "mmt2")
            mg = P.sb(st, [128, 8, 512], BF16, "mmg")
            h2f = P.sb(st, [128, 8, 512], F32, "h2f")
            h2b = P.sb(st, [128, 8, 512], BF16, "h2b")
            pgt = [P.ps(st, [128, 512], F32, "pgt") for _ in range(3)]
            pbt = [P.ps(st, [128, 512], F32, "pbt") for _ in range(2)]
            px = P.ps(st, [128, 512], F32, "px")
            prt = P.ps(st, [128, 512], F32, "prt")
            rt = {n: P.sb(st, [128, w], F32, "r_" + n) for n, w in
                  (("l36", 36), ("gmax", 1), ("eg", 4), ("gsum", 1), ("og", 4), ("pen", 4), ("lem", 32), ("m1", 1), ("oh1", 32),
                   ("lem2", 32), ("m2", 1), ("oh2", 32), ("r", 1), ("w1", 1), ("w2", 1), ("G", 64))}
            P.memset(rt["G"][:], 0.0, ["G"])
            gts = P.sb(st, [32, 128], F32, "gts")
            ident = self.cst[:, C_IDENT:C_IDENT + 128]
            ktr = ((0, 2), (2, 5), (5, 8))
            nbr = 0
            for bi, (t0, n) in enumerate(self.blocks):
                who = 1 if bi == 0 else 0
                P.dma("sync", hT[:, :, :n], hTv[:, :, t0:t0 + n], writes=["mhT"])
                P.dma("sync", yb[:, :, :n], YBv[:, :, t0:t0 + n], writes=["myb"])
                P.dma("sync", xt[:, :, :n], XTv[:, :, t0:t0 + n], writes=["mxt"])
                for ft in range(8):
                    fs = slice(ft * 128, (ft + 1) * 128)
                    for j in range(3):
                        for kt in range(8):
                            P.mm(pgt[j][:, :n], gz[:, kt, j * 1024 + ft * 128:j * 1024 + (ft + 1) * 128], hT[:, kt, :n], kt == 0, kt == 7,
                                 [("gz", j), "mhT"], [("pgt", j)])
                        P.act(sig[:, j, :n], pgt[j][:, :n], AF.Sigmoid, [("pgt", j)], [("msig", j)])
                    for j in range(3):
                        pb = nbr % 2
                        nbr += 1
                        k0, k1 = ktr[j]
                        for kt in range(k0, k1):
                            P.mm(pbt[pb][:, :n], wbr[:, kt, fs], yb[:, kt, :n], kt == k0, kt == k1 - 1, [("wbr", j), "myb"], [("pbt", pb)])
                        if j == 0:
                            P.tt(mt[:, :n], pbt[pb][:, :n], sig[:, j, :n], ALU.mult, [("pbt", pb), ("msig", j)], ["mmt"])
                        else:
                            P.tt(mt2[:, :n], pbt[pb][:, :n], sig[:, j, :n], ALU.mult, [("pbt", pb), ("msig", j)], ["mmt2"])
                            if j == 1:
                                P.tt(mt[:, :n], mt[:, :n], mt2[:, :n], ALU.add, ["mmt", "mmt2"], ["mmt"])
                            else:
                                P.tt(mg[:, ft, :n], mt[:, :n], mt2[:, :n], ALU.add, ["mmt", "mmt2"], [("mmg", ft)])
                mk = [("mmg", ft) for ft in range(8)]
                for ft in range(8):
                    for kt in range(8):
                        P.mm(px[:, :n], wout[:, kt, ft * 128:(ft + 1) * 128], mg[:, kt, :n], kt == 0, kt == 7, ["wout"] + mk, ["px"])
                    P.stt(xt[:, ft, :n], px[:, :n], self.mod[:, 2, ft, who:who + 1], xt[:, ft, :n], ALU.mult, ALU.add,
                          ["px", ("mod", 2, who), "mxt"], ["mxt"])
                P.dma("sync", XTv[:, :, t0:t0 + n], xt[:, :, :n], reads=["mxt"], writes=[("XTw", bi)])
                self.norm_block(nb, xt, "norm2_gs", 3, who, h2f, ["mxt"], "h2f", n)
                P.copy(h2b[:, :, :n], h2f[:, :, :n], ["h2f"], ["h2b"], eng="gpsimd")
                P.dma("sync", h2v[:, :, t0:t0 + n], h2b[:, :, :n], reads=["h2b"], writes=[("h2T", bi)])
                for sblk in range(n // 128):
                    ss = slice(sblk * 128, (sblk + 1) * 128)
                    for kt in range(8):
                        P.mm(prt[:, 0:36], h2f[:, kt, ss], wr[:, kt, :], kt == 0, kt == 7, ["h2f", ("wr", 0), ("wr", 1)], ["prt"])
                    r_ = rt
                    P.tt(r_["l36"][:], prt[:, 0:36], brow[:], ALU.add, ["prt", ("brow", 0), ("brow", 1)], ["l36"])
                    lg4 = r_["l36"][:, 0:4]
                    le = r_["l36"][:, 4:36]
                    P.op("vector", (lambda e, o=r_["gmax"][:], i=lg4: e.tensor_reduce(o, i, AX.X, ALU.max)), ["l36"], ["gmax"])
                    P.ts(r_["eg"][:], lg4, r_["gmax"][:, 0:1], None, ALU.subtract, None, ["l36", "gmax"], ["eg"])
                    P.act(r_["eg"][:], r_["eg"][:], AF.Exp, ["eg"], ["eg"])
                    P.op("vector", (lambda e, o=r_["gsum"][:], i=r_["eg"][:]: e.tensor_reduce(o, i, AX.X, ALU.add)), ["eg"], ["gsum"])
                    P.recip(r_["gsum"][:], r_["gsum"][:], ["gsum"], ["gsum"])
                    P.ts(r_["og"][:], lg4, r_["gmax"][:, 0:1], None, ALU.is_ge, None, ["l36", "gmax"], ["og"])
                    P.ts(r_["pen"][:], r_["og"][:], -1.0, 1.0e4, ALU.add, ALU.mult, ["og"], ["pen"])
                    P.tt(r_["lem"][:].rearrange("p (g j) -> p g j", g=4), le.rearrange("p (g j) -> p g j", g=4),
                         r_["pen"][:].unsqueeze(2).to_broadcast([128, 4, 8]), ALU.add, ["l36", "pen"], ["lem"])
                    P.op("vector", (lambda e, o=r_["m1"][:], i=r_["lem"][:]: e.tensor_reduce(o, i, AX.X, ALU.max)), ["lem"], ["m1"])
                    P.ts(r_["oh1"][:], r_["lem"][:], r_["m1"][:, 0:1], None, ALU.is_ge, None, ["lem", "m1"], ["oh1"])
                    P.stt(r_["lem2"][:], r_["oh1"][:], -1.0e4, r_["lem"][:], ALU.mult, ALU.add, ["oh1", "lem"], ["lem2"])
                    P.op("vector", (lambda e, o=r_["m2"][:], i=r_["lem2"][:]: e.tensor_reduce(o, i, AX.X, ALU.max)), ["lem2"], ["m2"])
                    P.ts(r_["oh2"][:], r_["lem2"][:], r_["m2"][:, 0:1], None, ALU.is_ge, None, ["lem2", "m2"], ["oh2"])
                    P.tt(r_["r"][:], r_["m2"][:], r_["m1"][:], ALU.subtract, ["m1", "m2"], ["r"])
                    P.act(r_["r"][:], r_["r"][:], AF.Exp, ["r"], ["r"])
                    P.ts(r_["w1"][:], r_["r"][:], 1.0, None, ALU.add, None, ["r"], ["w1"])
                    P.recip(r_["w1"][:], r_["w1"][:], ["w1"], ["w1"])
                    P.tt(r_["w1"][:], r_["w1"][:], r_["gsum"][:], ALU.mult, ["w1", "gsum"], ["w1"])
                    P.tt(r_["w2"][:], r_["w1"][:], r_["r"][:], ALU.mult, ["w1", "r"], ["w2"])
                    P.ts(r_["G"][:, 0:32], r_["oh1"][:], r_["w1"][:, 0:1], None, ALU.mult, None, ["oh1", "w1"], ["G"])
                    P.stt(r_["G"][:, 0:32], r_["oh2"][:], r_["w2"][:, 0:1], r_["G"][:, 0:32], ALU.mult, ALU.add, ["oh2", "w2", "G"], ["G"])
                    P.mm(prt[0:64, 128:256], r_["G"][:], ident, True, True, ["G", "cst"], ["prt"])
                    P.copy(gts[:], prt[0:32, 128:256], ["prt"], ["gts"], eng="scalar")
                    c0 = t0 + sblk * 128
                    P.dma("sync", self.GTT[:, c0:c0 + 128], gts[:], reads=["gts"], writes=[("GTT", c0)])

    def phase_moe(self, l):
        P, nc = self.P, self.nc
        T, L = self.T, self.L
        XTv = self.XT.rearrange("(ft p) t -> p ft t", p=128)
        h2v = self.h2T_scr.rearrange("(ft p) t -> p ft t", p=128)
        groups, cur, tot = [], [], 0
        for b in self.blocks:
            if tot + b[1] > 1536:
                groups.append(cur)
                cur, tot = [], 0
            cur.append(b)
            tot += b[1]
        groups.append(cur)
        with ExitStack() as st:
            h2 = P.sb(st, [128, 8, 1536], BF16, "eh2")
            acc = P.sb(st, [128, 8, 1536], F32, "eacc")
            grep = [P.sb(st, [128, 1536], F32, "egrep") for _ in range(2)]
            wg = [P.sb(st, [128, 8, 512], BF16, "ewg") for _ in range(2)]
            wu = [P.sb(st, [128, 8, 512], BF16, "ewu") for _ in range(2)]
            wd = [P.sb(st, [128, 4, 1024], BF16, "ewd") for _ in range(2)]
            sg = [P.sb(st, [128, 512], F32, "esg") for _ in range(2)]
            hu = [P.sb(st, [128, 512], F32, "ehu") for _ in range(2)]
            hid = P.sb(st, [128, 4, 512], BF16, "ehid")
            xt = P.sb(st, [128, 8, 512], F32, "ext")
            pg = [P.ps(st, [128, 512], F32, "epg") for _ in range(2)]
            pu = [P.ps(st, [128, 512], F32, "epu") for _ in range(2)]
            pd = [P.ps(st, [128, 2, 512], F32, "epd") for _ in range(2)]
            nj = 0
            nf = 0
            for grp in groups:
                g0 = grp[0][0]
                ng = sum(b[1] for b in grp)
                P.dma("sync", h2[:, :, :ng], h2v[:, :, g0:g0 + ng], writes=["eh2"])
                P.memset(acc[:], 0.0, ["eacc"])
                for e in range(NE):
                    s_ = e % 2
                    P.dma("gpsimd", wg[s_][:], self.din["moe_w_gate"][l, e].rearrange("(kt p) f -> p kt f", p=128), writes=[("ewg", s_)])
                    P.dma("gpsimd", wu[s_][:], self.din["moe_w_up"][l, e].rearrange("(kt p) f -> p kt f", p=128), writes=[("ewu", s_)])
                    P.dma("gpsimd", wd[s_][:], self.din["moe_w_down"][l, e].rearrange("(jt p) f -> p jt f", p=128), writes=[("ewd", s_)])
                    P.dma("sync", grep[s_][:, :ng], self.GTT[e:e + 1, g0:g0 + ng].partition_broadcast(128), writes=[("egrep", s_)])
                    for (t0, n) in grp:
                        o = t0 - g0
                        for jt in range(4):
                            u = nj % 2
                            nj += 1
                            for kt in range(8):
                                P.mm(pg[u][:, :n], wg[s_][:, kt, jt * 128:(jt + 1) * 128], h2[:, kt, o:o + n], kt == 0, kt == 7,
                                     [("ewg", s_), "eh2"], [("epg", u)])
                            for kt in range(8):
                                P.mm(pu[u][:, :n], wu[s_][:, kt, jt * 128:(jt + 1) * 128], h2[:, kt, o:o + n], kt == 0, kt == 7,
                                     [("ewu", s_), "eh2"], [("epu", u)])
                            P.act(sg[u][:, :n], pg[u][:, :n], AF.Silu, [("epg", u)], [("esg", u)])
                            P.tt(hu[u][:, :n], pu[u][:, :n], sg[u][:, :n], ALU.mult, [("epu", u), ("esg", u)], [("ehu", u)])
                            P.tt(hid[:, jt, :n], hu[u][:, :n], grep[s_][:, o:o + n], ALU.mult, [("ehu", u), ("egrep", s_)], [("ehid", jt)],
                                 eng="gpsimd")
                        hk = [("ehid", jt) for jt in range(4)]
                        for fp in range(4):
                            u = nf % 2
                            nf += 1
                            for fq in range(2):
                                ft = fp * 2 + fq
                                for jt in range(4):
                                    P.mm(pd[u][:, fq, :n], wd[s_][:, jt, ft * 128:(ft + 1) * 128], hid[:, jt, :n], jt == 0, jt == 3,
                                         [("ewd", s_)] + hk, [("epd", u, fq)])
                            P.tt(acc[:, fp * 2:(fp + 1) * 2, o:o + n], acc[:, fp * 2:(fp + 1) * 2, o:o + n], pd[u][:, :, :n], ALU.add,
                                 ["eacc", ("epd", u, 0), ("epd", u, 1)], ["eacc"])
                for (t0, n) in grp:
                    o = t0 - g0
                    who = 1 if t0 == 0 else 0
                    P.dma("sync", xt[:, :, :n], XTv[:, :, t0:t0 + n], writes=["ext"])
                    for ft in range(8):
                        P.stt(xt[:, ft, :n], acc[:, ft, o:o + n], self.mod[:, 5, ft, who:who + 1], xt[:, ft, :n], ALU.mult, ALU.add,
                              ["eacc", ("mod", 5, who), "ext"], ["ext"])
                    P.dma("sync", XTv[:, :, t0:t0 + n], xt[:, :, :n], reads=["ext"], writes=[("XTe", t0)])


_CACHE = {}


def kernel(**inputs):
    L = inputs["x"].shape[1]
    B = inputs["x"].shape[0]
    depth = inputs["w_in"].shape[0]
    shapes = {n: tuple(inputs[n].shape) for n in PARAM_NAMES}
    shapes["c"] = (D,)
    key = (L, depth)
    if key not in _CACHE:
        _CACHE[key] = Builder(L, depth, shapes).build()
    nc = _CACHE[key]
    cst, cmask, pos = host_consts(L)
    shared = {n: np.ascontiguousarray(np.asarray(inputs[n], dtype=np.float32)) for n in PARAM_NAMES if n != "c"}
    shared["cst"] = cst
    shared["cmask"] = cmask
    shared["pos"] = pos
    in_maps = []
    for b in range(B):
        m = dict(shared)
        m["x"] = np.ascontiguousarray(np.asarray(inputs["x"][b], dtype=np.float32))
        m["ctx"] = np.ascontiguousarray(np.asarray(inputs["ctx"][b], dtype=np.float32))
        m["c"] = np.ascontiguousarray(np.asarray(inputs["c"][b], dtype=np.float32))
        in_maps.append(m)
    res = run_bass_kernel_spmd(nc, in_maps, core_ids=list(range(B)))
    out = np.stack([np.asarray(r["out"], dtype=np.float32) for r in res.results], axis=0)
    return out
```

```python
import math
import numpy as np
from contextlib import ExitStack
import concourse.bass as bass
import concourse.mybir as mybir
from concourse.bass_utils import run_bass_kernel_spmd

F32 = mybir.dt.float32
BF16 = mybir.dt.bfloat16
I32 = mybir.dt.int32
AF = mybir.ActivationFunctionType
ALU = mybir.AluOpType
AX = mybir.AxisListType

COMPUTE = ("tensor", "vector", "scalar", "gpsimd")
ALLENG = ("tensor", "vector", "scalar", "gpsimd", "sync")
NDMASEM = 6

D = 1024
NCTX = 256
IN_SIZES = (256, 384, 384, 384, 384, 384, 384, 384, 384, 384, 3072)
IN_OFF = [0]
for _s in IN_SIZES:
    IN_OFF.append(IN_OFF[-1] + _s)
(O_U, O_HQ, O_HFF, O_HFB, O_HV, O_HG, O_RQ, O_RK, O_RV, O_RG, O_GZ) = IN_OFF[:11]
IN_WIDTH = IN_OFF[-1]
NE = 32
EH = 512
EPS = 1e-6


class Prog:
    def __init__(self, nc, stack):
        self.nc = nc
        self.stack = stack
        self.ops = {e: [] for e in ALLENG}
        self.sem = {}
        self.cnt = {}
        for e in COMPUTE:
            self.sem[e] = stack.enter_context(nc.semaphore("s_" + e))
            self.cnt[e] = 0
        self.dsem = {}
        self.dcnt = {}
        self.dnext = {}
        for q in ("sync", "gpsimd"):
            self.dsem[q] = [stack.enter_context(nc.semaphore("d_%s%d" % (q, i))) for i in range(NDMASEM)]
            self.dcnt[q] = [0] * NDMASEM
            self.dnext[q] = 0
        self.semid = {}
        self.last_write = {}
        self.readers = {}
        self.seen = {e: {} for e in ALLENG}
        self.nalloc = 0
        self.out_tokens = []

    def sb(self, stack, shape, dtype=F32, name=None):
        self.nalloc += 1
        name = (name or "t") + "_%d" % self.nalloc
        return stack.enter_context(self.nc.sbuf_tensor(name, list(shape), dtype))

    def ps(self, stack, shape, dtype=F32, name=None):
        self.nalloc += 1
        name = (name or "p") + "_%d" % self.nalloc
        return stack.enter_context(self.nc.psum_tensor(name, list(shape), dtype))

    def _deps(self, eng, reads, writes):
        need = {}

        def add(tok):
            s, v = tok
            k = id(s)
            self.semid[k] = s
            if need.get(k, 0) < v:
                need[k] = v

        for k in reads:
            if k in self.last_write:
                add(self.last_write[k])
        for k in writes:
            if k in self.last_write:
                add(self.last_write[k])
            for t in self.readers.get(k, ()):
                add(t)
        waits = []
        for k, v in need.items():
            s = self.semid[k]
            if eng == "tensor" and s is self.sem["tensor"]:
                continue
            if self.seen[eng].get(k, 0) >= v:
                continue
            self.seen[eng][k] = v
            waits.append((s, v))
        return waits

    def _commit(self, tok, reads, writes):
        for k in reads:
            self.readers.setdefault(k, []).append(tok)
        for k in writes:
            self.last_write[k] = tok
            self.readers[k] = []

    def op(self, eng, fn, reads=(), writes=()):
        waits = self._deps(eng, reads, writes)
        self.cnt[eng] += 1
        tok = (self.sem[eng], self.cnt[eng])
        self.ops[eng].append((fn, waits, self.sem[eng], 1))
        self._commit(tok, reads, writes)
        return tok

    def dma(self, q, out, in_, reads=(), writes=(), is_output=False):
        i = self.dnext[q]
        self.dnext[q] = (i + 1) % NDMASEM
        s = self.dsem[q][i]
        waits = self._deps(q, reads, writes)
        prev = self.dcnt[q][i]
        k = id(s)
        self.semid[k] = s
        if prev > 0 and self.seen[q].get(k, 0) < prev:
            self.seen[q][k] = prev
            waits.append((s, prev))
        self.dcnt[q][i] = prev + 16
        tok = (s, prev + 16)

        def fn(e, out=out, in_=in_):
            return e.dma_start(out=out, in_=in_)

        self.ops[q].append((fn, waits, s, 16))
        self._commit(tok, reads, writes)
        if is_output:
            self.out_tokens.append(tok)
        return tok

    def barrier(self):
        toks = []
        for e in COMPUTE:
            if self.cnt[e] > 0:
                toks.append((self.sem[e], self.cnt[e]))
        for q in self.dsem:
            for i, s in enumerate(self.dsem[q]):
                if self.dcnt[q][i] > 0:
                    toks.append((s, self.dcnt[q][i]))
        for eng in ALLENG:
            waits = []
            for s, v in toks:
                k = id(s)
                self.semid[k] = s
                if eng in COMPUTE and s is self.sem[eng]:
                    if eng == "tensor":
                        continue
                if self.seen[eng].get(k, 0) >= v:
                    continue
                self.seen[eng][k] = v
                waits.append((s, v))
            if waits:
                self.ops[eng].append((None, waits, None, 0))
        self.last_write = {}
        self.readers = {}

    def emit(self):
        nc = self.nc
        fin = list(self.out_tokens)
        with nc.Block() as block:
            def mk(eng):
                def body(e):
                    for fn, waits, s, inc in self.ops[eng]:
                        for ws, wv in waits:
                            e.wait_ge(ws, wv)
                        if fn is not None:
                            ins = fn(e)
                            ins.then_inc(s, inc)
                    if eng == "sync":
                        for ws, wv in fin:
                            e.wait_ge(ws, wv)
                return body
            block.sync(mk("sync"))
            block.tensor(mk("tensor"))
            block.vector(mk("vector"))
            block.scalar(mk("scalar"))
            block.gpsimd(mk("gpsimd"))

    def mm(self, out, lhsT, rhs, start, stop, reads, writes):
        return self.op("tensor", lambda e: e.matmul(out, lhsT, rhs, start=start, stop=stop), reads, writes)

    def act(self, out, in_, func, reads, writes, bias=None, scale=None):
        kw = {}
        if bias is not None:
            kw["bias"] = bias
        if scale is not None:
            kw["scale"] = scale
        return self.op("scalar", lambda e: e.activation(out, in_, func, **kw), reads, writes)

    def tt(self, out, in0, in1, op, reads, writes, eng="vector"):
        return self.op(eng, lambda e: e.tensor_tensor(out, in0, in1, op), reads, writes)

    def ts(self, out, in0, s1, s2, op0, op1, reads, writes, eng="vector"):
        if s2 is None:
            return self.op(eng, lambda e: e.tensor_scalar(out, in0, s1, None, op0), reads, writes)
        return self.op(eng, lambda e: e.tensor_scalar(out, in0, s1, s2, op0, op1), reads, writes)

    def stt(self, out, in0, scalar, in1, op0, op1, reads, writes):
        return self.op("vector", lambda e: e.scalar_tensor_tensor(out, in0, scalar, in1, op0, op1), reads, writes)

    def copy(self, out, in_, reads, writes, eng="vector"):
        if eng == "scalar":
            return self.op("scalar", lambda e: e.copy(out, in_), reads, writes)
        return self.op(eng, lambda e: e.tensor_copy(out, in_), reads, writes)

    def memset(self, ap, val, writes, eng="vector"):
        return self.op(eng, lambda e: e.memset(ap, val), (), writes)

    def recip(self, out, in_, reads, writes):
        return self.op("vector", lambda e: e.reciprocal(out, in_), reads, writes)


def rev_ap(a):
    ap = [list(d) for d in a.ap]
    n = ap[-1][1]
    st = ap[-1][0]
    ap[-1] = [-st, n]
    return bass.AP(a.tensor, a.offset + st * (n - 1), ap)


C_IDENT = 0
C_RESET = 128
C_IOTA = 640
C_MR = 896
C_MC = 897
C_FIDX = 898
C_SIGN = 899
C_PIDX = 900
C_RESET32 = 904
C_W = 904 + 512

M_FWD = 0
M_BWD = 128
M_BWDS = 256
M32_FWD = 384
M32_BWD = 448
M_W = 512


def host_consts(L):
    c = np.zeros((128, C_W), np.float32)
    c[:, C_IDENT:C_IDENT + 128] = np.eye(128, dtype=np.float32)
    t = np.arange(512)
    c[:, C_RESET:C_RESET + 512] = (t % 64 != 0).astype(np.float32)[None, :]
    c[:, C_IOTA:C_IOTA + 256] = np.arange(256, dtype=np.float32)[None, :]
    c[:, C_RESET32:C_RESET32 + 512] = (t % 32 != 0).astype(np.float32)[None, :]
    p = np.arange(128)
    j = p % 32
    c[:, C_MR] = (j < 16)
    c[:, C_MC] = (j >= 16)
    c[:, C_FIDX] = p % 16
    c[:, C_SIGN] = np.where((p % 64) < 32, -1.0, 1.0)
    c[:, C_PIDX] = p
    m = np.zeros((128, M_W), np.int32)
    s = (p % 64)[:, None]
    tt = np.arange(64)[None, :]
    m[:, M_FWD:M_FWD + 128] = np.tile((s <= tt).astype(np.int32), (1, 2))
    m[:, M_BWD:M_BWD + 128] = np.tile((s >= tt).astype(np.int32), (1, 2))
    m[:, M_BWDS:M_BWDS + 128] = np.tile((s > tt).astype(np.int32), (1, 2))
    s32 = (p % 32)[:, None]
    t32 = np.arange(32)[None, :]
    m[:, M32_FWD:M32_FWD + 64] = np.tile((s32 <= t32).astype(np.int32), (1, 2))
    m[:, M32_BWD:M32_BWD + 64] = np.tile((s32 >= t32).astype(np.int32), (1, 2))
    tl = np.arange(L)
    pos = np.stack([tl // 64, tl % 64]).astype(np.float32)
    return c, m, pos


PARAM_NAMES = ["c", "c_ctx", "w_ada", "b_ada", "norm1_g", "norm2_g", "w_in", "s5_lam_re", "s5_lam_im",
               "s5_log_dt", "s5_b_re", "s5_b_im", "s5_c_re", "s5_c_im", "s5_d", "s5_w_glu", "hgrn_lb_raw",
               "w_branch_s5", "w_branch_hgrn", "w_branch_ret", "w_out", "moe_w_group", "moe_b_group",
               "moe_w_expert", "moe_b_expert", "moe_w_gate", "moe_w_up", "moe_w_down", "final_norm_g"]


class Builder:
    def __init__(self, L, depth, shapes, debug=()):
        self.L = L
        self.T = NCTX + L
        self.depth = depth
        self.debug = set(debug)
        nc = bass.Bass("TRN2", target_bir_lowering=False)
        self.nc = nc
        T = self.T
        self.din = {}
        self.din["x"] = nc.dram_tensor("x", [L, D], F32, kind="ExternalInput").ap()
        self.din["ctx"] = nc.dram_tensor("ctx", [NCTX, D], F32, kind="ExternalInput").ap()
        for n in PARAM_NAMES:
            self.din[n] = nc.dram_tensor(n, list(shapes[n]), F32, kind="ExternalInput").ap()
        self.din["cst"] = nc.dram_tensor("cst", [128, C_W], F32, kind="ExternalInput").ap()
        self.din["cmask"] = nc.dram_tensor("cmask", [128, M_W], I32, kind="ExternalInput").ap()
        self.din["pos"] = nc.dram_tensor("pos", [2, L], F32, kind="ExternalInput").ap()
        self.out = nc.dram_tensor("out", [L, D], F32, kind="ExternalOutput").ap()
        self.scr = {}
        self.blocks = [(0, NCTX)] + [(NCTX + 512 * i, 512) for i in range(L // 512)]
        assert L % 512 == 0

    def dump(self, name, ap, shape, dtype, reads):
        if "dumps" not in self.debug:
            return
        t = self.nc.dram_tensor("dbg_" + name, list(shape), dtype, kind="ExternalOutput").ap()
        self.P.dma("sync", t, ap, reads=reads, writes=["dbg_" + name])

    def scratch(self, name, shape, dtype):
        kind = "ExternalOutput" if name in self.debug else "Internal"
        t = self.nc.dram_tensor("scr_" + name, list(shape), dtype, kind=kind).ap()
        self.scr[name] = t
        return t

    def build(self):
        nc = self.nc
        T, L = self.T, self.L
        with ExitStack() as top, nc.allow_non_contiguous_dma(reason="small strided parameter loads"), \
                nc.allow_low_precision(reason="bf16 matmul operands"):
            P = Prog(nc, top)
            self.P = P
            self.cst = P.sb(top, [128, C_W], F32, "cst")
            self.cmask = P.sb(top, [128, M_W], I32, "cmask")
            P.dma("sync", self.cst[:], self.din["cst"], writes=["cst"])
            P.dma("sync", self.cmask[:], self.din["cmask"], writes=["cmask"])
            self.identb = P.sb(top, [128, 128], BF16, "identb")
            P.copy(self.identb[:], self.cst[:, C_IDENT:C_IDENT + 128], ["cst"], ["identb"])
            self.onesb = P.sb(top, [128, 128], BF16, "onesb")
            P.memset(self.onesb[:], 1.0, ["onesb"])
            self.onesf = P.sb(top, [128, 128], F32, "onesf")
            P.memset(self.onesf[:], 1.0, ["onesf"])
            self.epsc = P.sb(top, [128, 1], F32, "epsc")
            P.memset(self.epsc[:], EPS, ["epsc"])
            self.mod = P.sb(top, [128, 6, 8, 2], F32, "mod")
            self.g1s = P.sb(top, [128, 8, 2], F32, "g1s")
            self.g2s = P.sb(top, [128, 8, 2], F32, "g2s")
            self.XT = self.scratch("XT", [D, T], F32)
            self.PJ = self.scratch("PJ", [O_GZ, T], BF16)
            self.LF = self.scratch("LF", [2, 384, T], F32)
            self.VT = self.scratch("VT", [2, T, 384], BF16)
            self.OA = self.scratch("OA", [2, 384, T], F32)
            self.YB = self.scratch("YB", [D, T], BF16)
            self.GT = self.scratch("GT", [T, NE], F32)
            self.phase_input()
            P.barrier()
            self.phase_rope()
            P.barrier()
            for l in range(self.depth):
                self.l = l
                self.phase_mod(l)
                P.barrier()
                self.phase_proj(l)
                P.barrier()
                if "stop_proj" in self.debug:
                    break
                self.phase_s5(l)
                P.barrier()
                if "stop_s5" in self.debug:
                    break
                if "skip_hg" not in self.debug:
                    self.phase_gla(l, 0)
                    P.barrier()
                if "skip_ret" not in self.debug:
                    self.phase_gla(l, 1)
                    P.barrier()
                if "stop_mix" in self.debug:
                    break
                self.phase_merge(l)
                P.barrier()
                if "stop_merge" in self.debug:
                    break
                self.phase_moe(l)
                P.barrier()
            self.phase_output()
            P.emit()
        return nc

    def phase_input(self):
        P, nc = self.P, self.nc
        with ExitStack() as st:
            ident = self.cst[:, C_IDENT:C_IDENT + 128]
            xin = [P.sb(st, [128, D], F32, "xin") for _ in range(2)]
            xo = [P.sb(st, [128, 8, 128], F32, "xo") for _ in range(2)]
            pt = [P.ps(st, [128, 4, 128], F32, "pt") for _ in range(2)]
            ntile = self.T // 128
            for i in range(ntile):
                b = i % 2
                if i < NCTX // 128:
                    src = self.din["ctx"][i * 128:(i + 1) * 128, :]
                else:
                    j = i - NCTX // 128
                    src = self.din["x"][j * 128:(j + 1) * 128, :]
                P.dma("sync", xin[b][:], src, writes=[("xin", b)])
                for hf in range(2):
                    for q in range(4):
                        ft = hf * 4 + q
                        P.mm(pt[hf][:, q, :], xin[b][:, ft * 128:(ft + 1) * 128], ident, True, True,
                             [("xin", b), "cst"], [("pt", hf, q)])
                    P.copy(xo[b][:, hf * 4:(hf + 1) * 4, :], pt[hf][:], [("pt", hf, q) for q in range(4)],
                           [("xo", b, hf)], eng=("vector" if hf == 0 else "scalar"))
                dst = self.XT.rearrange("(ft p) t -> p ft t", p=128)[:, :, i * 128:(i + 1) * 128]
                P.dma("sync", dst, xo[b][:], reads=[("xo", b, 0), ("xo", b, 1)], writes=[("XT", i)])

    def phase_rope(self):
        P = self.P
        L = self.L
        self.ROPE = self.scratch("ROPE", [2, 128, L], F32)
        with ExitStack() as st:
            ropc = P.sb(st, [128, L], F32, "ropc")
            rops = P.sb(st, [128, L], F32, "rops")
            posr = P.sb(st, [128, L], F32, "posr")
            posc = P.sb(st, [128, L], F32, "posc")
            P.dma("sync", posr[:], self.din["pos"][0:1, :].partition_broadcast(128), writes=["posr"])
            P.dma("sync", posc[:], self.din["pos"][1:2, :].partition_broadcast(128), writes=["posc"])
            invf = P.sb(st, [128, 1], F32, "invf")
            P.act(invf[:], self.cst[:, C_FIDX:C_FIDX + 1], AF.Exp, ["cst"], ["invf"], scale=-math.log(10000.0) / 16.0)
            P.ts(posr[:], posr[:], self.cst[:, C_MR:C_MR + 1], None, ALU.mult, None, ["posr", "cst"], ["posr"])
            P.stt(posr[:], posc[:], self.cst[:, C_MC:C_MC + 1], posr[:], ALU.mult, ALU.add, ["posc", "posr", "cst"], ["posr"])
            P.ts(posr[:], posr[:], invf[:, 0:1], None, ALU.mult, None, ["posr", "invf"], ["posr"])
            self.sincos(st, posr, ropc, rops, L, "posr", "ropc", "rops")
            P.ts(rops[:], rops[:], self.cst[:, C_SIGN:C_SIGN + 1], None, ALU.mult, None, ["rops", "cst"], ["rops"])
            P.dma("sync", self.ROPE[0], ropc[:], reads=["ropc"], writes=["ROPE0"])
            P.dma("sync", self.ROPE[1], rops[:], reads=["rops"], writes=["ROPE1"])

    def phase_mod(self, l):
        P, nc = self.P, self.nc
        with ExitStack() as st:
            cc = P.sb(st, [128, 8, 2], F32, "cc")
            P.dma("sync", cc[:, :, 0], self.din["c"].rearrange("(kt p) -> p kt", p=128), writes=["cc0"])
            P.dma("sync", cc[:, :, 1], self.din["c_ctx"].rearrange("(kt p) -> p kt", p=128), writes=["cc1"])
            sc = P.sb(st, [128, 8, 2], F32, "sc")
            P.act(sc[:], cc[:], AF.Silu, ["cc0", "cc1"], ["sc"])
            bada = P.sb(st, [128, 6, 8], F32, "bada")
            P.dma("sync", bada[:], self.din["b_ada"][l].rearrange("(j ft p) -> p j ft", p=128, ft=8), writes=["bada"])
            wa = [P.sb(st, [128, 8, 1024], F32, "wa") for _ in range(2)]
            pm = P.ps(st, [128, 8, 2], F32, "pm")
            for j in range(6):
                b = j % 2
                src = self.din["w_ada"][l].rearrange("(kt p) f -> p kt f", p=128)[:, :, j * 1024:(j + 1) * 1024]
                P.dma("sync", wa[b][:], src, writes=[("wa", b)])
                for ft in range(8):
                    for kt in range(8):
                        P.mm(pm[:, ft, :], wa[b][:, kt, ft * 128:(ft + 1) * 128], sc[:, kt, :], kt == 0, kt == 7,
                             [("wa", b), "sc"], [("pm", ft)])
                for w in range(2):
                    P.tt(self.mod[:, j, :, w], pm[:, :, w], bada[:, j, :], ALU.add,
                         [("pm", ft) for ft in range(8)] + ["bada"], [("mod", j, w)])
            for (gname, gdst, jsc) in (("norm1_g", self.g1s, 1), ("norm2_g", self.g2s, 4)):
                g = P.sb(st, [128, 8], F32, "g")
                P.dma("sync", g[:], self.din[gname][l].rearrange("(ft p) -> p ft", p=128), writes=[gname])
                for w in range(2):
                    P.stt(gdst[:, :, w], self.mod[:, jsc, :, w], 1.0, g[:], ALU.add, ALU.mult,
                          [("mod", jsc, w), gname], [(gname + "s", w)])

    def norm_block(self, st_bufs, xt, gs, jshift, who, hout, keys_in, key_out, n):
        P = self.P
        sq, pss, rstd, tmp = st_bufs
        P.act(sq[:, :, :n], xt[:, :, :n], AF.Square, keys_in, ["nb_sq"])
        for kt in range(8):
            P.mm(pss[:, :n], self.onesb[:], sq[:, kt, :n], kt == 0, kt == 7, ["nb_sq", "onesb"], ["nb_ps"])
        P.act(rstd[:, :n], pss[:, :n], AF.Sqrt, ["nb_ps"], ["nb_rstd"], bias=self.epsc[:, 0:1], scale=1.0 / D)
        P.recip(rstd[:, :n], rstd[:, :n], ["nb_rstd"], ["nb_rstd"])
        for ft in range(8):
            P.tt(tmp[:, ft, :n], xt[:, ft, :n], rstd[:, :n], ALU.mult, keys_in + ["nb_rstd"], [("nb_tmp", ft)])
            P.act(hout[:, ft, :n], tmp[:, ft, :n], AF.Identity, [("nb_tmp", ft), (gs, who), ("mod", jshift, who)],
                  [key_out], bias=self.mod[:, jshift, ft, who:who + 1],
                  scale=(self.g1s if gs == "norm1_gs" else self.g2s)[:, ft, who:who + 1])

    def alloc_norm_bufs(self, st):
        P = self.P
        sq = P.sb(st, [128, 8, 512], BF16, "nsq")
        pss = P.ps(st, [128, 512], F32, "npss")
        rstd = P.sb(st, [128, 512], F32, "nrstd")
        tmp = P.sb(st, [128, 8, 512], F32, "ntmp")
        return (sq, pss, rstd, tmp)

    def phase_proj(self, l):
        P, nc = self.P, self.nc
        T, L = self.T, self.L
        w_in = self.din["w_in"][l].rearrange("(kt p) f -> p kt f", p=128)
        XTv = self.XT.rearrange("(ft p) t -> p ft t", p=128)
        PJv = self.PJ.rearrange("(ft p) t -> p ft t", p=128)
        self.hT_scr = self.scr.get("hT") or self.scratch("hT", [D, T], BF16)
        hTv = self.hT_scr.rearrange("(ft p) t -> p ft t", p=128)
        with ExitStack() as st:
            nb = self.alloc_norm_bufs(st)
            hT = P.sb(st, [128, 8, T], BF16, "hT")
            xt = [P.sb(st, [128, 8, 512], F32, "xt") for _ in range(2)]
            for bi, (t0, n) in enumerate(self.blocks):
                b = bi % 2
                P.dma("sync", xt[b][:, :, :n], XTv[:, :, t0:t0 + n], reads=["XTall"], writes=[("xt", b)])
                self.norm_block(nb, xt[b], "norm1_gs", 0, 1 if bi == 0 else 0, hT[:, :, t0:t0 + n],
                                [("xt", b)], ("hT", bi), n)
                P.dma("sync", hTv[:, :, t0:t0 + n], hT[:, :, t0:t0 + n], reads=[("hT", bi)], writes=[("hTd", bi)])
            hkeys = [("hT", bi) for bi in range(len(self.blocks))]
            lbr = P.sb(st, [128, 3, 2, 4], F32, "lbr")
            for li in range(4):
                for d in range(2):
                    P.dma("sync", lbr[:, :, d, li], self.din["hgrn_lb_raw"][li, d].rearrange("(ft p) -> p ft", p=128),
                          writes=[("lbr", li, d)])
            lbk = [("lbr", li, d) for li in range(4) for d in range(2)]
            lbe = P.sb(st, [128, 3, 2, 4], F32, "lbe")
            P.act(lbe[:], lbr[:], AF.Exp, lbk, ["lbe"])
            lsum = P.sb(st, [128, 3, 2], F32, "lsum")
            P.op("vector", lambda e: e.tensor_reduce(lsum[:], lbe[:], AX.X, ALU.add), ["lbe"], ["lsum"])
            P.recip(lsum[:], lsum[:], ["lsum"], ["lsum"])
            lb = P.sb(st, [128, 3, 2], F32, "lb")
            oml = P.sb(st, [128, 3, 2], F32, "oml")
            P.memset(lb[:], 0.0, ["lb"])
            for li in range(1, l + 1):
                P.tt(lb[:], lb[:], lbe[:, :, :, li], ALU.add, ["lb", "lbe"], ["lb"])
            P.tt(lb[:], lb[:], lsum[:], ALU.mult, ["lb", "lsum"], ["lb"])
            P.ts(oml[:], lb[:], -1.0, 1.0, ALU.mult, ALU.add, ["lb"], ["oml"])
            rcb = [P.sb(st, [128, 512], F32, "rcb") for _ in range(2)]
            rsb = [P.sb(st, [128, 512], F32, "rsb") for _ in range(2)]
            wb = [P.sb(st, [128, 8, 384], BF16, "wb") for _ in range(3)]
            pp = [P.ps(st, [128, 512], F32, "pp") for _ in range(4)]
            ob = [P.sb(st, [128, 3, 512], BF16, "ob") for _ in range(2)]
            of = [P.sb(st, [128, 3, 512], F32, "of") for _ in range(2)]
            t1 = P.sb(st, [128, 512], F32, "pt1")
            t2 = P.sb(st, [128, 512], F32, "pt2")
            cnt = {"pp": 0, "ob": 0}

            def load_w(slot, c0, ncol, swap=False):
                if not swap:
                    P.dma("gpsimd", wb[slot][:, :, :ncol], w_in[:, :, c0:c0 + ncol], reads=[], writes=[("wb", slot)])
                else:
                    src = w_in[:, :, c0:c0 + ncol].rearrange("p kt (h two j) -> p kt h two j", two=2, j=32)
                    dst = wb[slot][:, :, :ncol].rearrange("p kt (h two j) -> p kt h two j", two=2, j=32)
                    for kt in range(8):
                        P.dma("gpsimd", dst[:, kt, :, 0, :], src[:, kt, :, 1, :], reads=[], writes=[("wb", slot, kt, 0)])
                        P.dma("gpsimd", dst[:, kt, :, 1, :], src[:, kt, :, 0, :], reads=[], writes=[("wb", slot, kt, 1)])

            def wkeys(slot, swap=False):
                if not swap:
                    return [("wb", slot)]
                return [("wb", slot, kt, x) for kt in range(8) for x in range(2)]

            def fm_proj(slot, ft, t0, n, wk):
                i = cnt["pp"] % 4
                cnt["pp"] += 1
                for kt in range(8):
                    P.mm(pp[i][:, :n], wb[slot][:, kt, ft * 128:(ft + 1) * 128], hT[:, kt, t0:t0 + n], kt == 0, kt == 7,
                         wk + hkeys, [("pp", i)])
                return i

            def feature_group(c0, nft, row0, post, extra_w=None):
                load_w(0, c0, nft * 128)
                for bi, (t0, n) in enumerate(self.blocks):
                    o = cnt["ob"] % 2
                    cnt["ob"] += 1
                    for ft in range(nft):
                        i = fm_proj(0, ft, t0, n, wkeys(0))
                        post(i, ft, n, ob[o], o, bi, t0)
                    P.dma("sync", PJv[:, row0 // 128:row0 // 128 + nft, t0:t0 + n], ob[o][:, :nft, :n],
                          reads=[("ob", o, ft) for ft in range(nft)], writes=[("PJ", row0, bi)])

            def post_copy(i, ft, n, obt, o, bi, t0):
                P.copy(obt[:, ft, :n], pp[i][:, :n], [("pp", i)], [("ob", o, ft)], eng="scalar")

            def post_silu(i, ft, n, obt, o, bi, t0):
                P.act(obt[:, ft, :n], pp[i][:, :n], AF.Silu, [("pp", i)], [("ob", o, ft)])

            feature_group(O_U, 2, O_U, post_copy)
            feature_group(O_HQ, 3, O_HQ, post_copy)
            feature_group(O_HG, 3, O_HG, post_silu)
            feature_group(O_RG, 3, O_RG, post_silu)
            LFv = self.LF.rearrange("d (ft p) t -> d p ft t", p=128)
            for d, c0 in ((0, O_HFF), (1, O_HFB)):
                load_w(0, c0, 384)
                for bi, (t0, n) in enumerate(self.blocks):
                    o = cnt["ob"] % 2
                    cnt["ob"] += 1
                    for ft in range(3):
                        i = fm_proj(0, ft, t0, n, wkeys(0))
                        P.act(t1[:, :n], pp[i][:, :n], AF.Sigmoid, [("pp", i)], ["pt1"])
                        P.ts(t1[:, :n], t1[:, :n], oml[:, ft, d:d + 1], lb[:, ft, d:d + 1], ALU.mult, ALU.add,
                             ["pt1", "oml", "lb"], ["pt1"])
                        P.act(of[o][:, ft, :n], t1[:, :n], AF.Ln, ["pt1"], [("of", o, ft)])
                        P.ts(ob[o][:, ft, :n], t1[:, :n], -1.0, 1.0, ALU.mult, ALU.add, ["pt1"], [("ob", o, ft)])
                    P.dma("sync", PJv[:, c0 // 128:c0 // 128 + 3, t0:t0 + n], ob[o][:, :3, :n],
                          reads=[("ob", o, ft) for ft in range(3)], writes=[("PJ", c0, bi)])
                    P.dma("sync", LFv[d][:, :, t0:t0 + n], of[o][:, :3, :n],
                          reads=[("of", o, ft) for ft in range(3)], writes=[("LF", d, bi)])
            for c0, scl in ((O_RQ, 1.0), (O_RK, 0.125)):
                load_w(0, c0, 384)
                load_w(1, c0, 384, swap=True)
                for bi, (t0, n) in enumerate(self.blocks):
                    o = cnt["ob"] % 2
                    cnt["ob"] += 1
                    if bi > 0:
                        l0 = t0 - NCTX
                        P.dma("sync", rcb[bi % 2][:, :n], self.ROPE[0, :, l0:l0 + n], writes=[("rcb", bi % 2)])
                        P.dma("sync", rsb[bi % 2][:, :n], self.ROPE[1, :, l0:l0 + n], writes=[("rsb", bi % 2)])
                    for ft in range(3):
                        i = fm_proj(0, ft, t0, n, wkeys(0))
                        if bi == 0:
                            P.act(ob[o][:, ft, :n], pp[i][:, :n], AF.Identity, [("pp", i)], [("ob", o, ft)], scale=scl)
                        else:
                            i2 = fm_proj(1, ft, t0, n, wkeys(1, True))
                            rb_ = bi % 2
                            P.tt(t1[:, :n], pp[i][:, :n], rcb[rb_][:, :n], ALU.mult, [("pp", i), ("rcb", rb_)], ["pt1"])
                            P.tt(t2[:, :n], pp[i2][:, :n], rsb[rb_][:, :n], ALU.mult, [("pp", i2), ("rsb", rb_)], ["pt2"])
                            P.tt(t1[:, :n], t1[:, :n], t2[:, :n], ALU.add, ["pt1", "pt2"], ["pt1"])
                            P.act(ob[o][:, ft, :n], t1[:, :n], AF.Identity, ["pt1"], [("ob", o, ft)], scale=scl)
                    P.dma("sync", PJv[:, c0 // 128:c0 // 128 + 3, t0:t0 + n], ob[o][:, :3, :n],
                          reads=[("ob", o, ft) for ft in range(3)], writes=[("PJ", c0, bi)])
            vb = [P.sb(st, [128, 384], BF16, "vb") for _ in range(2)]
            for vi, c0 in ((0, O_HV), (1, O_RV)):
                load_w(2, c0, 384)
                for tt_ in range(T // 128):
                    i = cnt["pp"] % 4
                    cnt["pp"] += 1
                    for kt in range(8):
                        P.mm(pp[i][:, :384], hT[:, kt, tt_ * 128:(tt_ + 1) * 128], wb[2][:, kt, :384], kt == 0, kt == 7,
                             [("wb", 2)] + hkeys, [("pp", i)])
                    o = tt_ % 2
                    P.copy(vb[o][:], pp[i][:, :384], [("pp", i)], [("vb", o)], eng=("vector" if o == 0 else "scalar"))
                    P.dma("sync", self.VT[vi, tt_ * 128:(tt_ + 1) * 128, :], vb[o][:], reads=[("vb", o)], writes=[("VT", vi, tt_)])

    def sincos(self, st, ang, cosd, sind, n, akey, kc, ks, bufs=None):
        P = self.P
        if bufs is None:
            ki = P.sb(st, [128, n], I32, "sc_ki")
            kf = P.sb(st, [128, n], F32, "sc_kf")
        else:
            ki, kf = bufs[0][:, :n], bufs[1][:, :n]
        P.ts(kf[:], ang[:, :n], 1.0 / (2 * math.pi), None, ALU.mult, None, [akey], ["sc_kf"])
        P.copy(ki[:], kf[:], ["sc_kf"], ["sc_ki"])
        P.copy(kf[:], ki[:], ["sc_ki"], ["sc_kf"])
        P.stt(ang[:, :n], kf[:], -2 * math.pi, ang[:, :n], ALU.mult, ALU.add, ["sc_kf", akey], [akey])
        P.ts(kf[:], ang[:, :n], math.pi, -2 * math.pi, ALU.is_gt, ALU.mult, [akey], ["sc_kf"])
        P.tt(ang[:, :n], ang[:, :n], kf[:], ALU.add, [akey, "sc_kf"], [akey])
        P.ts(kf[:], ang[:, :n], -math.pi, 2 * math.pi, ALU.is_lt, ALU.mult, [akey], ["sc_kf"])
        P.tt(ang[:, :n], ang[:, :n], kf[:], ALU.add, [akey, "sc_kf"], [akey])
        P.ts(ang[:, :n], ang[:, :n], math.pi, -math.pi, ALU.min, ALU.max, [akey], [akey])
        P.act(sind[:, :n], ang[:, :n], AF.Sin, [akey], [ks])
        P.act(kf[:], ang[:, :n], AF.Abs, [akey], ["sc_kf"])
        P.ts(kf[:], kf[:], -1.0, math.pi / 2, ALU.mult, ALU.add, ["sc_kf"], ["sc_kf"])
        P.act(cosd[:, :n], kf[:], AF.Sin, ["sc_kf"], [kc])

    def phase_output(self):
        P, nc = self.P, self.nc
        T, L = self.T, self.L
        XTv = self.XT.rearrange("(ft p) t -> p ft t", p=128)
        with ExitStack() as st:
            nb = self.alloc_norm_bufs(st)
            sq, pss, rstd, tmp = nb
            ident = self.cst[:, C_IDENT:C_IDENT + 128]
            fg = P.sb(st, [128, 8], F32, "fg")
            P.dma("sync", fg[:], self.din["final_norm_g"].rearrange("(ft p) -> p ft", p=128), writes=["fg"])
            xt = [P.sb(st, [128, 8, 512], F32, "oxt") for _ in range(2)]
            xn = [P.sb(st, [128, 8, 512], F32, "oxn") for _ in range(2)]
            po = [P.ps(st, [128, 4, 128], F32, "opo") for _ in range(2)]
            ot = [P.sb(st, [128, D], F32, "oot") for _ in range(2)]
            k = 0
            for bi, (t0, n) in enumerate(self.blocks):
                if bi == 0:
                    continue
                b = bi % 2
                P.dma("sync", xt[b][:, :, :n], XTv[:, :, t0:t0 + n], reads=["XTall"], writes=[("oxt", b)])
                P.act(sq[:, :, :n], xt[b][:, :, :n], AF.Square, [("oxt", b)], ["nb_sq"])
                for kt in range(8):
                    P.mm(pss[:, :n], self.onesb[:], sq[:, kt, :n], kt == 0, kt == 7, ["nb_sq", "onesb"], ["nb_ps"])
                P.act(rstd[:, :n], pss[:, :n], AF.Sqrt, ["nb_ps"], ["nb_rstd"], bias=self.epsc[:, 0:1], scale=1.0 / D)
                P.recip(rstd[:, :n], rstd[:, :n], ["nb_rstd"], ["nb_rstd"])
                for ft in range(8):
                    P.stt(xn[b][:, ft, :n], xt[b][:, ft, :n], fg[:, ft:ft + 1], rstd[:, :n], ALU.mult, ALU.mult,
                          [("oxt", b), "fg", "nb_rstd"], [("oxn", b, ft)])
                for s in range(n // 128):
                    o = k % 2
                    k += 1
                    for hf in range(2):
                        for q in range(4):
                            ft = hf * 4 + q
                            P.mm(po[hf][:, q, :], xn[b][:, ft, s * 128:(s + 1) * 128], ident, True, True,
                                 [("oxn", b, ft), "cst"], [("opo", hf, q)])
                        P.copy(ot[o][:, hf * 512:(hf + 1) * 512], po[hf][:].rearrange("p a b -> p (a b)"),
                               [("opo", hf, q) for q in range(4)], [("oot", o, hf)], eng=("vector" if hf == 0 else "scalar"))
                    r0 = t0 - NCTX + s * 128
                    P.dma("sync", self.out[r0:r0 + 128, :], ot[o][:], reads=[("oot", o, 0), ("oot", o, 1)],
                          writes=[("out", r0)], is_output=True)


    def phase_s5(self, l):
        P, nc = self.P, self.nc
        T, L = self.T, self.L
        PJv = self.PJ.rearrange("(ft p) t -> p ft t", p=128)
        YBv = self.YB.rearrange("(ft p) t -> p ft t", p=128)
        nblk = T // 256
        with ExitStack() as st:
            uT = P.sb(st, [128, 2, T], BF16, "uT")
            P.dma("sync", uT[:], PJv[:, 0:2, :], writes=["uT"])
            yacc = P.sb(st, [128, 2, T], F32, "yacc")
            Ec = P.sb(st, [128, 8, 256], F32, "Ec")
            Es = P.sb(st, [128, 8, 256], F32, "Es")
            Fc = P.sb(st, [128, 8, 256], F32, "Fc")
            Fs = P.sb(st, [128, 8, 256], F32, "Fs")
            ang = P.sb(st, [128, 8, 256], F32, "ang")
            tA = P.sb(st, [128, 4, 256], F32, "tA")
            tB = P.sb(st, [128, 4, 256], F32, "tB")
            tC = P.sb(st, [128, 4, 256], F32, "tC")
            tD = P.sb(st, [128, 4, 256], F32, "tD")
            xt_ = P.sb(st, [128, 4, 2, 256], F32, "xtl")
            M = P.sb(st, [128, 4, 2, 256], F32, "M")
            hb = P.sb(st, [128, 4, 2, 256], BF16, "hb")
            BD = [P.sb(st, [128, 2, 512], BF16, "BD%d" % c) for c in range(2)]
            CT = [P.sb(st, [128, 8, 128], BF16, "CT%d" % c) for c in range(2)]
            sm = {n: P.sb(st, [128, 8], F32, "s5" + n) for n in
                  ("lr", "li", "dt", "th", "mag", "c", "s", "ar", "ai", "den", "am1", "zr", "zi", "t1", "t2", "a256", "Rc", "Rs")}
            init = [P.sb(st, [128, 8, 2], F32, "init%d" % i) for i in range(2)]
            pbu = P.ps(st, [128, 4, 2, 256], F32, "pbu")
            py = P.ps(st, [128, 256], F32, "py")
            iota = self.cst[:, C_IOTA:C_IOTA + 256]
            scb = (P.sb(st, [128, 2048], I32, "sc_ki"), P.sb(st, [128, 2048], F32, "sc_kf"))
            magf = P.sb(st, [128, 8, 256], F32, "magf")
            for d in range(2):
                P.dma("sync", sm["lr"][:], self.din["s5_lam_re"][l, d].rearrange("g p -> (g p)").rearrange("(nt p) -> p nt", p=128), writes=["lr"])
                P.dma("sync", sm["li"][:], self.din["s5_lam_im"][l, d].rearrange("g p -> (g p)").rearrange("(nt p) -> p nt", p=128), writes=["li"])
                ld = self.din["s5_log_dt"][l, d]
                for half in range(2):
                    src = bass.AP(ld.tensor, ld.offset + half, [[0, 64], [2, 8]])
                    P.dma("sync", sm["dt"][half * 64:(half + 1) * 64, :], src, writes=[("dt", half)])
                P.act(sm["dt"][:], sm["dt"][:], AF.Exp, [("dt", 0), ("dt", 1)], ["dt"])
                P.tt(sm["th"][:], sm["li"][:], sm["dt"][:], ALU.mult, ["li", "dt"], ["th"])
                P.tt(sm["mag"][:], sm["lr"][:], sm["dt"][:], ALU.mult, ["lr", "dt"], ["mag"])
                P.act(sm["mag"][:], sm["mag"][:], AF.Exp, ["mag"], ["mag"])
                P.ts(sm["a256"][:], sm["th"][:], 256.0, None, ALU.mult, None, ["th"], ["a256"])
                P.copy(sm["t1"][:], sm["th"][:], ["th"], ["t1"])
                with ExitStack() as st2:
                    self.sincos(st2, sm["t1"], sm["c"], sm["s"], 8, "t1", "c", "s", scb)
                    self.sincos(st2, sm["a256"], sm["Rc"], sm["Rs"], 8, "a256", "Rc", "Rs", scb)
                    P.tt(sm["ar"][:], sm["mag"][:], sm["c"][:], ALU.mult, ["mag", "c"], ["ar"])
                    P.tt(sm["ai"][:], sm["mag"][:], sm["s"][:], ALU.mult, ["mag", "s"], ["ai"])
                    P.tt(sm["den"][:], sm["lr"][:], sm["lr"][:], ALU.mult, ["lr"], ["den"])
                    P.tt(sm["t2"][:], sm["li"][:], sm["li"][:], ALU.mult, ["li"], ["t2"])
                    P.tt(sm["den"][:], sm["den"][:], sm["t2"][:], ALU.add, ["den", "t2"], ["den"])
                    P.recip(sm["den"][:], sm["den"][:], ["den"], ["den"])
                    P.ts(sm["am1"][:], sm["ar"][:], -1.0, None, ALU.add, None, ["ar"], ["am1"])
                    P.tt(sm["zr"][:], sm["am1"][:], sm["lr"][:], ALU.mult, ["am1", "lr"], ["zr"])
                    P.tt(sm["t2"][:], sm["ai"][:], sm["li"][:], ALU.mult, ["ai", "li"], ["t2"])
                    P.tt(sm["zr"][:], sm["zr"][:], sm["t2"][:], ALU.add, ["zr", "t2"], ["zr"])
                    P.tt(sm["zr"][:], sm["zr"][:], sm["den"][:], ALU.mult, ["zr", "den"], ["zr"])
                    P.tt(sm["zi"][:], sm["ai"][:], sm["lr"][:], ALU.mult, ["ai", "lr"], ["zi"])
                    P.tt(sm["t2"][:], sm["am1"][:], sm["li"][:], ALU.mult, ["am1", "li"], ["t2"])
                    P.tt(sm["zi"][:], sm["zi"][:], sm["t2"][:], ALU.subtract, ["zi", "t2"], ["zi"])
                    P.tt(sm["zi"][:], sm["zi"][:], sm["den"][:], ALU.mult, ["zi", "den"], ["zi"])
                    P.tt(ang[:], iota.unsqueeze(1).to_broadcast([128, 8, 256]), sm["th"][:].unsqueeze(2).to_broadcast([128, 8, 256]),
                         ALU.mult, ["cst", "th"], ["ang"])
                    a2 = ang[:].rearrange("p a b -> p (a b)")
                    self.sincos(st2, ang[:].rearrange("p a b -> p (a b)"), Ec[:].rearrange("p a b -> p (a b)"),
                                Es[:].rearrange("p a b -> p (a b)"), 2048, "ang", "Ec", "Es", scb)
                for nt in range(8):
                    P.ts(magf[:, nt, :], self.onesf[:, 0:128].unsqueeze(1).to_broadcast([128, 2, 128]).rearrange("p a b -> p (a b)") if False else Ec[:, nt, :],
                         0.0, sm["mag"][:, nt:nt + 1], ALU.mult, ALU.add, ["Ec", "mag"], ["magf"])
                zrb = sm["zr"][:].unsqueeze(2).to_broadcast([128, 8, 256])
                zib = sm["zi"][:].unsqueeze(2).to_broadcast([128, 8, 256])
                P.tt(Fc[:], Ec[:], zrb, ALU.mult, ["Ec", "zr"], ["Fc"])
                P.tt(ang[:], Es[:], zib, ALU.mult, ["Es", "zi"], ["ang"])
                P.tt(Fc[:], Fc[:], ang[:], ALU.add, ["Fc", "ang"], ["Fc"])
                P.tt(Fs[:], Ec[:], zib, ALU.mult, ["Ec", "zi"], ["Fs"])
                P.tt(ang[:], Es[:], zrb, ALU.mult, ["Es", "zr"], ["ang"])
                P.tt(Fs[:], Fs[:], ang[:], ALU.subtract, ["Fs", "ang"], ["Fs"])
                for c, nm in ((0, "s5_b_re"), (1, "s5_b_im")):
                    P.memset(BD[c][:], 0.0, [("BD", c)])
                    for g in range(16):
                        kt, gl = g // 8, g % 8
                        P.dma("gpsimd", BD[c][gl * 16:(gl + 1) * 16, kt, gl * 64:(gl + 1) * 64],
                              self.din[nm][l, d, g].rearrange("p h -> h p"), reads=[], writes=[("BD", c)])
                for c, nm in ((0, "s5_c_re"), (1, "s5_c_im")):
                    P.memset(CT[c][:], 0.0, [("CT", c)])
                    for g in range(16):
                        nt, g2, gl = g // 2, g % 2, g % 8
                        P.dma("gpsimd", CT[c][g2 * 64:(g2 + 1) * 64, nt, gl * 16:(gl + 1) * 16],
                              self.din[nm][l, d, g].rearrange("h p -> p h"), reads=[], writes=[("CT", c)])
                P.ts(CT[1][:], CT[1][:], -1.0, None, ALU.mult, None, [("CT", 1)], [("CT", 1)])
                if d == 0:
                    for nm_ in ("th", "mag", "zr", "zi", "Rc", "Rs", "dt", "lr", "li"):
                        self.dump(nm_, sm[nm_][:], [128, 8], F32, [nm_])
                    self.dump("Ec", Ec[:], [128, 8, 256], F32, ["Ec"])
                    self.dump("Es", Es[:], [128, 8, 256], F32, ["Es"])
                    self.dump("Fc", Fc[:], [128, 8, 256], F32, ["Fc"])
                    self.dump("BD0", BD[0][:], [128, 2, 512], BF16, [("BD", 0)])
                    self.dump("CT0", CT[0][:], [128, 8, 128], BF16, [("CT", 0)])
                    self.dump("CT1", CT[1][:], [128, 8, 128], BF16, [("CT", 1)])
                if d == 1 and "YD" in self.debug:
                    YD = self.scratch("YD", [256, T], F32)
                    P.dma("sync", YD.rearrange("(q p) t -> p q t", p=128), yacc[:], reads=[("yacc", q, b2) for q in range(2) for b2 in range(nblk)], writes=["YD"])
                order = list(range(nblk)) if d == 0 else [0] + list(range(nblk - 1, 0, -1))
                P.memset(init[0][:], 0.0, [("init", 0)])
                last = 255 if d == 0 else 0
                R = (lambda a: a) if d == 0 else rev_ap
                for bi, blk in enumerate(order):
                    t0 = blk * 256
                    ii, io = bi % 2, (bi + 1) % 2
                    for q in range(2):
                        for j in range(4):
                            for c in range(2):
                                P.mm(pbu[:, j, c, :], BD[c][:, q, j * 128:(j + 1) * 128], uT[:, q, t0:t0 + 256], True, True,
                                     [("BD", c), "uT"], [("pbu", j, c)])
                        pk = [("pbu", j, c) for j in range(4) for c in range(2)]
                        fc = R(Fc[:, 4 * q:4 * q + 4, :])
                        fs = R(Fs[:, 4 * q:4 * q + 4, :])
                        ec = R(Ec[:, 4 * q:4 * q + 4, :])
                        es = R(Es[:, 4 * q:4 * q + 4, :])
                        P.tt(tA[:], pbu[:, :, 0, :], fc, ALU.mult, pk + ["Fc"], ["tA"])
                        P.tt(tB[:], pbu[:, :, 1, :], fs, ALU.mult, pk + ["Fs"], ["tB"])
                        P.tt(xt_[:, :, 0, :], tA[:], tB[:], ALU.subtract, ["tA", "tB"], [("xtl", 0)])
                        P.tt(tA[:], pbu[:, :, 1, :], fc, ALU.mult, pk + ["Fc"], ["tA"])
                        P.tt(tB[:], pbu[:, :, 0, :], fs, ALU.mult, pk + ["Fs"], ["tB"])
                        P.tt(xt_[:, :, 1, :], tA[:], tB[:], ALU.add, ["tA", "tB"], [("xtl", 1)])
                        for j in range(4):
                            nt = 4 * q + j
                            for c in range(2):
                                def f(e, j=j, c=c, nt=nt, ii=ii, R=R):
                                    return e.tensor_tensor_scan(R(M[:, j, c, :]), magf[:, nt, :],
                                                                R(xt_[:, j, c, :]), init[ii][:, nt, c:c + 1], ALU.mult, ALU.add)
                                P.op("vector", f, [("xtl", c), "magf", ("init", ii)], [("M", c)])
                        rc = sm["Rc"][:, 4 * q:4 * q + 4]
                        rs = sm["Rs"][:, 4 * q:4 * q + 4]
                        mre = M[:, :, 0, last]
                        mim = M[:, :, 1, last]
                        t1 = sm["t1"][:, 0:4]
                        t2 = sm["t2"][:, 0:4]
                        P.tt(t1, mre, rc, ALU.mult, [("M", 0), "Rc"], ["t1"])
                        P.tt(t2, mim, rs, ALU.mult, [("M", 1), "Rs"], ["t2"])
                        P.tt(init[io][:, 4 * q:4 * q + 4, 0], t1, t2, ALU.subtract, ["t1", "t2"], [("init", io)])
                        P.tt(t1, mim, rc, ALU.mult, [("M", 1), "Rc"], ["t1"])
                        P.tt(t2, mre, rs, ALU.mult, [("M", 0), "Rs"], ["t2"])
                        P.tt(init[io][:, 4 * q:4 * q + 4, 1], t1, t2, ALU.add, ["t1", "t2"], [("init", io)])
                        G_ = "vector"
                        P.tt(tC[:], M[:, :, 0, :], ec, ALU.mult, [("M", 0), "Ec"], ["tC"], eng=G_)
                        P.tt(tD[:], M[:, :, 1, :], es, ALU.mult, [("M", 1), "Es"], ["tD"], eng=G_)
                        P.tt(hb[:, :, 0, :], tC[:], tD[:], ALU.subtract, ["tC", "tD"], [("hb", 0)], eng=G_)
                        P.tt(tC[:], M[:, :, 1, :], ec, ALU.mult, [("M", 1), "Ec"], ["tC"], eng=G_)
                        P.tt(tD[:], M[:, :, 0, :], es, ALU.mult, [("M", 0), "Es"], ["tD"], eng=G_)
                        P.tt(hb[:, :, 1, :], tC[:], tD[:], ALU.add, ["tC", "tD"], [("hb", 1)], eng=G_)
                        k = 0
                        for j in range(4):
                            for c in range(2):
                                P.mm(py[:], CT[c][:, 4 * q + j, :], hb[:, j, c, :], k == 0, k == 7,
                                     [("CT", c), ("hb", c)], ["py"])
                                k += 1
                        if d == 0:
                            P.copy(yacc[:, q, t0:t0 + 256], py[:], ["py"], [("yacc", q, blk)], eng="scalar")
                            if bi == 0 and q == 0:
                                self.dump("pbu", xt_[:], [128, 4, 2, 256], F32, [("xtl", 0), ("xtl", 1)])
                                self.dump("M", M[:], [128, 4, 2, 256], F32, [("M", 0), ("M", 1)])
                                self.dump("hb", hb[:], [128, 4, 2, 256], BF16, [("hb", 0), ("hb", 1)])
                        else:
                            P.tt(yacc[:, q, t0:t0 + 256], yacc[:, q, t0:t0 + 256], py[:], ALU.add, ["py", ("yacc", q, blk)], [("yacc", q, blk)])
            if False:
                YD = self.scratch("YD", [256, T], F32)
                P.dma("sync", YD.rearrange("(q p) t -> p q t", p=128), yacc[:], reads=[("yacc", q, b2) for q in range(2) for b2 in range(nblk)] + [("yg", 0), ("yg", 1)], writes=["YD"])
            dsk = P.sb(st, [128, 2], F32, "dsk")
            P.dma("sync", dsk[:], self.din["s5_d"][l].rearrange("(kt p) -> p kt", p=128), writes=["dsk"])
            wg = P.sb(st, [128, 2, 256], BF16, "wglu")
            P.dma("gpsimd", wg[:], self.din["s5_w_glu"][l].rearrange("(kt p) f -> p kt f", p=128), reads=[], writes=["wglu"])
            yb = P.sb(st, [128, 2, 512], BF16, "ybf")
            yo = [P.sb(st, [128, 2, 512], BF16, "yo") for _ in range(2)]
            sg = P.sb(st, [128, 512], F32, "sg")
            pg = P.ps(st, [128, 512], F32, "pg")
            for bi, (t0, n) in enumerate(self.blocks):
                o = bi % 2
                yk = [("yacc", q, b2) for q in range(2) for b2 in range(nblk)]
                for q in range(2):
                    P.stt(yacc[:, q, t0:t0 + n], uT[:, q, t0:t0 + n], dsk[:, q:q + 1], yacc[:, q, t0:t0 + n], ALU.mult, ALU.add,
                          ["uT", "dsk"] + yk, [("yg", q)])
                    P.act(yacc[:, q, t0:t0 + n], yacc[:, q, t0:t0 + n], AF.Gelu_apprx_tanh, [("yg", q)], [("yg", q)])
                    P.copy(yb[:, q, :n], yacc[:, q, t0:t0 + n], [("yg", q)], [("ybf", q)])
                for ft in range(2):
                    for kt in range(2):
                        P.mm(pg[:, :n], wg[:, kt, ft * 128:(ft + 1) * 128], yb[:, kt, :n], kt == 0, kt == 1,
                             ["wglu", ("ybf", 0), ("ybf", 1)], ["pg"])
                    P.act(sg[:, :n], pg[:, :n], AF.Sigmoid, ["pg"], ["sg"])
                    P.tt(yo[o][:, ft, :n], yacc[:, ft, t0:t0 + n], sg[:, :n], ALU.mult, ["sg", ("yg", ft)], [("yo", o, ft)])
                P.dma("sync", YBv[:, 0:2, t0:t0 + n], yo[o][:, :, :n], reads=[("yo", o, 0), ("yo", o, 1)], writes=[("YB", 0, bi)])

    def phase_gla(self, l, which):
        P, nc = self.P, self.nc
        T, L = self.T, self.L
        PJ = self.PJ
        qoff = O_HQ if which == 0 else O_RQ
        goff = O_HG if which == 0 else O_RG
        yrow0 = 256 + which * 384
        CS = 32 if which == 0 else 64
        NH = 6
        with ExitStack() as st:
            def mk(shape, dt, nm):
                return [P.sb(st, shape, dt, nm) for _ in range(NH)]
            qf = mk([64, 512], BF16, "qf")
            kf = mk([64, 512], BF16, "kf")
            qt = mk([64, 512], BF16, "qt")
            ktl = mk([64, 512 + 64], BF16, "ktl")
            qh = mk([64, 512], BF16, "qh")
            kd = mk([64, 512 + 64], BF16, "kd")
            kdT = mk([64, 512 // CS, 64], BF16, "kdT")
            vv = mk([64, 512 // CS, 64], BF16, "vv")
            S = mk([64, 64], F32, "S")
            Sb = mk([64, 64], BF16, "Sb")
            ob = mk([64, 512], F32, "obk")
            oa = mk([64, 512], F32, "oak")
            ebend = mk([64, 16], F32, "ebend")
            Asb = [[[P.sb(st, [64, CS], BF16, "Asb") for _ in range(2)] for _ in range(2)] for _ in range(NH)]
            for hd in range(NH):
                P.memset(ktl[hd][:], 0.0, [("ktl", hd)])
                P.memset(kd[hd][:], 0.0, [("kd", hd)])
                for dd in range(2):
                    for sl in range(2):
                        P.memset(Asb[hd][dd][sl][:], 0.0, [("Asb", hd, dd, sl)])
            lf = [P.sb(st, [64, 512], F32, "lf") for _ in range(2)]
            bb = [P.sb(st, [64, 512], F32, "bb") for _ in range(2)]
            d1 = [P.sb(st, [64, 512], F32, "d1") for _ in range(2)]
            ex = [P.sb(st, [64, 512], F32, "ex") for _ in range(2)]
            gsb = [P.sb(st, [64, 512], BF16, "gsb") for _ in range(2)]
            osq = [P.sb(st, [64, 512], BF16, "osq") for _ in range(2)]
            rs_ = [P.sb(st, [64, 512], F32, "rs_") for _ in range(2)]
            yo = [P.sb(st, [64, 512], BF16, "yo") for _ in range(2)]
            LB = [P.ps(st, [128, 512], F32, "LB") for _ in range(NH)]
            PT = P.ps(st, [128, 512], F32, "PT")
            PN = P.ps(st, [128, 512], F32, "PN")
            if which == 1:
                tb = [{n: P.sb(st, [64, 64], F32, "rt" + n) for n in ("b", "q", "k", "e", "d")} for _ in range(NH)]
                ebr = mk([64, 1], F32, "ebr")
                for hd in range(NH):
                    lgc = math.log(1.0 - 2.0 ** (-5.0 - hd))
                    t_ = tb[hd]
                    P.ts(t_["b"][:], self.cst[0:64, C_IOTA:C_IOTA + 64], 1.0, lgc, ALU.add, ALU.mult, ["cst"], [("rtb", hd)])
                    P.act(t_["e"][:], t_["b"][:], AF.Exp, [("rtb", hd)], [("rte", hd)])
                    P.ts(t_["q"][:], t_["b"][:], t_["b"][:, 31:32], None, ALU.subtract, None, [("rtb", hd)], [("rtq", hd)])
                    P.act(t_["k"][:], t_["q"][:], AF.Exp, [("rtq", hd)], [("rtk", hd)], scale=-1.0)
                    P.act(t_["q"][:], t_["q"][:], AF.Exp, [("rtq", hd)], [("rtq", hd)])
                    P.ts(t_["d"][:], t_["b"][:], t_["b"][:, 63:64], None, ALU.subtract, None, [("rtb", hd)], [("rtd", hd)])
                    P.act(t_["d"][:], t_["d"][:], AF.Exp, [("rtd", hd)], [("rtd", hd)], scale=-1.0)
                    P.copy(ebr[hd][:], t_["e"][:, 63:64], [("rte", hd)], [("ebr", hd)])
            if which == 0:
                mask_f = self.cmask[0:CS, M32_FWD:M32_FWD + 32]
                mask_b = self.cmask[0:CS, M32_BWD:M32_BWD + 32]
            else:
                mask_f = self.cmask[0:CS, M_FWD:M_FWD + 64]
                mask_b = self.cmask[0:CS, M_BWDS:M_BWDS + 64]
            reset = self.cst[0:64, C_RESET32:C_RESET32 + 512]
            nstep = 0
            npre = 0
            for d in range(2):
                order = list(range(len(self.blocks))) if d == 0 else [0] + list(range(len(self.blocks) - 1, 0, -1))
                R = (lambda a: a) if d == 0 else rev_ap
                mask = mask_f if d == 0 else mask_b
                for hd in range(NH):
                    P.memset(S[hd][:], 0.0, [("S", hd)])
                    P.memset(Sb[hd][:], 0.0, [("Sb", hd)])
                for blk in order:
                    t0, n = self.blocks[blk]
                    nch = n // CS
                    pm = (CS // 2 - 1) if d == 0 else CS // 2
                    pe = (CS - 1) if d == 0 else 0
                    for hd in range(NH):
                        u = npre % 2
                        npre += 1
                        koff = (O_HFF if d == 0 else O_HFB) if which == 0 else O_RK
                        r0 = qoff + hd * 64
                        P.dma("sync", qf[hd][:, :n], PJ[r0:r0 + 64, t0:t0 + n], writes=[("qf", hd)])
                        r1 = koff + hd * 64
                        P.dma("sync", kf[hd][:, :n], PJ[r1:r1 + 64, t0:t0 + n], writes=[("kf", hd)])
                        P.dma("sync", vv[hd][0:CS, :n // CS, :],
                              self.VT[which, t0:t0 + n, hd * 64:(hd + 1) * 64].rearrange("(a p) c -> p a c", p=CS), writes=[("vv", hd)])
                        if d == 1:
                            P.dma("sync", oa[hd][:, :n], self.OA[which, hd * 64:(hd + 1) * 64, t0:t0 + n], writes=[("oak", hd)])
                        if which == 0:
                            P.dma("sync", lf[u][:, :n], self.LF[d, hd * 64:(hd + 1) * 64, t0:t0 + n], writes=[("lf", u)])
                            rr, rb, rl = reset[:, :n], R(bb[u][:, :n]), R(lf[u][:, :n])
                            P.op("vector", (lambda e, rr=rr, rb=rb, rl=rl: e.tensor_tensor_scan(rb, rr, rl, 0.0, ALU.mult, ALU.add)),
                                 [("lf", u), "cst"], [("bb", u)])
                            b3 = bb[u][:, :n].rearrange("p (c i) -> p c i", i=CS)
                            d3 = d1[u][:, :n].rearrange("p (c i) -> p c i", i=CS)
                            kb, kd1, kex = ("bb", u), ("d1", u), ("ex", u)
                            P.tt(d3, b3, b3[:, :, pm:pm + 1].to_broadcast([64, nch, CS]), ALU.subtract, [kb], [kd1])
                            P.act(ex[u][:, :n], d1[u][:, :n], AF.Exp, [kd1], [kex])
                            P.tt(qt[hd][:, :n], qf[hd][:, :n], ex[u][:, :n], ALU.mult, [("qf", hd), kex], [("qt", hd)])
                            P.act(ex[u][:, :n], d1[u][:, :n], AF.Exp, [kd1], [kex], scale=-1.0)
                            P.tt(ktl[hd][:, :n], kf[hd][:, :n], ex[u][:, :n], ALU.mult, [("kf", hd), kex], [("ktl", hd)])
                            P.act(ex[u][:, :n], bb[u][:, :n], AF.Exp, [kb], [kex])
                            P.tt(qh[hd][:, :n], qf[hd][:, :n], ex[u][:, :n], ALU.mult, [("qf", hd), kex], [("qh", hd)])
                            P.tt(d3, b3, b3[:, :, pe:pe + 1].to_broadcast([64, nch, CS]), ALU.subtract, [kb], [kd1])
                            P.act(ex[u][:, :n], d1[u][:, :n], AF.Exp, [kd1], [kex], scale=-1.0)
                            P.tt(kd[hd][:, :n], kf[hd][:, :n], ex[u][:, :n], ALU.mult, [("kf", hd), kex], [("kd", hd)])
                            P.act(ebend[hd][:, :nch], b3[:, :, pe], AF.Exp, [kb], [("ebend", hd)])
                        else:
                            q3 = qf[hd][:, :n].rearrange("p (c i) -> p c i", i=CS)
                            k3 = kf[hd][:, :n].rearrange("p (c i) -> p c i", i=CS)

                            def tbc(nm, R=R, nch=nch, hd=hd):
                                return R(tb[hd][nm][:]).unsqueeze(1).to_broadcast([64, nch, CS])
                            P.tt(qt[hd][:, :n].rearrange("p (c i) -> p c i", i=CS), q3, tbc("q"), ALU.mult, [("qf", hd), ("rtq", hd)], [("qt", hd)])
                            P.tt(ktl[hd][:, :n].rearrange("p (c i) -> p c i", i=CS), k3, tbc("k"), ALU.mult, [("kf", hd), ("rtk", hd)], [("ktl", hd)])
                            P.tt(qh[hd][:, :n].rearrange("p (c i) -> p c i", i=CS), q3, tbc("e"), ALU.mult, [("qf", hd), ("rte", hd)], [("qh", hd)])
                            P.tt(kd[hd][:, :n].rearrange("p (c i) -> p c i", i=CS), k3, tbc("d"), ALU.mult, [("kf", hd), ("rtd", hd)], [("kd", hd)])
                        for a in range(n // CS):
                            pc = (a % 4) * 64
                            P.mm(PT[0:64, pc:pc + 64], kd[hd][:, a * CS:a * CS + 64], self.identb[0:64, 0:64], True, True,
                                 [("kd", hd), "identb"], ["PT"])
                            if a % 4 == 3 or a == n // CS - 1:
                                a0 = a - (a % 4)
                                na = a - a0 + 1
                                P.copy(kdT[hd][0:CS, a0:a0 + na, :], PT[0:CS, 0:na * 64].rearrange("p (a k) -> p a k", k=64), ["PT"],
                                       [("kdT", hd)], eng="scalar")
                    corder = list(range(nch)) if d == 0 else list(range(nch - 1, -1, -1))
                    for c in corder:
                        sl = nstep % 2
                        nstep += 1
                        cs = slice(c * CS, (c + 1) * CS)
                        def regs(hd):
                            return (LB[hd][0:64, sl * 192:sl * 192 + CS], LB[hd][0:64, sl * 192 + 64:sl * 192 + 64 + CS],
                                    LB[hd][0:64, 384:448], Asb[hd][d][sl], ("Asb", hd, d, sl), ("LB", hd))
                        for hd in range(NH):
                            PAr, POr, PSr, A, ak, lk = regs(hd)
                            P.mm(PAr, ktl[hd][:, c * CS:c * CS + 64], qt[hd][:, cs], True, True, [("ktl", hd), ("qt", hd)], [lk])
                        for hd in range(NH):
                            PAr, POr, PSr, A, ak, lk = regs(hd)
                            P.op("vector", (lambda e, A=A, PAr=PAr, mask=mask: e.copy_predicated(A[0:CS, :], mask, PAr[0:CS, :])),
                                 [lk, "cmask", ak], [ak])
                        for hd in range(NH):
                            PAr, POr, PSr, A, ak, lk = regs(hd)
                            P.mm(POr, vv[hd][0:CS, c, :], A[0:CS, :], True, False, [("vv", hd), ak], [lk])
                            P.mm(POr, Sb[hd][:, :], qh[hd][:, cs], False, True, [("Sb", hd), ("qh", hd)], [lk])
                        for hd in range(NH):
                            PAr, POr, PSr, A, ak, lk = regs(hd)
                            if d == 0:
                                P.copy(ob[hd][:, cs], POr, [lk], [("obk", hd)], eng="scalar")
                            else:
                                P.tt(ob[hd][:, cs], POr, oa[hd][:, cs], ALU.add, [lk, ("oak", hd)], [("obk", hd)])
                        for hd in range(NH):
                            PAr, POr, PSr, A, ak, lk = regs(hd)
                            P.mm(PSr, kdT[hd][0:CS, c, :], vv[hd][0:CS, c, :], True, True, [("kdT", hd), ("vv", hd)], [lk])
                        for hd in range(NH):
                            PAr, POr, PSr, A, ak, lk = regs(hd)
                            esc = ebend[hd][:, c:c + 1] if which == 0 else ebr[hd][:, 0:1]
                            P.stt(S[hd][:], S[hd][:], esc, PSr, ALU.mult, ALU.add,
                                  [("S", hd), ("ebend", hd) if which == 0 else ("ebr", hd), lk], [("S", hd)])
                        for hd in range(NH):
                            P.copy(Sb[hd][:], S[hd][:], [("S", hd)], [("Sb", hd)], eng="scalar")
                    for hd in range(NH):
                        u = hd % 2
                        if d == 0:
                            P.dma("sync", self.OA[which, hd * 64:(hd + 1) * 64, t0:t0 + n], ob[hd][:, :n], reads=[("obk", hd)],
                                  writes=[("OA", blk, hd)])
                        else:
                            gr = goff + hd * 64
                            P.dma("sync", gsb[u][:, :n], PJ[gr:gr + 64, t0:t0 + n], writes=[("gsb", u)])
                            P.act(osq[u][:, :n], ob[hd][:, :n], AF.Square, [("obk", hd)], [("osq", u)])
                            P.mm(PN[0:64, :n], self.onesb[0:64, 0:64], osq[u][:, :n], True, True, [("osq", u), "onesb"], ["PN"])
                            P.act(rs_[u][:, :n], PN[0:64, :n], AF.Sqrt, ["PN"], [("rs_", u)], bias=self.epsc[0:64, 0:1], scale=1.0 / 64)
                            P.recip(rs_[u][:, :n], rs_[u][:, :n], [("rs_", u)], [("rs_", u)])
                            P.tt(rs_[u][:, :n], rs_[u][:, :n], ob[hd][:, :n], ALU.mult, [("rs_", u), ("obk", hd)], [("rs_", u)])
                            P.tt(yo[u][:, :n], rs_[u][:, :n], gsb[u][:, :n], ALU.mult, [("rs_", u), ("gsb", u)], [("yo", u)])
                            yr = yrow0 + hd * 64
                            P.dma("sync", self.YB[yr:yr + 64, t0:t0 + n], yo[u][:, :n], reads=[("yo", u)], writes=[("YBg", blk, hd)])

    def phase_merge(self, l):
        P, nc = self.P, self.nc
        T, L = self.T, self.L
        XTv = self.XT.rearrange("(ft p) t -> p ft t", p=128)
        YBv = self.YB.rearrange("(ft p) t -> p ft t", p=128)
        hTv = self.hT_scr.rearrange("(ft p) t -> p ft t", p=128)
        self.h2T_scr = self.scr.get("h2T") or self.scratch("h2T", [D, T], BF16)
        self.GTT = self.scr.get("GTT") or self.scratch("GTT", [NE, T], F32)
        h2v = self.h2T_scr.rearrange("(ft p) t -> p ft t", p=128)
        w_in = self.din["w_in"][l].rearrange("(kt p) f -> p kt f", p=128)
        with ExitStack() as st:
            nb = self.alloc_norm_bufs(st)
            gz = P.sb(st, [128, 8, 3072], BF16, "gz")
            for j in range(3):
                P.dma("gpsimd", gz[:, :, j * 1024:(j + 1) * 1024], w_in[:, :, O_GZ + j * 1024:O_GZ + (j + 1) * 1024], writes=[("gz", j)])
            wbr = P.sb(st, [128, 8, 1024], BF16, "wbr")
            P.dma("gpsimd", wbr[:, 0:2, :], self.din["w_branch_s5"][l].rearrange("(kt p) f -> p kt f", p=128), writes=[("wbr", 0)])
            P.dma("gpsimd", wbr[:, 2:5, :], self.din["w_branch_hgrn"][l].rearrange("(kt p) f -> p kt f", p=128), writes=[("wbr", 1)])
            P.dma("gpsimd", wbr[:, 5:8, :], self.din["w_branch_ret"][l].rearrange("(kt p) f -> p kt f", p=128), writes=[("wbr", 2)])
            wout = P.sb(st, [128, 8, 1024], BF16, "wout")
            P.dma("gpsimd", wout[:], self.din["w_out"][l].rearrange("(kt p) f -> p kt f", p=128), writes=["wout"])
            wr = P.sb(st, [128, 8, 36], F32, "wr")
            P.dma("sync", wr[:, :, 0:4], self.din["moe_w_group"][l].rearrange("(kt p) e -> p kt e", p=128), writes=[("wr", 0)])
            P.dma("sync", wr[:, :, 4:36], self.din["moe_w_expert"][l].rearrange("(kt p) e -> p kt e", p=128), writes=[("wr", 1)])
            brow = P.sb(st, [128, 36], F32, "brow")
            P.dma("sync", brow[:, 0:4], self.din["moe_b_group"][l:l + 1, :].partition_broadcast(128), writes=[("brow", 0)])
            P.dma("sync", brow[:, 4:36], self.din["moe_b_expert"][l:l + 1, :].partition_broadcast(128), writes=[("brow", 1)])
            hT = P.sb(st, [128, 8, 512], BF16, "mhT")
            yb = P.sb(st, [128, 8, 512], BF16, "myb")
            xt = P.sb(st, [128, 8, 512], F32, "mxt")
            sig = P.sb(st, [128, 3, 512], F32, "msig")
            mt = P.sb(st, [128, 512], F32, "mmt")
            mt2 = P.sb(st, [128, 512], F32, "mmt2")
            mg = P.sb(st, [128, 8, 512], BF16, "mmg")
            h2f = P.sb(st, [128, 8, 512], F32, "h2f")
            h2b = P.sb(st, [128, 8, 512], BF16, "h2b")
            pgt = [P.ps(st, [128, 512], F32, "pgt") for _ in range(3)]
            pbt = [P.ps(st, [128, 512], F32, "pbt") for _ in range(2)]
            px = P.ps(st, [128, 512], F32, "px")
            prt = P.ps(st, [128, 512], F32, "prt")
            rt = {n: P.sb(st, [128, w], F32, "r_" + n) for n, w in
                  (("l36", 36), ("gmax", 1), ("eg", 4), ("gsum", 1), ("og", 4), ("pen", 4), ("lem", 32), ("m1", 1), ("oh1", 32),
                   ("lem2", 32), ("m2", 1), ("oh2", 32), ("r", 1), ("w1", 1), ("w2", 1), ("G", 64))}
            P.memset(rt["G"][:], 0.0, ["G"])
            gts = P.sb(st, [32, 128], F32, "gts")
            ident = self.cst[:, C_IDENT:C_IDENT + 128]
            ktr = ((0, 2), (2, 5), (5, 8))
            nbr = 0
            for bi, (t0, n) in enumerate(self.blocks):
                who = 1 if bi == 0 else 0
                P.dma("sync", hT[:, :, :n], hTv[:, :, t0:t0 + n], writes=["mhT"])
                P.dma("sync", yb[:, :, :n], YBv[:, :, t0:t0 + n], writes=["myb"])
                P.dma("sync", xt[:, :, :n], XTv[:, :, t0:t0 + n], writes=["mxt"])
                for ft in range(8):
                    fs = slice(ft * 128, (ft + 1) * 128)
                    for j in range(3):
                        for kt in range(8):
                            P.mm(pgt[j][:, :n], gz[:, kt, j * 1024 + ft * 128:j * 1024 + (ft + 1) * 128], hT[:, kt, :n], kt == 0, kt == 7,
                                 [("gz", j), "mhT"], [("pgt", j)])
                        P.act(sig[:, j, :n], pgt[j][:, :n], AF.Sigmoid, [("pgt", j)], [("msig", j)])
                    for j in range(3):
                        pb = nbr % 2
                        nbr += 1
                        k0, k1 = ktr[j]
                        for kt in range(k0, k1):
                            P.mm(pbt[pb][:, :n], wbr[:, kt, fs], yb[:, kt, :n], kt == k0, kt == k1 - 1, [("wbr", j), "myb"], [("pbt", pb)])
                        if j == 0:
                            P.tt(mt[:, :n], pbt[pb][:, :n], sig[:, j, :n], ALU.mult, [("pbt", pb), ("msig", j)], ["mmt"])
                        else:
                            P.tt(mt2[:, :n], pbt[pb][:, :n], sig[:, j, :n], ALU.mult, [("pbt", pb), ("msig", j)], ["mmt2"])
                            if j == 1:
                                P.tt(mt[:, :n], mt[:, :n], mt2[:, :n], ALU.add, ["mmt", "mmt2"], ["mmt"])
                            else:
                                P.tt(mg[:, ft, :n], mt[:, :n], mt2[:, :n], ALU.add, ["mmt", "mmt2"], [("mmg", ft)])
                mk = [("mmg", ft) for ft in range(8)]
                for ft in range(8):
                    for kt in range(8):
                        P.mm(px[:, :n], wout[:, kt, ft * 128:(ft + 1) * 128], mg[:, kt, :n], kt == 0, kt == 7, ["wout"] + mk, ["px"])
                    P.stt(xt[:, ft, :n], px[:, :n], self.mod[:, 2, ft, who:who + 1], xt[:, ft, :n], ALU.mult, ALU.add,
                          ["px", ("mod", 2, who), "mxt"], ["mxt"])
                P.dma("sync", XTv[:, :, t0:t0 + n], xt[:, :, :n], reads=["mxt"], writes=[("XTw", bi)])
                self.norm_block(nb, xt, "norm2_gs", 3, who, h2f, ["mxt"], "h2f", n)
                P.copy(h2b[:, :, :n], h2f[:, :, :n], ["h2f"], ["h2b"], eng="gpsimd")
                P.dma("sync", h2v[:, :, t0:t0 + n], h2b[:, :, :n], reads=["h2b"], writes=[("h2T", bi)])
                for sblk in range(n // 128):
                    ss = slice(sblk * 128, (sblk + 1) * 128)
                    for kt in range(8):
                        P.mm(prt[:, 0:36], h2f[:, kt, ss], wr[:, kt, :], kt == 0, kt == 7, ["h2f", ("wr", 0), ("wr", 1)], ["prt"])
                    r_ = rt
                    P.tt(r_["l36"][:], prt[:, 0:36], brow[:], ALU.add, ["prt", ("brow", 0), ("brow", 1)], ["l36"])
                    lg4 = r_["l36"][:, 0:4]
                    le = r_["l36"][:, 4:36]
                    P.op("vector", (lambda e, o=r_["gmax"][:], i=lg4: e.tensor_reduce(o, i, AX.X, ALU.max)), ["l36"], ["gmax"])
                    P.ts(r_["eg"][:], lg4, r_["gmax"][:, 0:1], None, ALU.subtract, None, ["l36", "gmax"], ["eg"])
                    P.act(r_["eg"][:], r_["eg"][:], AF.Exp, ["eg"], ["eg"])
                    P.op("vector", (lambda e, o=r_["gsum"][:], i=r_["eg"][:]: e.tensor_reduce(o, i, AX.X, ALU.add)), ["eg"], ["gsum"])
                    P.recip(r_["gsum"][:], r_["gsum"][:], ["gsum"], ["gsum"])
                    P.ts(r_["og"][:], lg4, r_["gmax"][:, 0:1], None, ALU.is_ge, None, ["l36", "gmax"], ["og"])
                    P.ts(r_["pen"][:], r_["og"][:], -1.0, 1.0e4, ALU.add, ALU.mult, ["og"], ["pen"])
                    P.tt(r_["lem"][:].rearrange("p (g j) -> p g j", g=4), le.rearrange("p (g j) -> p g j", g=4),
                         r_["pen"][:].unsqueeze(2).to_broadcast([128, 4, 8]), ALU.add, ["l36", "pen"], ["lem"])
                    P.op("vector", (lambda e, o=r_["m1"][:], i=r_["lem"][:]: e.tensor_reduce(o, i, AX.X, ALU.max)), ["lem"], ["m1"])
                    P.ts(r_["oh1"][:], r_["lem"][:], r_["m1"][:, 0:1], None, ALU.is_ge, None, ["lem", "m1"], ["oh1"])
                    P.stt(r_["lem2"][:], r_["oh1"][:], -1.0e4, r_["lem"][:], ALU.mult, ALU.add, ["oh1", "lem"], ["lem2"])
                    P.op("vector", (lambda e, o=r_["m2"][:], i=r_["lem2"][:]: e.tensor_reduce(o, i, AX.X, ALU.max)), ["lem2"], ["m2"])
                    P.ts(r_["oh2"][:], r_["lem2"][:], r_["m2"][:, 0:1], None, ALU.is_ge, None, ["lem2", "m2"], ["oh2"])
                    P.tt(r_["r"][:], r_["m2"][:], r_["m1"][:], ALU.subtract, ["m1", "m2"], ["r"])
                    P.act(r_["r"][:], r_["r"][:], AF.Exp, ["r"], ["r"])
                    P.ts(r_["w1"][:], r_["r"][:], 1.0, None, ALU.add, None, ["r"], ["w1"])
                    P.recip(r_["w1"][:], r_["w1"][:], ["w1"], ["w1"])
                    P.tt(r_["w1"][:], r_["w1"][:], r_["gsum"][:], ALU.mult, ["w1", "gsum"], ["w1"])
                    P.tt(r_["w2"][:], r_["w1"][:], r_["r"][:], ALU.mult, ["w1", "r"], ["w2"])
                    P.ts(r_["G"][:, 0:32], r_["oh1"][:], r_["w1"][:, 0:1], None, ALU.mult, None, ["oh1", "w1"], ["G"])
                    P.stt(r_["G"][:, 0:32], r_["oh2"][:], r_["w2"][:, 0:1], r_["G"][:, 0:32], ALU.mult, ALU.add, ["oh2", "w2", "G"], ["G"])
                    P.mm(prt[0:64, 128:256], r_["G"][:], ident, True, True, ["G", "cst"], ["prt"])
                    P.copy(gts[:], prt[0:32, 128:256], ["prt"], ["gts"], eng="scalar")
                    c0 = t0 + sblk * 128
                    P.dma("sync", self.GTT[:, c0:c0 + 128], gts[:], reads=["gts"], writes=[("GTT", c0)])

    def phase_moe(self, l):
        P, nc = self.P, self.nc
        T, L = self.T, self.L
        XTv = self.XT.rearrange("(ft p) t -> p ft t", p=128)
        h2v = self.h2T_scr.rearrange("(ft p) t -> p ft t", p=128)
        groups, cur, tot = [], [], 0
        for b in self.blocks:
            if tot + b[1] > 1536:
                groups.append(cur)
                cur, tot = [], 0
            cur.append(b)
            tot += b[1]
        groups.append(cur)
        with ExitStack() as st:
            h2 = P.sb(st, [128, 8, 1536], BF16, "eh2")
            acc = P.sb(st, [128, 8, 1536], F32, "eacc")
            grep = [P.sb(st, [128, 1536], F32, "egrep") for _ in range(2)]
            wg = [P.sb(st, [128, 8, 512], BF16, "ewg") for _ in range(2)]
            wu = [P.sb(st, [128, 8, 512], BF16, "ewu") for _ in range(2)]
            wd = [P.sb(st, [128, 4, 1024], BF16, "ewd") for _ in range(2)]
            sg = [P.sb(st, [128, 512], F32, "esg") for _ in range(2)]
            hu = [P.sb(st, [128, 512], F32, "ehu") for _ in range(2)]
            hid = P.sb(st, [128, 4, 512], BF16, "ehid")
            xt = P.sb(st, [128, 8, 512], F32, "ext")
            pg = [P.ps(st, [128, 512], F32, "epg") for _ in range(2)]
            pu = [P.ps(st, [128, 512], F32, "epu") for _ in range(2)]
            pd = [P.ps(st, [128, 2, 512], F32, "epd") for _ in range(2)]
            nj = 0
            nf = 0
            for grp in groups:
                g0 = grp[0][0]
                ng = sum(b[1] for b in grp)
                P.dma("sync", h2[:, :, :ng], h2v[:, :, g0:g0 + ng], writes=["eh2"])
                P.memset(acc[:], 0.0, ["eacc"])
                for e in range(NE):
                    s_ = e % 2
                    P.dma("gpsimd", wg[s_][:], self.din["moe_w_gate"][l, e].rearrange("(kt p) f -> p kt f", p=128), writes=[("ewg", s_)])
                    P.dma("gpsimd", wu[s_][:], self.din["moe_w_up"][l, e].rearrange("(kt p) f -> p kt f", p=128), writes=[("ewu", s_)])
                    P.dma("gpsimd", wd[s_][:], self.din["moe_w_down"][l, e].rearrange("(jt p) f -> p jt f", p=128), writes=[("ewd", s_)])
                    P.dma("sync", grep[s_][:, :ng], self.GTT[e:e + 1, g0:g0 + ng].partition_broadcast(128), writes=[("egrep", s_)])
                    for (t0, n) in grp:
                        o = t0 - g0
                        for jt in range(4):
                            u = nj % 2
                            nj += 1
                            for kt in range(8):
                                P.mm(pg[u][:, :n], wg[s_][:, kt, jt * 128:(jt + 1) * 128], h2[:, kt, o:o + n], kt == 0, kt == 7,
                                     [("ewg", s_), "eh2"], [("epg", u)])
                            for kt in range(8):
                                P.mm(pu[u][:, :n], wu[s_][:, kt, jt * 128:(jt + 1) * 128], h2[:, kt, o:o + n], kt == 0, kt == 7,
                                     [("ewu", s_), "eh2"], [("epu", u)])
                            P.act(sg[u][:, :n], pg[u][:, :n], AF.Silu, [("epg", u)], [("esg", u)])
                            P.tt(hu[u][:, :n], pu[u][:, :n], sg[u][:, :n], ALU.mult, [("epu", u), ("esg", u)], [("ehu", u)])
                            P.tt(hid[:, jt, :n], hu[u][:, :n], grep[s_][:, o:o + n], ALU.mult, [("ehu", u), ("egrep", s_)], [("ehid", jt)],
                                 eng="gpsimd")
                        hk = [("ehid", jt) for jt in range(4)]
                        for fp in range(4):
                            u = nf % 2
                            nf += 1
                            for fq in range(2):
                                ft = fp * 2 + fq
                                for jt in range(4):
                                    P.mm(pd[u][:, fq, :n], wd[s_][:, jt, ft * 128:(ft + 1) * 128], hid[:, jt, :n], jt == 0, jt == 3,
                                         [("ewd", s_)] + hk, [("epd", u, fq)])
                            P.tt(acc[:, fp * 2:(fp + 1) * 2, o:o + n], acc[:, fp * 2:(fp + 1) * 2, o:o + n], pd[u][:, :, :n], ALU.add,
                                 ["eacc", ("epd", u, 0), ("epd", u, 1)], ["eacc"])
                for (t0, n) in grp:
                    o = t0 - g0
                    who = 1 if t0 == 0 else 0
                    P.dma("sync", xt[:, :, :n], XTv[:, :, t0:t0 + n], writes=["ext"])
                    for ft in range(8):
                        P.stt(xt[:, ft, :n], acc[:, ft, o:o + n], self.mod[:, 5, ft, who:who + 1], xt[:, ft, :n], ALU.mult, ALU.add,
                              ["eacc", ("mod", 5, who), "ext"], ["ext"])
                    P.dma("sync", XTv[:, :, t0:t0 + n], xt[:, :, :n], reads=["ext"], writes=[("XTe", t0)])


_CACHE = {}


def kernel(**inputs):
    L = inputs["x"].shape[1]
    B = inputs["x"].shape[0]
    depth = inputs["w_in"].shape[0]
    shapes = {n: tuple(inputs[n].shape) for n in PARAM_NAMES}
    shapes["c"] = (D,)
    key = (L, depth)
    if key not in _CACHE:
        _CACHE[key] = Builder(L, depth, shapes).build()
    nc = _CACHE[key]
    cst, cmask, pos = host_consts(L)
    shared = {n: np.ascontiguousarray(np.asarray(inputs[n], dtype=np.float32)) for n in PARAM_NAMES if n != "c"}
    shared["cst"] = cst
    shared["cmask"] = cmask
    shared["pos"] = pos
    in_maps = []
    for b in range(B):
        m = dict(shared)
        m["x"] = np.ascontiguousarray(np.asarray(inputs["x"][b], dtype=np.float32))
        m["ctx"] = np.ascontiguousarray(np.asarray(inputs["ctx"][b], dtype=np.float32))
        m["c"] = np.ascontiguousarray(np.asarray(inputs["c"][b], dtype=np.float32))
        in_maps.append(m)
    res = run_bass_kernel_spmd(nc, in_maps, core_ids=list(range(B)))
    out = np.stack([np.asarray(r["out"], dtype=np.float32) for r in res.results], axis=0)
    return out
```

```python
import math
import numpy as np
from contextlib import ExitStack
import concourse.bass as bass
import concourse.mybir as mybir
from concourse.bass_utils import run_bass_kernel_spmd

F32 = mybir.dt.float32
BF16 = mybir.dt.bfloat16
I32 = mybir.dt.int32
AF = mybir.ActivationFunctionType
ALU = mybir.AluOpType
AX = mybir.AxisListType

COMPUTE = ("tensor", "vector", "scalar", "gpsimd")
ALLENG = ("tensor", "vector", "scalar", "gpsimd", "sync")
NDMASEM = 6

D = 1024
NCTX = 256
IN_SIZES = (256, 384, 384, 384, 384, 384, 384, 384, 384, 384, 3072)
IN_OFF = [0]
for _s in IN_SIZES:
    IN_OFF.append(IN_OFF[-1] + _s)
(O_U, O_HQ, O_HFF, O_HFB, O_HV, O_HG, O_RQ, O_RK, O_RV, O_RG, O_GZ) = IN_OFF[:11]
IN_WIDTH = IN_OFF[-1]
NE = 32
EH = 512
EPS = 1e-6


class Prog:
    def __init__(self, nc, stack):
        self.nc = nc
        self.stack = stack
        self.ops = {e: [] for e in ALLENG}
        self.sem = {}
        self.cnt = {}
        for e in COMPUTE:
            self.sem[e] = stack.enter_context(nc.semaphore("s_" + e))
            self.cnt[e] = 0
        self.dsem = {}
        self.dcnt = {}
        self.dnext = {}
        for q in ("sync", "gpsimd"):
            self.dsem[q] = [stack.enter_context(nc.semaphore("d_%s%d" % (q, i))) for i in range(NDMASEM)]
            self.dcnt[q] = [0] * NDMASEM
            self.dnext[q] = 0
        self.semid = {}
        self.last_write = {}
        self.readers = {}
        self.seen = {e: {} for e in ALLENG}
        self.nalloc = 0
        self.out_tokens = []

    def sb(self, stack, shape, dtype=F32, name=None):
        self.nalloc += 1
        name = (name or "t") + "_%d" % self.nalloc
        return stack.enter_context(self.nc.sbuf_tensor(name, list(shape), dtype))

    def ps(self, stack, shape, dtype=F32, name=None):
        self.nalloc += 1
        name = (name or "p") + "_%d" % self.nalloc
        return stack.enter_context(self.nc.psum_tensor(name, list(shape), dtype))

    def _deps(self, eng, reads, writes):
        need = {}

        def add(tok):
            s, v = tok
            k = id(s)
            self.semid[k] = s
            if need.get(k, 0) < v:
                need[k] = v

        for k in reads:
            if k in self.last_write:
                add(self.last_write[k])
        for k in writes:
            if k in self.last_write:
                add(self.last_write[k])
            for t in self.readers.get(k, ()):
                add(t)
        waits = []
        for k, v in need.items():
            s = self.semid[k]
            if eng == "tensor" and s is self.sem["tensor"]:
                continue
            if self.seen[eng].get(k, 0) >= v:
                continue
            self.seen[eng][k] = v
            waits.append((s, v))
        return waits

    def _commit(self, tok, reads, writes):
        for k in reads:
            self.readers.setdefault(k, []).append(tok)
        for k in writes:
            self.last_write[k] = tok
            self.readers[k] = []

    def op(self, eng, fn, reads=(), writes=()):
        waits = self._deps(eng, reads, writes)
        self.cnt[eng] += 1
        tok = (self.sem[eng], self.cnt[eng])
        self.ops[eng].append((fn, waits, self.sem[eng], 1))
        self._commit(tok, reads, writes)
        return tok

    def dma(self, q, out, in_, reads=(), writes=(), is_output=False):
        i = self.dnext[q]
        self.dnext[q] = (i + 1) % NDMASEM
        s = self.dsem[q][i]
        waits = self._deps(q, reads, writes)
        prev = self.dcnt[q][i]
        k = id(s)
        self.semid[k] = s
        if prev > 0 and self.seen[q].get(k, 0) < prev:
            self.seen[q][k] = prev
            waits.append((s, prev))
        self.dcnt[q][i] = prev + 16
        tok = (s, prev + 16)

        def fn(e, out=out, in_=in_):
            return e.dma_start(out=out, in_=in_)

        self.ops[q].append((fn, waits, s, 16))
        self._commit(tok, reads, writes)
        if is_output:
            self.out_tokens.append(tok)
        return tok

    def barrier(self):
        toks = []
        for e in COMPUTE:
            if self.cnt[e] > 0:
                toks.append((self.sem[e], self.cnt[e]))
        for q in self.dsem:
            for i, s in enumerate(self.dsem[q]):
                if self.dcnt[q][i] > 0:
                    toks.append((s, self.dcnt[q][i]))
        for eng in ALLENG:
            waits = []
            for s, v in toks:
                k = id(s)
                self.semid[k] = s
                if eng in COMPUTE and s is self.sem[eng]:
                    if eng == "tensor":
                        continue
                if self.seen[eng].get(k, 0) >= v:
                    continue
                self.seen[eng][k] = v
                waits.append((s, v))
            if waits:
                self.ops[eng].append((None, waits, None, 0))
        self.last_write = {}
        self.readers = {}

    def emit(self):
        nc = self.nc
        fin = list(self.out_tokens)
        with nc.Block() as block:
            def mk(eng):
                def body(e):
                    for fn, waits, s, inc in self.ops[eng]:
                        for ws, wv in waits:
                            e.wait_ge(ws, wv)
                        if fn is not None:
                            ins = fn(e)
                            ins.then_inc(s, inc)
                    if eng == "sync":
                        for ws, wv in fin:
                            e.wait_ge(ws, wv)
                return body
            block.sync(mk("sync"))
            block.tensor(mk("tensor"))
            block.vector(mk("vector"))
            block.scalar(mk("scalar"))
            block.gpsimd(mk("gpsimd"))

    def mm(self, out, lhsT, rhs, start, stop, reads, writes):
        return self.op("tensor", lambda e: e.matmul(out, lhsT, rhs, start=start, stop=stop), reads, writes)

    def act(self, out, in_, func, reads, writes, bias=None, scale=None):
        kw = {}
        if bias is not None:
            kw["bias"] = bias
        if scale is not None:
            kw["scale"] = scale
        return self.op("scalar", lambda e: e.activation(out, in_, func, **kw), reads, writes)

    def tt(self, out, in0, in1, op, reads, writes, eng="vector"):
        return self.op(eng, lambda e: e.tensor_tensor(out, in0, in1, op), reads, writes)

    def ts(self, out, in0, s1, s2, op0, op1, reads, writes, eng="vector"):
        if s2 is None:
            return self.op(eng, lambda e: e.tensor_scalar(out, in0, s1, None, op0), reads, writes)
        return self.op(eng, lambda e: e.tensor_scalar(out, in0, s1, s2, op0, op1), reads, writes)

    def stt(self, out, in0, scalar, in1, op0, op1, reads, writes):
        return self.op("vector", lambda e: e.scalar_tensor_tensor(out, in0, scalar, in1, op0, op1), reads, writes)

    def copy(self, out, in_, reads, writes, eng="vector"):
        if eng == "scalar":
            return self.op("scalar", lambda e: e.copy(out, in_), reads, writes)
        return self.op(eng, lambda e: e.tensor_copy(out, in_), reads, writes)

    def memset(self, ap, val, writes, eng="vector"):
        return self.op(eng, lambda e: e.memset(ap, val), (), writes)

    def recip(self, out, in_, reads, writes):
        return self.op("vector", lambda e: e.reciprocal(out, in_), reads, writes)


def rev_ap(a):
    ap = [list(d) for d in a.ap]
    n = ap[-1][1]
    st = ap[-1][0]
    ap[-1] = [-st, n]
    return bass.AP(a.tensor, a.offset + st * (n - 1), ap)


C_IDENT = 0
C_RESET = 128
C_IOTA = 640
C_MR = 896
C_MC = 897
C_FIDX = 898
C_SIGN = 899
C_PIDX = 900
C_RESET32 = 904
C_W = 904 + 512

M_FWD = 0
M_BWD = 128
M_BWDS = 256
M32_FWD = 384
M32_BWD = 448
M_W = 512


def host_consts(L):
    c = np.zeros((128, C_W), np.float32)
    c[:, C_IDENT:C_IDENT + 128] = np.eye(128, dtype=np.float32)
    t = np.arange(512)
    c[:, C_RESET:C_RESET + 512] = (t % 64 != 0).astype(np.float32)[None, :]
    c[:, C_IOTA:C_IOTA + 256] = np.arange(256, dtype=np.float32)[None, :]
    c[:, C_RESET32:C_RESET32 + 512] = (t % 32 != 0).astype(np.float32)[None, :]
    p = np.arange(128)
    j = p % 32
    c[:, C_MR] = (j < 16)
    c[:, C_MC] = (j >= 16)
    c[:, C_FIDX] = p % 16
    c[:, C_SIGN] = np.where((p % 64) < 32, -1.0, 1.0)
    c[:, C_PIDX] = p
    m = np.zeros((128, M_W), np.int32)
    s = (p % 64)[:, None]
    tt = np.arange(64)[None, :]
    m[:, M_FWD:M_FWD + 128] = np.tile((s <= tt).astype(np.int32), (1, 2))
    m[:, M_BWD:M_BWD + 128] = np.tile((s >= tt).astype(np.int32), (1, 2))
    m[:, M_BWDS:M_BWDS + 128] = np.tile((s > tt).astype(np.int32), (1, 2))
    s32 = (p % 32)[:, None]
    t32 = np.arange(32)[None, :]
    m[:, M32_FWD:M32_FWD + 64] = np.tile((s32 <= t32).astype(np.int32), (1, 2))
    m[:, M32_BWD:M32_BWD + 64] = np.tile((s32 >= t32).astype(np.int32), (1, 2))
    tl = np.arange(L)
    pos = np.stack([tl // 64, tl % 64]).astype(np.float32)
    return c, m, pos


PARAM_NAMES = ["c", "c_ctx", "w_ada", "b_ada", "norm1_g", "norm2_g", "w_in", "s5_lam_re", "s5_lam_im",
               "s5_log_dt", "s5_b_re", "s5_b_im", "s5_c_re", "s5_c_im", "s5_d", "s5_w_glu", "hgrn_lb_raw",
               "w_branch_s5", "w_branch_hgrn", "w_branch_ret", "w_out", "moe_w_group", "moe_b_group",
               "moe_w_expert", "moe_b_expert", "moe_w_gate", "moe_w_up", "moe_w_down", "final_norm_g"]


class Builder:
    def __init__(self, L, depth, shapes, debug=()):
        self.L = L
        self.T = NCTX + L
        self.depth = depth
        self.debug = set(debug)
        nc = bass.Bass("TRN2", target_bir_lowering=False)
        self.nc = nc
        T = self.T
        self.din = {}
        self.din["x"] = nc.dram_tensor("x", [L, D], F32, kind="ExternalInput").ap()
        self.din["ctx"] = nc.dram_tensor("ctx", [NCTX, D], F32, kind="ExternalInput").ap()
        for n in PARAM_NAMES:
            self.din[n] = nc.dram_tensor(n, list(shapes[n]), F32, kind="ExternalInput").ap()
        self.din["cst"] = nc.dram_tensor("cst", [128, C_W], F32, kind="ExternalInput").ap()
        self.din["cmask"] = nc.dram_tensor("cmask", [128, M_W], I32, kind="ExternalInput").ap()
        self.din["pos"] = nc.dram_tensor("pos", [2, L], F32, kind="ExternalInput").ap()
        self.out = nc.dram_tensor("out", [L, D], F32, kind="ExternalOutput").ap()
        self.scr = {}
        self.blocks = [(0, NCTX)] + [(NCTX + 512 * i, 512) for i in range(L // 512)]
        assert L % 512 == 0

    def dump(self, name, ap, shape, dtype, reads):
        if "dumps" not in self.debug:
            return
        t = self.nc.dram_tensor("dbg_" + name, list(shape), dtype, kind="ExternalOutput").ap()
        self.P.dma("sync", t, ap, reads=reads, writes=["dbg_" + name])

    def scratch(self, name, shape, dtype):
        kind = "ExternalOutput" if name in self.debug else "Internal"
        t = self.nc.dram_tensor("scr_" + name, list(shape), dtype, kind=kind).ap()
        self.scr[name] = t
        return t

    def build(self):
        nc = self.nc
        T, L = self.T, self.L
        with ExitStack() as top, nc.allow_non_contiguous_dma(reason="small strided parameter loads"), \
                nc.allow_low_precision(reason="bf16 matmul operands"):
            P = Prog(nc, top)
            self.P = P
            self.cst = P.sb(top, [128, C_W], F32, "cst")
            self.cmask = P.sb(top, [128, M_W], I32, "cmask")
            P.dma("sync", self.cst[:], self.din["cst"], writes=["cst"])
            P.dma("sync", self.cmask[:], self.din["cmask"], writes=["cmask"])
            self.identb = P.sb(top, [128, 128], BF16, "identb")
            P.copy(self.identb[:], self.cst[:, C_IDENT:C_IDENT + 128], ["cst"], ["identb"])
            self.onesb = P.sb(top, [128, 128], BF16, "onesb")
            P.memset(self.onesb[:], 1.0, ["onesb"])
            self.onesf = P.sb(top, [128, 128], F32, "onesf")
            P.memset(self.onesf[:], 1.0, ["onesf"])
            self.epsc = P.sb(top, [128, 1], F32, "epsc")
            P.memset(self.epsc[:], EPS, ["epsc"])
            self.mod = P.sb(top, [128, 6, 8, 2], F32, "mod")
            self.g1s = P.sb(top, [128, 8, 2], F32, "g1s")
            self.g2s = P.sb(top, [128, 8, 2], F32, "g2s")
            self.XT = self.scratch("XT", [D, T], F32)
            self.PJ = self.scratch("PJ", [O_GZ, T], BF16)
            self.LF = self.scratch("LF", [2, 384, T], F32)
            self.VT = self.scratch("VT", [2, T, 384], BF16)
            self.OA = self.scratch("OA", [2, 384, T], F32)
            self.YB = self.scratch("YB", [D, T], BF16)
            self.GT = self.scratch("GT", [T, NE], F32)
            self.phase_input()
            P.barrier()
            self.phase_rope()
            P.barrier()
            for l in range(self.depth):
                self.l = l
                self.phase_mod(l)
                P.barrier()
                self.phase_proj(l)
                P.barrier()
                if "stop_proj" in self.debug:
                    break
                self.phase_s5(l)
                P.barrier()
                if "stop_s5" in self.debug:
                    break
                if "skip_hg" not in self.debug:
                    self.phase_gla(l, 0)
                    P.barrier()
                if "skip_ret" not in self.debug:
                    self.phase_gla(l, 1)
                    P.barrier()
                if "stop_mix" in self.debug:
                    break
                self.phase_merge(l)
                P.barrier()
                if "stop_merge" in self.debug:
                    break
                self.phase_moe(l)
                P.barrier()
            self.phase_output()
            P.emit()
        return nc

    def phase_input(self):
        P, nc = self.P, self.nc
        with ExitStack() as st:
            ident = self.cst[:, C_IDENT:C_IDENT + 128]
            xin = [P.sb(st, [128, D], F32, "xin") for _ in range(2)]
            xo = [P.sb(st, [128, 8, 128], F32, "xo") for _ in range(2)]
            pt = [P.ps(st, [128, 4, 128], F32, "pt") for _ in range(2)]
            ntile = self.T // 128
            for i in range(ntile):
                b = i % 2
                if i < NCTX // 128:
                    src = self.din["ctx"][i * 128:(i + 1) * 128, :]
                else:
                    j = i - NCTX // 128
                    src = self.din["x"][j * 128:(j + 1) * 128, :]
                P.dma("sync", xin[b][:], src, writes=[("xin", b)])
                for hf in range(2):
                    for q in range(4):
                        ft = hf * 4 + q
                        P.mm(pt[hf][:, q, :], xin[b][:, ft * 128:(ft + 1) * 128], ident, True, True,
                             [("xin", b), "cst"], [("pt", hf, q)])
                    P.copy(xo[b][:, hf * 4:(hf + 1) * 4, :], pt[hf][:], [("pt", hf, q) for q in range(4)],
                           [("xo", b, hf)], eng=("vector" if hf == 0 else "scalar"))
                dst = self.XT.rearrange("(ft p) t -> p ft t", p=128)[:, :, i * 128:(i + 1) * 128]
                P.dma("sync", dst, xo[b][:], reads=[("xo", b, 0), ("xo", b, 1)], writes=[("XT", i)])

    def phase_rope(self):
        P = self.P
        L = self.L
        self.ROPE = self.scratch("ROPE", [2, 128, L], F32)
        with ExitStack() as st:
            ropc = P.sb(st, [128, L], F32, "ropc")
            rops = P.sb(st, [128, L], F32, "rops")
            posr = P.sb(st, [128, L], F32, "posr")
            posc = P.sb(st, [128, L], F32, "posc")
            P.dma("sync", posr[:], self.din["pos"][0:1, :].partition_broadcast(128), writes=["posr"])
            P.dma("sync", posc[:], self.din["pos"][1:2, :].partition_broadcast(128), writes=["posc"])
            invf = P.sb(st, [128, 1], F32, "invf")
            P.act(invf[:], self.cst[:, C_FIDX:C_FIDX + 1], AF.Exp, ["cst"], ["invf"], scale=-math.log(10000.0) / 16.0)
            P.ts(posr[:], posr[:], self.cst[:, C_MR:C_MR + 1], None, ALU.mult, None, ["posr", "cst"], ["posr"])
            P.stt(posr[:], posc[:], self.cst[:, C_MC:C_MC + 1], posr[:], ALU.mult, ALU.add, ["posc", "posr", "cst"], ["posr"])
            P.ts(posr[:], posr[:], invf[:, 0:1], None, ALU.mult, None, ["posr", "invf"], ["posr"])
            self.sincos(st, posr, ropc, rops, L, "posr", "ropc", "rops")
            P.ts(rops[:], rops[:], self.cst[:, C_SIGN:C_SIGN + 1], None, ALU.mult, None, ["rops", "cst"], ["rops"])
            P.dma("sync", self.ROPE[0], ropc[:], reads=["ropc"], writes=["ROPE0"])
            P.dma("sync", self.ROPE[1], rops[:], reads=["rops"], writes=["ROPE1"])

    def phase_mod(self, l):
        P, nc = self.P, self.nc
        with ExitStack() as st:
            cc = P.sb(st, [128, 8, 2], F32, "cc")
            P.dma("sync", cc[:, :, 0], self.din["c"].rearrange("(kt p) -> p kt", p=128), writes=["cc0"])
            P.dma("sync", cc[:, :, 1], self.din["c_ctx"].rearrange("(kt p) -> p kt", p=128), writes=["cc1"])
            sc = P.sb(st, [128, 8, 2], F32, "sc")
            P.act(sc[:], cc[:], AF.Silu, ["cc0", "cc1"], ["sc"])
            bada = P.sb(st, [128, 6, 8], F32, "bada")
            P.dma("sync", bada[:], self.din["b_ada"][l].rearrange("(j ft p) -> p j ft", p=128, ft=8), writes=["bada"])
            wa = [P.sb(st, [128, 8, 1024], F32, "wa") for _ in range(2)]
            pm = P.ps(st, [128, 8, 2], F32, "pm")
            for j in range(6):
                b = j % 2
                src = self.din["w_ada"][l].rearrange("(kt p) f -> p kt f", p=128)[:, :, j * 1024:(j + 1) * 1024]
                P.dma("sync", wa[b][:], src, writes=[("wa", b)])
                for ft in range(8):
                    for kt in range(8):
                        P.mm(pm[:, ft, :], wa[b][:, kt, ft * 128:(ft + 1) * 128], sc[:, kt, :], kt == 0, kt == 7,
                             [("wa", b), "sc"], [("pm", ft)])
                for w in range(2):
                    P.tt(self.mod[:, j, :, w], pm[:, :, w], bada[:, j, :], ALU.add,
                         [("pm", ft) for ft in range(8)] + ["bada"], [("mod", j, w)])
            for (gname, gdst, jsc) in (("norm1_g", self.g1s, 1), ("norm2_g", self.g2s, 4)):
                g = P.sb(st, [128, 8], F32, "g")
                P.dma("sync", g[:], self.din[gname][l].rearrange("(ft p) -> p ft", p=128), writes=[gname])
                for w in range(2):
                    P.stt(gdst[:, :, w], self.mod[:, jsc, :, w], 1.0, g[:], ALU.add, ALU.mult,
                          [("mod", jsc, w), gname], [(gname + "s", w)])

    def norm_block(self, st_bufs, xt, gs, jshift, who, hout, keys_in, key_out, n):
        P = self.P
        sq, pss, rstd, tmp = st_bufs
        P.act(sq[:, :, :n], xt[:, :, :n], AF.Square, keys_in, ["nb_sq"])
        for kt in range(8):
            P.mm(pss[:, :n], self.onesb[:], sq[:, kt, :n], kt == 0, kt == 7, ["nb_sq", "onesb"], ["nb_ps"])
        P.act(rstd[:, :n], pss[:, :n], AF.Sqrt, ["nb_ps"], ["nb_rstd"], bias=self.epsc[:, 0:1], scale=1.0 / D)
        P.recip(rstd[:, :n], rstd[:, :n], ["nb_rstd"], ["nb_rstd"])
        for ft in range(8):
            P.tt(tmp[:, ft, :n], xt[:, ft, :n], rstd[:, :n], ALU.mult, keys_in + ["nb_rstd"], [("nb_tmp", ft)])
            P.act(hout[:, ft, :n], tmp[:, ft, :n], AF.Identity, [("nb_tmp", ft), (gs, who), ("mod", jshift, who)],
                  [key_out], bias=self.mod[:, jshift, ft, who:who + 1],
                  scale=(self.g1s if gs == "norm1_gs" else self.g2s)[:, ft, who:who + 1])

    def alloc_norm_bufs(self, st):
        P = self.P
        sq = P.sb(st, [128, 8, 512], BF16, "nsq")
        pss = P.ps(st, [128, 512], F32, "npss")
        rstd = P.sb(st, [128, 512], F32, "nrstd")
        tmp = P.sb(st, [128, 8, 512], F32, "ntmp")
        return (sq, pss, rstd, tmp)

    def phase_proj(self, l):
        P, nc = self.P, self.nc
        T, L = self.T, self.L
        w_in = self.din["w_in"][l].rearrange("(kt p) f -> p kt f", p=128)
        XTv = self.XT.rearrange("(ft p) t -> p ft t", p=128)
        PJv = self.PJ.rearrange("(ft p) t -> p ft t", p=128)
        self.hT_scr = self.scr.get("hT") or self.scratch("hT", [D, T], BF16)
        hTv = self.hT_scr.rearrange("(ft p) t -> p ft t", p=128)
        with ExitStack() as st:
            nb = self.alloc_norm_bufs(st)
            hT = P.sb(st, [128, 8, T], BF16, "hT")
            xt = [P.sb(st, [128, 8, 512], F32, "xt") for _ in range(2)]
            for bi, (t0, n) in enumerate(self.blocks):
                b = bi % 2
                P.dma("sync", xt[b][:, :, :n], XTv[:, :, t0:t0 + n], reads=["XTall"], writes=[("xt", b)])
                self.norm_block(nb, xt[b], "norm1_gs", 0, 1 if bi == 0 else 0, hT[:, :, t0:t0 + n],
                                [("xt", b)], ("hT", bi), n)
                P.dma("sync", hTv[:, :, t0:t0 + n], hT[:, :, t0:t0 + n], reads=[("hT", bi)], writes=[("hTd", bi)])
            hkeys = [("hT", bi) for bi in range(len(self.blocks))]
            lbr = P.sb(st, [128, 3, 2, 4], F32, "lbr")
            for li in range(4):
                for d in range(2):
                    P.dma("sync", lbr[:, :, d, li], self.din["hgrn_lb_raw"][li, d].rearrange("(ft p) -> p ft", p=128),
                          writes=[("lbr", li, d)])
            lbk = [("lbr", li, d) for li in range(4) for d in range(2)]
            lbe = P.sb(st, [128, 3, 2, 4], F32, "lbe")
            P.act(lbe[:], lbr[:], AF.Exp, lbk, ["lbe"])
            lsum = P.sb(st, [128, 3, 2], F32, "lsum")
            P.op("vector", lambda e: e.tensor_reduce(lsum[:], lbe[:], AX.X, ALU.add), ["lbe"], ["lsum"])
            P.recip(lsum[:], lsum[:], ["lsum"], ["lsum"])
            lb = P.sb(st, [128, 3, 2], F32, "lb")
            oml = P.sb(st, [128, 3, 2], F32, "oml")
            P.memset(lb[:], 0.0, ["lb"])
            for li in range(1, l + 1):
                P.tt(lb[:], lb[:], lbe[:, :, :, li], ALU.add, ["lb", "lbe"], ["lb"])
            P.tt(lb[:], lb[:], lsum[:], ALU.mult, ["lb", "lsum"], ["lb"])
            P.ts(oml[:], lb[:], -1.0, 1.0, ALU.mult, ALU.add, ["lb"], ["oml"])
            rcb = [P.sb(st, [128, 512], F32, "rcb") for _ in range(2)]
            rsb = [P.sb(st, [128, 512], F32, "rsb") for _ in range(2)]
            wb = [P.sb(st, [128, 8, 384], BF16, "wb") for _ in range(3)]
            pp = [P.ps(st, [128, 512], F32, "pp") for _ in range(4)]
            ob = [P.sb(st, [128, 3, 512], BF16, "ob") for _ in range(2)]
            of = [P.sb(st, [128, 3, 512], F32, "of") for _ in range(2)]
            t1 = P.sb(st, [128, 512], F32, "pt1")
            t2 = P.sb(st, [128, 512], F32, "pt2")
            cnt = {"pp": 0, "ob": 0}

            def load_w(slot, c0, ncol, swap=False):
                if not swap:
                    P.dma("gpsimd", wb[slot][:, :, :ncol], w_in[:, :, c0:c0 + ncol], reads=[], writes=[("wb", slot)])
                else:
                    src = w_in[:, :, c0:c0 + ncol].rearrange("p kt (h two j) -> p kt h two j", two=2, j=32)
                    dst = wb[slot][:, :, :ncol].rearrange("p kt (h two j) -> p kt h two j", two=2, j=32)
                    for kt in range(8):
                        P.dma("gpsimd", dst[:, kt, :, 0, :], src[:, kt, :, 1, :], reads=[], writes=[("wb", slot, kt, 0)])
                        P.dma("gpsimd", dst[:, kt, :, 1, :], src[:, kt, :, 0, :], reads=[], writes=[("wb", slot, kt, 1)])

            def wkeys(slot, swap=False):
                if not swap:
                    return [("wb", slot)]
                return [("wb", slot, kt, x) for kt in range(8) for x in range(2)]

            def fm_proj(slot, ft, t0, n, wk):
                i = cnt["pp"] % 4
                cnt["pp"] += 1
                for kt in range(8):
                    P.mm(pp[i][:, :n], wb[slot][:, kt, ft * 128:(ft + 1) * 128], hT[:, kt, t0:t0 + n], kt == 0, kt == 7,
                         wk + hkeys, [("pp", i)])
                return i

            def feature_group(c0, nft, row0, post, extra_w=None):
                load_w(0, c0, nft * 128)
                for bi, (t0, n) in enumerate(self.blocks):
                    o = cnt["ob"] % 2
                    cnt["ob"] += 1
                    for ft in range(nft):
                        i = fm_proj(0, ft, t0, n, wkeys(0))
                        post(i, ft, n, ob[o], o, bi, t0)
                    P.dma("sync", PJv[:, row0 // 128:row0 // 128 + nft, t0:t0 + n], ob[o][:, :nft, :n],
                          reads=[("ob", o, ft) for ft in range(nft)], writes=[("PJ", row0, bi)])

            def post_copy(i, ft, n, obt, o, bi, t0):
                P.copy(obt[:, ft, :n], pp[i][:, :n], [("pp", i)], [("ob", o, ft)], eng="scalar")

            def post_silu(i, ft, n, obt, o, bi, t0):
                P.act(obt[:, ft, :n], pp[i][:, :n], AF.Silu, [("pp", i)], [("ob", o, ft)])

            feature_group(O_U, 2, O_U, post_copy)
            feature_group(O_HQ, 3, O_HQ, post_copy)
            feature_group(O_HG, 3, O_HG, post_silu)
            feature_group(O_RG, 3, O_RG, post_silu)
            LFv = self.LF.rearrange("d (ft p) t -> d p ft t", p=128)
            for d, c0 in ((0, O_HFF), (1, O_HFB)):
                load_w(0, c0, 384)
                for bi, (t0, n) in enumerate(self.blocks):
                    o = cnt["ob"] % 2
                    cnt["ob"] += 1
                    for ft in range(3):
                        i = fm_proj(0, ft, t0, n, wkeys(0))
                        P.act(t1[:, :n], pp[i][:, :n], AF.Sigmoid, [("pp", i)], ["pt1"])
                        P.ts(t1[:, :n], t1[:, :n], oml[:, ft, d:d + 1], lb[:, ft, d:d + 1], ALU.mult, ALU.add,
                             ["pt1", "oml", "lb"], ["pt1"])
                        P.act(of[o][:, ft, :n], t1[:, :n], AF.Ln, ["pt1"], [("of", o, ft)])
                        P.ts(ob[o][:, ft, :n], t1[:, :n], -1.0, 1.0, ALU.mult, ALU.add, ["pt1"], [("ob", o, ft)])
                    P.dma("sync", PJv[:, c0 // 128:c0 // 128 + 3, t0:t0 + n], ob[o][:, :3, :n],
                          reads=[("ob", o, ft) for ft in range(3)], writes=[("PJ", c0, bi)])
                    P.dma("sync", LFv[d][:, :, t0:t0 + n], of[o][:, :3, :n],
                          reads=[("of", o, ft) for ft in range(3)], writes=[("LF", d, bi)])
            for c0, scl in ((O_RQ, 1.0), (O_RK, 0.125)):
                load_w(0, c0, 384)
                load_w(1, c0, 384, swap=True)
                for bi, (t0, n) in enumerate(self.blocks):
                    o = cnt["ob"] % 2
                    cnt["ob"] += 1
                    if bi > 0:
                        l0 = t0 - NCTX
                        P.dma("sync", rcb[bi % 2][:, :n], self.ROPE[0, :, l0:l0 + n], writes=[("rcb", bi % 2)])
                        P.dma("sync", rsb[bi % 2][:, :n], self.ROPE[1, :, l0:l0 + n], writes=[("rsb", bi % 2)])
                    for ft in range(3):
                        i = fm_proj(0, ft, t0, n, wkeys(0))
                        if bi == 0:
                            P.act(ob[o][:, ft, :n], pp[i][:, :n], AF.Identity, [("pp", i)], [("ob", o, ft)], scale=scl)
                        else:
                            i2 = fm_proj(1, ft, t0, n, wkeys(1, True))
                            rb_ = bi % 2
                            P.tt(t1[:, :n], pp[i][:, :n], rcb[rb_][:, :n], ALU.mult, [("pp", i), ("rcb", rb_)], ["pt1"])
                            P.tt(t2[:, :n], pp[i2][:, :n], rsb[rb_][:, :n], ALU.mult, [("pp", i2), ("rsb", rb_)], ["pt2"])
                            P.tt(t1[:, :n], t1[:, :n], t2[:, :n], ALU.add, ["pt1", "pt2"], ["pt1"])
                            P.act(ob[o][:, ft, :n], t1[:, :n], AF.Identity, ["pt1"], [("ob", o, ft)], scale=scl)
                    P.dma("sync", PJv[:, c0 // 128:c0 // 128 + 3, t0:t0 + n], ob[o][:, :3, :n],
                          reads=[("ob", o, ft) for ft in range(3)], writes=[("PJ", c0, bi)])
            vb = [P.sb(st, [128, 384], BF16, "vb") for _ in range(2)]
            for vi, c0 in ((0, O_HV), (1, O_RV)):
                load_w(2, c0, 384)
                for tt_ in range(T // 128):
                    i = cnt["pp"] % 4
                    cnt["pp"] += 1
                    for kt in range(8):
                        P.mm(pp[i][:, :384], hT[:, kt, tt_ * 128:(tt_ + 1) * 128], wb[2][:, kt, :384], kt == 0, kt == 7,
                             [("wb", 2)] + hkeys, [("pp", i)])
                    o = tt_ % 2
                    P.copy(vb[o][:], pp[i][:, :384], [("pp", i)], [("vb", o)], eng=("vector" if o == 0 else "scalar"))
                    P.dma("sync", self.VT[vi, tt_ * 128:(tt_ + 1) * 128, :], vb[o][:], reads=[("vb", o)], writes=[("VT", vi, tt_)])

    def sincos(self, st, ang, cosd, sind, n, akey, kc, ks, bufs=None):
        P = self.P
        if bufs is None:
            ki = P.sb(st, [128, n], I32, "sc_ki")
            kf = P.sb(st, [128, n], F32, "sc_kf")
        else:
            ki, kf = bufs[0][:, :n], bufs[1][:, :n]
        P.ts(kf[:], ang[:, :n], 1.0 / (2 * math.pi), None, ALU.mult, None, [akey], ["sc_kf"])
        P.copy(ki[:], kf[:], ["sc_kf"], ["sc_ki"])
        P.copy(kf[:], ki[:], ["sc_ki"], ["sc_kf"])
        P.stt(ang[:, :n], kf[:], -2 * math.pi, ang[:, :n], ALU.mult, ALU.add, ["sc_kf", akey], [akey])
        P.ts(kf[:], ang[:, :n], math.pi, -2 * math.pi, ALU.is_gt, ALU.mult, [akey], ["sc_kf"])
        P.tt(ang[:, :n], ang[:, :n], kf[:], ALU.add, [akey, "sc_kf"], [akey])
        P.ts(kf[:], ang[:, :n], -math.pi, 2 * math.pi, ALU.is_lt, ALU.mult, [akey], ["sc_kf"])
        P.tt(ang[:, :n], ang[:, :n], kf[:], ALU.add, [akey, "sc_kf"], [akey])
        P.ts(ang[:, :n], ang[:, :n], math.pi, -math.pi, ALU.min, ALU.max, [akey], [akey])
        P.act(sind[:, :n], ang[:, :n], AF.Sin, [akey], [ks])
        P.act(kf[:], ang[:, :n], AF.Abs, [akey], ["sc_kf"])
        P.ts(kf[:], kf[:], -1.0, math.pi / 2, ALU.mult, ALU.add, ["sc_kf"], ["sc_kf"])
        P.act(cosd[:, :n], kf[:], AF.Sin, ["sc_kf"], [kc])

    def phase_output(self):
        P, nc = self.P, self.nc
        T, L = self.T, self.L
        XTv = self.XT.rearrange("(ft p) t -> p ft t", p=128)
        with ExitStack() as st:
            nb = self.alloc_norm_bufs(st)
            sq, pss, rstd, tmp = nb
            ident = self.cst[:, C_IDENT:C_IDENT + 128]
            fg = P.sb(st, [128, 8], F32, "fg")
            P.dma("sync", fg[:], self.din["final_norm_g"].rearrange("(ft p) -> p ft", p=128), writes=["fg"])
            xt = [P.sb(st, [128, 8, 512], F32, "oxt") for _ in range(2)]
            xn = [P.sb(st, [128, 8, 512], F32, "oxn") for _ in range(2)]
            po = [P.ps(st, [128, 4, 128], F32, "opo") for _ in range(2)]
            ot = [P.sb(st, [128, D], F32, "oot") for _ in range(2)]
            k = 0
            for bi, (t0, n) in enumerate(self.blocks):
                if bi == 0:
                    continue
                b = bi % 2
                P.dma("sync", xt[b][:, :, :n], XTv[:, :, t0:t0 + n], reads=["XTall"], writes=[("oxt", b)])
                P.act(sq[:, :, :n], xt[b][:, :, :n], AF.Square, [("oxt", b)], ["nb_sq"])
                for kt in range(8):
                    P.mm(pss[:, :n], self.onesb[:], sq[:, kt, :n], kt == 0, kt == 7, ["nb_sq", "onesb"], ["nb_ps"])
                P.act(rstd[:, :n], pss[:, :n], AF.Sqrt, ["nb_ps"], ["nb_rstd"], bias=self.epsc[:, 0:1], scale=1.0 / D)
                P.recip(rstd[:, :n], rstd[:, :n], ["nb_rstd"], ["nb_rstd"])
                for ft in range(8):
                    P.stt(xn[b][:, ft, :n], xt[b][:, ft, :n], fg[:, ft:ft + 1], rstd[:, :n], ALU.mult, ALU.mult,
                          [("oxt", b), "fg", "nb_rstd"], [("oxn", b, ft)])
                for s in range(n // 128):
                    o = k % 2
                    k += 1
                    for hf in range(2):
                        for q in range(4):
                            ft = hf * 4 + q
                            P.mm(po[hf][:, q, :], xn[b][:, ft, s * 128:(s + 1) * 128], ident, True, True,
                                 [("oxn", b, ft), "cst"], [("opo", hf, q)])
                        P.copy(ot[o][:, hf * 512:(hf + 1) * 512], po[hf][:].rearrange("p a b -> p (a b)"),
                               [("opo", hf, q) for q in range(4)], [("oot", o, hf)], eng=("vector" if hf == 0 else "scalar"))
                    r0 = t0 - NCTX + s * 128
                    P.dma("sync", self.out[r0:r0 + 128, :], ot[o][:], reads=[("oot", o, 0), ("oot", o, 1)],
                          writes=[("out", r0)], is_output=True)


    def phase_s5(self, l):
        P, nc = self.P, self.nc
        T, L = self.T, self.L
        PJv = self.PJ.rearrange("(ft p) t -> p ft t", p=128)
        YBv = self.YB.rearrange("(ft p) t -> p ft t", p=128)
        nblk = T // 256
        with ExitStack() as st:
            uT = P.sb(st, [128, 2, T], BF16, "uT")
            P.dma("sync", uT[:], PJv[:, 0:2, :], writes=["uT"])
            yacc = P.sb(st, [128, 2, T], F32, "yacc")
            Ec = P.sb(st, [128, 8, 256], F32, "Ec")
            Es = P.sb(st, [128, 8, 256], F32, "Es")
            Fc = P.sb(st, [128, 8, 256], F32, "Fc")
            Fs = P.sb(st, [128, 8, 256], F32, "Fs")
            ang = P.sb(st, [128, 8, 256], F32, "ang")
            tA = P.sb(st, [128, 4, 256], F32, "tA")
            tB = P.sb(st, [128, 4, 256], F32, "tB")
            tC = P.sb(st, [128, 4, 256], F32, "tC")
            tD = P.sb(st, [128, 4, 256], F32, "tD")
            xt_ = P.sb(st, [128, 4, 2, 256], F32, "xtl")
            M = P.sb(st, [128, 4, 2, 256], F32, "M")
            hb = P.sb(st, [128, 4, 2, 256], BF16, "hb")
            BD = [P.sb(st, [128, 2, 512], BF16, "BD%d" % c) for c in range(2)]
            CT = [P.sb(st, [128, 8, 128], BF16, "CT%d" % c) for c in range(2)]
            sm = {n: P.sb(st, [128, 8], F32, "s5" + n) for n in
                  ("lr", "li", "dt", "th", "mag", "c", "s", "ar", "ai", "den", "am1", "zr", "zi", "t1", "t2", "a256", "Rc", "Rs")}
            init = [P.sb(st, [128, 8, 2], F32, "init%d" % i) for i in range(2)]
            pbu = P.ps(st, [128, 4, 2, 256], F32, "pbu")
            py = P.ps(st, [128, 256], F32, "py")
            iota = self.cst[:, C_IOTA:C_IOTA + 256]
            scb = (P.sb(st, [128, 2048], I32, "sc_ki"), P.sb(st, [128, 2048], F32, "sc_kf"))
            magf = P.sb(st, [128, 8, 256], F32, "magf")
            for d in range(2):
                P.dma("sync", sm["lr"][:], self.din["s5_lam_re"][l, d].rearrange("g p -> (g p)").rearrange("(nt p) -> p nt", p=128), writes=["lr"])
                P.dma("sync", sm["li"][:], self.din["s5_lam_im"][l, d].rearrange("g p -> (g p)").rearrange("(nt p) -> p nt", p=128), writes=["li"])
                ld = self.din["s5_log_dt"][l, d]
                for half in range(2):
                    src = bass.AP(ld.tensor, ld.offset + half, [[0, 64], [2, 8]])
                    P.dma("sync", sm["dt"][half * 64:(half + 1) * 64, :], src, writes=[("dt", half)])
                P.act(sm["dt"][:], sm["dt"][:], AF.Exp, [("dt", 0), ("dt", 1)], ["dt"])
                P.tt(sm["th"][:], sm["li"][:], sm["dt"][:], ALU.mult, ["li", "dt"], ["th"])
                P.tt(sm["mag"][:], sm["lr"][:], sm["dt"][:], ALU.mult, ["lr", "dt"], ["mag"])
                P.act(sm["mag"][:], sm["mag"][:], AF.Exp, ["mag"], ["mag"])
                P.ts(sm["a256"][:], sm["th"][:], 256.0, None, ALU.mult, None, ["th"], ["a256"])
                P.copy(sm["t1"][:], sm["th"][:], ["th"], ["t1"])
                with ExitStack() as st2:
                    self.sincos(st2, sm["t1"], sm["c"], sm["s"], 8, "t1", "c", "s", scb)
                    self.sincos(st2, sm["a256"], sm["Rc"], sm["Rs"], 8, "a256", "Rc", "Rs", scb)
                    P.tt(sm["ar"][:], sm["mag"][:], sm["c"][:], ALU.mult, ["mag", "c"], ["ar"])
                    P.tt(sm["ai"][:], sm["mag"][:], sm["s"][:], ALU.mult, ["mag", "s"], ["ai"])
                    P.tt(sm["den"][:], sm["lr"][:], sm["lr"][:], ALU.mult, ["lr"], ["den"])
                    P.tt(sm["t2"][:], sm["li"][:], sm["li"][:], ALU.mult, ["li"], ["t2"])
                    P.tt(sm["den"][:], sm["den"][:], sm["t2"][:], ALU.add, ["den", "t2"], ["den"])
                    P.recip(sm["den"][:], sm["den"][:], ["den"], ["den"])
                    P.ts(sm["am1"][:], sm["ar"][:], -1.0, None, ALU.add, None, ["ar"], ["am1"])
                    P.tt(sm["zr"][:], sm["am1"][:], sm["lr"][:], ALU.mult, ["am1", "lr"], ["zr"])
                    P.tt(sm["t2"][:], sm["ai"][:], sm["li"][:], ALU.mult, ["ai", "li"], ["t2"])
                    P.tt(sm["zr"][:], sm["zr"][:], sm["t2"][:], ALU.add, ["zr", "t2"], ["zr"])
                    P.tt(sm["zr"][:], sm["zr"][:], sm["den"][:], ALU.mult, ["zr", "den"], ["zr"])
                    P.tt(sm["zi"][:], sm["ai"][:], sm["lr"][:], ALU.mult, ["ai", "lr"], ["zi"])
                    P.tt(sm["t2"][:], sm["am1"][:], sm["li"][:], ALU.mult, ["am1", "li"], ["t2"])
                    P.tt(sm["zi"][:], sm["zi"][:], sm["t2"][:], ALU.subtract, ["zi", "t2"], ["zi"])
                    P.tt(sm["zi"][:], sm["zi"][:], sm["den"][:], ALU.mult, ["zi", "den"], ["zi"])
                    P.tt(ang[:], iota.unsqueeze(1).to_broadcast([128, 8, 256]), sm["th"][:].unsqueeze(2).to_broadcast([128, 8, 256]),
                         ALU.mult, ["cst", "th"], ["ang"])
                    a2 = ang[:].rearrange("p a b -> p (a b)")
                    self.sincos(st2, ang[:].rearrange("p a b -> p (a b)"), Ec[:].rearrange("p a b -> p (a b)"),
                                Es[:].rearrange("p a b -> p (a b)"), 2048, "ang", "Ec", "Es", scb)
                for nt in range(8):
                    P.ts(magf[:, nt, :], self.onesf[:, 0:128].unsqueeze(1).to_broadcast([128, 2, 128]).rearrange("p a b -> p (a b)") if False else Ec[:, nt, :],
                         0.0, sm["mag"][:, nt:nt + 1], ALU.mult, ALU.add, ["Ec", "mag"], ["magf"])
                zrb = sm["zr"][:].unsqueeze(2).to_broadcast([128, 8, 256])
                zib = sm["zi"][:].unsqueeze(2).to_broadcast([128, 8, 256])
                P.tt(Fc[:], Ec[:], zrb, ALU.mult, ["Ec", "zr"], ["Fc"])
                P.tt(ang[:], Es[:], zib, ALU.mult, ["Es", "zi"], ["ang"])
                P.tt(Fc[:], Fc[:], ang[:], ALU.add, ["Fc", "ang"], ["Fc"])
                P.tt(Fs[:], Ec[:], zib, ALU.mult, ["Ec", "zi"], ["Fs"])
                P.tt(ang[:], Es[:], zrb, ALU.mult, ["Es", "zr"], ["ang"])
                P.tt(Fs[:], Fs[:], ang[:], ALU.subtract, ["Fs", "ang"], ["Fs"])
                for c, nm in ((0, "s5_b_re"), (1, "s5_b_im")):
                    P.memset(BD[c][:], 0.0, [("BD", c)])
                    for g in range(16):
                        kt, gl = g // 8, g % 8
                        P.dma("gpsimd", BD[c][gl * 16:(gl + 1) * 16, kt, gl * 64:(gl + 1) * 64],
                              self.din[nm][l, d, g].rearrange("p h -> h p"), reads=[], writes=[("BD", c)])
                for c, nm in ((0, "s5_c_re"), (1, "s5_c_im")):
                    P.memset(CT[c][:], 0.0, [("CT", c)])
                    for g in range(16):
                        nt, g2, gl = g // 2, g % 2, g % 8
                        P.dma("gpsimd", CT[c][g2 * 64:(g2 + 1) * 64, nt, gl * 16:(gl + 1) * 16],
                              self.din[nm][l, d, g].rearrange("h p -> p h"), reads=[], writes=[("CT", c)])
                P.ts(CT[1][:], CT[1][:], -1.0, None, ALU.mult, None, [("CT", 1)], [("CT", 1)])
                if d == 0:
                    for nm_ in ("th", "mag", "zr", "zi", "Rc", "Rs", "dt", "lr", "li"):
                        self.dump(nm_, sm[nm_][:], [128, 8], F32, [nm_])
                    self.dump("Ec", Ec[:], [128, 8, 256], F32, ["Ec"])
                    self.dump("Es", Es[:], [128, 8, 256], F32, ["Es"])
                    self.dump("Fc", Fc[:], [128, 8, 256], F32, ["Fc"])
                    self.dump("BD0", BD[0][:], [128, 2, 512], BF16, [("BD", 0)])
                    self.dump("CT0", CT[0][:], [128, 8, 128], BF16, [("CT", 0)])
                    self.dump("CT1", CT[1][:], [128, 8, 128], BF16, [("CT", 1)])
                if d == 1 and "YD" in self.debug:
                    YD = self.scratch("YD", [256, T], F32)
                    P.dma("sync", YD.rearrange("(q p) t -> p q t", p=128), yacc[:], reads=[("yacc", q, b2) for q in range(2) for b2 in range(nblk)], writes=["YD"])
                order = list(range(nblk)) if d == 0 else [0] + list(range(nblk - 1, 0, -1))
                P.memset(init[0][:], 0.0, [("init", 0)])
                last = 255 if d == 0 else 0
                R = (lambda a: a) if d == 0 else rev_ap
                for bi, blk in enumerate(order):
                    t0 = blk * 256
                    ii, io = bi % 2, (bi + 1) % 2
                    for q in range(2):
                        for j in range(4):
                            for c in range(2):
                                P.mm(pbu[:, j, c, :], BD[c][:, q, j * 128:(j + 1) * 128], uT[:, q, t0:t0 + 256], True, True,
                                     [("BD", c), "uT"], [("pbu", j, c)])
                        pk = [("pbu", j, c) for j in range(4) for c in range(2)]
                        fc = R(Fc[:, 4 * q:4 * q + 4, :])
                        fs = R(Fs[:, 4 * q:4 * q + 4, :])
                        ec = R(Ec[:, 4 * q:4 * q + 4, :])
                        es = R(Es[:, 4 * q:4 * q + 4, :])
                        P.tt(tA[:], pbu[:, :, 0, :], fc, ALU.mult, pk + ["Fc"], ["tA"])
                        P.tt(tB[:], pbu[:, :, 1, :], fs, ALU.mult, pk + ["Fs"], ["tB"])
                        P.tt(xt_[:, :, 0, :], tA[:], tB[:], ALU.subtract, ["tA", "tB"], [("xtl", 0)])
                        P.tt(tA[:], pbu[:, :, 1, :], fc, ALU.mult, pk + ["Fc"], ["tA"])
                        P.tt(tB[:], pbu[:, :, 0, :], fs, ALU.mult, pk + ["Fs"], ["tB"])
                        P.tt(xt_[:, :, 1, :], tA[:], tB[:], ALU.add, ["tA", "tB"], [("xtl", 1)])
                        for j in range(4):
                            nt = 4 * q + j
                            for c in range(2):
                                def f(e, j=j, c=c, nt=nt, ii=ii, R=R):
                                    return e.tensor_tensor_scan(R(M[:, j, c, :]), magf[:, nt, :],
                                                                R(xt_[:, j, c, :]), init[ii][:, nt, c:c + 1], ALU.mult, ALU.add)
                                P.op("vector", f, [("xtl", c), "magf", ("init", ii)], [("M", c)])
                        rc = sm["Rc"][:, 4 * q:4 * q + 4]
                        rs = sm["Rs"][:, 4 * q:4 * q + 4]
                        mre = M[:, :, 0, last]
                        mim = M[:, :, 1, last]
                        t1 = sm["t1"][:, 0:4]
                        t2 = sm["t2"][:, 0:4]
                        P.tt(t1, mre, rc, ALU.mult, [("M", 0), "Rc"], ["t1"])
                        P.tt(t2, mim, rs, ALU.mult, [("M", 1), "Rs"], ["t2"])
                        P.tt(init[io][:, 4 * q:4 * q + 4, 0], t1, t2, ALU.subtract, ["t1", "t2"], [("init", io)])
                        P.tt(t1, mim, rc, ALU.mult, [("M", 1), "Rc"], ["t1"])
                        P.tt(t2, mre, rs, ALU.mult, [("M", 0), "Rs"], ["t2"])
                        P.tt(init[io][:, 4 * q:4 * q + 4, 1], t1, t2, ALU.add, ["t1", "t2"], [("init", io)])
                        G_ = "vector"
                        P.tt(tC[:], M[:, :, 0, :], ec, ALU.mult, [("M", 0), "Ec"], ["tC"], eng=G_)
                        P.tt(tD[:], M[:, :, 1, :], es, ALU.mult, [("M", 1), "Es"], ["tD"], eng=G_)
                        P.tt(hb[:, :, 0, :], tC[:], tD[:], ALU.subtract, ["tC", "tD"], [("hb", 0)], eng=G_)
                        P.tt(tC[:], M[:, :, 1, :], ec, ALU.mult, [("M", 1), "Ec"], ["tC"], eng=G_)
                        P.tt(tD[:], M[:, :, 0, :], es, ALU.mult, [("M", 0), "Es"], ["tD"], eng=G_)
                        P.tt(hb[:, :, 1, :], tC[:], tD[:], ALU.add, ["tC", "tD"], [("hb", 1)], eng=G_)
                        k = 0
                        for j in range(4):
                            for c in range(2):
                                P.mm(py[:], CT[c][:, 4 * q + j, :], hb[:, j, c, :], k == 0, k == 7,
                                     [("CT", c), ("hb", c)], ["py"])
                                k += 1
                        if d == 0:
                            P.copy(yacc[:, q, t0:t0 + 256], py[:], ["py"], [("yacc", q, blk)], eng="scalar")
                            if bi == 0 and q == 0:
                                self.dump("pbu", xt_[:], [128, 4, 2, 256], F32, [("xtl", 0), ("xtl", 1)])
                                self.dump("M", M[:], [128, 4, 2, 256], F32, [("M", 0), ("M", 1)])
                                self.dump("hb", hb[:], [128, 4, 2, 256], BF16, [("hb", 0), ("hb", 1)])
                        else:
                            P.tt(yacc[:, q, t0:t0 + 256], yacc[:, q, t0:t0 + 256], py[:], ALU.add, ["py", ("yacc", q, blk)], [("yacc", q, blk)])
            if False:
                YD = self.scratch("YD", [256, T], F32)
                P.dma("sync", YD.rearrange("(q p) t -> p q t", p=128), yacc[:], reads=[("yacc", q, b2) for q in range(2) for b2 in range(nblk)] + [("yg", 0), ("yg", 1)], writes=["YD"])
            dsk = P.sb(st, [128, 2], F32, "dsk")
            P.dma("sync", dsk[:], self.din["s5_d"][l].rearrange("(kt p) -> p kt", p=128), writes=["dsk"])
            wg = P.sb(st, [128, 2, 256], BF16, "wglu")
            P.dma("gpsimd", wg[:], self.din["s5_w_glu"][l].rearrange("(kt p) f -> p kt f", p=128), reads=[], writes=["wglu"])
            yb = P.sb(st, [128, 2, 512], BF16, "ybf")
            yo = [P.sb(st, [128, 2, 512], BF16, "yo") for _ in range(2)]
            sg = P.sb(st, [128, 512], F32, "sg")
            pg = P.ps(st, [128, 512], F32, "pg")
            for bi, (t0, n) in enumerate(self.blocks):
                o = bi % 2
                yk = [("yacc", q, b2) for q in range(2) for b2 in range(nblk)]
                for q in range(2):
                    P.stt(yacc[:, q, t0:t0 + n], uT[:, q, t0:t0 + n], dsk[:, q:q + 1], yacc[:, q, t0:t0 + n], ALU.mult, ALU.add,
                          ["uT", "dsk"] + yk, [("yg", q)])
                    P.act(yacc[:, q, t0:t0 + n], yacc[:, q, t0:t0 + n], AF.Gelu_apprx_tanh, [("yg", q)], [("yg", q)])
                    P.copy(yb[:, q, :n], yacc[:, q, t0:t0 + n], [("yg", q)], [("ybf", q)])
                for ft in range(2):
                    for kt in range(2):
                        P.mm(pg[:, :n], wg[:, kt, ft * 128:(ft + 1) * 128], yb[:, kt, :n], kt == 0, kt == 1,
                             ["wglu", ("ybf", 0), ("ybf", 1)], ["pg"])
                    P.act(sg[:, :n], pg[:, :n], AF.Sigmoid, ["pg"], ["sg"])
                    P.tt(yo[o][:, ft, :n], yacc[:, ft, t0:t0 + n], sg[:, :n], ALU.mult, ["sg", ("yg", ft)], [("yo", o, ft)])
                P.dma("sync", YBv[:, 0:2, t0:t0 + n], yo[o][:, :, :n], reads=[("yo", o, 0), ("yo", o, 1)], writes=[("YB", 0, bi)])

    def phase_gla(self, l, which):
        P, nc = self.P, self.nc
        T, L = self.T, self.L
        PJ = self.PJ
        qoff = O_HQ if which == 0 else O_RQ
        goff = O_HG if which == 0 else O_RG
        yrow0 = 256 + which * 384
        CS = 32 if which == 0 else 64
        NH = 6
        with ExitStack() as st:
            def mk(shape, dt, nm):
                return [P.sb(st, shape, dt, nm) for _ in range(NH)]
            qf = mk([64, 512], BF16, "qf")
            kf = mk([64, 512], BF16, "kf")
            qt = mk([64, 512], BF16, "qt")
            ktl = mk([64, 512 + 64], BF16, "ktl")
            qh = mk([64, 512], BF16, "qh")
            kd = mk([64, 512 + 64], BF16, "kd")
            kdT = mk([64, 512 // CS, 64], BF16, "kdT")
            vv = mk([64, 512 // CS, 64], BF16, "vv")
            S = mk([64, 64], F32, "S")
            Sb = mk([64, 64], BF16, "Sb")
            ob = mk([64, 512], F32, "obk")
            oa = mk([64, 512], F32, "oak")
            ebend = mk([64, 16], F32, "ebend")
            Asb = [[[P.sb(st, [64, CS], BF16, "Asb") for _ in range(2)] for _ in range(2)] for _ in range(NH)]
            for hd in range(NH):
                P.memset(ktl[hd][:], 0.0, [("ktl", hd)])
                P.memset(kd[hd][:], 0.0, [("kd", hd)])
                for dd in range(2):
                    for sl in range(2):
                        P.memset(Asb[hd][dd][sl][:], 0.0, [("Asb", hd, dd, sl)])
            lf = [P.sb(st, [64, 512], F32, "lf") for _ in range(2)]
            bb = [P.sb(st, [64, 512], F32, "bb") for _ in range(2)]
            d1 = [P.sb(st, [64, 512], F32, "d1") for _ in range(2)]
            ex = [P.sb(st, [64, 512], F32, "ex") for _ in range(2)]
            gsb = [P.sb(st, [64, 512], BF16, "gsb") for _ in range(2)]
            osq = [P.sb(st, [64, 512], BF16, "osq") for _ in range(2)]
            rs_ = [P.sb(st, [64, 512], F32, "rs_") for _ in range(2)]
            yo = [P.sb(st, [64, 512], BF16, "yo") for _ in range(2)]
            LB = [P.ps(st, [128, 512], F32, "LB") for _ in range(NH)]
            PT = P.ps(st, [128, 512], F32, "PT")
            PN = P.ps(st, [128, 512], F32, "PN")
            if which == 1:
                tb = [{n: P.sb(st, [64, 64], F32, "rt" + n) for n in ("b", "q", "k", "e", "d")} for _ in range(NH)]
                ebr = mk([64, 1], F32, "ebr")
                for hd in range(NH):
                    lgc = math.log(1.0 - 2.0 ** (-5.0 - hd))
                    t_ = tb[hd]
                    P.ts(t_["b"][:], self.cst[0:64, C_IOTA:C_IOTA + 64], 1.0, lgc, ALU.add, ALU.mult, ["cst"], [("rtb", hd)])
                    P.act(t_["e"][:], t_["b"][:], AF.Exp, [("rtb", hd)], [("rte", hd)])
                    P.ts(t_["q"][:], t_["b"][:], t_["b"][:, 31:32], None, ALU.subtract, None, [("rtb", hd)], [("rtq", hd)])
                    P.act(t_["k"][:], t_["q"][:], AF.Exp, [("rtq", hd)], [("rtk", hd)], scale=-1.0)
                    P.act(t_["q"][:], t_["q"][:], AF.Exp, [("rtq", hd)], [("rtq", hd)])
                    P.ts(t_["d"][:], t_["b"][:], t_["b"][:, 63:64], None, ALU.subtract, None, [("rtb", hd)], [("rtd", hd)])
                    P.act(t_["d"][:], t_["d"][:], AF.Exp, [("rtd", hd)], [("rtd", hd)], scale=-1.0)
                    P.copy(ebr[hd][:], t_["e"][:, 63:64], [("rte", hd)], [("ebr", hd)])
            if which == 0:
                mask_f = self.cmask[0:CS, M32_FWD:M32_FWD + 32]
                mask_b = self.cmask[0:CS, M32_BWD:M32_BWD + 32]
            else:
                mask_f = self.cmask[0:CS, M_FWD:M_FWD + 64]
                mask_b = self.cmask[0:CS, M_BWDS:M_BWDS + 64]
            reset = self.cst[0:64, C_RESET32:C_RESET32 + 512]
            nstep = 0
            npre = 0
            for d in range(2):
                order = list(range(len(self.blocks))) if d == 0 else [0] + list(range(len(self.blocks) - 1, 0, -1))
                R = (lambda a: a) if d == 0 else rev_ap
                mask = mask_f if d == 0 else mask_b
                for hd in range(NH):
                    P.memset(S[hd][:], 0.0, [("S", hd)])
                    P.memset(Sb[hd][:], 0.0, [("Sb", hd)])
                for blk in order:
                    t0, n = self.blocks[blk]
                    nch = n // CS
                    pm = (CS // 2 - 1) if d == 0 else CS // 2
                    pe = (CS - 1) if d == 0 else 0
                    for hd in range(NH):
                        u = npre % 2
                        npre += 1
                        koff = (O_HFF if d == 0 else O_HFB) if which == 0 else O_RK
                        r0 = qoff + hd * 64
                        P.dma("sync", qf[hd][:, :n], PJ[r0:r0 + 64, t0:t0 + n], writes=[("qf", hd)])
                        r1 = koff + hd * 64
                        P.dma("sync", kf[hd][:, :n], PJ[r1:r1 + 64, t0:t0 + n], writes=[("kf", hd)])
                        P.dma("sync", vv[hd][0:CS, :n // CS, :],
                              self.VT[which, t0:t0 + n, hd * 64:(hd + 1) * 64].rearrange("(a p) c -> p a c", p=CS), writes=[("vv", hd)])
                        if d == 1:
                            P.dma("sync", oa[hd][:, :n], self.OA[which, hd * 64:(hd + 1) * 64, t0:t0 + n], writes=[("oak", hd)])
                        if which == 0:
                            P.dma("sync", lf[u][:, :n], self.LF[d, hd * 64:(hd + 1) * 64, t0:t0 + n], writes=[("lf", u)])
                            rr, rb, rl = reset[:, :n], R(bb[u][:, :n]), R(lf[u][:, :n])
                            P.op("vector", (lambda e, rr=rr, rb=rb, rl=rl: e.tensor_tensor_scan(rb, rr, rl, 0.0, ALU.mult, ALU.add)),
                                 [("lf", u), "cst"], [("bb", u)])
                            b3 = bb[u][:, :n].rearrange("p (c i) -> p c i", i=CS)
                            d3 = d1[u][:, :n].rearrange("p (c i) -> p c i", i=CS)
                            kb, kd1, kex = ("bb", u), ("d1", u), ("ex", u)
                            P.tt(d3, b3, b3[:, :, pm:pm + 1].to_broadcast([64, nch, CS]), ALU.subtract, [kb], [kd1])
                            P.act(ex[u][:, :n], d1[u][:, :n], AF.Exp, [kd1], [kex])
                            P.tt(qt[hd][:, :n], qf[hd][:, :n], ex[u][:, :n], ALU.mult, [("qf", hd), kex], [("qt", hd)])
                            P.act(ex[u][:, :n], d1[u][:, :n], AF.Exp, [kd1], [kex], scale=-1.0)
                            P.tt(ktl[hd][:, :n], kf[hd][:, :n], ex[u][:, :n], ALU.mult, [("kf", hd), kex], [("ktl", hd)])
                            P.act(ex[u][:, :n], bb[u][:, :n], AF.Exp, [kb], [kex])
                            P.tt(qh[hd][:, :n], qf[hd][:, :n], ex[u][:, :n], ALU.mult, [("qf", hd), kex], [("qh", hd)])
                            P.tt(d3, b3, b3[:, :, pe:pe + 1].to_broadcast([64, nch, CS]), ALU.subtract, [kb], [kd1])
                            P.act(ex[u][:, :n], d1[u][:, :n], AF.Exp, [kd1], [kex], scale=-1.0)
                            P.tt(kd[hd][:, :n], kf[hd][:, :n], ex[u][:, :n], ALU.mult, [("kf", hd), kex], [("kd", hd)])
                            P.act(ebend[hd][:, :nch], b3[:, :, pe], AF.Exp, [kb], [("ebend", hd)])
                        else:
                            q3 = qf[hd][:, :n].rearrange("p (c i) -> p c i", i=CS)
                            k3 = kf[hd][:, :n].rearrange("p (c i) -> p c i", i=CS)

                            def tbc(nm, R=R, nch=nch, hd=hd):
                                return R(tb[hd][nm][:]).unsqueeze(1).to_broadcast([64, nch, CS])
                            P.tt(qt[hd][:, :n].rearrange("p (c i) -> p c i", i=CS), q3, tbc("q"), ALU.mult, [("qf", hd), ("rtq", hd)], [("qt", hd)])
                            P.tt(ktl[hd][:, :n].rearrange("p (c i) -> p c i", i=CS), k3, tbc("k"), ALU.mult, [("kf", hd), ("rtk", hd)], [("ktl", hd)])
                            P.tt(qh[hd][:, :n].rearrange("p (c i) -> p c i", i=CS), q3, tbc("e"), ALU.mult, [("qf", hd), ("rte", hd)], [("qh", hd)])
                            P.tt(kd[hd][:, :n].rearrange("p (c i) -> p c i", i=CS), k3, tbc("d"), ALU.mult, [("kf", hd), ("rtd", hd)], [("kd", hd)])
                        for a in range(n // CS):
                            pc = (a % 4) * 64
                            P.mm(PT[0:64, pc:pc + 64], kd[hd][:, a * CS:a * CS + 64], self.identb[0:64, 0:64], True, True,
                                 [("kd", hd), "identb"], ["PT"])
                            if a % 4 == 3 or a == n // CS - 1:
                                a0 = a - (a % 4)
                                na = a - a0 + 1
                                P.copy(kdT[hd][0:CS, a0:a0 + na, :], PT[0:CS, 0:na * 64].rearrange("p (a k) -> p a k", k=64), ["PT"],
                                       [("kdT", hd)], eng="scalar")
                    corder = list(range(nch)) if d == 0 else list(range(nch - 1, -1, -1))
                    for c in corder:
                        sl = nstep % 2
                        nstep += 1
                        cs = slice(c * CS, (c + 1) * CS)
                        def regs(hd):
                            return (LB[hd][0:64, sl * 192:sl * 192 + CS], LB[hd][0:64, sl * 192 + 64:sl * 192 + 64 + CS],
                                    LB[hd][0:64, 384:448], Asb[hd][d][sl], ("Asb", hd, d, sl), ("LB", hd))
                        for hd in range(NH):
                            PAr, POr, PSr, A, ak, lk = regs(hd)
                            P.mm(PAr, ktl[hd][:, c * CS:c * CS + 64], qt[hd][:, cs], True, True, [("ktl", hd), ("qt", hd)], [lk])
                        for hd in range(NH):
                            PAr, POr, PSr, A, ak, lk = regs(hd)
                            P.op("vector", (lambda e, A=A, PAr=PAr, mask=mask: e.copy_predicated(A[0:CS, :], mask, PAr[0:CS, :])),
                                 [lk, "cmask", ak], [ak])
                        for hd in range(NH):
                            PAr, POr, PSr, A, ak, lk = regs(hd)
                            P.mm(POr, vv[hd][0:CS, c, :], A[0:CS, :], True, False, [("vv", hd), ak], [lk])
                            P.mm(POr, Sb[hd][:, :], qh[hd][:, cs], False, True, [("Sb", hd), ("qh", hd)], [lk])
                        for hd in range(NH):
                            PAr, POr, PSr, A, ak, lk = regs(hd)
                            if d == 0:
                                P.copy(ob[hd][:, cs], POr, [lk], [("obk", hd)], eng="scalar")
                            else:
                                P.tt(ob[hd][:, cs], POr, oa[hd][:, cs], ALU.add, [lk, ("oak", hd)], [("obk", hd)])
                        for hd in range(NH):
                            PAr, POr, PSr, A, ak, lk = regs(hd)
                            P.mm(PSr, kdT[hd][0:CS, c, :], vv[hd][0:CS, c, :], True, True, [("kdT", hd), ("vv", hd)], [lk])
                        for hd in range(NH):
                            PAr, POr, PSr, A, ak, lk = regs(hd)
                            esc = ebend[hd][:, c:c + 1] if which == 0 else ebr[hd][:, 0:1]
                            P.stt(S[hd][:], S[hd][:], esc, PSr, ALU.mult, ALU.add,
                                  [("S", hd), ("ebend", hd) if which == 0 else ("ebr", hd), lk], [("S", hd)])
                        for hd in range(NH):
                            P.copy(Sb[hd][:], S[hd][:], [("S", hd)], [("Sb", hd)], eng="scalar")
                    for hd in range(NH):
                        u = hd % 2
                        if d == 0:
                            P.dma("sync", self.OA[which, hd * 64:(hd + 1) * 64, t0:t0 + n], ob[hd][:, :n], reads=[("obk", hd)],
                                  writes=[("OA", blk, hd)])
                        else:
                            gr = goff + hd * 64
                            P.dma("sync", gsb[u][:, :n], PJ[gr:gr + 64, t0:t0 + n], writes=[("gsb", u)])
                            P.act(osq[u][:, :n], ob[hd][:, :n], AF.Square, [("obk", hd)], [("osq", u)])
                            P.mm(PN[0:64, :n], self.onesb[0:64, 0:64], osq[u][:, :n], True, True, [("osq", u), "onesb"], ["PN"])
                            P.act(rs_[u][:, :n], PN[0:64, :n], AF.Sqrt, ["PN"], [("rs_", u)], bias=self.epsc[0:64, 0:1], scale=1.0 / 64)
                            P.recip(rs_[u][:, :n], rs_[u][:, :n], [("rs_", u)], [("rs_", u)])
                            P.tt(rs_[u][:, :n], rs_[u][:, :n], ob[hd][:, :n], ALU.mult, [("rs_", u), ("obk", hd)], [("rs_", u)])
                            P.tt(yo[u][:, :n], rs_[u][:, :n], gsb[u][:, :n], ALU.mult, [("rs_", u), ("gsb", u)], [("yo", u)])
                            yr = yrow0 + hd * 64
                            P.dma("sync", self.YB[yr:yr + 64, t0:t0 + n], yo[u][:, :n], reads=[("yo", u)], writes=[("YBg", blk, hd)])

    def phase_merge(self, l):
        P, nc = self.P, self.nc
        T, L = self.T, self.L
        XTv = self.XT.rearrange("(ft p) t -> p ft t", p=128)
        YBv = self.YB.rearrange("(ft p) t -> p ft t", p=128)
        hTv = self.hT_scr.rearrange("(ft p) t -> p ft t", p=128)
        self.h2T_scr = self.scr.get("h2T") or self.scratch("h2T", [D, T], BF16)
        self.GTT = self.scr.get("GTT") or self.scratch("GTT", [NE, T], F32)
        h2v = self.h2T_scr.rearrange("(ft p) t -> p ft t", p=128)
        w_in = self.din["w_in"][l].rearrange("(kt p) f -> p kt f", p=128)
        with ExitStack() as st:
            nb = self.alloc_norm_bufs(st)
            gz = P.sb(st, [128, 8, 3072], BF16, "gz")
            for j in range(3):
                P.dma("gpsimd", gz[:, :, j * 1024:(j + 1) * 1024], w_in[:, :, O_GZ + j * 1024:O_GZ + (j + 1) * 1024], writes=[("gz", j)])
            wbr = P.sb(st, [128, 8, 1024], BF16, "wbr")
            P.dma("gpsimd", wbr[:, 0:2, :], self.din["w_branch_s5"][l].rearrange("(kt p) f -> p kt f", p=128), writes=[("wbr", 0)])
            P.dma("gpsimd", wbr[:, 2:5, :], self.din["w_branch_hgrn"][l].rearrange("(kt p) f -> p kt f", p=128), writes=[("wbr", 1)])
            P.dma("gpsimd", wbr[:, 5:8, :], self.din["w_branch_ret"][l].rearrange("(kt p) f -> p kt f", p=128), writes=[("wbr", 2)])
            wout = P.sb(st, [128, 8, 1024], BF16, "wout")
            P.dma("gpsimd", wout[:], self.din["w_out"][l].rearrange("(kt p) f -> p kt f", p=128), writes=["wout"])
            wr = P.sb(st, [128, 8, 36], F32, "wr")
            P.dma("sync", wr[:, :, 0:4], self.din["moe_w_group"][l].rearrange("(kt p) e -> p kt e", p=128), writes=[("wr", 0)])
            P.dma("sync", wr[:, :, 4:36], self.din["moe_w_expert"][l].rearrange("(kt p) e -> p kt e", p=128), writes=[("wr", 1)])
            brow = P.sb(st, [128, 36], F32, "brow")
            P.dma("sync", brow[:, 0:4], self.din["moe_b_group"][l:l + 1, :].partition_broadcast(128), writes=[("brow", 0)])
            P.dma("sync", brow[:, 4:36], self.din["moe_b_expert"][l:l + 1, :].partition_broadcast(128), writes=[("brow", 1)])
            hT = P.sb(st, [128, 8, 512], BF16, "mhT")
            yb = P.sb(st, [128, 8, 512], BF16, "myb")
            xt = P.sb(st, [128, 8, 512], F32, "mxt")
            sig = P.sb(st, [128, 3, 512], F32, "msig")
            mt = P.sb(st, [128, 512], F32, "mmt")
            mt2 = P.sb(st, [128, 512], F32, "mmt2")
            mg = P.sb(st, [128, 8, 512], BF16, "mmg")
            h2f = P.sb(st, [128, 8, 512], F32, "h2f")
            h2b = P.sb(st, [128, 8, 512], BF16, "h2b")
            pgt = [P.ps(st, [128, 512], F32, "pgt") for _ in range(3)]
            pbt = [P.ps(st, [128, 512], F32, "pbt") for _ in range(2)]
            px = P.ps(st, [128, 512], F32, "px")
            prt = P.ps(st, [128, 512], F32, "prt")
            rt = {n: P.sb(st, [128, w], F32, "r_" + n) for n, w in
                  (("l36", 36), ("gmax", 1), ("eg", 4), ("gsum", 1), ("og", 4), ("pen", 4), ("lem", 32), ("m1", 1), ("oh1", 32),
                   ("lem2", 32), ("m2", 1), ("oh2", 32), ("r", 1), ("w1", 1), ("w2", 1), ("G", 64))}
            P.memset(rt["G"][:], 0.0, ["G"])
            gts = P.sb(st, [32, 128], F32, "gts")
            ident = self.cst[:, C_IDENT:C_IDENT + 128]
            ktr = ((0, 2), (2, 5), (5, 8))
            nbr = 0
            for bi, (t0, n) in enumerate(self.blocks):
                who = 1 if bi == 0 else 0
                P.dma("sync", hT[:, :, :n], hTv[:, :, t0:t0 + n], writes=["mhT"])
                P.dma("sync", yb[:, :, :n], YBv[:, :, t0:t0 + n], writes=["myb"])
                P.dma("sync", xt[:, :, :n], XTv[:, :, t0:t0 + n], writes=["mxt"])
                for ft in range(8):
                    fs = slice(ft * 128, (ft + 1) * 128)
                    for j in range(3):
                        for kt in range(8):
                            P.mm(pgt[j][:, :n], gz[:, kt, j * 1024 + ft * 128:j * 1024 + (ft + 1) * 128], hT[:, kt, :n], kt == 0, kt == 7,
                                 [("gz", j), "mhT"], [("pgt", j)])
                        P.act(sig[:, j, :n], pgt[j][:, :n], AF.Sigmoid, [("pgt", j)], [("msig", j)])
                    for j in range(3):
                        pb = nbr % 2
                        nbr += 1
                        k0, k1 = ktr[j]
                        for kt in range(k0, k1):
                            P.mm(pbt[pb][:, :n], wbr[:, kt, fs], yb[:, kt, :n], kt == k0, kt == k1 - 1, [("wbr", j), "myb"], [("pbt", pb)])
                        if j == 0:
                            P.tt(mt[:, :n], pbt[pb][:, :n], sig[:, j, :n], ALU.mult, [("pbt", pb), ("msig", j)], ["mmt"])
                        else:
                            P.tt(mt2[:, :n], pbt[pb][:, :n], sig[:, j, :n], ALU.mult, [("pbt", pb), ("msig", j)], ["mmt2"])
                            if j == 1:
                                P.tt(mt[:, :n], mt[:, :n], mt2[:, :n], ALU.add, ["mmt", "mmt2"], ["mmt"])
                            else:
                                P.tt(mg[:, ft, :n], mt[:, :n], mt2[:, :n], ALU.add, ["mmt", "mmt2"], [("mmg", ft)])
                mk = [("mmg", ft) for ft in range(8)]
                for ft in range(8):
                    for kt in range(8):
                        P.mm(px[:, :n], wout[:, kt, ft * 128:(ft + 1) * 128], mg[:, kt, :n], kt == 0, kt == 7, ["wout"] + mk, ["px"])
                    P.stt(xt[:, ft, :n], px[:, :n], self.mod[:, 2, ft, who:who + 1], xt[:, ft, :n], ALU.mult, ALU.add,
                          ["px", ("mod", 2, who), "mxt"], ["mxt"])
                P.dma("sync", XTv[:, :, t0:t0 + n], xt[:, :, :n], reads=["mxt"], writes=[("XTw", bi)])
                self.norm_block(nb, xt, "norm2_gs", 3, who, h2f, ["mxt"], "h2f", n)
                P.copy(h2b[:, :, :n], h2f[:, :, :n], ["h2f"], ["h2b"], eng="gpsimd")
                P.dma("sync", h2v[:, :, t0:t0 + n], h2b[:, :, :n], reads=["h2b"], writes=[("h2T", bi)])
                for sblk in range(n // 128):
                    ss = slice(sblk * 128, (sblk + 1) * 128)
                    for kt in range(8):
                        P.mm(prt[:, 0:36], h2f[:, kt, ss], wr[:, kt, :], kt == 0, kt == 7, ["h2f", ("wr", 0), ("wr", 1)], ["prt"])
                    r_ = rt
                    P.tt(r_["l36"][:], prt[:, 0:36], brow[:], ALU.add, ["prt", ("brow", 0), ("brow", 1)], ["l36"])
                    lg4 = r_["l36"][:, 0:4]
                    le = r_["l36"][:, 4:36]
                    P.op("vector", (lambda e, o=r_["gmax"][:], i=lg4: e.tensor_reduce(o, i, AX.X, ALU.max)), ["l36"], ["gmax"])
                    P.ts(r_["eg"][:], lg4, r_["gmax"][:, 0:1], None, ALU.subtract, None, ["l36", "gmax"], ["eg"])
                    P.act(r_["eg"][:], r_["eg"][:], AF.Exp, ["eg"], ["eg"])
                    P.op("vector", (lambda e, o=r_["gsum"][:], i=r_["eg"][:]: e.tensor_reduce(o, i, AX.X, ALU.add)), ["eg"], ["gsum"])
                    P.recip(r_["gsum"][:], r_["gsum"][:], ["gsum"], ["gsum"])
                    P.ts(r_["og"][:], lg4, r_["gmax"][:, 0:1], None, ALU.is_ge, None, ["l36", "gmax"], ["og"])
                    P.ts(r_["pen"][:], r_["og"][:], -1.0, 1.0e4, ALU.add, ALU.mult, ["og"], ["pen"])
                    P.tt(r_["lem"][:].rearrange("p (g j) -> p g j", g=4), le.rearrange("p (g j) -> p g j", g=4),
                         r_["pen"][:].unsqueeze(2).to_broadcast([128, 4, 8]), ALU.add, ["l36", "pen"], ["lem"])
                    P.op("vector", (lambda e, o=r_["m1"][:], i=r_["lem"][:]: e.tensor_reduce(o, i, AX.X, ALU.max)), ["lem"], ["m1"])
                    P.ts(r_["oh1"][:], r_["lem"][:], r_["m1"][:, 0:1], None, ALU.is_ge, None, ["lem", "m1"], ["oh1"])
                    P.stt(r_["lem2"][:], r_["oh1"][:], -1.0e4, r_["lem"][:], ALU.mult, ALU.add, ["oh1", "lem"], ["lem2"])
                    P.op("vector", (lambda e, o=r_["m2"][:], i=r_["lem2"][:]: e.tensor_reduce(o, i, AX.X, ALU.max)), ["lem2"], ["m2"])
                    P.ts(r_["oh2"][:], r_["lem2"][:], r_["m2"][:, 0:1], None, ALU.is_ge, None, ["lem2", "m2"], ["oh2"])
                    P.tt(r_["r"][:], r_["m2"][:], r_["m1"][:], ALU.subtract, ["m1", "m2"], ["r"])
                    P.act(r_["r"][:], r_["r"][:], AF.Exp, ["r"], ["r"])
                    P.ts(r_["w1"][:], r_["r"][:], 1.0, None, ALU.add, None, ["r"], ["w1"])
                    P.recip(r_["w1"][:], r_["w1"][:], ["w1"], ["w1"])
                    P.tt(r_["w1"][:], r_["w1"][:], r_["gsum"][:], ALU.mult, ["w1", "gsum"], ["w1"])
                    P.tt(r_["w2"][:], r_["w1"][:], r_["r"][:], ALU.mult, ["w1", "r"], ["w2"])
                    P.ts(r_["G"][:, 0:32], r_["oh1"][:], r_["w1"][:, 0:1], None, ALU.mult, None, ["oh1", "w1"], ["G"])
                    P.stt(r_["G"][:, 0:32], r_["oh2"][:], r_["w2"][:, 0:1], r_["G"][:, 0:32], ALU.mult, ALU.add, ["oh2", "w2", "G"], ["G"])
                    P.mm(prt[0:64, 128:256], r_["G"][:], ident, True, True, ["G", "cst"], ["prt"])
                    P.copy(gts[:], prt[0:32, 128:256], ["prt"], ["gts"], eng="scalar")
                    c0 = t0 + sblk * 128
                    P.dma("sync", self.GTT[:, c0:c0 + 128], gts[:], reads=["gts"], writes=[("GTT", c0)])

    def phase_moe(self, l):
        P, nc = self.P, self.nc
        T, L = self.T, self.L
        XTv = self.XT.rearrange("(ft p) t -> p ft t", p=128)
        h2v = self.h2T_scr.rearrange("(ft p) t -> p ft t", p=128)
        groups, cur, tot = [], [], 0
        for b in self.blocks:
            if tot + b[1] > 1536:
                groups.append(cur)
                cur, tot = [], 0
            cur.append(b)
            tot += b[1]
        groups.append(cur)
        with ExitStack() as st:
            h2 = P.sb(st, [128, 8, 1536], BF16, "eh2")
            acc = P.sb(st, [128, 8, 1536], F32, "eacc")
            grep = [P.sb(st, [128, 1536], F32, "egrep") for _ in range(2)]
            wg = [P.sb(st, [128, 8, 512], BF16, "ewg") for _ in range(2)]
            wu = [P.sb(st, [128, 8, 512], BF16, "ewu") for _ in range(2)]
            wd = [P.sb(st, [128, 4, 1024], BF16, "ewd") for _ in range(2)]
            sg = [P.sb(st, [128, 512], F32, "esg") for _ in range(2)]
            hu = [P.sb(st, [128, 512], F32, "ehu") for _ in range(2)]
            hid = [P.sb(st, [128, 4, 512], BF16, "ehid") for _ in range(2)]
            xt = P.sb(st, [128, 8, 512], F32, "ext")
            pg = [P.ps(st, [128, 512], F32, "epg") for _ in range(2)]
            pu = [P.ps(st, [128, 512], F32, "epu") for _ in range(2)]
            pd = [P.ps(st, [128, 2, 512], F32, "epd") for _ in range(2)]
            nj = 0
            nf = 0
            for grp in groups:
                g0 = grp[0][0]
                ng = sum(b[1] for b in grp)
                P.dma("sync", h2[:, :, :ng], h2v[:, :, g0:g0 + ng], writes=["eh2"])
                P.memset(acc[:], 0.0, ["eacc"])
                units = [(e, t0, n) for e in range(NE) for (t0, n) in grp]

                def emit_gu(ui):
                    nonlocal nj
                    e, t0, n = units[ui]
                    s_ = e % 2
                    hs_ = ui % 2
                    o = t0 - g0
                    if t0 == grp[0][0]:
                        P.dma("gpsimd", wg[s_][:], self.din["moe_w_gate"][l, e].rearrange("(kt p) f -> p kt f", p=128), writes=[("ewg", s_)])
                        P.dma("gpsimd", wu[s_][:], self.din["moe_w_up"][l, e].rearrange("(kt p) f -> p kt f", p=128), writes=[("ewu", s_)])
                        P.dma("gpsimd", wd[s_][:], self.din["moe_w_down"][l, e].rearrange("(jt p) f -> p jt f", p=128), writes=[("ewd", s_)])
                        P.dma("sync", grep[s_][:, :ng], self.GTT[e:e + 1, g0:g0 + ng].partition_broadcast(128), writes=[("egrep", s_)])
                    for jt in range(4):
                        u = nj % 2
                        nj += 1
                        for kt in range(8):
                            P.mm(pg[u][:, :n], wg[s_][:, kt, jt * 128:(jt + 1) * 128], h2[:, kt, o:o + n], kt == 0, kt == 7,
                                 [("ewg", s_), "eh2"], [("epg", u)])
                        for kt in range(8):
                            P.mm(pu[u][:, :n], wu[s_][:, kt, jt * 128:(jt + 1) * 128], h2[:, kt, o:o + n], kt == 0, kt == 7,
                                 [("ewu", s_), "eh2"], [("epu", u)])
                        P.act(sg[u][:, :n], pg[u][:, :n], AF.Silu, [("epg", u)], [("esg", u)])
                        P.tt(hu[u][:, :n], pu[u][:, :n], sg[u][:, :n], ALU.mult, [("epu", u), ("esg", u)], [("ehu", u)])
                        P.tt(hid[hs_][:, jt, :n], hu[u][:, :n], grep[s_][:, o:o + n], ALU.mult, [("ehu", u), ("egrep", s_)], [("ehid", hs_, jt)],
                             eng="gpsimd")

                def emit_d(ui):
                    nonlocal nf
                    e, t0, n = units[ui]
                    s_ = e % 2
                    hs_ = ui % 2
                    o = t0 - g0
                    hk = [("ehid", hs_, jt) for jt in range(4)]
                    for fp in range(4):
                        u = nf % 2
                        nf += 1
                        for fq in range(2):
                            ft = fp * 2 + fq
                            for jt in range(4):
                                P.mm(pd[u][:, fq, :n], wd[s_][:, jt, ft * 128:(ft + 1) * 128], hid[hs_][:, jt, :n], jt == 0, jt == 3,
                                     [("ewd", s_)] + hk, [("epd", u, fq)])
                        P.tt(acc[:, fp * 2:(fp + 1) * 2, o:o + n], acc[:, fp * 2:(fp + 1) * 2, o:o + n], pd[u][:, :, :n], ALU.add,
                             ["eacc", ("epd", u, 0), ("epd", u, 1)], ["eacc"])

                for ui in range(len(units)):
                    emit_gu(ui)
                    if ui > 0:
                        emit_d(ui - 1)
                emit_d(len(units) - 1)
                for (t0, n) in grp:
                    o = t0 - g0
                    who = 1 if t0 == 0 else 0
                    P.dma("sync", xt[:, :, :n], XTv[:, :, t0:t0 + n], writes=["ext"])
                    for ft in range(8):
                        P.stt(xt[:, ft, :n], acc[:, ft, o:o + n], self.mod[:, 5, ft, who:who + 1], xt[:, ft, :n], ALU.mult, ALU.add,
                              ["eacc", ("mod", 5, who), "ext"], ["ext"])
                    P.dma("sync", XTv[:, :, t0:t0 + n], xt[:, :, :n], reads=["ext"], writes=[("XTe", t0)])


_CACHE = {}


def kernel(**inputs):
    L = inputs["x"].shape[1]
    B = inputs["x"].shape[0]
    depth = inputs["w_in"].shape[0]
    shapes = {n: tuple(inputs[n].shape) for n in PARAM_NAMES}
    shapes["c"] = (D,)
    key = (L, depth)
    if key not in _CACHE:
        _CACHE[key] = Builder(L, depth, shapes).build()
    nc = _CACHE[key]
    cst, cmask, pos = host_consts(L)
    shared = {n: np.ascontiguousarray(np.asarray(inputs[n], dtype=np.float32)) for n in PARAM_NAMES if n != "c"}
    shared["cst"] = cst
    shared["cmask"] = cmask
    shared["pos"] = pos
    in_maps = []
    for b in range(B):
        m = dict(shared)
        m["x"] = np.ascontiguousarray(np.asarray(inputs["x"][b], dtype=np.float32))
        m["ctx"] = np.ascontiguousarray(np.asarray(inputs["ctx"][b], dtype=np.float32))
        m["c"] = np.ascontiguousarray(np.asarray(inputs["c"][b], dtype=np.float32))
        in_maps.append(m)
    res = run_bass_kernel_spmd(nc, in_maps, core_ids=list(range(B)))
    out = np.stack([np.asarray(r["out"], dtype=np.float32) for r in res.results], axis=0)
    return out
```

```python
import math
import numpy as np
from contextlib import ExitStack
import concourse.bass as bass
import concourse.mybir as mybir
from concourse.bass_utils import run_bass_kernel_spmd

F32 = mybir.dt.float32
BF16 = mybir.dt.bfloat16
I32 = mybir.dt.int32
AF = mybir.ActivationFunctionType
ALU = mybir.AluOpType
AX = mybir.AxisListType

COMPUTE = ("tensor", "vector", "scalar", "gpsimd")
ALLENG = ("tensor", "vector", "scalar", "gpsimd", "sync")
NDMASEM = 6

D = 1024
NCTX = 256
IN_SIZES = (256, 384, 384, 384, 384, 384, 384, 384, 384, 384, 3072)
IN_OFF = [0]
for _s in IN_SIZES:
    IN_OFF.append(IN_OFF[-1] + _s)
(O_U, O_HQ, O_HFF, O_HFB, O_HV, O_HG, O_RQ, O_RK, O_RV, O_RG, O_GZ) = IN_OFF[:11]
IN_WIDTH = IN_OFF[-1]
NE = 32
EH = 512
EPS = 1e-6


class Prog:
    def __init__(self, nc, stack):
        self.nc = nc
        self.stack = stack
        self.ops = {e: [] for e in ALLENG}
        self.sem = {}
        self.cnt = {}
        for e in COMPUTE:
            self.sem[e] = stack.enter_context(nc.semaphore("s_" + e))
            self.cnt[e] = 0
        self.dsem = {}
        self.dcnt = {}
        self.dnext = {}
        for q in ("sync", "gpsimd"):
            self.dsem[q] = [stack.enter_context(nc.semaphore("d_%s%d" % (q, i))) for i in range(NDMASEM)]
            self.dcnt[q] = [0] * NDMASEM
            self.dnext[q] = 0
        self.semid = {}
        self.last_write = {}
        self.readers = {}
        self.seen = {e: {} for e in ALLENG}
        self.nalloc = 0
        self.out_tokens = []

    def sb(self, stack, shape, dtype=F32, name=None):
        self.nalloc += 1
        name = (name or "t") + "_%d" % self.nalloc
        return stack.enter_context(self.nc.sbuf_tensor(name, list(shape), dtype))

    def ps(self, stack, shape, dtype=F32, name=None):
        self.nalloc += 1
        name = (name or "p") + "_%d" % self.nalloc
        return stack.enter_context(self.nc.psum_tensor(name, list(shape), dtype))

    def _deps(self, eng, reads, writes):
        need = {}

        def add(tok):
            s, v = tok
            k = id(s)
            self.semid[k] = s
            if need.get(k, 0) < v:
                need[k] = v

        for k in reads:
            if k in self.last_write:
                add(self.last_write[k])
        for k in writes:
            if k in self.last_write:
                add(self.last_write[k])
            for t in self.readers.get(k, ()):
                add(t)
        waits = []
        for k, v in need.items():
            s = self.semid[k]
            if eng == "tensor" and s is self.sem["tensor"]:
                continue
            if self.seen[eng].get(k, 0) >= v:
                continue
            self.seen[eng][k] = v
            waits.append((s, v))
        return waits

    def _commit(self, tok, reads, writes):
        for k in reads:
            self.readers.setdefault(k, []).append(tok)
        for k in writes:
            self.last_write[k] = tok
            self.readers[k] = []

    def op(self, eng, fn, reads=(), writes=()):
        waits = self._deps(eng, reads, writes)
        self.cnt[eng] += 1
        tok = (self.sem[eng], self.cnt[eng])
        self.ops[eng].append((fn, waits, self.sem[eng], 1))
        self._commit(tok, reads, writes)
        return tok

    def dma(self, q, out, in_, reads=(), writes=(), is_output=False):
        i = self.dnext[q]
        self.dnext[q] = (i + 1) % NDMASEM
        s = self.dsem[q][i]
        waits = self._deps(q, reads, writes)
        prev = self.dcnt[q][i]
        k = id(s)
        self.semid[k] = s
        if prev > 0 and self.seen[q].get(k, 0) < prev:
            self.seen[q][k] = prev
            waits.append((s, prev))
        self.dcnt[q][i] = prev + 16
        tok = (s, prev + 16)

        def fn(e, out=out, in_=in_):
            return e.dma_start(out=out, in_=in_)

        self.ops[q].append((fn, waits, s, 16))
        self._commit(tok, reads, writes)
        if is_output:
            self.out_tokens.append(tok)
        return tok

    def barrier(self):
        toks = []
        for e in COMPUTE:
            if self.cnt[e] > 0:
                toks.append((self.sem[e], self.cnt[e]))
        for q in self.dsem:
            for i, s in enumerate(self.dsem[q]):
                if self.dcnt[q][i] > 0:
                    toks.append((s, self.dcnt[q][i]))
        for eng in ALLENG:
            waits = []
            for s, v in toks:
                k = id(s)
                self.semid[k] = s
                if eng in COMPUTE and s is self.sem[eng]:
                    if eng == "tensor":
                        continue
                if self.seen[eng].get(k, 0) >= v:
                    continue
                self.seen[eng][k] = v
                waits.append((s, v))
            if waits:
                self.ops[eng].append((None, waits, None, 0))
        self.last_write = {}
        self.readers = {}

    def emit(self):
        nc = self.nc
        fin = list(self.out_tokens)
        with nc.Block() as block:
            def mk(eng):
                def body(e):
                    for fn, waits, s, inc in self.ops[eng]:
                        for ws, wv in waits:
                            e.wait_ge(ws, wv)
                        if fn is not None:
                            ins = fn(e)
                            ins.then_inc(s, inc)
                    if eng == "sync":
                        for ws, wv in fin:
                            e.wait_ge(ws, wv)
                return body
            block.sync(mk("sync"))
            block.tensor(mk("tensor"))
            block.vector(mk("vector"))
            block.scalar(mk("scalar"))
            block.gpsimd(mk("gpsimd"))

    def mm(self, out, lhsT, rhs, start, stop, reads, writes):
        return self.op("tensor", lambda e: e.matmul(out, lhsT, rhs, start=start, stop=stop), reads, writes)

    def act(self, out, in_, func, reads, writes, bias=None, scale=None):
        kw = {}
        if bias is not None:
            kw["bias"] = bias
        if scale is not None:
            kw["scale"] = scale
        return self.op("scalar", lambda e: e.activation(out, in_, func, **kw), reads, writes)

    def tt(self, out, in0, in1, op, reads, writes, eng="vector"):
        return self.op(eng, lambda e: e.tensor_tensor(out, in0, in1, op), reads, writes)

    def ts(self, out, in0, s1, s2, op0, op1, reads, writes, eng="vector"):
        if s2 is None:
            return self.op(eng, lambda e: e.tensor_scalar(out, in0, s1, None, op0), reads, writes)
        return self.op(eng, lambda e: e.tensor_scalar(out, in0, s1, s2, op0, op1), reads, writes)

    def stt(self, out, in0, scalar, in1, op0, op1, reads, writes):
        return self.op("vector", lambda e: e.scalar_tensor_tensor(out, in0, scalar, in1, op0, op1), reads, writes)

    def copy(self, out, in_, reads, writes, eng="vector"):
        if eng == "scalar":
            return self.op("scalar", lambda e: e.copy(out, in_), reads, writes)
        return self.op(eng, lambda e: e.tensor_copy(out, in_), reads, writes)

    def memset(self, ap, val, writes, eng="vector"):
        return self.op(eng, lambda e: e.memset(ap, val), (), writes)

    def recip(self, out, in_, reads, writes):
        return self.op("vector", lambda e: e.reciprocal(out, in_), reads, writes)


def rev_ap(a):
    ap = [list(d) for d in a.ap]
    n = ap[-1][1]
    st = ap[-1][0]
    ap[-1] = [-st, n]
    return bass.AP(a.tensor, a.offset + st * (n - 1), ap)


C_IDENT = 0
C_RESET = 128
C_IOTA = 640
C_MR = 896
C_MC = 897
C_FIDX = 898
C_SIGN = 899
C_PIDX = 900
C_RESET32 = 904
C_W = 904 + 512

M_FWD = 0
M_BWD = 128
M_BWDS = 256
M32_FWD = 384
M32_BWD = 448
M_W = 512


def host_consts(L):
    c = np.zeros((128, C_W), np.float32)
    c[:, C_IDENT:C_IDENT + 128] = np.eye(128, dtype=np.float32)
    t = np.arange(512)
    c[:, C_RESET:C_RESET + 512] = (t % 64 != 0).astype(np.float32)[None, :]
    c[:, C_IOTA:C_IOTA + 256] = np.arange(256, dtype=np.float32)[None, :]
    c[:, C_RESET32:C_RESET32 + 512] = (t % 32 != 0).astype(np.float32)[None, :]
    p = np.arange(128)
    j = p % 32
    c[:, C_MR] = (j < 16)
    c[:, C_MC] = (j >= 16)
    c[:, C_FIDX] = p % 16
    c[:, C_SIGN] = np.where((p % 64) < 32, -1.0, 1.0)
    c[:, C_PIDX] = p
    m = np.zeros((128, M_W), np.int32)
    s = (p % 64)[:, None]
    tt = np.arange(64)[None, :]
    m[:, M_FWD:M_FWD + 128] = np.tile((s <= tt).astype(np.int32), (1, 2))
    m[:, M_BWD:M_BWD + 128] = np.tile((s >= tt).astype(np.int32), (1, 2))
    m[:, M_BWDS:M_BWDS + 128] = np.tile((s > tt).astype(np.int32), (1, 2))
    s32 = (p % 32)[:, None]
    t32 = np.arange(32)[None, :]
    m[:, M32_FWD:M32_FWD + 64] = np.tile((s32 <= t32).astype(np.int32), (1, 2))
    m[:, M32_BWD:M32_BWD + 64] = np.tile((s32 >= t32).astype(np.int32), (1, 2))
    tl = np.arange(L)
    pos = np.stack([tl // 64, tl % 64]).astype(np.float32)
    return c, m, pos


PARAM_NAMES = ["c", "c_ctx", "w_ada", "b_ada", "norm1_g", "norm2_g", "w_in", "s5_lam_re", "s5_lam_im",
               "s5_log_dt", "s5_b_re", "s5_b_im", "s5_c_re", "s5_c_im", "s5_d", "s5_w_glu", "hgrn_lb_raw",
               "w_branch_s5", "w_branch_hgrn", "w_branch_ret", "w_out", "moe_w_group", "moe_b_group",
               "moe_w_expert", "moe_b_expert", "moe_w_gate", "moe_w_up", "moe_w_down", "final_norm_g"]


class Builder:
    def __init__(self, L, depth, shapes, debug=()):
        self.L = L
        self.T = NCTX + L
        self.depth = depth
        self.debug = set(debug)
        nc = bass.Bass("TRN2", target_bir_lowering=False)
        self.nc = nc
        T = self.T
        self.din = {}
        self.din["x"] = nc.dram_tensor("x", [L, D], F32, kind="ExternalInput").ap()
        self.din["ctx"] = nc.dram_tensor("ctx", [NCTX, D], F32, kind="ExternalInput").ap()
        for n in PARAM_NAMES:
            self.din[n] = nc.dram_tensor(n, list(shapes[n]), F32, kind="ExternalInput").ap()
        self.din["cst"] = nc.dram_tensor("cst", [128, C_W], F32, kind="ExternalInput").ap()
        self.din["cmask"] = nc.dram_tensor("cmask", [128, M_W], I32, kind="ExternalInput").ap()
        self.din["pos"] = nc.dram_tensor("pos", [2, L], F32, kind="ExternalInput").ap()
        self.out = nc.dram_tensor("out", [L, D], F32, kind="ExternalOutput").ap()
        self.scr = {}
        self.blocks = [(0, NCTX)] + [(NCTX + 512 * i, 512) for i in range(L // 512)]
        assert L % 512 == 0

    def dump(self, name, ap, shape, dtype, reads):
        if "dumps" not in self.debug:
            return
        t = self.nc.dram_tensor("dbg_" + name, list(shape), dtype, kind="ExternalOutput").ap()
        self.P.dma("sync", t, ap, reads=reads, writes=["dbg_" + name])

    def scratch(self, name, shape, dtype):
        kind = "ExternalOutput" if name in self.debug else "Internal"
        t = self.nc.dram_tensor("scr_" + name, list(shape), dtype, kind=kind).ap()
        self.scr[name] = t
        return t

    def build(self):
        nc = self.nc
        T, L = self.T, self.L
        with ExitStack() as top, nc.allow_non_contiguous_dma(reason="small strided parameter loads"), \
                nc.allow_low_precision(reason="bf16 matmul operands"):
            P = Prog(nc, top)
            self.P = P
            self.cst = P.sb(top, [128, C_W], F32, "cst")
            self.cmask = P.sb(top, [128, M_W], I32, "cmask")
            P.dma("sync", self.cst[:], self.din["cst"], writes=["cst"])
            P.dma("sync", self.cmask[:], self.din["cmask"], writes=["cmask"])
            self.identb = P.sb(top, [128, 128], BF16, "identb")
            P.copy(self.identb[:], self.cst[:, C_IDENT:C_IDENT + 128], ["cst"], ["identb"])
            self.onesb = P.sb(top, [128, 128], BF16, "onesb")
            P.memset(self.onesb[:], 1.0, ["onesb"])
            self.onesf = P.sb(top, [128, 128], F32, "onesf")
            P.memset(self.onesf[:], 1.0, ["onesf"])
            self.epsc = P.sb(top, [128, 1], F32, "epsc")
            P.memset(self.epsc[:], EPS, ["epsc"])
            self.mod = P.sb(top, [128, 6, 8, 2], F32, "mod")
            self.g1s = P.sb(top, [128, 8, 2], F32, "g1s")
            self.g2s = P.sb(top, [128, 8, 2], F32, "g2s")
            self.XT = self.scratch("XT", [D, T], F32)
            self.PJ = self.scratch("PJ", [O_GZ, T], BF16)
            self.LF = self.scratch("LF", [2, 384, T], F32)
            self.VT = self.scratch("VT", [2, T, 384], BF16)
            self.OA = self.scratch("OA", [2, 384, T], F32)
            self.YB = self.scratch("YB", [D, T], BF16)
            self.GT = self.scratch("GT", [T, NE], F32)
            self.phase_input()
            P.barrier()
            self.phase_rope()
            P.barrier()
            for l in range(self.depth):
                self.l = l
                self.phase_mod(l)
                P.barrier()
                self.phase_proj(l)
                P.barrier()
                if "stop_proj" in self.debug:
                    break
                self.phase_s5(l)
                P.barrier()
                if "stop_s5" in self.debug:
                    break
                if "skip_hg" not in self.debug:
                    self.phase_gla(l, 0)
                    P.barrier()
                if "skip_ret" not in self.debug:
                    self.phase_gla(l, 1)
                    P.barrier()
                if "stop_mix" in self.debug:
                    break
                self.phase_merge(l)
                P.barrier()
                if "stop_merge" in self.debug:
                    break
                self.phase_moe(l)
                P.barrier()
            self.phase_output()
            P.emit()
        return nc

    def phase_input(self):
        P, nc = self.P, self.nc
        with ExitStack() as st:
            ident = self.cst[:, C_IDENT:C_IDENT + 128]
            xin = [P.sb(st, [128, D], F32, "xin") for _ in range(2)]
            xo = [P.sb(st, [128, 8, 128], F32, "xo") for _ in range(2)]
            pt = [P.ps(st, [128, 4, 128], F32, "pt") for _ in range(2)]
            ntile = self.T // 128
            for i in range(ntile):
                b = i % 2
                if i < NCTX // 128:
                    src = self.din["ctx"][i * 128:(i + 1) * 128, :]
                else:
                    j = i - NCTX // 128
                    src = self.din["x"][j * 128:(j + 1) * 128, :]
                P.dma("sync", xin[b][:], src, writes=[("xin", b)])
                for hf in range(2):
                    for q in range(4):
                        ft = hf * 4 + q
                        P.mm(pt[hf][:, q, :], xin[b][:, ft * 128:(ft + 1) * 128], ident, True, True,
                             [("xin", b), "cst"], [("pt", hf, q)])
                    P.copy(xo[b][:, hf * 4:(hf + 1) * 4, :], pt[hf][:], [("pt", hf, q) for q in range(4)],
                           [("xo", b, hf)], eng=("vector" if hf == 0 else "scalar"))
                dst = self.XT.rearrange("(ft p) t -> p ft t", p=128)[:, :, i * 128:(i + 1) * 128]
                P.dma("sync", dst, xo[b][:], reads=[("xo", b, 0), ("xo", b, 1)], writes=[("XT", i)])

    def phase_rope(self):
        P = self.P
        L = self.L
        self.ROPE = self.scratch("ROPE", [2, 128, L], F32)
        with ExitStack() as st:
            ropc = P.sb(st, [128, L], F32, "ropc")
            rops = P.sb(st, [128, L], F32, "rops")
            posr = P.sb(st, [128, L], F32, "posr")
            posc = P.sb(st, [128, L], F32, "posc")
            P.dma("sync", posr[:], self.din["pos"][0:1, :].partition_broadcast(128), writes=["posr"])
            P.dma("sync", posc[:], self.din["pos"][1:2, :].partition_broadcast(128), writes=["posc"])
            invf = P.sb(st, [128, 1], F32, "invf")
            P.act(invf[:], self.cst[:, C_FIDX:C_FIDX + 1], AF.Exp, ["cst"], ["invf"], scale=-math.log(10000.0) / 16.0)
            P.ts(posr[:], posr[:], self.cst[:, C_MR:C_MR + 1], None, ALU.mult, None, ["posr", "cst"], ["posr"])
            P.stt(posr[:], posc[:], self.cst[:, C_MC:C_MC + 1], posr[:], ALU.mult, ALU.add, ["posc", "posr", "cst"], ["posr"])
            P.ts(posr[:], posr[:], invf[:, 0:1], None, ALU.mult, None, ["posr", "invf"], ["posr"])
            self.sincos(st, posr, ropc, rops, L, "posr", "ropc", "rops")
            P.ts(rops[:], rops[:], self.cst[:, C_SIGN:C_SIGN + 1], None, ALU.mult, None, ["rops", "cst"], ["rops"])
            P.dma("sync", self.ROPE[0], ropc[:], reads=["ropc"], writes=["ROPE0"])
            P.dma("sync", self.ROPE[1], rops[:], reads=["rops"], writes=["ROPE1"])

    def phase_mod(self, l):
        P, nc = self.P, self.nc
        with ExitStack() as st:
            cc = P.sb(st, [128, 8, 2], F32, "cc")
            P.dma("sync", cc[:, :, 0], self.din["c"].rearrange("(kt p) -> p kt", p=128), writes=["cc0"])
            P.dma("sync", cc[:, :, 1], self.din["c_ctx"].rearrange("(kt p) -> p kt", p=128), writes=["cc1"])
            sc = P.sb(st, [128, 8, 2], F32, "sc")
            P.act(sc[:], cc[:], AF.Silu, ["cc0", "cc1"], ["sc"])
            bada = P.sb(st, [128, 6, 8], F32, "bada")
            P.dma("sync", bada[:], self.din["b_ada"][l].rearrange("(j ft p) -> p j ft", p=128, ft=8), writes=["bada"])
            wa = [P.sb(st, [128, 8, 1024], F32, "wa") for _ in range(2)]
            pm = P.ps(st, [128, 8, 2], F32, "pm")
            for j in range(6):
                b = j % 2
                src = self.din["w_ada"][l].rearrange("(kt p) f -> p kt f", p=128)[:, :, j * 1024:(j + 1) * 1024]
                P.dma("sync", wa[b][:], src, writes=[("wa", b)])
                for ft in range(8):
                    for kt in range(8):
                        P.mm(pm[:, ft, :], wa[b][:, kt, ft * 128:(ft + 1) * 128], sc[:, kt, :], kt == 0, kt == 7,
                             [("wa", b), "sc"], [("pm", ft)])
                for w in range(2):
                    P.tt(self.mod[:, j, :, w], pm[:, :, w], bada[:, j, :], ALU.add,
                         [("pm", ft) for ft in range(8)] + ["bada"], [("mod", j, w)])
            for (gname, gdst, jsc) in (("norm1_g", self.g1s, 1), ("norm2_g", self.g2s, 4)):
                g = P.sb(st, [128, 8], F32, "g")
                P.dma("sync", g[:], self.din[gname][l].rearrange("(ft p) -> p ft", p=128), writes=[gname])
                for w in range(2):
                    P.stt(gdst[:, :, w], self.mod[:, jsc, :, w], 1.0, g[:], ALU.add, ALU.mult,
                          [("mod", jsc, w), gname], [(gname + "s", w)])

    def norm_block(self, st_bufs, xt, gs, jshift, who, hout, keys_in, key_out, n):
        P = self.P
        sq, pss, rstd, tmp = st_bufs
        P.act(sq[:, :, :n], xt[:, :, :n], AF.Square, keys_in, ["nb_sq"])
        for kt in range(8):
            P.mm(pss[:, :n], self.onesb[:], sq[:, kt, :n], kt == 0, kt == 7, ["nb_sq", "onesb"], ["nb_ps"])
        P.act(rstd[:, :n], pss[:, :n], AF.Sqrt, ["nb_ps"], ["nb_rstd"], bias=self.epsc[:, 0:1], scale=1.0 / D)
        P.recip(rstd[:, :n], rstd[:, :n], ["nb_rstd"], ["nb_rstd"])
        for ft in range(8):
            P.tt(tmp[:, ft, :n], xt[:, ft, :n], rstd[:, :n], ALU.mult, keys_in + ["nb_rstd"], [("nb_tmp", ft)])
            P.act(hout[:, ft, :n], tmp[:, ft, :n], AF.Identity, [("nb_tmp", ft), (gs, who), ("mod", jshift, who)],
                  [key_out], bias=self.mod[:, jshift, ft, who:who + 1],
                  scale=(self.g1s if gs == "norm1_gs" else self.g2s)[:, ft, who:who + 1])

    def alloc_norm_bufs(self, st):
        P = self.P
        sq = P.sb(st, [128, 8, 512], BF16, "nsq")
        pss = P.ps(st, [128, 512], F32, "npss")
        rstd = P.sb(st, [128, 512], F32, "nrstd")
        tmp = P.sb(st, [128, 8, 512], F32, "ntmp")
        return (sq, pss, rstd, tmp)

    def phase_proj(self, l):
        P, nc = self.P, self.nc
        T, L = self.T, self.L
        w_in = self.din["w_in"][l].rearrange("(kt p) f -> p kt f", p=128)
        XTv = self.XT.rearrange("(ft p) t -> p ft t", p=128)
        PJv = self.PJ.rearrange("(ft p) t -> p ft t", p=128)
        self.hT_scr = self.scr.get("hT") or self.scratch("hT", [D, T], BF16)
        hTv = self.hT_scr.rearrange("(ft p) t -> p ft t", p=128)
        with ExitStack() as st:
            nb = self.alloc_norm_bufs(st)
            hT = P.sb(st, [128, 8, T], BF16, "hT")
            xt = [P.sb(st, [128, 8, 512], F32, "xt") for _ in range(2)]
            for bi, (t0, n) in enumerate(self.blocks):
                b = bi % 2
                P.dma("sync", xt[b][:, :, :n], XTv[:, :, t0:t0 + n], reads=["XTall"], writes=[("xt", b)])
                self.norm_block(nb, xt[b], "norm1_gs", 0, 1 if bi == 0 else 0, hT[:, :, t0:t0 + n],
                                [("xt", b)], ("hT", bi), n)
                P.dma("sync", hTv[:, :, t0:t0 + n], hT[:, :, t0:t0 + n], reads=[("hT", bi)], writes=[("hTd", bi)])
            hkeys = [("hT", bi) for bi in range(len(self.blocks))]
            lbr = P.sb(st, [128, 3, 2, 4], F32, "lbr")
            for li in range(4):
                for d in range(2):
                    P.dma("sync", lbr[:, :, d, li], self.din["hgrn_lb_raw"][li, d].rearrange("(ft p) -> p ft", p=128),
                          writes=[("lbr", li, d)])
            lbk = [("lbr", li, d) for li in range(4) for d in range(2)]
            lbe = P.sb(st, [128, 3, 2, 4], F32, "lbe")
            P.act(lbe[:], lbr[:], AF.Exp, lbk, ["lbe"])
            lsum = P.sb(st, [128, 3, 2], F32, "lsum")
            P.op("vector", lambda e: e.tensor_reduce(lsum[:], lbe[:], AX.X, ALU.add), ["lbe"], ["lsum"])
            P.recip(lsum[:], lsum[:], ["lsum"], ["lsum"])
            lb = P.sb(st, [128, 3, 2], F32, "lb")
            oml = P.sb(st, [128, 3, 2], F32, "oml")
            P.memset(lb[:], 0.0, ["lb"])
            for li in range(1, l + 1):
                P.tt(lb[:], lb[:], lbe[:, :, :, li], ALU.add, ["lb", "lbe"], ["lb"])
            P.tt(lb[:], lb[:], lsum[:], ALU.mult, ["lb", "lsum"], ["lb"])
            P.ts(oml[:], lb[:], -1.0, 1.0, ALU.mult, ALU.add, ["lb"], ["oml"])
            rcb = [P.sb(st, [128, 512], F32, "rcb") for _ in range(2)]
            rsb = [P.sb(st, [128, 512], F32, "rsb") for _ in range(2)]
            wb = [P.sb(st, [128, 8, 384], BF16, "wb") for _ in range(3)]
            pp = [P.ps(st, [128, 512], F32, "pp") for _ in range(4)]
            ob = [P.sb(st, [128, 3, 512], BF16, "ob") for _ in range(2)]
            of = [P.sb(st, [128, 3, 512], F32, "of") for _ in range(2)]
            t1 = P.sb(st, [128, 512], F32, "pt1")
            t2 = P.sb(st, [128, 512], F32, "pt2")
            cnt = {"pp": 0, "ob": 0}

            def load_w(slot, c0, ncol, swap=False):
                if not swap:
                    P.dma("gpsimd", wb[slot][:, :, :ncol], w_in[:, :, c0:c0 + ncol], reads=[], writes=[("wb", slot)])
                else:
                    src = w_in[:, :, c0:c0 + ncol].rearrange("p kt (h two j) -> p kt h two j", two=2, j=32)
                    dst = wb[slot][:, :, :ncol].rearrange("p kt (h two j) -> p kt h two j", two=2, j=32)
                    for kt in range(8):
                        P.dma("gpsimd", dst[:, kt, :, 0, :], src[:, kt, :, 1, :], reads=[], writes=[("wb", slot, kt, 0)])
                        P.dma("gpsimd", dst[:, kt, :, 1, :], src[:, kt, :, 0, :], reads=[], writes=[("wb", slot, kt, 1)])

            def wkeys(slot, swap=False):
                if not swap:
                    return [("wb", slot)]
                return [("wb", slot, kt, x) for kt in range(8) for x in range(2)]

            def fm_proj(slot, ft, t0, n, wk):
                i = cnt["pp"] % 4
                cnt["pp"] += 1
                for kt in range(8):
                    P.mm(pp[i][:, :n], wb[slot][:, kt, ft * 128:(ft + 1) * 128], hT[:, kt, t0:t0 + n], kt == 0, kt == 7,
                         wk + hkeys, [("pp", i)])
                return i

            def feature_group(c0, nft, row0, post, extra_w=None):
                load_w(0, c0, nft * 128)
                for bi, (t0, n) in enumerate(self.blocks):
                    o = cnt["ob"] % 2
                    cnt["ob"] += 1
                    for ft in range(nft):
                        i = fm_proj(0, ft, t0, n, wkeys(0))
                        post(i, ft, n, ob[o], o, bi, t0)
                    P.dma("sync", PJv[:, row0 // 128:row0 // 128 + nft, t0:t0 + n], ob[o][:, :nft, :n],
                          reads=[("ob", o, ft) for ft in range(nft)], writes=[("PJ", row0, bi)])

            def post_copy(i, ft, n, obt, o, bi, t0):
                P.copy(obt[:, ft, :n], pp[i][:, :n], [("pp", i)], [("ob", o, ft)], eng="scalar")

            def post_silu(i, ft, n, obt, o, bi, t0):
                P.act(obt[:, ft, :n], pp[i][:, :n], AF.Silu, [("pp", i)], [("ob", o, ft)])

            feature_group(O_U, 2, O_U, post_copy)
            feature_group(O_HQ, 3, O_HQ, post_copy)
            feature_group(O_HG, 3, O_HG, post_silu)
            feature_group(O_RG, 3, O_RG, post_silu)
            LFv = self.LF.rearrange("d (ft p) t -> d p ft t", p=128)
            for d, c0 in ((0, O_HFF), (1, O_HFB)):
                load_w(0, c0, 384)
                for bi, (t0, n) in enumerate(self.blocks):
                    o = cnt["ob"] % 2
                    cnt["ob"] += 1
                    for ft in range(3):
                        i = fm_proj(0, ft, t0, n, wkeys(0))
                        P.act(t1[:, :n], pp[i][:, :n], AF.Sigmoid, [("pp", i)], ["pt1"])
                        P.ts(t1[:, :n], t1[:, :n], oml[:, ft, d:d + 1], lb[:, ft, d:d + 1], ALU.mult, ALU.add,
                             ["pt1", "oml", "lb"], ["pt1"])
                        P.act(of[o][:, ft, :n], t1[:, :n], AF.Ln, ["pt1"], [("of", o, ft)])
                        P.ts(ob[o][:, ft, :n], t1[:, :n], -1.0, 1.0, ALU.mult, ALU.add, ["pt1"], [("ob", o, ft)])
                    P.dma("sync", PJv[:, c0 // 128:c0 // 128 + 3, t0:t0 + n], ob[o][:, :3, :n],
                          reads=[("ob", o, ft) for ft in range(3)], writes=[("PJ", c0, bi)])
                    P.dma("sync", LFv[d][:, :, t0:t0 + n], of[o][:, :3, :n],
                          reads=[("of", o, ft) for ft in range(3)], writes=[("LF", d, bi)])
            for c0, scl in ((O_RQ, 1.0), (O_RK, 0.125)):
                load_w(0, c0, 384)
                load_w(1, c0, 384, swap=True)
                for bi, (t0, n) in enumerate(self.blocks):
                    o = cnt["ob"] % 2
                    cnt["ob"] += 1
                    if bi > 0:
                        l0 = t0 - NCTX
                        P.dma("sync", rcb[bi % 2][:, :n], self.ROPE[0, :, l0:l0 + n], writes=[("rcb", bi % 2)])
                        P.dma("sync", rsb[bi % 2][:, :n], self.ROPE[1, :, l0:l0 + n], writes=[("rsb", bi % 2)])
                    for ft in range(3):
                        i = fm_proj(0, ft, t0, n, wkeys(0))
                        if bi == 0:
                            P.act(ob[o][:, ft, :n], pp[i][:, :n], AF.Identity, [("pp", i)], [("ob", o, ft)], scale=scl)
                        else:
                            i2 = fm_proj(1, ft, t0, n, wkeys(1, True))
                            rb_ = bi % 2
                            P.tt(t1[:, :n], pp[i][:, :n], rcb[rb_][:, :n], ALU.mult, [("pp", i), ("rcb", rb_)], ["pt1"])
                            P.tt(t2[:, :n], pp[i2][:, :n], rsb[rb_][:, :n], ALU.mult, [("pp", i2), ("rsb", rb_)], ["pt2"])
                            P.tt(t1[:, :n], t1[:, :n], t2[:, :n], ALU.add, ["pt1", "pt2"], ["pt1"])
                            P.act(ob[o][:, ft, :n], t1[:, :n], AF.Identity, ["pt1"], [("ob", o, ft)], scale=scl)
                    P.dma("sync", PJv[:, c0 // 128:c0 // 128 + 3, t0:t0 + n], ob[o][:, :3, :n],
                          reads=[("ob", o, ft) for ft in range(3)], writes=[("PJ", c0, bi)])
            vb = [P.sb(st, [128, 384], BF16, "vb") for _ in range(2)]
            for vi, c0 in ((0, O_HV), (1, O_RV)):
                load_w(2, c0, 384)
                for tt_ in range(T // 128):
                    i = cnt["pp"] % 4
                    cnt["pp"] += 1
                    for kt in range(8):
                        P.mm(pp[i][:, :384], hT[:, kt, tt_ * 128:(tt_ + 1) * 128], wb[2][:, kt, :384], kt == 0, kt == 7,
                             [("wb", 2)] + hkeys, [("pp", i)])
                    o = tt_ % 2
                    P.copy(vb[o][:], pp[i][:, :384], [("pp", i)], [("vb", o)], eng=("vector" if o == 0 else "scalar"))
                    P.dma("sync", self.VT[vi, tt_ * 128:(tt_ + 1) * 128, :], vb[o][:], reads=[("vb", o)], writes=[("VT", vi, tt_)])

    def sincos(self, st, ang, cosd, sind, n, akey, kc, ks, bufs=None):
        P = self.P
        if bufs is None:
            ki = P.sb(st, [128, n], I32, "sc_ki")
            kf = P.sb(st, [128, n], F32, "sc_kf")
        else:
            ki, kf = bufs[0][:, :n], bufs[1][:, :n]
        P.ts(kf[:], ang[:, :n], 1.0 / (2 * math.pi), None, ALU.mult, None, [akey], ["sc_kf"])
        P.copy(ki[:], kf[:], ["sc_kf"], ["sc_ki"])
        P.copy(kf[:], ki[:], ["sc_ki"], ["sc_kf"])
        P.stt(ang[:, :n], kf[:], -2 * math.pi, ang[:, :n], ALU.mult, ALU.add, ["sc_kf", akey], [akey])
        P.ts(kf[:], ang[:, :n], math.pi, -2 * math.pi, ALU.is_gt, ALU.mult, [akey], ["sc_kf"])
        P.tt(ang[:, :n], ang[:, :n], kf[:], ALU.add, [akey, "sc_kf"], [akey])
        P.ts(kf[:], ang[:, :n], -math.pi, 2 * math.pi, ALU.is_lt, ALU.mult, [akey], ["sc_kf"])
        P.tt(ang[:, :n], ang[:, :n], kf[:], ALU.add, [akey, "sc_kf"], [akey])
        P.ts(ang[:, :n], ang[:, :n], math.pi, -math.pi, ALU.min, ALU.max, [akey], [akey])
        P.act(sind[:, :n], ang[:, :n], AF.Sin, [akey], [ks])
        P.act(kf[:], ang[:, :n], AF.Abs, [akey], ["sc_kf"])
        P.ts(kf[:], kf[:], -1.0, math.pi / 2, ALU.mult, ALU.add, ["sc_kf"], ["sc_kf"])
        P.act(cosd[:, :n], kf[:], AF.Sin, ["sc_kf"], [kc])

    def phase_output(self):
        P, nc = self.P, self.nc
        T, L = self.T, self.L
        XTv = self.XT.rearrange("(ft p) t -> p ft t", p=128)
        with ExitStack() as st:
            nb = self.alloc_norm_bufs(st)
            sq, pss, rstd, tmp = nb
            ident = self.cst[:, C_IDENT:C_IDENT + 128]
            fg = P.sb(st, [128, 8], F32, "fg")
            P.dma("sync", fg[:], self.din["final_norm_g"].rearrange("(ft p) -> p ft", p=128), writes=["fg"])
            xt = [P.sb(st, [128, 8, 512], F32, "oxt") for _ in range(2)]
            xn = [P.sb(st, [128, 8, 512], F32, "oxn") for _ in range(2)]
            po = [P.ps(st, [128, 4, 128], F32, "opo") for _ in range(2)]
            ot = [P.sb(st, [128, D], F32, "oot") for _ in range(2)]
            k = 0
            for bi, (t0, n) in enumerate(self.blocks):
                if bi == 0:
                    continue
                b = bi % 2
                P.dma("sync", xt[b][:, :, :n], XTv[:, :, t0:t0 + n], reads=["XTall"], writes=[("oxt", b)])
                P.act(sq[:, :, :n], xt[b][:, :, :n], AF.Square, [("oxt", b)], ["nb_sq"])
                for kt in range(8):
                    P.mm(pss[:, :n], self.onesb[:], sq[:, kt, :n], kt == 0, kt == 7, ["nb_sq", "onesb"], ["nb_ps"])
                P.act(rstd[:, :n], pss[:, :n], AF.Sqrt, ["nb_ps"], ["nb_rstd"], bias=self.epsc[:, 0:1], scale=1.0 / D)
                P.recip(rstd[:, :n], rstd[:, :n], ["nb_rstd"], ["nb_rstd"])
                for ft in range(8):
                    P.stt(xn[b][:, ft, :n], xt[b][:, ft, :n], fg[:, ft:ft + 1], rstd[:, :n], ALU.mult, ALU.mult,
                          [("oxt", b), "fg", "nb_rstd"], [("oxn", b, ft)])
                for s in range(n // 128):
                    o = k % 2
                    k += 1
                    for hf in range(2):
                        for q in range(4):
                            ft = hf * 4 + q
                            P.mm(po[hf][:, q, :], xn[b][:, ft, s * 128:(s + 1) * 128], ident, True, True,
                                 [("oxn", b, ft), "cst"], [("opo", hf, q)])
                        P.copy(ot[o][:, hf * 512:(hf + 1) * 512], po[hf][:].rearrange("p a b -> p (a b)"),
                               [("opo", hf, q) for q in range(4)], [("oot", o, hf)], eng=("vector" if hf == 0 else "scalar"))
                    r0 = t0 - NCTX + s * 128
                    P.dma("sync", self.out[r0:r0 + 128, :], ot[o][:], reads=[("oot", o, 0), ("oot", o, 1)],
                          writes=[("out", r0)], is_output=True)


    def phase_s5(self, l):
        P, nc = self.P, self.nc
        T, L = self.T, self.L
        PJv = self.PJ.rearrange("(ft p) t -> p ft t", p=128)
        YBv = self.YB.rearrange("(ft p) t -> p ft t", p=128)
        nblk = T // 256
        with ExitStack() as st:
            uT = P.sb(st, [128, 2, T], BF16, "uT")
            P.dma("sync", uT[:], PJv[:, 0:2, :], writes=["uT"])
            yacc = P.sb(st, [128, 2, T], F32, "yacc")
            Ec = P.sb(st, [128, 8, 256], F32, "Ec")
            Es = P.sb(st, [128, 8, 256], F32, "Es")
            Fc = P.sb(st, [128, 8, 256], F32, "Fc")
            Fs = P.sb(st, [128, 8, 256], F32, "Fs")
            ang = P.sb(st, [128, 8, 256], F32, "ang")
            tA = P.sb(st, [128, 4, 256], F32, "tA")
            tB = P.sb(st, [128, 4, 256], F32, "tB")
            tC = P.sb(st, [128, 4, 256], F32, "tC")
            tD = P.sb(st, [128, 4, 256], F32, "tD")
            xt_ = P.sb(st, [128, 4, 2, 256], F32, "xtl")
            M = P.sb(st, [128, 4, 2, 256], F32, "M")
            hb = P.sb(st, [128, 4, 2, 256], BF16, "hb")
            BD = [P.sb(st, [128, 2, 512], BF16, "BD%d" % c) for c in range(2)]
            CT = [P.sb(st, [128, 8, 128], BF16, "CT%d" % c) for c in range(2)]
            sm = {n: P.sb(st, [128, 8], F32, "s5" + n) for n in
                  ("lr", "li", "dt", "th", "mag", "c", "s", "ar", "ai", "den", "am1", "zr", "zi", "t1", "t2", "a256", "Rc", "Rs")}
            init = [P.sb(st, [128, 8, 2], F32, "init%d" % i) for i in range(2)]
            pbu = P.ps(st, [128, 4, 2, 256], F32, "pbu")
            py = P.ps(st, [128, 256], F32, "py")
            iota = self.cst[:, C_IOTA:C_IOTA + 256]
            scb = (P.sb(st, [128, 2048], I32, "sc_ki"), P.sb(st, [128, 2048], F32, "sc_kf"))
            magf = P.sb(st, [128, 8, 256], F32, "magf")
            for d in range(2):
                P.dma("sync", sm["lr"][:], self.din["s5_lam_re"][l, d].rearrange("g p -> (g p)").rearrange("(nt p) -> p nt", p=128), writes=["lr"])
                P.dma("sync", sm["li"][:], self.din["s5_lam_im"][l, d].rearrange("g p -> (g p)").rearrange("(nt p) -> p nt", p=128), writes=["li"])
                ld = self.din["s5_log_dt"][l, d]
                for half in range(2):
                    src = bass.AP(ld.tensor, ld.offset + half, [[0, 64], [2, 8]])
                    P.dma("sync", sm["dt"][half * 64:(half + 1) * 64, :], src, writes=[("dt", half)])
                P.act(sm["dt"][:], sm["dt"][:], AF.Exp, [("dt", 0), ("dt", 1)], ["dt"])
                P.tt(sm["th"][:], sm["li"][:], sm["dt"][:], ALU.mult, ["li", "dt"], ["th"])
                P.tt(sm["mag"][:], sm["lr"][:], sm["dt"][:], ALU.mult, ["lr", "dt"], ["mag"])
                P.act(sm["mag"][:], sm["mag"][:], AF.Exp, ["mag"], ["mag"])
                P.ts(sm["a256"][:], sm["th"][:], 256.0, None, ALU.mult, None, ["th"], ["a256"])
                P.copy(sm["t1"][:], sm["th"][:], ["th"], ["t1"])
                with ExitStack() as st2:
                    self.sincos(st2, sm["t1"], sm["c"], sm["s"], 8, "t1", "c", "s", scb)
                    self.sincos(st2, sm["a256"], sm["Rc"], sm["Rs"], 8, "a256", "Rc", "Rs", scb)
                    P.tt(sm["ar"][:], sm["mag"][:], sm["c"][:], ALU.mult, ["mag", "c"], ["ar"])
                    P.tt(sm["ai"][:], sm["mag"][:], sm["s"][:], ALU.mult, ["mag", "s"], ["ai"])
                    P.tt(sm["den"][:], sm["lr"][:], sm["lr"][:], ALU.mult, ["lr"], ["den"])
                    P.tt(sm["t2"][:], sm["li"][:], sm["li"][:], ALU.mult, ["li"], ["t2"])
                    P.tt(sm["den"][:], sm["den"][:], sm["t2"][:], ALU.add, ["den", "t2"], ["den"])
                    P.recip(sm["den"][:], sm["den"][:], ["den"], ["den"])
                    P.ts(sm["am1"][:], sm["ar"][:], -1.0, None, ALU.add, None, ["ar"], ["am1"])
                    P.tt(sm["zr"][:], sm["am1"][:], sm["lr"][:], ALU.mult, ["am1", "lr"], ["zr"])
                    P.tt(sm["t2"][:], sm["ai"][:], sm["li"][:], ALU.mult, ["ai", "li"], ["t2"])
                    P.tt(sm["zr"][:], sm["zr"][:], sm["t2"][:], ALU.add, ["zr", "t2"], ["zr"])
                    P.tt(sm["zr"][:], sm["zr"][:], sm["den"][:], ALU.mult, ["zr", "den"], ["zr"])
                    P.tt(sm["zi"][:], sm["ai"][:], sm["lr"][:], ALU.mult, ["ai", "lr"], ["zi"])
                    P.tt(sm["t2"][:], sm["am1"][:], sm["li"][:], ALU.mult, ["am1", "li"], ["t2"])
                    P.tt(sm["zi"][:], sm["zi"][:], sm["t2"][:], ALU.subtract, ["zi", "t2"], ["zi"])
                    P.tt(sm["zi"][:], sm["zi"][:], sm["den"][:], ALU.mult, ["zi", "den"], ["zi"])
                    P.tt(ang[:], iota.unsqueeze(1).to_broadcast([128, 8, 256]), sm["th"][:].unsqueeze(2).to_broadcast([128, 8, 256]),
                         ALU.mult, ["cst", "th"], ["ang"])
                    a2 = ang[:].rearrange("p a b -> p (a b)")
                    self.sincos(st2, ang[:].rearrange("p a b -> p (a b)"), Ec[:].rearrange("p a b -> p (a b)"),
                                Es[:].rearrange("p a b -> p (a b)"), 2048, "ang", "Ec", "Es", scb)
                for nt in range(8):
                    P.ts(magf[:, nt, :], self.onesf[:, 0:128].unsqueeze(1).to_broadcast([128, 2, 128]).rearrange("p a b -> p (a b)") if False else Ec[:, nt, :],
                         0.0, sm["mag"][:, nt:nt + 1], ALU.mult, ALU.add, ["Ec", "mag"], ["magf"])
                zrb = sm["zr"][:].unsqueeze(2).to_broadcast([128, 8, 256])
                zib = sm["zi"][:].unsqueeze(2).to_broadcast([128, 8, 256])
                P.tt(Fc[:], Ec[:], zrb, ALU.mult, ["Ec", "zr"], ["Fc"])
                P.tt(ang[:], Es[:], zib, ALU.mult, ["Es", "zi"], ["ang"])
                P.tt(Fc[:], Fc[:], ang[:], ALU.add, ["Fc", "ang"], ["Fc"])
                P.tt(Fs[:], Ec[:], zib, ALU.mult, ["Ec", "zi"], ["Fs"])
                P.tt(ang[:], Es[:], zrb, ALU.mult, ["Es", "zr"], ["ang"])
                P.tt(Fs[:], Fs[:], ang[:], ALU.subtract, ["Fs", "ang"], ["Fs"])
                for c, nm in ((0, "s5_b_re"), (1, "s5_b_im")):
                    P.memset(BD[c][:], 0.0, [("BD", c)] + [("BDg", c, g) for g in range(16)])
                    for g in range(16):
                        kt, gl = g // 8, g % 8
                        P.dma("gpsimd", BD[c][gl * 16:(gl + 1) * 16, kt, gl * 64:(gl + 1) * 64],
                              self.din[nm][l, d, g].rearrange("p h -> h p"), reads=[("BD", c)], writes=[("BDg", c, g)])
                for c, nm in ((0, "s5_c_re"), (1, "s5_c_im")):
                    P.memset(CT[c][:], 0.0, [("CT", c)] + [("CTg", c, g) for g in range(16)])
                    for g in range(16):
                        nt, g2, gl = g // 2, g % 2, g % 8
                        P.dma("gpsimd", CT[c][g2 * 64:(g2 + 1) * 64, nt, gl * 16:(gl + 1) * 16],
                              self.din[nm][l, d, g].rearrange("h p -> p h"), reads=[("CT", c)], writes=[("CTg", c, g)])
                P.ts(CT[1][:], CT[1][:], -1.0, None, ALU.mult, None, [("CT", 1)] + [("CTg", 1, g) for g in range(16)],
                     [("CT", 1)] + [("CTg", 1, g) for g in range(16)])
                if d == 0:
                    for nm_ in ("th", "mag", "zr", "zi", "Rc", "Rs", "dt", "lr", "li"):
                        self.dump(nm_, sm[nm_][:], [128, 8], F32, [nm_])
                    self.dump("Ec", Ec[:], [128, 8, 256], F32, ["Ec"])
                    self.dump("Es", Es[:], [128, 8, 256], F32, ["Es"])
                    self.dump("Fc", Fc[:], [128, 8, 256], F32, ["Fc"])
                    self.dump("BD0", BD[0][:], [128, 2, 512], BF16, [("BD", 0)] + [("BDg", 0, g) for g in range(16)])
                    self.dump("CT0", CT[0][:], [128, 8, 128], BF16, [("CT", 0)] + [("CTg", 0, g) for g in range(16)])
                    self.dump("CT1", CT[1][:], [128, 8, 128], BF16, [("CT", 1)] + [("CTg", 1, g) for g in range(16)])
                if d == 1 and "YD" in self.debug:
                    YD = self.scratch("YD", [256, T], F32)
                    P.dma("sync", YD.rearrange("(q p) t -> p q t", p=128), yacc[:], reads=[("yacc", q, b2) for q in range(2) for b2 in range(nblk)], writes=["YD"])
                order = list(range(nblk)) if d == 0 else [0] + list(range(nblk - 1, 0, -1))
                P.memset(init[0][:], 0.0, [("init", 0)])
                last = 255 if d == 0 else 0
                R = (lambda a: a) if d == 0 else rev_ap
                for bi, blk in enumerate(order):
                    t0 = blk * 256
                    ii, io = bi % 2, (bi + 1) % 2
                    for q in range(2):
                        for j in range(4):
                            for c in range(2):
                                P.mm(pbu[:, j, c, :], BD[c][:, q, j * 128:(j + 1) * 128], uT[:, q, t0:t0 + 256], True, True,
                                     [("BD", c), "uT"] + [("BDg", c, g) for g in range(16)], [("pbu", j, c)])
                        pk = [("pbu", j, c) for j in range(4) for c in range(2)]
                        fc = R(Fc[:, 4 * q:4 * q + 4, :])
                        fs = R(Fs[:, 4 * q:4 * q + 4, :])
                        ec = R(Ec[:, 4 * q:4 * q + 4, :])
                        es = R(Es[:, 4 * q:4 * q + 4, :])
                        P.tt(tA[:], pbu[:, :, 0, :], fc, ALU.mult, pk + ["Fc"], ["tA"])
                        P.tt(tB[:], pbu[:, :, 1, :], fs, ALU.mult, pk + ["Fs"], ["tB"])
                        P.tt(xt_[:, :, 0, :], tA[:], tB[:], ALU.subtract, ["tA", "tB"], [("xtl", 0)])
                        P.tt(tA[:], pbu[:, :, 1, :], fc, ALU.mult, pk + ["Fc"], ["tA"])
                        P.tt(tB[:], pbu[:, :, 0, :], fs, ALU.mult, pk + ["Fs"], ["tB"])
                        P.tt(xt_[:, :, 1, :], tA[:], tB[:], ALU.add, ["tA", "tB"], [("xtl", 1)])
                        for j in range(4):
                            nt = 4 * q + j
                            for c in range(2):
                                def f(e, j=j, c=c, nt=nt, ii=ii, R=R):
                                    return e.tensor_tensor_scan(R(M[:, j, c, :]), magf[:, nt, :],
                                                                R(xt_[:, j, c, :]), init[ii][:, nt, c:c + 1], ALU.mult, ALU.add)
                                P.op("vector", f, [("xtl", c), "magf", ("init", ii)], [("M", c)])
                        rc = sm["Rc"][:, 4 * q:4 * q + 4]
                        rs = sm["Rs"][:, 4 * q:4 * q + 4]
                        mre = M[:, :, 0, last]
                        mim = M[:, :, 1, last]
                        t1 = sm["t1"][:, 0:4]
                        t2 = sm["t2"][:, 0:4]
                        P.tt(t1, mre, rc, ALU.mult, [("M", 0), "Rc"], ["t1"])
                        P.tt(t2, mim, rs, ALU.mult, [("M", 1), "Rs"], ["t2"])
                        P.tt(init[io][:, 4 * q:4 * q + 4, 0], t1, t2, ALU.subtract, ["t1", "t2"], [("init", io)])
                        P.tt(t1, mim, rc, ALU.mult, [("M", 1), "Rc"], ["t1"])
                        P.tt(t2, mre, rs, ALU.mult, [("M", 0), "Rs"], ["t2"])
                        P.tt(init[io][:, 4 * q:4 * q + 4, 1], t1, t2, ALU.add, ["t1", "t2"], [("init", io)])
                        G_ = "vector"
                        P.tt(tC[:], M[:, :, 0, :], ec, ALU.mult, [("M", 0), "Ec"], ["tC"], eng=G_)
                        P.tt(tD[:], M[:, :, 1, :], es, ALU.mult, [("M", 1), "Es"], ["tD"], eng=G_)
                        P.tt(hb[:, :, 0, :], tC[:], tD[:], ALU.subtract, ["tC", "tD"], [("hb", 0)], eng=G_)
                        P.tt(tC[:], M[:, :, 1, :], ec, ALU.mult, [("M", 1), "Ec"], ["tC"], eng=G_)
                        P.tt(tD[:], M[:, :, 0, :], es, ALU.mult, [("M", 0), "Es"], ["tD"], eng=G_)
                        P.tt(hb[:, :, 1, :], tC[:], tD[:], ALU.add, ["tC", "tD"], [("hb", 1)], eng=G_)
                        k = 0
                        for j in range(4):
                            for c in range(2):
                                P.mm(py[:], CT[c][:, 4 * q + j, :], hb[:, j, c, :], k == 0, k == 7,
                                     [("CT", c), ("hb", c)] + [("CTg", c, g) for g in range(16)], ["py"])
                                k += 1
                        if d == 0:
                            P.copy(yacc[:, q, t0:t0 + 256], py[:], ["py"], [("yacc", q, blk)], eng="scalar")
                            if bi == 0 and q == 0:
                                self.dump("pbu", xt_[:], [128, 4, 2, 256], F32, [("xtl", 0), ("xtl", 1)])
                                self.dump("M", M[:], [128, 4, 2, 256], F32, [("M", 0), ("M", 1)])
                                self.dump("hb", hb[:], [128, 4, 2, 256], BF16, [("hb", 0), ("hb", 1)])
                        else:
                            P.tt(yacc[:, q, t0:t0 + 256], yacc[:, q, t0:t0 + 256], py[:], ALU.add, ["py", ("yacc", q, blk)], [("yacc", q, blk)])
            if False:
                YD = self.scratch("YD", [256, T], F32)
                P.dma("sync", YD.rearrange("(q p) t -> p q t", p=128), yacc[:], reads=[("yacc", q, b2) for q in range(2) for b2 in range(nblk)] + [("yg", 0), ("yg", 1)], writes=["YD"])
            dsk = P.sb(st, [128, 2], F32, "dsk")
            P.dma("sync", dsk[:], self.din["s5_d"][l].rearrange("(kt p) -> p kt", p=128), writes=["dsk"])
            wg = P.sb(st, [128, 2, 256], BF16, "wglu")
            P.dma("gpsimd", wg[:], self.din["s5_w_glu"][l].rearrange("(kt p) f -> p kt f", p=128), reads=[], writes=["wglu"])
            yb = P.sb(st, [128, 2, 512], BF16, "ybf")
            yo = [P.sb(st, [128, 2, 512], BF16, "yo") for _ in range(2)]
            sg = P.sb(st, [128, 512], F32, "sg")
            pg = P.ps(st, [128, 512], F32, "pg")
            for bi, (t0, n) in enumerate(self.blocks):
                o = bi % 2
                yk = [("yacc", q, b2) for q in range(2) for b2 in range(nblk)]
                for q in range(2):
                    P.stt(yacc[:, q, t0:t0 + n], uT[:, q, t0:t0 + n], dsk[:, q:q + 1], yacc[:, q, t0:t0 + n], ALU.mult, ALU.add,
                          ["uT", "dsk"] + yk, [("yg", q)])
                    P.act(yacc[:, q, t0:t0 + n], yacc[:, q, t0:t0 + n], AF.Gelu_apprx_tanh, [("yg", q)], [("yg", q)])
                    P.copy(yb[:, q, :n], yacc[:, q, t0:t0 + n], [("yg", q)], [("ybf", q)])
                for ft in range(2):
                    for kt in range(2):
                        P.mm(pg[:, :n], wg[:, kt, ft * 128:(ft + 1) * 128], yb[:, kt, :n], kt == 0, kt == 1,
                             ["wglu", ("ybf", 0), ("ybf", 1)], ["pg"])
                    P.act(sg[:, :n], pg[:, :n], AF.Sigmoid, ["pg"], ["sg"])
                    P.tt(yo[o][:, ft, :n], yacc[:, ft, t0:t0 + n], sg[:, :n], ALU.mult, ["sg", ("yg", ft)], [("yo", o, ft)])
                P.dma("sync", YBv[:, 0:2, t0:t0 + n], yo[o][:, :, :n], reads=[("yo", o, 0), ("yo", o, 1)], writes=[("YB", 0, bi)])

    def phase_gla(self, l, which):
        P, nc = self.P, self.nc
        T, L = self.T, self.L
        PJ = self.PJ
        qoff = O_HQ if which == 0 else O_RQ
        goff = O_HG if which == 0 else O_RG
        yrow0 = 256 + which * 384
        CS = 32 if which == 0 else 64
        NH = 6
        with ExitStack() as st:
            def mk(shape, dt, nm):
                return [P.sb(st, shape, dt, nm) for _ in range(NH)]
            qf = mk([64, 512], BF16, "qf")
            kf = mk([64, 512], BF16, "kf")
            qt = mk([64, 512], BF16, "qt")
            ktl = mk([64, 512 + 64], BF16, "ktl")
            qh = mk([64, 512], BF16, "qh")
            kd = mk([64, 512 + 64], BF16, "kd")
            kdT = mk([64, 512 // CS, 64], BF16, "kdT")
            vv = mk([64, 512 // CS, 64], BF16, "vv")
            S = mk([64, 64], F32, "S")
            Sb = mk([64, 64], BF16, "Sb")
            ob = mk([64, 512], F32, "obk")
            oa = mk([64, 512], F32, "oak")
            ebend = mk([64, 16], F32, "ebend")
            Asb = [[[P.sb(st, [64, CS], BF16, "Asb") for _ in range(2)] for _ in range(2)] for _ in range(NH)]
            for hd in range(NH):
                P.memset(ktl[hd][:], 0.0, [("ktl", hd)])
                P.memset(kd[hd][:], 0.0, [("kd", hd)])
                for dd in range(2):
                    for sl in range(2):
                        P.memset(Asb[hd][dd][sl][:], 0.0, [("Asb", hd, dd, sl)])
            lf = [P.sb(st, [64, 512], F32, "lf") for _ in range(2)]
            bb = [P.sb(st, [64, 512], F32, "bb") for _ in range(2)]
            d1 = [P.sb(st, [64, 512], F32, "d1") for _ in range(2)]
            ex = [P.sb(st, [64, 512], F32, "ex") for _ in range(2)]
            gsb = [P.sb(st, [64, 512], BF16, "gsb") for _ in range(2)]
            osq = [P.sb(st, [64, 512], BF16, "osq") for _ in range(2)]
            rs_ = [P.sb(st, [64, 512], F32, "rs_") for _ in range(2)]
            yo = [P.sb(st, [64, 512], BF16, "yo") for _ in range(2)]
            LB = [P.ps(st, [128, 512], F32, "LB") for _ in range(NH)]
            PT = P.ps(st, [128, 512], F32, "PT")
            PN = P.ps(st, [128, 512], F32, "PN")
            if which == 1:
                tb = [{n: P.sb(st, [64, 64], F32, "rt" + n) for n in ("b", "q", "k", "e", "d")} for _ in range(NH)]
                ebr = mk([64, 1], F32, "ebr")
                for hd in range(NH):
                    lgc = math.log(1.0 - 2.0 ** (-5.0 - hd))
                    t_ = tb[hd]
                    P.ts(t_["b"][:], self.cst[0:64, C_IOTA:C_IOTA + 64], 1.0, lgc, ALU.add, ALU.mult, ["cst"], [("rtb", hd)])
                    P.act(t_["e"][:], t_["b"][:], AF.Exp, [("rtb", hd)], [("rte", hd)])
                    P.ts(t_["q"][:], t_["b"][:], t_["b"][:, 31:32], None, ALU.subtract, None, [("rtb", hd)], [("rtq", hd)])
                    P.act(t_["k"][:], t_["q"][:], AF.Exp, [("rtq", hd)], [("rtk", hd)], scale=-1.0)
                    P.act(t_["q"][:], t_["q"][:], AF.Exp, [("rtq", hd)], [("rtq", hd)])
                    P.ts(t_["d"][:], t_["b"][:], t_["b"][:, 63:64], None, ALU.subtract, None, [("rtb", hd)], [("rtd", hd)])
                    P.act(t_["d"][:], t_["d"][:], AF.Exp, [("rtd", hd)], [("rtd", hd)], scale=-1.0)
                    P.copy(ebr[hd][:], t_["e"][:, 63:64], [("rte", hd)], [("ebr", hd)])
            if which == 0:
                mask_f = self.cmask[0:CS, M32_FWD:M32_FWD + 32]
                mask_b = self.cmask[0:CS, M32_BWD:M32_BWD + 32]
            else:
                mask_f = self.cmask[0:CS, M_FWD:M_FWD + 64]
                mask_b = self.cmask[0:CS, M_BWDS:M_BWDS + 64]
            reset = self.cst[0:64, C_RESET32:C_RESET32 + 512]
            nstep = 0
            npre = 0
            for d in range(2):
                order = list(range(len(self.blocks))) if d == 0 else [0] + list(range(len(self.blocks) - 1, 0, -1))
                R = (lambda a: a) if d == 0 else rev_ap
                mask = mask_f if d == 0 else mask_b
                for hd in range(NH):
                    P.memset(S[hd][:], 0.0, [("S", hd)])
                    P.memset(Sb[hd][:], 0.0, [("Sb", hd)])
                for blk in order:
                    t0, n = self.blocks[blk]
                    nch = n // CS
                    pm = (CS // 2 - 1) if d == 0 else CS // 2
                    pe = (CS - 1) if d == 0 else 0
                    for hd in range(NH):
                        u = npre % 2
                        npre += 1
                        koff = (O_HFF if d == 0 else O_HFB) if which == 0 else O_RK
                        r0 = qoff + hd * 64
                        P.dma("sync", qf[hd][:, :n], PJ[r0:r0 + 64, t0:t0 + n], writes=[("qf", hd)])
                        r1 = koff + hd * 64
                        P.dma("sync", kf[hd][:, :n], PJ[r1:r1 + 64, t0:t0 + n], writes=[("kf", hd)])
                        P.dma("sync", vv[hd][0:CS, :n // CS, :],
                              self.VT[which, t0:t0 + n, hd * 64:(hd + 1) * 64].rearrange("(a p) c -> p a c", p=CS), writes=[("vv", hd)])
                        if d == 1:
                            P.dma("sync", oa[hd][:, :n], self.OA[which, hd * 64:(hd + 1) * 64, t0:t0 + n], writes=[("oak", hd)])
                        if which == 0:
                            P.dma("sync", lf[u][:, :n], self.LF[d, hd * 64:(hd + 1) * 64, t0:t0 + n], writes=[("lf", u)])
                            rr, rb, rl = reset[:, :n], R(bb[u][:, :n]), R(lf[u][:, :n])
                            P.op("vector", (lambda e, rr=rr, rb=rb, rl=rl: e.tensor_tensor_scan(rb, rr, rl, 0.0, ALU.mult, ALU.add)),
                                 [("lf", u), "cst"], [("bb", u)])
                            b3 = bb[u][:, :n].rearrange("p (c i) -> p c i", i=CS)
                            d3 = d1[u][:, :n].rearrange("p (c i) -> p c i", i=CS)
                            kb, kd1, kex = ("bb", u), ("d1", u), ("ex", u)
                            P.tt(d3, b3, b3[:, :, pm:pm + 1].to_broadcast([64, nch, CS]), ALU.subtract, [kb], [kd1])
                            P.act(ex[u][:, :n], d1[u][:, :n], AF.Exp, [kd1], [kex])
                            P.tt(qt[hd][:, :n], qf[hd][:, :n], ex[u][:, :n], ALU.mult, [("qf", hd), kex], [("qt", hd)])
                            P.act(ex[u][:, :n], d1[u][:, :n], AF.Exp, [kd1], [kex], scale=-1.0)
                            P.tt(ktl[hd][:, :n], kf[hd][:, :n], ex[u][:, :n], ALU.mult, [("kf", hd), kex], [("ktl", hd)])
                            P.act(ex[u][:, :n], bb[u][:, :n], AF.Exp, [kb], [kex])
                            P.tt(qh[hd][:, :n], qf[hd][:, :n], ex[u][:, :n], ALU.mult, [("qf", hd), kex], [("qh", hd)])
                            P.tt(d3, b3, b3[:, :, pe:pe + 1].to_broadcast([64, nch, CS]), ALU.subtract, [kb], [kd1])
                            P.act(ex[u][:, :n], d1[u][:, :n], AF.Exp, [kd1], [kex], scale=-1.0)
                            P.tt(kd[hd][:, :n], kf[hd][:, :n], ex[u][:, :n], ALU.mult, [("kf", hd), kex], [("kd", hd)])
                            P.act(ebend[hd][:, :nch], b3[:, :, pe], AF.Exp, [kb], [("ebend", hd)])
                        else:
                            q3 = qf[hd][:, :n].rearrange("p (c i) -> p c i", i=CS)
                            k3 = kf[hd][:, :n].rearrange("p (c i) -> p c i", i=CS)

                            def tbc(nm, R=R, nch=nch, hd=hd):
                                return R(tb[hd][nm][:]).unsqueeze(1).to_broadcast([64, nch, CS])
                            P.tt(qt[hd][:, :n].rearrange("p (c i) -> p c i", i=CS), q3, tbc("q"), ALU.mult, [("qf", hd), ("rtq", hd)], [("qt", hd)])
                            P.tt(ktl[hd][:, :n].rearrange("p (c i) -> p c i", i=CS), k3, tbc("k"), ALU.mult, [("kf", hd), ("rtk", hd)], [("ktl", hd)])
                            P.tt(qh[hd][:, :n].rearrange("p (c i) -> p c i", i=CS), q3, tbc("e"), ALU.mult, [("qf", hd), ("rte", hd)], [("qh", hd)])
                            P.tt(kd[hd][:, :n].rearrange("p (c i) -> p c i", i=CS), k3, tbc("d"), ALU.mult, [("kf", hd), ("rtd", hd)], [("kd", hd)])
                        for a in range(n // CS):
                            pc = (a % 4) * 64
                            P.mm(PT[0:64, pc:pc + 64], kd[hd][:, a * CS:a * CS + 64], self.identb[0:64, 0:64], True, True,
                                 [("kd", hd), "identb"], ["PT"])
                            if a % 4 == 3 or a == n // CS - 1:
                                a0 = a - (a % 4)
                                na = a - a0 + 1
                                P.copy(kdT[hd][0:CS, a0:a0 + na, :], PT[0:CS, 0:na * 64].rearrange("p (a k) -> p a k", k=64), ["PT"],
                                       [("kdT", hd)], eng="scalar")
                    corder = list(range(nch)) if d == 0 else list(range(nch - 1, -1, -1))
                    for c in corder:
                        sl = nstep % 2
                        nstep += 1
                        cs = slice(c * CS, (c + 1) * CS)
                        def regs(hd):
                            return (LB[hd][0:64, sl * 192:sl * 192 + CS], LB[hd][0:64, sl * 192 + 64:sl * 192 + 64 + CS],
                                    LB[hd][0:64, 384:448], Asb[hd][d][sl], ("Asb", hd, d, sl), ("LB", hd))
                        for hd in range(NH):
                            PAr, POr, PSr, A, ak, lk = regs(hd)
                            P.mm(PAr, ktl[hd][:, c * CS:c * CS + 64], qt[hd][:, cs], True, True, [("ktl", hd), ("qt", hd)], [lk])
                        for hd in range(NH):
                            PAr, POr, PSr, A, ak, lk = regs(hd)
                            P.op("vector", (lambda e, A=A, PAr=PAr, mask=mask: e.copy_predicated(A[0:CS, :], mask, PAr[0:CS, :])),
                                 [lk, "cmask", ak], [ak])
                        for hd in range(NH):
                            PAr, POr, PSr, A, ak, lk = regs(hd)
                            P.mm(POr, vv[hd][0:CS, c, :], A[0:CS, :], True, False, [("vv", hd), ak], [lk])
                            P.mm(POr, Sb[hd][:, :], qh[hd][:, cs], False, True, [("Sb", hd), ("qh", hd)], [lk])
                        for hd in range(NH):
                            PAr, POr, PSr, A, ak, lk = regs(hd)
                            if d == 0:
                                P.copy(ob[hd][:, cs], POr, [lk], [("obk", hd)], eng="scalar")
                            else:
                                P.tt(ob[hd][:, cs], POr, oa[hd][:, cs], ALU.add, [lk, ("oak", hd)], [("obk", hd)])
                        for hd in range(NH):
                            PAr, POr, PSr, A, ak, lk = regs(hd)
                            P.mm(PSr, kdT[hd][0:CS, c, :], vv[hd][0:CS, c, :], True, True, [("kdT", hd), ("vv", hd)], [lk])
                        for hd in range(NH):
                            PAr, POr, PSr, A, ak, lk = regs(hd)
                            esc = ebend[hd][:, c:c + 1] if which == 0 else ebr[hd][:, 0:1]
                            P.stt(S[hd][:], S[hd][:], esc, PSr, ALU.mult, ALU.add,
                                  [("S", hd), ("ebend", hd) if which == 0 else ("ebr", hd), lk], [("S", hd)])
                        for hd in range(NH):
                            P.copy(Sb[hd][:], S[hd][:], [("S", hd)], [("Sb", hd)], eng="scalar")
                    for hd in range(NH):
                        u = hd % 2
                        if d == 0:
                            P.dma("sync", self.OA[which, hd * 64:(hd + 1) * 64, t0:t0 + n], ob[hd][:, :n], reads=[("obk", hd)],
                                  writes=[("OA", blk, hd)])
                        else:
                            gr = goff + hd * 64
                            P.dma("sync", gsb[u][:, :n], PJ[gr:gr + 64, t0:t0 + n], writes=[("gsb", u)])
                            P.act(osq[u][:, :n], ob[hd][:, :n], AF.Square, [("obk", hd)], [("osq", u)])
                            P.mm(PN[0:64, :n], self.onesb[0:64, 0:64], osq[u][:, :n], True, True, [("osq", u), "onesb"], ["PN"])
                            P.act(rs_[u][:, :n], PN[0:64, :n], AF.Sqrt, ["PN"], [("rs_", u)], bias=self.epsc[0:64, 0:1], scale=1.0 / 64)
                            P.recip(rs_[u][:, :n], rs_[u][:, :n], [("rs_", u)], [("rs_", u)])
                            P.tt(rs_[u][:, :n], rs_[u][:, :n], ob[hd][:, :n], ALU.mult, [("rs_", u), ("obk", hd)], [("rs_", u)])
                            P.tt(yo[u][:, :n], rs_[u][:, :n], gsb[u][:, :n], ALU.mult, [("rs_", u), ("gsb", u)], [("yo", u)])
                            yr = yrow0 + hd * 64
                            P.dma("sync", self.YB[yr:yr + 64, t0:t0 + n], yo[u][:, :n], reads=[("yo", u)], writes=[("YBg", blk, hd)])

    def phase_merge(self, l):
        P, nc = self.P, self.nc
        T, L = self.T, self.L
        XTv = self.XT.rearrange("(ft p) t -> p ft t", p=128)
        YBv = self.YB.rearrange("(ft p) t -> p ft t", p=128)
        hTv = self.hT_scr.rearrange("(ft p) t -> p ft t", p=128)
        self.h2T_scr = self.scr.get("h2T") or self.scratch("h2T", [D, T], BF16)
        self.GTT = self.scr.get("GTT") or self.scratch("GTT", [NE, T], F32)
        h2v = self.h2T_scr.rearrange("(ft p) t -> p ft t", p=128)
        w_in = self.din["w_in"][l].rearrange("(kt p) f -> p kt f", p=128)
        with ExitStack() as st:
            nb = self.alloc_norm_bufs(st)
            gz = P.sb(st, [128, 8, 3072], BF16, "gz")
            for j in range(3):
                P.dma("gpsimd", gz[:, :, j * 1024:(j + 1) * 1024], w_in[:, :, O_GZ + j * 1024:O_GZ + (j + 1) * 1024], writes=[("gz", j)])
            wbr = P.sb(st, [128, 8, 1024], BF16, "wbr")
            P.dma("gpsimd", wbr[:, 0:2, :], self.din["w_branch_s5"][l].rearrange("(kt p) f -> p kt f", p=128), writes=[("wbr", 0)])
            P.dma("gpsimd", wbr[:, 2:5, :], self.din["w_branch_hgrn"][l].rearrange("(kt p) f -> p kt f", p=128), writes=[("wbr", 1)])
            P.dma("gpsimd", wbr[:, 5:8, :], self.din["w_branch_ret"][l].rearrange("(kt p) f -> p kt f", p=128), writes=[("wbr", 2)])
            wout = P.sb(st, [128, 8, 1024], BF16, "wout")
            P.dma("gpsimd", wout[:], self.din["w_out"][l].rearrange("(kt p) f -> p kt f", p=128), writes=["wout"])
            wr = P.sb(st, [128, 8, 36], F32, "wr")
            P.dma("sync", wr[:, :, 0:4], self.din["moe_w_group"][l].rearrange("(kt p) e -> p kt e", p=128), writes=[("wr", 0)])
            P.dma("sync", wr[:, :, 4:36], self.din["moe_w_expert"][l].rearrange("(kt p) e -> p kt e", p=128), writes=[("wr", 1)])
            brow = P.sb(st, [128, 36], F32, "brow")
            P.dma("sync", brow[:, 0:4], self.din["moe_b_group"][l:l + 1, :].partition_broadcast(128), writes=[("brow", 0)])
            P.dma("sync", brow[:, 4:36], self.din["moe_b_expert"][l:l + 1, :].partition_broadcast(128), writes=[("brow", 1)])
            hT = P.sb(st, [128, 8, 512], BF16, "mhT")
            yb = P.sb(st, [128, 8, 512], BF16, "myb")
            xt = P.sb(st, [128, 8, 512], F32, "mxt")
            sig = P.sb(st, [128, 3, 512], F32, "msig")
            mt = P.sb(st, [128, 512], F32, "mmt")
            mt2 = P.sb(st, [128, 512], F32, "mmt2")
            mg = P.sb(st, [128, 8, 512], BF16, "mmg")
            h2f = P.sb(st, [128, 8, 512], F32, "h2f")
            h2b = P.sb(st, [128, 8, 512], BF16, "h2b")
            pgt = [P.ps(st, [128, 512], F32, "pgt") for _ in range(3)]
            pbt = [P.ps(st, [128, 512], F32, "pbt") for _ in range(2)]
            px = P.ps(st, [128, 512], F32, "px")
            prt = P.ps(st, [128, 512], F32, "prt")
            rt = {n: P.sb(st, [128, w], F32, "r_" + n) for n, w in
                  (("l36", 36), ("gmax", 1), ("eg", 4), ("gsum", 1), ("og", 4), ("pen", 4), ("lem", 32), ("m1", 1), ("oh1", 32),
                   ("lem2", 32), ("m2", 1), ("oh2", 32), ("r", 1), ("w1", 1), ("w2", 1), ("G", 64))}
            P.memset(rt["G"][:], 0.0, ["G"])
            gts = P.sb(st, [32, 128], F32, "gts")
            ident = self.cst[:, C_IDENT:C_IDENT + 128]
            ktr = ((0, 2), (2, 5), (5, 8))
            nbr = 0
            for bi, (t0, n) in enumerate(self.blocks):
                who = 1 if bi == 0 else 0
                P.dma("sync", hT[:, :, :n], hTv[:, :, t0:t0 + n], writes=["mhT"])
                P.dma("sync", yb[:, :, :n], YBv[:, :, t0:t0 + n], writes=["myb"])
                P.dma("sync", xt[:, :, :n], XTv[:, :, t0:t0 + n], writes=["mxt"])
                for ft in range(8):
                    fs = slice(ft * 128, (ft + 1) * 128)
                    for j in range(3):
                        for kt in range(8):
                            P.mm(pgt[j][:, :n], gz[:, kt, j * 1024 + ft * 128:j * 1024 + (ft + 1) * 128], hT[:, kt, :n], kt == 0, kt == 7,
                                 [("gz", j), "mhT"], [("pgt", j)])
                        P.act(sig[:, j, :n], pgt[j][:, :n], AF.Sigmoid, [("pgt", j)], [("msig", j)])
                    for j in range(3):
                        pb = nbr % 2
                        nbr += 1
                        k0, k1 = ktr[j]
                        for kt in range(k0, k1):
                            P.mm(pbt[pb][:, :n], wbr[:, kt, fs], yb[:, kt, :n], kt == k0, kt == k1 - 1, [("wbr", j), "myb"], [("pbt", pb)])
                        if j == 0:
                            P.tt(mt[:, :n], pbt[pb][:, :n], sig[:, j, :n], ALU.mult, [("pbt", pb), ("msig", j)], ["mmt"])
                        else:
                            P.tt(mt2[:, :n], pbt[pb][:, :n], sig[:, j, :n], ALU.mult, [("pbt", pb), ("msig", j)], ["mmt2"])
                            if j == 1:
                                P.tt(mt[:, :n], mt[:, :n], mt2[:, :n], ALU.add, ["mmt", "mmt2"], ["mmt"])
                            else:
                                P.tt(mg[:, ft, :n], mt[:, :n], mt2[:, :n], ALU.add, ["mmt", "mmt2"], [("mmg", ft)])
                mk = [("mmg", ft) for ft in range(8)]
                for ft in range(8):
                    for kt in range(8):
                        P.mm(px[:, :n], wout[:, kt, ft * 128:(ft + 1) * 128], mg[:, kt, :n], kt == 0, kt == 7, ["wout"] + mk, ["px"])
                    P.stt(xt[:, ft, :n], px[:, :n], self.mod[:, 2, ft, who:who + 1], xt[:, ft, :n], ALU.mult, ALU.add,
                          ["px", ("mod", 2, who), "mxt"], ["mxt"])
                P.dma("sync", XTv[:, :, t0:t0 + n], xt[:, :, :n], reads=["mxt"], writes=[("XTw", bi)])
                self.norm_block(nb, xt, "norm2_gs", 3, who, h2f, ["mxt"], "h2f", n)
                P.copy(h2b[:, :, :n], h2f[:, :, :n], ["h2f"], ["h2b"], eng="gpsimd")
                P.dma("sync", h2v[:, :, t0:t0 + n], h2b[:, :, :n], reads=["h2b"], writes=[("h2T", bi)])
                for sblk in range(n // 128):
                    ss = slice(sblk * 128, (sblk + 1) * 128)
                    for kt in range(8):
                        P.mm(prt[:, 0:36], h2f[:, kt, ss], wr[:, kt, :], kt == 0, kt == 7, ["h2f", ("wr", 0), ("wr", 1)], ["prt"])
                    r_ = rt
                    P.tt(r_["l36"][:], prt[:, 0:36], brow[:], ALU.add, ["prt", ("brow", 0), ("brow", 1)], ["l36"])
                    lg4 = r_["l36"][:, 0:4]
                    le = r_["l36"][:, 4:36]
                    P.op("vector", (lambda e, o=r_["gmax"][:], i=lg4: e.tensor_reduce(o, i, AX.X, ALU.max)), ["l36"], ["gmax"])
                    P.ts(r_["eg"][:], lg4, r_["gmax"][:, 0:1], None, ALU.subtract, None, ["l36", "gmax"], ["eg"])
                    P.act(r_["eg"][:], r_["eg"][:], AF.Exp, ["eg"], ["eg"])
                    P.op("vector", (lambda e, o=r_["gsum"][:], i=r_["eg"][:]: e.tensor_reduce(o, i, AX.X, ALU.add)), ["eg"], ["gsum"])
                    P.recip(r_["gsum"][:], r_["gsum"][:], ["gsum"], ["gsum"])
                    P.ts(r_["og"][:], lg4, r_["gmax"][:, 0:1], None, ALU.is_ge, None, ["l36", "gmax"], ["og"])
                    P.ts(r_["pen"][:], r_["og"][:], -1.0, 1.0e4, ALU.add, ALU.mult, ["og"], ["pen"])
                    P.tt(r_["lem"][:].rearrange("p (g j) -> p g j", g=4), le.rearrange("p (g j) -> p g j", g=4),
                         r_["pen"][:].unsqueeze(2).to_broadcast([128, 4, 8]), ALU.add, ["l36", "pen"], ["lem"])
                    P.op("vector", (lambda e, o=r_["m1"][:], i=r_["lem"][:]: e.tensor_reduce(o, i, AX.X, ALU.max)), ["lem"], ["m1"])
                    P.ts(r_["oh1"][:], r_["lem"][:], r_["m1"][:, 0:1], None, ALU.is_ge, None, ["lem", "m1"], ["oh1"])
                    P.stt(r_["lem2"][:], r_["oh1"][:], -1.0e4, r_["lem"][:], ALU.mult, ALU.add, ["oh1", "lem"], ["lem2"])
                    P.op("vector", (lambda e, o=r_["m2"][:], i=r_["lem2"][:]: e.tensor_reduce(o, i, AX.X, ALU.max)), ["lem2"], ["m2"])
                    P.ts(r_["oh2"][:], r_["lem2"][:], r_["m2"][:, 0:1], None, ALU.is_ge, None, ["lem2", "m2"], ["oh2"])
                    P.tt(r_["r"][:], r_["m2"][:], r_["m1"][:], ALU.subtract, ["m1", "m2"], ["r"])
                    P.act(r_["r"][:], r_["r"][:], AF.Exp, ["r"], ["r"])
                    P.ts(r_["w1"][:], r_["r"][:], 1.0, None, ALU.add, None, ["r"], ["w1"])
                    P.recip(r_["w1"][:], r_["w1"][:], ["w1"], ["w1"])
                    P.tt(r_["w1"][:], r_["w1"][:], r_["gsum"][:], ALU.mult, ["w1", "gsum"], ["w1"])
                    P.tt(r_["w2"][:], r_["w1"][:], r_["r"][:], ALU.mult, ["w1", "r"], ["w2"])
                    P.ts(r_["G"][:, 0:32], r_["oh1"][:], r_["w1"][:, 0:1], None, ALU.mult, None, ["oh1", "w1"], ["G"])
                    P.stt(r_["G"][:, 0:32], r_["oh2"][:], r_["w2"][:, 0:1], r_["G"][:, 0:32], ALU.mult, ALU.add, ["oh2", "w2", "G"], ["G"])
                    P.mm(prt[0:64, 128:256], r_["G"][:], ident, True, True, ["G", "cst"], ["prt"])
                    P.copy(gts[:], prt[0:32, 128:256], ["prt"], ["gts"], eng="scalar")
                    c0 = t0 + sblk * 128
                    P.dma("sync", self.GTT[:, c0:c0 + 128], gts[:], reads=["gts"], writes=[("GTT", c0)])

    def phase_moe(self, l):
        P, nc = self.P, self.nc
        T, L = self.T, self.L
        XTv = self.XT.rearrange("(ft p) t -> p ft t", p=128)
        h2v = self.h2T_scr.rearrange("(ft p) t -> p ft t", p=128)
        groups, cur, tot = [], [], 0
        for b in self.blocks:
            if tot + b[1] > 1536:
                groups.append(cur)
                cur, tot = [], 0
            cur.append(b)
            tot += b[1]
        groups.append(cur)
        with ExitStack() as st:
            h2 = P.sb(st, [128, 8, 1536], BF16, "eh2")
            acc = P.sb(st, [128, 8, 1536], F32, "eacc")
            grep = [P.sb(st, [128, 1536], F32, "egrep") for _ in range(2)]
            wg = [P.sb(st, [128, 8, 512], BF16, "ewg") for _ in range(2)]
            wu = [P.sb(st, [128, 8, 512], BF16, "ewu") for _ in range(2)]
            wd = [P.sb(st, [128, 4, 1024], BF16, "ewd") for _ in range(2)]
            sg = [P.sb(st, [128, 512], F32, "esg") for _ in range(2)]
            hu = [P.sb(st, [128, 512], F32, "ehu") for _ in range(2)]
            hid = [P.sb(st, [128, 4, 512], BF16, "ehid") for _ in range(2)]
            xt = P.sb(st, [128, 8, 512], F32, "ext")
            pg = [P.ps(st, [128, 512], F32, "epg") for _ in range(2)]
            pu = [P.ps(st, [128, 512], F32, "epu") for _ in range(2)]
            pd = [P.ps(st, [128, 2, 512], F32, "epd") for _ in range(2)]
            nj = 0
            nf = 0
            for grp in groups:
                g0 = grp[0][0]
                ng = sum(b[1] for b in grp)
                P.dma("sync", h2[:, :, :ng], h2v[:, :, g0:g0 + ng], writes=["eh2"])
                P.memset(acc[:], 0.0, ["eacc"])
                units = [(e, t0, n) for e in range(NE) for (t0, n) in grp]

                def emit_gu(ui):
                    nonlocal nj
                    e, t0, n = units[ui]
                    s_ = e % 2
                    hs_ = ui % 2
                    o = t0 - g0
                    if t0 == grp[0][0]:
                        P.dma("gpsimd", wg[s_][:], self.din["moe_w_gate"][l, e].rearrange("(kt p) f -> p kt f", p=128), writes=[("ewg", s_)])
                        P.dma("gpsimd", wu[s_][:], self.din["moe_w_up"][l, e].rearrange("(kt p) f -> p kt f", p=128), writes=[("ewu", s_)])
                        P.dma("gpsimd", wd[s_][:], self.din["moe_w_down"][l, e].rearrange("(jt p) f -> p jt f", p=128), writes=[("ewd", s_)])
                        P.dma("sync", grep[s_][:, :ng], self.GTT[e:e + 1, g0:g0 + ng].partition_broadcast(128), writes=[("egrep", s_)])
                    for jt in range(4):
                        u = nj % 2
                        nj += 1
                        for kt in range(8):
                            P.mm(pg[u][:, :n], wg[s_][:, kt, jt * 128:(jt + 1) * 128], h2[:, kt, o:o + n], kt == 0, kt == 7,
                                 [("ewg", s_), "eh2"], [("epg", u)])
                        for kt in range(8):
                            P.mm(pu[u][:, :n], wu[s_][:, kt, jt * 128:(jt + 1) * 128], h2[:, kt, o:o + n], kt == 0, kt == 7,
                                 [("ewu", s_), "eh2"], [("epu", u)])
                        P.act(sg[u][:, :n], pg[u][:, :n], AF.Silu, [("epg", u)], [("esg", u)])
                        P.tt(hu[u][:, :n], pu[u][:, :n], sg[u][:, :n], ALU.mult, [("epu", u), ("esg", u)], [("ehu", u)])
                        P.tt(hid[hs_][:, jt, :n], hu[u][:, :n], grep[s_][:, o:o + n], ALU.mult, [("ehu", u), ("egrep", s_)], [("ehid", hs_, jt)],
                             eng="gpsimd")

                def emit_d(ui):
                    nonlocal nf
                    e, t0, n = units[ui]
                    s_ = e % 2
                    hs_ = ui % 2
                    o = t0 - g0
                    hk = [("ehid", hs_, jt) for jt in range(4)]
                    for fp in range(4):
                        u = nf % 2
                        nf += 1
                        for fq in range(2):
                            ft = fp * 2 + fq
                            for jt in range(4):
                                P.mm(pd[u][:, fq, :n], wd[s_][:, jt, ft * 128:(ft + 1) * 128], hid[hs_][:, jt, :n], jt == 0, jt == 3,
                                     [("ewd", s_)] + hk, [("epd", u, fq)])
                        P.tt(acc[:, fp * 2:(fp + 1) * 2, o:o + n], acc[:, fp * 2:(fp + 1) * 2, o:o + n], pd[u][:, :, :n], ALU.add,
                             ["eacc", ("epd", u, 0), ("epd", u, 1)], ["eacc"])

                for ui in range(len(units)):
                    emit_gu(ui)
                    if ui > 0:
                        emit_d(ui - 1)
                emit_d(len(units) - 1)
                for (t0, n) in grp:
                    o = t0 - g0
                    who = 1 if t0 == 0 else 0
                    P.dma("sync", xt[:, :, :n], XTv[:, :, t0:t0 + n], writes=["ext"])
                    for ft in range(8):
                        P.stt(xt[:, ft, :n], acc[:, ft, o:o + n], self.mod[:, 5, ft, who:who + 1], xt[:, ft, :n], ALU.mult, ALU.add,
                              ["eacc", ("mod", 5, who), "ext"], ["ext"])
                    P.dma("sync", XTv[:, :, t0:t0 + n], xt[:, :, :n], reads=["ext"], writes=[("XTe", t0)])


_CACHE = {}


def kernel(**inputs):
    L = inputs["x"].shape[1]
    B = inputs["x"].shape[0]
    depth = inputs["w_in"].shape[0]
    shapes = {n: tuple(inputs[n].shape) for n in PARAM_NAMES}
    shapes["c"] = (D,)
    key = (L, depth)
    if key not in _CACHE:
        _CACHE[key] = Builder(L, depth, shapes).build()
    nc = _CACHE[key]
    cst, cmask, pos = host_consts(L)
    shared = {n: np.ascontiguousarray(np.asarray(inputs[n], dtype=np.float32)) for n in PARAM_NAMES if n != "c"}
    shared["cst"] = cst
    shared["cmask"] = cmask
    shared["pos"] = pos
    in_maps = []
    for b in range(B):
        m = dict(shared)
        m["x"] = np.ascontiguousarray(np.asarray(inputs["x"][b], dtype=np.float32))
        m["ctx"] = np.ascontiguousarray(np.asarray(inputs["ctx"][b], dtype=np.float32))
        m["c"] = np.ascontiguousarray(np.asarray(inputs["c"][b], dtype=np.float32))
        in_maps.append(m)
    res = run_bass_kernel_spmd(nc, in_maps, core_ids=list(range(B)))
    out = np.stack([np.asarray(r["out"], dtype=np.float32) for r in res.results], axis=0)
    return out
```

```python
import math
import numpy as np
from contextlib import ExitStack
import concourse.bass as bass
import concourse.mybir as mybir
from concourse.bass_utils import run_bass_kernel_spmd

F32 = mybir.dt.float32
BF16 = mybir.dt.bfloat16
I32 = mybir.dt.int32
AF = mybir.ActivationFunctionType
ALU = mybir.AluOpType
AX = mybir.AxisListType

COMPUTE = ("tensor", "vector", "scalar", "gpsimd")
ALLENG = ("tensor", "vector", "scalar", "gpsimd", "sync")
NDMASEM = 6

D = 1024
NCTX = 256
IN_SIZES = (256, 384, 384, 384, 384, 384, 384, 384, 384, 384, 3072)
IN_OFF = [0]
for _s in IN_SIZES:
    IN_OFF.append(IN_OFF[-1] + _s)
(O_U, O_HQ, O_HFF, O_HFB, O_HV, O_HG, O_RQ, O_RK, O_RV, O_RG, O_GZ) = IN_OFF[:11]
IN_WIDTH = IN_OFF[-1]
NE = 32
EH = 512
EPS = 1e-6


class Prog:
    def __init__(self, nc, stack):
        self.nc = nc
        self.stack = stack
        self.ops = {e: [] for e in ALLENG}
        self.sem = {}
        self.cnt = {}
        for e in COMPUTE:
            self.sem[e] = stack.enter_context(nc.semaphore("s_" + e))
            self.cnt[e] = 0
        self.dsem = {}
        self.dcnt = {}
        self.dnext = {}
        for q in ("sync", "gpsimd"):
            self.dsem[q] = [stack.enter_context(nc.semaphore("d_%s%d" % (q, i))) for i in range(NDMASEM)]
            self.dcnt[q] = [0] * NDMASEM
            self.dnext[q] = 0
        self.semid = {}
        self.last_write = {}
        self.readers = {}
        self.seen = {e: {} for e in ALLENG}
        self.nalloc = 0
        self.out_tokens = []

    def sb(self, stack, shape, dtype=F32, name=None):
        self.nalloc += 1
        name = (name or "t") + "_%d" % self.nalloc
        return stack.enter_context(self.nc.sbuf_tensor(name, list(shape), dtype))

    def ps(self, stack, shape, dtype=F32, name=None):
        self.nalloc += 1
        name = (name or "p") + "_%d" % self.nalloc
        return stack.enter_context(self.nc.psum_tensor(name, list(shape), dtype))

    def _deps(self, eng, reads, writes):
        need = {}

        def add(tok):
            s, v = tok
            k = id(s)
            self.semid[k] = s
            if need.get(k, 0) < v:
                need[k] = v

        for k in reads:
            if k in self.last_write:
                add(self.last_write[k])
        for k in writes:
            if k in self.last_write:
                add(self.last_write[k])
            for t in self.readers.get(k, ()):
                add(t)
        waits = []
        for k, v in need.items():
            s = self.semid[k]
            if eng == "tensor" and s is self.sem["tensor"]:
                continue
            if self.seen[eng].get(k, 0) >= v:
                continue
            self.seen[eng][k] = v
            waits.append((s, v))
        return waits

    def _commit(self, tok, reads, writes):
        for k in reads:
            self.readers.setdefault(k, []).append(tok)
        for k in writes:
            self.last_write[k] = tok
            self.readers[k] = []

    def op(self, eng, fn, reads=(), writes=()):
        waits = self._deps(eng, reads, writes)
        self.cnt[eng] += 1
        tok = (self.sem[eng], self.cnt[eng])
        self.ops[eng].append((fn, waits, self.sem[eng], 1))
        self._commit(tok, reads, writes)
        return tok

    def dma(self, q, out, in_, reads=(), writes=(), is_output=False):
        i = self.dnext[q]
        self.dnext[q] = (i + 1) % NDMASEM
        s = self.dsem[q][i]
        waits = self._deps(q, reads, writes)
        prev = self.dcnt[q][i]
        k = id(s)
        self.semid[k] = s
        if prev > 0 and self.seen[q].get(k, 0) < prev:
            self.seen[q][k] = prev
            waits.append((s, prev))
        self.dcnt[q][i] = prev + 16
        tok = (s, prev + 16)

        def fn(e, out=out, in_=in_):
            return e.dma_start(out=out, in_=in_)

        self.ops[q].append((fn, waits, s, 16))
        self._commit(tok, reads, writes)
        if is_output:
            self.out_tokens.append(tok)
        return tok

    def barrier(self):
        toks = []
        for e in COMPUTE:
            if self.cnt[e] > 0:
                toks.append((self.sem[e], self.cnt[e]))
        for q in self.dsem:
            for i, s in enumerate(self.dsem[q]):
                if self.dcnt[q][i] > 0:
                    toks.append((s, self.dcnt[q][i]))
        for eng in ALLENG:
            waits = []
            for s, v in toks:
                k = id(s)
                self.semid[k] = s
                if eng in COMPUTE and s is self.sem[eng]:
                    if eng == "tensor":
                        continue
                if self.seen[eng].get(k, 0) >= v:
                    continue
                self.seen[eng][k] = v
                waits.append((s, v))
            if waits:
                self.ops[eng].append((None, waits, None, 0))
        self.last_write = {}
        self.readers = {}

    def emit(self):
        nc = self.nc
        fin = list(self.out_tokens)
        with nc.Block() as block:
            def mk(eng):
                def body(e):
                    for fn, waits, s, inc in self.ops[eng]:
                        for ws, wv in waits:
                            e.wait_ge(ws, wv)
                        if fn is not None:
                            ins = fn(e)
                            ins.then_inc(s, inc)
                    if eng == "sync":
                        for ws, wv in fin:
                            e.wait_ge(ws, wv)
                return body
            block.sync(mk("sync"))
            block.tensor(mk("tensor"))
            block.vector(mk("vector"))
            block.scalar(mk("scalar"))
            block.gpsimd(mk("gpsimd"))

    def mm(self, out, lhsT, rhs, start, stop, reads, writes):
        return self.op("tensor", lambda e: e.matmul(out, lhsT, rhs, start=start, stop=stop), reads, writes)

    def act(self, out, in_, func, reads, writes, bias=None, scale=None):
        kw = {}
        if bias is not None:
            kw["bias"] = bias
        if scale is not None:
            kw["scale"] = scale
        return self.op("scalar", lambda e: e.activation(out, in_, func, **kw), reads, writes)

    def tt(self, out, in0, in1, op, reads, writes, eng="vector"):
        return self.op(eng, lambda e: e.tensor_tensor(out, in0, in1, op), reads, writes)

    def ts(self, out, in0, s1, s2, op0, op1, reads, writes, eng="vector"):
        if s2 is None:
            return self.op(eng, lambda e: e.tensor_scalar(out, in0, s1, None, op0), reads, writes)
        return self.op(eng, lambda e: e.tensor_scalar(out, in0, s1, s2, op0, op1), reads, writes)

    def stt(self, out, in0, scalar, in1, op0, op1, reads, writes):
        return self.op("vector", lambda e: e.scalar_tensor_tensor(out, in0, scalar, in1, op0, op1), reads, writes)

    def copy(self, out, in_, reads, writes, eng="vector"):
        if eng == "scalar":
            return self.op("scalar", lambda e: e.copy(out, in_), reads, writes)
        return self.op(eng, lambda e: e.tensor_copy(out, in_), reads, writes)

    def memset(self, ap, val, writes, eng="vector"):
        return self.op(eng, lambda e: e.memset(ap, val), (), writes)

    def recip(self, out, in_, reads, writes):
        return self.op("vector", lambda e: e.reciprocal(out, in_), reads, writes)


def rev_ap(a):
    ap = [list(d) for d in a.ap]
    n = ap[-1][1]
    st = ap[-1][0]
    ap[-1] = [-st, n]
    return bass.AP(a.tensor, a.offset + st * (n - 1), ap)


C_IDENT = 0
C_RESET = 128
C_IOTA = 640
C_MR = 896
C_MC = 897
C_FIDX = 898
C_SIGN = 899
C_PIDX = 900
C_RESET32 = 904
C_W = 904 + 512

M_FWD = 0
M_BWD = 128
M_BWDS = 256
M32_FWD = 384
M32_BWD = 448
M_W = 512


def host_consts(L):
    c = np.zeros((128, C_W), np.float32)
    c[:, C_IDENT:C_IDENT + 128] = np.eye(128, dtype=np.float32)
    t = np.arange(512)
    c[:, C_RESET:C_RESET + 512] = (t % 64 != 0).astype(np.float32)[None, :]
    c[:, C_IOTA:C_IOTA + 256] = np.arange(256, dtype=np.float32)[None, :]
    c[:, C_RESET32:C_RESET32 + 512] = (t % 32 != 0).astype(np.float32)[None, :]
    p = np.arange(128)
    j = p % 32
    c[:, C_MR] = (j < 16)
    c[:, C_MC] = (j >= 16)
    c[:, C_FIDX] = p % 16
    c[:, C_SIGN] = np.where((p % 64) < 32, -1.0, 1.0)
    c[:, C_PIDX] = p
    m = np.zeros((128, M_W), np.int32)
    s = (p % 64)[:, None]
    tt = np.arange(64)[None, :]
    m[:, M_FWD:M_FWD + 128] = np.tile((s <= tt).astype(np.int32), (1, 2))
    m[:, M_BWD:M_BWD + 128] = np.tile((s >= tt).astype(np.int32), (1, 2))
    m[:, M_BWDS:M_BWDS + 128] = np.tile((s > tt).astype(np.int32), (1, 2))
    s32 = (p % 32)[:, None]
    t32 = np.arange(32)[None, :]
    m[:, M32_FWD:M32_FWD + 64] = np.tile((s32 <= t32).astype(np.int32), (1, 2))
    m[:, M32_BWD:M32_BWD + 64] = np.tile((s32 >= t32).astype(np.int32), (1, 2))
    tl = np.arange(L)
    pos = np.stack([tl // 64, tl % 64]).astype(np.float32)
    return c, m, pos


PARAM_NAMES = ["c", "c_ctx", "w_ada", "b_ada", "norm1_g", "norm2_g", "w_in", "s5_lam_re", "s5_lam_im",
               "s5_log_dt", "s5_b_re", "s5_b_im", "s5_c_re", "s5_c_im", "s5_d", "s5_w_glu", "hgrn_lb_raw",
               "w_branch_s5", "w_branch_hgrn", "w_branch_ret", "w_out", "moe_w_group", "moe_b_group",
               "moe_w_expert", "moe_b_expert", "moe_w_gate", "moe_w_up", "moe_w_down", "final_norm_g"]


class Builder:
    def __init__(self, L, depth, shapes, debug=()):
        self.L = L
        self.T = NCTX + L
        self.depth = depth
        self.debug = set(debug)
        nc = bass.Bass("TRN2", target_bir_lowering=False)
        self.nc = nc
        T = self.T
        self.din = {}
        self.din["x"] = nc.dram_tensor("x", [L, D], F32, kind="ExternalInput").ap()
        self.din["ctx"] = nc.dram_tensor("ctx", [NCTX, D], F32, kind="ExternalInput").ap()
        for n in PARAM_NAMES:
            self.din[n] = nc.dram_tensor(n, list(shapes[n]), F32, kind="ExternalInput").ap()
        self.din["cst"] = nc.dram_tensor("cst", [128, C_W], F32, kind="ExternalInput").ap()
        self.din["cmask"] = nc.dram_tensor("cmask", [128, M_W], I32, kind="ExternalInput").ap()
        self.din["pos"] = nc.dram_tensor("pos", [2, L], F32, kind="ExternalInput").ap()
        self.out = nc.dram_tensor("out", [L, D], F32, kind="ExternalOutput").ap()
        self.scr = {}
        self.blocks = [(0, NCTX)] + [(NCTX + 512 * i, 512) for i in range(L // 512)]
        assert L % 512 == 0

    def dump(self, name, ap, shape, dtype, reads):
        if "dumps" not in self.debug:
            return
        t = self.nc.dram_tensor("dbg_" + name, list(shape), dtype, kind="ExternalOutput").ap()
        self.P.dma("sync", t, ap, reads=reads, writes=["dbg_" + name])

    def scratch(self, name, shape, dtype):
        kind = "ExternalOutput" if name in self.debug else "Internal"
        t = self.nc.dram_tensor("scr_" + name, list(shape), dtype, kind=kind).ap()
        self.scr[name] = t
        return t

    def build(self):
        nc = self.nc
        T, L = self.T, self.L
        with ExitStack() as top, nc.allow_non_contiguous_dma(reason="small strided parameter loads"), \
                nc.allow_low_precision(reason="bf16 matmul operands"):
            P = Prog(nc, top)
            self.P = P
            self.cst = P.sb(top, [128, C_W], F32, "cst")
            self.cmask = P.sb(top, [128, M_W], I32, "cmask")
            P.dma("sync", self.cst[:], self.din["cst"], writes=["cst"])
            P.dma("sync", self.cmask[:], self.din["cmask"], writes=["cmask"])
            self.identb = P.sb(top, [128, 128], BF16, "identb")
            P.copy(self.identb[:], self.cst[:, C_IDENT:C_IDENT + 128], ["cst"], ["identb"])
            self.onesb = P.sb(top, [128, 128], BF16, "onesb")
            P.memset(self.onesb[:], 1.0, ["onesb"])
            self.onesf = P.sb(top, [128, 128], F32, "onesf")
            P.memset(self.onesf[:], 1.0, ["onesf"])
            self.epsc = P.sb(top, [128, 1], F32, "epsc")
            P.memset(self.epsc[:], EPS, ["epsc"])
            self.mod = P.sb(top, [128, 6, 8, 2], F32, "mod")
            self.g1s = P.sb(top, [128, 8, 2], F32, "g1s")
            self.g2s = P.sb(top, [128, 8, 2], F32, "g2s")
            self.XT = self.scratch("XT", [D, T], F32)
            self.PJ = self.scratch("PJ", [O_GZ, T], BF16)
            self.LF = self.scratch("LF", [2, 384, T], F32)
            self.VT = self.scratch("VT", [2, T, 384], BF16)
            self.OA = self.scratch("OA", [2, 384, T], F32)
            self.YB = self.scratch("YB", [D, T], BF16)
            self.GT = self.scratch("GT", [T, NE], F32)
            self.phase_input()
            P.barrier()
            self.phase_rope()
            P.barrier()
            for l in range(self.depth):
                self.l = l
                self.phase_mod(l)
                P.barrier()
                self.phase_proj(l)
                P.barrier()
                if "stop_proj" in self.debug:
                    break
                self.phase_s5(l)
                P.barrier()
                if "stop_s5" in self.debug:
                    break
                if "skip_hg" not in self.debug:
                    self.phase_gla(l, 0)
                    P.barrier()
                if "skip_ret" not in self.debug:
                    self.phase_gla(l, 1)
                    P.barrier()
                if "stop_mix" in self.debug:
                    break
                self.phase_merge(l)
                P.barrier()
                if "stop_merge" in self.debug:
                    break
                self.phase_moe(l)
                P.barrier()
            self.phase_output()
            P.emit()
        return nc

    def phase_input(self):
        P, nc = self.P, self.nc
        with ExitStack() as st:
            ident = self.cst[:, C_IDENT:C_IDENT + 128]
            xin = [P.sb(st, [128, D], F32, "xin") for _ in range(2)]
            xo = [P.sb(st, [128, 8, 128], F32, "xo") for _ in range(2)]
            pt = [P.ps(st, [128, 4, 128], F32, "pt") for _ in range(2)]
            ntile = self.T // 128
            for i in range(ntile):
                b = i % 2
                if i < NCTX // 128:
                    src = self.din["ctx"][i * 128:(i + 1) * 128, :]
                else:
                    j = i - NCTX // 128
                    src = self.din["x"][j * 128:(j + 1) * 128, :]
                P.dma("sync", xin[b][:], src, writes=[("xin", b)])
                for hf in range(2):
                    for q in range(4):
                        ft = hf * 4 + q
                        P.mm(pt[hf][:, q, :], xin[b][:, ft * 128:(ft + 1) * 128], ident, True, True,
                             [("xin", b), "cst"], [("pt", hf, q)])
                    P.copy(xo[b][:, hf * 4:(hf + 1) * 4, :], pt[hf][:], [("pt", hf, q) for q in range(4)],
                           [("xo", b, hf)], eng=("vector" if hf == 0 else "scalar"))
                dst = self.XT.rearrange("(ft p) t -> p ft t", p=128)[:, :, i * 128:(i + 1) * 128]
                P.dma("sync", dst, xo[b][:], reads=[("xo", b, 0), ("xo", b, 1)], writes=[("XT", i)])

    def phase_rope(self):
        P = self.P
        L = self.L
        self.ROPE = self.scratch("ROPE", [2, 128, L], F32)
        with ExitStack() as st:
            ropc = P.sb(st, [128, L], F32, "ropc")
            rops = P.sb(st, [128, L], F32, "rops")
            posr = P.sb(st, [128, L], F32, "posr")
            posc = P.sb(st, [128, L], F32, "posc")
            P.dma("sync", posr[:], self.din["pos"][0:1, :].partition_broadcast(128), writes=["posr"])
            P.dma("sync", posc[:], self.din["pos"][1:2, :].partition_broadcast(128), writes=["posc"])
            invf = P.sb(st, [128, 1], F32, "invf")
            P.act(invf[:], self.cst[:, C_FIDX:C_FIDX + 1], AF.Exp, ["cst"], ["invf"], scale=-math.log(10000.0) / 16.0)
            P.ts(posr[:], posr[:], self.cst[:, C_MR:C_MR + 1], None, ALU.mult, None, ["posr", "cst"], ["posr"])
            P.stt(posr[:], posc[:], self.cst[:, C_MC:C_MC + 1], posr[:], ALU.mult, ALU.add, ["posc", "posr", "cst"], ["posr"])
            P.ts(posr[:], posr[:], invf[:, 0:1], None, ALU.mult, None, ["posr", "invf"], ["posr"])
            self.sincos(st, posr, ropc, rops, L, "posr", "ropc", "rops")
            P.ts(rops[:], rops[:], self.cst[:, C_SIGN:C_SIGN + 1], None, ALU.mult, None, ["rops", "cst"], ["rops"])
            P.dma("sync", self.ROPE[0], ropc[:], reads=["ropc"], writes=["ROPE0"])
            P.dma("sync", self.ROPE[1], rops[:], reads=["rops"], writes=["ROPE1"])

    def phase_mod(self, l):
        P, nc = self.P, self.nc
        with ExitStack() as st:
            cc = P.sb(st, [128, 8, 2], F32, "cc")
            P.dma("sync", cc[:, :, 0], self.din["c"].rearrange("(kt p) -> p kt", p=128), writes=["cc0"])
            P.dma("sync", cc[:, :, 1], self.din["c_ctx"].rearrange("(kt p) -> p kt", p=128), writes=["cc1"])
            sc = P.sb(st, [128, 8, 2], F32, "sc")
            P.act(sc[:], cc[:], AF.Silu, ["cc0", "cc1"], ["sc"])
            bada = P.sb(st, [128, 6, 8], F32, "bada")
            P.dma("sync", bada[:], self.din["b_ada"][l].rearrange("(j ft p) -> p j ft", p=128, ft=8), writes=["bada"])
            wa = [P.sb(st, [128, 8, 1024], F32, "wa") for _ in range(2)]
            pm = P.ps(st, [128, 8, 2], F32, "pm")
            for j in range(6):
                b = j % 2
                src = self.din["w_ada"][l].rearrange("(kt p) f -> p kt f", p=128)[:, :, j * 1024:(j + 1) * 1024]
                P.dma("sync", wa[b][:], src, writes=[("wa", b)])
                for ft in range(8):
                    for kt in range(8):
                        P.mm(pm[:, ft, :], wa[b][:, kt, ft * 128:(ft + 1) * 128], sc[:, kt, :], kt == 0, kt == 7,
                             [("wa", b), "sc"], [("pm", ft)])
                for w in range(2):
                    P.tt(self.mod[:, j, :, w], pm[:, :, w], bada[:, j, :], ALU.add,
                         [("pm", ft) for ft in range(8)] + ["bada"], [("mod", j, w)])
            for (gname, gdst, jsc) in (("norm1_g", self.g1s, 1), ("norm2_g", self.g2s, 4)):
                g = P.sb(st, [128, 8], F32, "g")
                P.dma("sync", g[:], self.din[gname][l].rearrange("(ft p) -> p ft", p=128), writes=[gname])
                for w in range(2):
                    P.stt(gdst[:, :, w], self.mod[:, jsc, :, w], 1.0, g[:], ALU.add, ALU.mult,
                          [("mod", jsc, w), gname], [(gname + "s", w)])

    def norm_block(self, st_bufs, xt, gs, jshift, who, hout, keys_in, key_out, n):
        P = self.P
        sq, pss, rstd, tmp = st_bufs
        P.act(sq[:, :, :n], xt[:, :, :n], AF.Square, keys_in, ["nb_sq"])
        for kt in range(8):
            P.mm(pss[:, :n], self.onesb[:], sq[:, kt, :n], kt == 0, kt == 7, ["nb_sq", "onesb"], ["nb_ps"])
        P.act(rstd[:, :n], pss[:, :n], AF.Sqrt, ["nb_ps"], ["nb_rstd"], bias=self.epsc[:, 0:1], scale=1.0 / D)
        P.recip(rstd[:, :n], rstd[:, :n], ["nb_rstd"], ["nb_rstd"])
        for ft in range(8):
            P.tt(tmp[:, ft, :n], xt[:, ft, :n], rstd[:, :n], ALU.mult, keys_in + ["nb_rstd"], [("nb_tmp", ft)])
            P.act(hout[:, ft, :n], tmp[:, ft, :n], AF.Identity, [("nb_tmp", ft), (gs, who), ("mod", jshift, who)],
                  [key_out], bias=self.mod[:, jshift, ft, who:who + 1],
                  scale=(self.g1s if gs == "norm1_gs" else self.g2s)[:, ft, who:who + 1])

    def alloc_norm_bufs(self, st):
        P = self.P
        sq = P.sb(st, [128, 8, 512], BF16, "nsq")
        pss = P.ps(st, [128, 512], F32, "npss")
        rstd = P.sb(st, [128, 512], F32, "nrstd")
        tmp = P.sb(st, [128, 8, 512], F32, "ntmp")
        return (sq, pss, rstd, tmp)

    def phase_proj(self, l):
        P, nc = self.P, self.nc
        T, L = self.T, self.L
        w_in = self.din["w_in"][l].rearrange("(kt p) f -> p kt f", p=128)
        XTv = self.XT.rearrange("(ft p) t -> p ft t", p=128)
        PJv = self.PJ.rearrange("(ft p) t -> p ft t", p=128)
        self.hT_scr = self.scr.get("hT") or self.scratch("hT", [D, T], BF16)
        hTv = self.hT_scr.rearrange("(ft p) t -> p ft t", p=128)
        with ExitStack() as st:
            nb = self.alloc_norm_bufs(st)
            hT = P.sb(st, [128, 8, T], BF16, "hT")
            xt = [P.sb(st, [128, 8, 512], F32, "xt") for _ in range(2)]
            for bi, (t0, n) in enumerate(self.blocks):
                b = bi % 2
                P.dma("sync", xt[b][:, :, :n], XTv[:, :, t0:t0 + n], reads=["XTall"], writes=[("xt", b)])
                self.norm_block(nb, xt[b], "norm1_gs", 0, 1 if bi == 0 else 0, hT[:, :, t0:t0 + n],
                                [("xt", b)], ("hT", bi), n)
                P.dma("sync", hTv[:, :, t0:t0 + n], hT[:, :, t0:t0 + n], reads=[("hT", bi)], writes=[("hTd", bi)])
            hkeys = [("hT", bi) for bi in range(len(self.blocks))]
            lbr = P.sb(st, [128, 3, 2, 4], F32, "lbr")
            for li in range(4):
                for d in range(2):
                    P.dma("sync", lbr[:, :, d, li], self.din["hgrn_lb_raw"][li, d].rearrange("(ft p) -> p ft", p=128),
                          writes=[("lbr", li, d)])
            lbk = [("lbr", li, d) for li in range(4) for d in range(2)]
            lbe = P.sb(st, [128, 3, 2, 4], F32, "lbe")
            P.act(lbe[:], lbr[:], AF.Exp, lbk, ["lbe"])
            lsum = P.sb(st, [128, 3, 2], F32, "lsum")
            P.op("vector", lambda e: e.tensor_reduce(lsum[:], lbe[:], AX.X, ALU.add), ["lbe"], ["lsum"])
            P.recip(lsum[:], lsum[:], ["lsum"], ["lsum"])
            lb = P.sb(st, [128, 3, 2], F32, "lb")
            oml = P.sb(st, [128, 3, 2], F32, "oml")
            P.memset(lb[:], 0.0, ["lb"])
            for li in range(1, l + 1):
                P.tt(lb[:], lb[:], lbe[:, :, :, li], ALU.add, ["lb", "lbe"], ["lb"])
            P.tt(lb[:], lb[:], lsum[:], ALU.mult, ["lb", "lsum"], ["lb"])
            P.ts(oml[:], lb[:], -1.0, 1.0, ALU.mult, ALU.add, ["lb"], ["oml"])
            rcb = [P.sb(st, [128, 512], F32, "rcb") for _ in range(2)]
            rsb = [P.sb(st, [128, 512], F32, "rsb") for _ in range(2)]
            wb = [P.sb(st, [128, 8, 384], BF16, "wb") for _ in range(3)]
            pp = [P.ps(st, [128, 512], F32, "pp") for _ in range(4)]
            ob = [P.sb(st, [128, 3, 512], BF16, "ob") for _ in range(2)]
            of = [P.sb(st, [128, 3, 512], F32, "of") for _ in range(2)]
            t1 = P.sb(st, [128, 512], F32, "pt1")
            t2 = P.sb(st, [128, 512], F32, "pt2")
            cnt = {"pp": 0, "ob": 0}

            def load_w(slot, c0, ncol, swap=False):
                if not swap:
                    P.dma("gpsimd", wb[slot][:, :, :ncol], w_in[:, :, c0:c0 + ncol], reads=[], writes=[("wb", slot)])
                else:
                    src = w_in[:, :, c0:c0 + ncol].rearrange("p kt (h two j) -> p kt h two j", two=2, j=32)
                    dst = wb[slot][:, :, :ncol].rearrange("p kt (h two j) -> p kt h two j", two=2, j=32)
                    for kt in range(8):
                        P.dma("gpsimd", dst[:, kt, :, 0, :], src[:, kt, :, 1, :], reads=[], writes=[("wb", slot, kt, 0)])
                        P.dma("gpsimd", dst[:, kt, :, 1, :], src[:, kt, :, 0, :], reads=[], writes=[("wb", slot, kt, 1)])

            def wkeys(slot, swap=False):
                if not swap:
                    return [("wb", slot)]
                return [("wb", slot, kt, x) for kt in range(8) for x in range(2)]

            def fm_proj(slot, ft, t0, n, wk):
                i = cnt["pp"] % 4
                cnt["pp"] += 1
                for kt in range(8):
                    P.mm(pp[i][:, :n], wb[slot][:, kt, ft * 128:(ft + 1) * 128], hT[:, kt, t0:t0 + n], kt == 0, kt == 7,
                         wk + hkeys, [("pp", i)])
                return i

            def feature_group(c0, nft, row0, post, extra_w=None):
                load_w(0, c0, nft * 128)
                for bi, (t0, n) in enumerate(self.blocks):
                    o = cnt["ob"] % 2
                    cnt["ob"] += 1
                    for ft in range(nft):
                        i = fm_proj(0, ft, t0, n, wkeys(0))
                        post(i, ft, n, ob[o], o, bi, t0)
                    P.dma("sync", PJv[:, row0 // 128:row0 // 128 + nft, t0:t0 + n], ob[o][:, :nft, :n],
                          reads=[("ob", o, ft) for ft in range(nft)], writes=[("PJ", row0, bi)])

            def post_copy(i, ft, n, obt, o, bi, t0):
                P.copy(obt[:, ft, :n], pp[i][:, :n], [("pp", i)], [("ob", o, ft)], eng="scalar")

            def post_silu(i, ft, n, obt, o, bi, t0):
                P.act(obt[:, ft, :n], pp[i][:, :n], AF.Silu, [("pp", i)], [("ob", o, ft)])

            feature_group(O_U, 2, O_U, post_copy)
            feature_group(O_HQ, 3, O_HQ, post_copy)
            feature_group(O_HG, 3, O_HG, post_silu)
            feature_group(O_RG, 3, O_RG, post_silu)
            LFv = self.LF.rearrange("d (ft p) t -> d p ft t", p=128)
            for d, c0 in ((0, O_HFF), (1, O_HFB)):
                load_w(0, c0, 384)
                for bi, (t0, n) in enumerate(self.blocks):
                    o = cnt["ob"] % 2
                    cnt["ob"] += 1
                    for ft in range(3):
                        i = fm_proj(0, ft, t0, n, wkeys(0))
                        P.act(t1[:, :n], pp[i][:, :n], AF.Sigmoid, [("pp", i)], ["pt1"])
                        P.ts(t1[:, :n], t1[:, :n], oml[:, ft, d:d + 1], lb[:, ft, d:d + 1], ALU.mult, ALU.add,
                             ["pt1", "oml", "lb"], ["pt1"])
                        P.act(of[o][:, ft, :n], t1[:, :n], AF.Ln, ["pt1"], [("of", o, ft)])
                        P.ts(ob[o][:, ft, :n], t1[:, :n], -1.0, 1.0, ALU.mult, ALU.add, ["pt1"], [("ob", o, ft)])
                    P.dma("sync", PJv[:, c0 // 128:c0 // 128 + 3, t0:t0 + n], ob[o][:, :3, :n],
                          reads=[("ob", o, ft) for ft in range(3)], writes=[("PJ", c0, bi)])
                    P.dma("sync", LFv[d][:, :, t0:t0 + n], of[o][:, :3, :n],
                          reads=[("of", o, ft) for ft in range(3)], writes=[("LF", d, bi)])
            for c0, scl in ((O_RQ, 1.0), (O_RK, 0.125)):
                load_w(0, c0, 384)
                load_w(1, c0, 384, swap=True)
                for bi, (t0, n) in enumerate(self.blocks):
                    o = cnt["ob"] % 2
                    cnt["ob"] += 1
                    if bi > 0:
                        l0 = t0 - NCTX
                        P.dma("sync", rcb[bi % 2][:, :n], self.ROPE[0, :, l0:l0 + n], writes=[("rcb", bi % 2)])
                        P.dma("sync", rsb[bi % 2][:, :n], self.ROPE[1, :, l0:l0 + n], writes=[("rsb", bi % 2)])
                    for ft in range(3):
                        i = fm_proj(0, ft, t0, n, wkeys(0))
                        if bi == 0:
                            P.act(ob[o][:, ft, :n], pp[i][:, :n], AF.Identity, [("pp", i)], [("ob", o, ft)], scale=scl)
                        else:
                            i2 = fm_proj(1, ft, t0, n, wkeys(1, True))
                            rb_ = bi % 2
                            P.tt(t1[:, :n], pp[i][:, :n], rcb[rb_][:, :n], ALU.mult, [("pp", i), ("rcb", rb_)], ["pt1"])
                            P.tt(t2[:, :n], pp[i2][:, :n], rsb[rb_][:, :n], ALU.mult, [("pp", i2), ("rsb", rb_)], ["pt2"])
                            P.tt(t1[:, :n], t1[:, :n], t2[:, :n], ALU.add, ["pt1", "pt2"], ["pt1"])
                            P.act(ob[o][:, ft, :n], t1[:, :n], AF.Identity, ["pt1"], [("ob", o, ft)], scale=scl)
                    P.dma("sync", PJv[:, c0 // 128:c0 // 128 + 3, t0:t0 + n], ob[o][:, :3, :n],
                          reads=[("ob", o, ft) for ft in range(3)], writes=[("PJ", c0, bi)])
            vb = [P.sb(st, [128, 384], BF16, "vb") for _ in range(2)]
            for vi, c0 in ((0, O_HV), (1, O_RV)):
                load_w(2, c0, 384)
                for tt_ in range(T // 128):
                    i = cnt["pp"] % 4
                    cnt["pp"] += 1
                    for kt in range(8):
                        P.mm(pp[i][:, :384], hT[:, kt, tt_ * 128:(tt_ + 1) * 128], wb[2][:, kt, :384], kt == 0, kt == 7,
                             [("wb", 2)] + hkeys, [("pp", i)])
                    o = tt_ % 2
                    P.copy(vb[o][:], pp[i][:, :384], [("pp", i)], [("vb", o)], eng=("vector" if o == 0 else "scalar"))
                    P.dma("sync", self.VT[vi, tt_ * 128:(tt_ + 1) * 128, :], vb[o][:], reads=[("vb", o)], writes=[("VT", vi, tt_)])

    def sincos(self, st, ang, cosd, sind, n, akey, kc, ks, bufs=None):
        P = self.P
        if bufs is None:
            ki = P.sb(st, [128, n], I32, "sc_ki")
            kf = P.sb(st, [128, n], F32, "sc_kf")
        else:
            ki, kf = bufs[0][:, :n], bufs[1][:, :n]
        P.ts(kf[:], ang[:, :n], 1.0 / (2 * math.pi), None, ALU.mult, None, [akey], ["sc_kf"])
        P.copy(ki[:], kf[:], ["sc_kf"], ["sc_ki"])
        P.copy(kf[:], ki[:], ["sc_ki"], ["sc_kf"])
        P.stt(ang[:, :n], kf[:], -2 * math.pi, ang[:, :n], ALU.mult, ALU.add, ["sc_kf", akey], [akey])
        P.ts(kf[:], ang[:, :n], math.pi, -2 * math.pi, ALU.is_gt, ALU.mult, [akey], ["sc_kf"])
        P.tt(ang[:, :n], ang[:, :n], kf[:], ALU.add, [akey, "sc_kf"], [akey])
        P.ts(kf[:], ang[:, :n], -math.pi, 2 * math.pi, ALU.is_lt, ALU.mult, [akey], ["sc_kf"])
        P.tt(ang[:, :n], ang[:, :n], kf[:], ALU.add, [akey, "sc_kf"], [akey])
        P.ts(ang[:, :n], ang[:, :n], math.pi, -math.pi, ALU.min, ALU.max, [akey], [akey])
        P.act(sind[:, :n], ang[:, :n], AF.Sin, [akey], [ks])
        P.act(kf[:], ang[:, :n], AF.Abs, [akey], ["sc_kf"])
        P.ts(kf[:], kf[:], -1.0, math.pi / 2, ALU.mult, ALU.add, ["sc_kf"], ["sc_kf"])
        P.act(cosd[:, :n], kf[:], AF.Sin, ["sc_kf"], [kc])

    def phase_output(self):
        P, nc = self.P, self.nc
        T, L = self.T, self.L
        XTv = self.XT.rearrange("(ft p) t -> p ft t", p=128)
        with ExitStack() as st:
            nb = self.alloc_norm_bufs(st)
            sq, pss, rstd, tmp = nb
            ident = self.cst[:, C_IDENT:C_IDENT + 128]
            fg = P.sb(st, [128, 8], F32, "fg")
            P.dma("sync", fg[:], self.din["final_norm_g"].rearrange("(ft p) -> p ft", p=128), writes=["fg"])
            xt = [P.sb(st, [128, 8, 512], F32, "oxt") for _ in range(2)]
            xn = [P.sb(st, [128, 8, 512], F32, "oxn") for _ in range(2)]
            po = [P.ps(st, [128, 4, 128], F32, "opo") for _ in range(2)]
            ot = [P.sb(st, [128, D], F32, "oot") for _ in range(2)]
            k = 0
            for bi, (t0, n) in enumerate(self.blocks):
                if bi == 0:
                    continue
                b = bi % 2
                P.dma("sync", xt[b][:, :, :n], XTv[:, :, t0:t0 + n], reads=["XTall"], writes=[("oxt", b)])
                P.act(sq[:, :, :n], xt[b][:, :, :n], AF.Square, [("oxt", b)], ["nb_sq"])
                for kt in range(8):
                    P.mm(pss[:, :n], self.onesb[:], sq[:, kt, :n], kt == 0, kt == 7, ["nb_sq", "onesb"], ["nb_ps"])
                P.act(rstd[:, :n], pss[:, :n], AF.Sqrt, ["nb_ps"], ["nb_rstd"], bias=self.epsc[:, 0:1], scale=1.0 / D)
                P.recip(rstd[:, :n], rstd[:, :n], ["nb_rstd"], ["nb_rstd"])
                for ft in range(8):
                    P.stt(xn[b][:, ft, :n], xt[b][:, ft, :n], fg[:, ft:ft + 1], rstd[:, :n], ALU.mult, ALU.mult,
                          [("oxt", b), "fg", "nb_rstd"], [("oxn", b, ft)])
                for s in range(n // 128):
                    o = k % 2
                    k += 1
                    for hf in range(2):
                        for q in range(4):
                            ft = hf * 4 + q
                            P.mm(po[hf][:, q, :], xn[b][:, ft, s * 128:(s + 1) * 128], ident, True, True,
                                 [("oxn", b, ft), "cst"], [("opo", hf, q)])
                        P.copy(ot[o][:, hf * 512:(hf + 1) * 512], po[hf][:].rearrange("p a b -> p (a b)"),
                               [("opo", hf, q) for q in range(4)], [("oot", o, hf)], eng=("vector" if hf == 0 else "scalar"))
                    r0 = t0 - NCTX + s * 128
                    P.dma("sync", self.out[r0:r0 + 128, :], ot[o][:], reads=[("oot", o, 0), ("oot", o, 1)],
                          writes=[("out", r0)], is_output=True)


    def phase_s5(self, l):
        P, nc = self.P, self.nc
        T, L = self.T, self.L
        PJv = self.PJ.rearrange("(ft p) t -> p ft t", p=128)
        YBv = self.YB.rearrange("(ft p) t -> p ft t", p=128)
        nblk = T // 256
        with ExitStack() as st:
            uT = P.sb(st, [128, 2, T], BF16, "uT")
            P.dma("sync", uT[:], PJv[:, 0:2, :], writes=["uT"])
            yacc = P.sb(st, [128, 2, T], F32, "yacc")
            Ec = P.sb(st, [128, 8, 256], F32, "Ec")
            Es = P.sb(st, [128, 8, 256], F32, "Es")
            Fc = P.sb(st, [128, 8, 256], F32, "Fc")
            Fs = P.sb(st, [128, 8, 256], F32, "Fs")
            ang = P.sb(st, [128, 8, 256], F32, "ang")
            tA = P.sb(st, [128, 4, 256], F32, "tA")
            tB = P.sb(st, [128, 4, 256], F32, "tB")
            tC = P.sb(st, [128, 4, 256], F32, "tC")
            tD = P.sb(st, [128, 4, 256], F32, "tD")
            xt_ = P.sb(st, [128, 4, 2, 256], F32, "xtl")
            M = P.sb(st, [128, 4, 2, 256], F32, "M")
            hb = P.sb(st, [128, 4, 2, 256], BF16, "hb")
            BD = [P.sb(st, [128, 2, 512], BF16, "BD%d" % c) for c in range(2)]
            CT = [P.sb(st, [128, 8, 128], BF16, "CT%d" % c) for c in range(2)]
            sm = {n: P.sb(st, [128, 8], F32, "s5" + n) for n in
                  ("lr", "li", "dt", "th", "mag", "c", "s", "ar", "ai", "den", "am1", "zr", "zi", "t1", "t2", "a256", "Rc", "Rs")}
            init = [P.sb(st, [128, 8, 2], F32, "init%d" % i) for i in range(2)]
            pbu = P.ps(st, [128, 4, 2, 256], F32, "pbu")
            py = P.ps(st, [128, 256], F32, "py")
            iota = self.cst[:, C_IOTA:C_IOTA + 256]
            scb = (P.sb(st, [128, 2048], I32, "sc_ki"), P.sb(st, [128, 2048], F32, "sc_kf"))
            magf = P.sb(st, [128, 8, 256], F32, "magf")
            for d in range(2):
                P.dma("sync", sm["lr"][:], self.din["s5_lam_re"][l, d].rearrange("g p -> (g p)").rearrange("(nt p) -> p nt", p=128), writes=["lr"])
                P.dma("sync", sm["li"][:], self.din["s5_lam_im"][l, d].rearrange("g p -> (g p)").rearrange("(nt p) -> p nt", p=128), writes=["li"])
                ld = self.din["s5_log_dt"][l, d]
                for half in range(2):
                    src = bass.AP(ld.tensor, ld.offset + half, [[0, 64], [2, 8]])
                    P.dma("sync", sm["dt"][half * 64:(half + 1) * 64, :], src, writes=[("dt", half)])
                P.act(sm["dt"][:], sm["dt"][:], AF.Exp, [("dt", 0), ("dt", 1)], ["dt"])
                P.tt(sm["th"][:], sm["li"][:], sm["dt"][:], ALU.mult, ["li", "dt"], ["th"])
                P.tt(sm["mag"][:], sm["lr"][:], sm["dt"][:], ALU.mult, ["lr", "dt"], ["mag"])
                P.act(sm["mag"][:], sm["mag"][:], AF.Exp, ["mag"], ["mag"])
                P.ts(sm["a256"][:], sm["th"][:], 256.0, None, ALU.mult, None, ["th"], ["a256"])
                P.copy(sm["t1"][:], sm["th"][:], ["th"], ["t1"])
                with ExitStack() as st2:
                    self.sincos(st2, sm["t1"], sm["c"], sm["s"], 8, "t1", "c", "s", scb)
                    self.sincos(st2, sm["a256"], sm["Rc"], sm["Rs"], 8, "a256", "Rc", "Rs", scb)
                    P.tt(sm["ar"][:], sm["mag"][:], sm["c"][:], ALU.mult, ["mag", "c"], ["ar"])
                    P.tt(sm["ai"][:], sm["mag"][:], sm["s"][:], ALU.mult, ["mag", "s"], ["ai"])
                    P.tt(sm["den"][:], sm["lr"][:], sm["lr"][:], ALU.mult, ["lr"], ["den"])
                    P.tt(sm["t2"][:], sm["li"][:], sm["li"][:], ALU.mult, ["li"], ["t2"])
                    P.tt(sm["den"][:], sm["den"][:], sm["t2"][:], ALU.add, ["den", "t2"], ["den"])
                    P.recip(sm["den"][:], sm["den"][:], ["den"], ["den"])
                    P.ts(sm["am1"][:], sm["ar"][:], -1.0, None, ALU.add, None, ["ar"], ["am1"])
                    P.tt(sm["zr"][:], sm["am1"][:], sm["lr"][:], ALU.mult, ["am1", "lr"], ["zr"])
                    P.tt(sm["t2"][:], sm["ai"][:], sm["li"][:], ALU.mult, ["ai", "li"], ["t2"])
                    P.tt(sm["zr"][:], sm["zr"][:], sm["t2"][:], ALU.add, ["zr", "t2"], ["zr"])
                    P.tt(sm["zr"][:], sm["zr"][:], sm["den"][:], ALU.mult, ["zr", "den"], ["zr"])
                    P.tt(sm["zi"][:], sm["ai"][:], sm["lr"][:], ALU.mult, ["ai", "lr"], ["zi"])
                    P.tt(sm["t2"][:], sm["am1"][:], sm["li"][:], ALU.mult, ["am1", "li"], ["t2"])
                    P.tt(sm["zi"][:], sm["zi"][:], sm["t2"][:], ALU.subtract, ["zi", "t2"], ["zi"])
                    P.tt(sm["zi"][:], sm["zi"][:], sm["den"][:], ALU.mult, ["zi", "den"], ["zi"])
                    P.tt(ang[:], iota.unsqueeze(1).to_broadcast([128, 8, 256]), sm["th"][:].unsqueeze(2).to_broadcast([128, 8, 256]),
                         ALU.mult, ["cst", "th"], ["ang"])
                    a2 = ang[:].rearrange("p a b -> p (a b)")
                    self.sincos(st2, ang[:].rearrange("p a b -> p (a b)"), Ec[:].rearrange("p a b -> p (a b)"),
                                Es[:].rearrange("p a b -> p (a b)"), 2048, "ang", "Ec", "Es", scb)
                for nt in range(8):
                    P.ts(magf[:, nt, :], self.onesf[:, 0:128].unsqueeze(1).to_broadcast([128, 2, 128]).rearrange("p a b -> p (a b)") if False else Ec[:, nt, :],
                         0.0, sm["mag"][:, nt:nt + 1], ALU.mult, ALU.add, ["Ec", "mag"], ["magf"])
                zrb = sm["zr"][:].unsqueeze(2).to_broadcast([128, 8, 256])
                zib = sm["zi"][:].unsqueeze(2).to_broadcast([128, 8, 256])
                P.tt(Fc[:], Ec[:], zrb, ALU.mult, ["Ec", "zr"], ["Fc"])
                P.tt(ang[:], Es[:], zib, ALU.mult, ["Es", "zi"], ["ang"])
                P.tt(Fc[:], Fc[:], ang[:], ALU.add, ["Fc", "ang"], ["Fc"])
                P.tt(Fs[:], Ec[:], zib, ALU.mult, ["Ec", "zi"], ["Fs"])
                P.tt(ang[:], Es[:], zrb, ALU.mult, ["Es", "zr"], ["ang"])
                P.tt(Fs[:], Fs[:], ang[:], ALU.subtract, ["Fs", "ang"], ["Fs"])
                for c, nm in ((0, "s5_b_re"), (1, "s5_b_im")):
                    P.memset(BD[c][:], 0.0, [("BD", c)] + [("BDg", c, g) for g in range(16)])
                    for g in range(16):
                        kt, gl = g // 8, g % 8
                        P.dma("gpsimd", BD[c][gl * 16:(gl + 1) * 16, kt, gl * 64:(gl + 1) * 64],
                              self.din[nm][l, d, g].rearrange("p h -> h p"), reads=[("BD", c)], writes=[("BDg", c, g)])
                for c, nm in ((0, "s5_c_re"), (1, "s5_c_im")):
                    P.memset(CT[c][:], 0.0, [("CT", c)] + [("CTg", c, g) for g in range(16)])
                    for g in range(16):
                        nt, g2, gl = g // 2, g % 2, g % 8
                        P.dma("gpsimd", CT[c][g2 * 64:(g2 + 1) * 64, nt, gl * 16:(gl + 1) * 16],
                              self.din[nm][l, d, g].rearrange("h p -> p h"), reads=[("CT", c)], writes=[("CTg", c, g)])
                P.ts(CT[1][:], CT[1][:], -1.0, None, ALU.mult, None, [("CT", 1)] + [("CTg", 1, g) for g in range(16)],
                     [("CT", 1)] + [("CTg", 1, g) for g in range(16)])
                if d == 0:
                    for nm_ in ("th", "mag", "zr", "zi", "Rc", "Rs", "dt", "lr", "li"):
                        self.dump(nm_, sm[nm_][:], [128, 8], F32, [nm_])
                    self.dump("Ec", Ec[:], [128, 8, 256], F32, ["Ec"])
                    self.dump("Es", Es[:], [128, 8, 256], F32, ["Es"])
                    self.dump("Fc", Fc[:], [128, 8, 256], F32, ["Fc"])
                    self.dump("BD0", BD[0][:], [128, 2, 512], BF16, [("BD", 0)] + [("BDg", 0, g) for g in range(16)])
                    self.dump("CT0", CT[0][:], [128, 8, 128], BF16, [("CT", 0)] + [("CTg", 0, g) for g in range(16)])
                    self.dump("CT1", CT[1][:], [128, 8, 128], BF16, [("CT", 1)] + [("CTg", 1, g) for g in range(16)])
                if d == 1 and "YD" in self.debug:
                    YD = self.scratch("YD", [256, T], F32)
                    P.dma("sync", YD.rearrange("(q p) t -> p q t", p=128), yacc[:], reads=[("yacc", q, b2) for q in range(2) for b2 in range(nblk)], writes=["YD"])
                order = list(range(nblk)) if d == 0 else [0] + list(range(nblk - 1, 0, -1))
                P.memset(init[0][:], 0.0, [("init", 0)])
                last = 255 if d == 0 else 0
                R = (lambda a: a) if d == 0 else rev_ap
                for bi, blk in enumerate(order):
                    t0 = blk * 256
                    ii, io = bi % 2, (bi + 1) % 2
                    for q in range(2):
                        for j in range(4):
                            for c in range(2):
                                P.mm(pbu[:, j, c, :], BD[c][:, q, j * 128:(j + 1) * 128], uT[:, q, t0:t0 + 256], True, True,
                                     [("BD", c), "uT"] + [("BDg", c, g) for g in range(16)], [("pbu", j, c)])
                        pk = [("pbu", j, c) for j in range(4) for c in range(2)]
                        fc = R(Fc[:, 4 * q:4 * q + 4, :])
                        fs = R(Fs[:, 4 * q:4 * q + 4, :])
                        ec = R(Ec[:, 4 * q:4 * q + 4, :])
                        es = R(Es[:, 4 * q:4 * q + 4, :])
                        P.tt(tA[:], pbu[:, :, 0, :], fc, ALU.mult, pk + ["Fc"], ["tA"])
                        P.tt(tB[:], pbu[:, :, 1, :], fs, ALU.mult, pk + ["Fs"], ["tB"])
                        P.tt(xt_[:, :, 0, :], tA[:], tB[:], ALU.subtract, ["tA", "tB"], [("xtl", 0)])
                        P.tt(tA[:], pbu[:, :, 1, :], fc, ALU.mult, pk + ["Fc"], ["tA"])
                        P.tt(tB[:], pbu[:, :, 0, :], fs, ALU.mult, pk + ["Fs"], ["tB"])
                        P.tt(xt_[:, :, 1, :], tA[:], tB[:], ALU.add, ["tA", "tB"], [("xtl", 1)])
                        for j in range(4):
                            nt = 4 * q + j
                            for c in range(2):
                                def f(e, j=j, c=c, nt=nt, ii=ii, R=R):
                                    return e.tensor_tensor_scan(R(M[:, j, c, :]), magf[:, nt, :],
                                                                R(xt_[:, j, c, :]), init[ii][:, nt, c:c + 1], ALU.mult, ALU.add)
                                P.op("vector", f, [("xtl", c), "magf", ("init", ii)], [("M", c)])
                        rc = sm["Rc"][:, 4 * q:4 * q + 4]
                        rs = sm["Rs"][:, 4 * q:4 * q + 4]
                        mre = M[:, :, 0, last]
                        mim = M[:, :, 1, last]
                        t1 = sm["t1"][:, 0:4]
                        t2 = sm["t2"][:, 0:4]
                        P.tt(t1, mre, rc, ALU.mult, [("M", 0), "Rc"], ["t1"])
                        P.tt(t2, mim, rs, ALU.mult, [("M", 1), "Rs"], ["t2"])
                        P.tt(init[io][:, 4 * q:4 * q + 4, 0], t1, t2, ALU.subtract, ["t1", "t2"], [("init", io)])
                        P.tt(t1, mim, rc, ALU.mult, [("M", 1), "Rc"], ["t1"])
                        P.tt(t2, mre, rs, ALU.mult, [("M", 0), "Rs"], ["t2"])
                        P.tt(init[io][:, 4 * q:4 * q + 4, 1], t1, t2, ALU.add, ["t1", "t2"], [("init", io)])
                        G_ = "vector"
                        P.tt(tC[:], M[:, :, 0, :], ec, ALU.mult, [("M", 0), "Ec"], ["tC"], eng=G_)
                        P.tt(tD[:], M[:, :, 1, :], es, ALU.mult, [("M", 1), "Es"], ["tD"], eng=G_)
                        P.tt(hb[:, :, 0, :], tC[:], tD[:], ALU.subtract, ["tC", "tD"], [("hb", 0)], eng=G_)
                        P.tt(tC[:], M[:, :, 1, :], ec, ALU.mult, [("M", 1), "Ec"], ["tC"], eng=G_)
                        P.tt(tD[:], M[:, :, 0, :], es, ALU.mult, [("M", 0), "Es"], ["tD"], eng=G_)
                        P.tt(hb[:, :, 1, :], tC[:], tD[:], ALU.add, ["tC", "tD"], [("hb", 1)], eng=G_)
                        k = 0
                        for j in range(4):
                            for c in range(2):
                                P.mm(py[:], CT[c][:, 4 * q + j, :], hb[:, j, c, :], k == 0, k == 7,
                                     [("CT", c), ("hb", c)] + [("CTg", c, g) for g in range(16)], ["py"])
                                k += 1
                        if d == 0:
                            P.copy(yacc[:, q, t0:t0 + 256], py[:], ["py"], [("yacc", q, blk)], eng="scalar")
                            if bi == 0 and q == 0:
                                self.dump("pbu", xt_[:], [128, 4, 2, 256], F32, [("xtl", 0), ("xtl", 1)])
                                self.dump("M", M[:], [128, 4, 2, 256], F32, [("M", 0), ("M", 1)])
                                self.dump("hb", hb[:], [128, 4, 2, 256], BF16, [("hb", 0), ("hb", 1)])
                        else:
                            P.tt(yacc[:, q, t0:t0 + 256], yacc[:, q, t0:t0 + 256], py[:], ALU.add, ["py", ("yacc", q, blk)], [("yacc", q, blk)])
            if False:
                YD = self.scratch("YD", [256, T], F32)
                P.dma("sync", YD.rearrange("(q p) t -> p q t", p=128), yacc[:], reads=[("yacc", q, b2) for q in range(2) for b2 in range(nblk)] + [("yg", 0), ("yg", 1)], writes=["YD"])
            dsk = P.sb(st, [128, 2], F32, "dsk")
            P.dma("sync", dsk[:], self.din["s5_d"][l].rearrange("(kt p) -> p kt", p=128), writes=["dsk"])
            wg = P.sb(st, [128, 2, 256], BF16, "wglu")
            P.dma("gpsimd", wg[:], self.din["s5_w_glu"][l].rearrange("(kt p) f -> p kt f", p=128), reads=[], writes=["wglu"])
            yb = P.sb(st, [128, 2, 512], BF16, "ybf")
            yo = [P.sb(st, [128, 2, 512], BF16, "yo") for _ in range(2)]
            sg = P.sb(st, [128, 512], F32, "sg")
            pg = P.ps(st, [128, 512], F32, "pg")
            for bi, (t0, n) in enumerate(self.blocks):
                o = bi % 2
                yk = [("yacc", q, b2) for q in range(2) for b2 in range(nblk)]
                for q in range(2):
                    P.stt(yacc[:, q, t0:t0 + n], uT[:, q, t0:t0 + n], dsk[:, q:q + 1], yacc[:, q, t0:t0 + n], ALU.mult, ALU.add,
                          ["uT", "dsk"] + yk, [("yg", q)])
                    P.act(yacc[:, q, t0:t0 + n], yacc[:, q, t0:t0 + n], AF.Gelu_apprx_tanh, [("yg", q)], [("yg", q)])
                    P.copy(yb[:, q, :n], yacc[:, q, t0:t0 + n], [("yg", q)], [("ybf", q)])
                for ft in range(2):
                    for kt in range(2):
                        P.mm(pg[:, :n], wg[:, kt, ft * 128:(ft + 1) * 128], yb[:, kt, :n], kt == 0, kt == 1,
                             ["wglu", ("ybf", 0), ("ybf", 1)], ["pg"])
                    P.act(sg[:, :n], pg[:, :n], AF.Sigmoid, ["pg"], ["sg"])
                    P.tt(yo[o][:, ft, :n], yacc[:, ft, t0:t0 + n], sg[:, :n], ALU.mult, ["sg", ("yg", ft)], [("yo", o, ft)])
                P.dma("sync", YBv[:, 0:2, t0:t0 + n], yo[o][:, :, :n], reads=[("yo", o, 0), ("yo", o, 1)], writes=[("YB", 0, bi)])

    def phase_gla(self, l, which):
        P, nc = self.P, self.nc
        T, L = self.T, self.L
        PJ = self.PJ
        qoff = O_HQ if which == 0 else O_RQ
        goff = O_HG if which == 0 else O_RG
        yrow0 = 256 + which * 384
        CS = 32 if which == 0 else 64
        NH = 6
        with ExitStack() as st:
            def mk(shape, dt, nm):
                return [P.sb(st, shape, dt, nm) for _ in range(NH)]
            qf = mk([64, 512], BF16, "qf")
            kf = mk([64, 512], BF16, "kf")
            qt = mk([64, 512], BF16, "qt")
            ktl = mk([64, 512 + 64], BF16, "ktl")
            qh = mk([64, 512], BF16, "qh")
            kd = mk([64, 512 + 64], BF16, "kd")
            kdT = mk([64, 512 // CS, 64], BF16, "kdT")
            vv = mk([64, 512 // CS, 64], BF16, "vv")
            S = mk([64, 64], F32, "S")
            Sb = mk([64, 64], BF16, "Sb")
            ob = mk([64, 512], F32, "obk")
            oa = mk([64, 512], F32, "oak")
            ebend = mk([64, 16], F32, "ebend")
            Asb = [[[P.sb(st, [64, CS], BF16, "Asb") for _ in range(2)] for _ in range(2)] for _ in range(NH)]
            for hd in range(NH):
                P.memset(ktl[hd][:], 0.0, [("ktl", hd)])
                P.memset(kd[hd][:], 0.0, [("kd", hd)])
                for dd in range(2):
                    for sl in range(2):
                        P.memset(Asb[hd][dd][sl][:], 0.0, [("Asb", hd, dd, sl)])
            lf = [P.sb(st, [64, 512], F32, "lf") for _ in range(2)]
            bb = [P.sb(st, [64, 512], F32, "bb") for _ in range(2)]
            d1 = [P.sb(st, [64, 512], F32, "d1") for _ in range(2)]
            ex = [P.sb(st, [64, 512], F32, "ex") for _ in range(2)]
            exk = [P.sb(st, [64, 512], F32, "exk") for _ in range(2)]
            exh = [P.sb(st, [64, 512], F32, "exh") for _ in range(2)]
            exd = [P.sb(st, [64, 512], F32, "exd") for _ in range(2)]
            d1b = [P.sb(st, [64, 512], F32, "d1b") for _ in range(2)]
            gsb = [P.sb(st, [64, 512], BF16, "gsb") for _ in range(2)]
            osq = [P.sb(st, [64, 512], BF16, "osq") for _ in range(2)]
            rs_ = [P.sb(st, [64, 512], F32, "rs_") for _ in range(2)]
            yo = [P.sb(st, [64, 512], BF16, "yo") for _ in range(2)]
            LB = [P.ps(st, [128, 512], F32, "LB") for _ in range(NH)]
            PT = P.ps(st, [128, 512], F32, "PT")
            PN = P.ps(st, [128, 512], F32, "PN")
            if which == 1:
                tb = [{n: P.sb(st, [64, 64], F32, "rt" + n) for n in ("b", "q", "k", "e", "d")} for _ in range(NH)]
                ebr = mk([64, 1], F32, "ebr")
                for hd in range(NH):
                    lgc = math.log(1.0 - 2.0 ** (-5.0 - hd))
                    t_ = tb[hd]
                    P.ts(t_["b"][:], self.cst[0:64, C_IOTA:C_IOTA + 64], 1.0, lgc, ALU.add, ALU.mult, ["cst"], [("rtb", hd)])
                    P.act(t_["e"][:], t_["b"][:], AF.Exp, [("rtb", hd)], [("rte", hd)])
                    P.ts(t_["q"][:], t_["b"][:], t_["b"][:, 31:32], None, ALU.subtract, None, [("rtb", hd)], [("rtq", hd)])
                    P.act(t_["k"][:], t_["q"][:], AF.Exp, [("rtq", hd)], [("rtk", hd)], scale=-1.0)
                    P.act(t_["q"][:], t_["q"][:], AF.Exp, [("rtq", hd)], [("rtq", hd)])
                    P.ts(t_["d"][:], t_["b"][:], t_["b"][:, 63:64], None, ALU.subtract, None, [("rtb", hd)], [("rtd", hd)])
                    P.act(t_["d"][:], t_["d"][:], AF.Exp, [("rtd", hd)], [("rtd", hd)], scale=-1.0)
                    P.copy(ebr[hd][:], t_["e"][:, 63:64], [("rte", hd)], [("ebr", hd)])
            if which == 0:
                mask_f = self.cmask[0:CS, M32_FWD:M32_FWD + 32]
                mask_b = self.cmask[0:CS, M32_BWD:M32_BWD + 32]
            else:
                mask_f = self.cmask[0:CS, M_FWD:M_FWD + 64]
                mask_b = self.cmask[0:CS, M_BWDS:M_BWDS + 64]
            reset = self.cst[0:64, C_RESET32:C_RESET32 + 512]
            nstep = 0
            npre = 0
            for d in range(2):
                order = list(range(len(self.blocks))) if d == 0 else [0] + list(range(len(self.blocks) - 1, 0, -1))
                R = (lambda a: a) if d == 0 else rev_ap
                mask = mask_f if d == 0 else mask_b
                for hd in range(NH):
                    P.memset(S[hd][:], 0.0, [("S", hd)])
                    P.memset(Sb[hd][:], 0.0, [("Sb", hd)])
                for blk in order:
                    t0, n = self.blocks[blk]
                    nch = n // CS
                    pm = (CS // 2 - 1) if d == 0 else CS // 2
                    pe = (CS - 1) if d == 0 else 0
                    for hd in range(NH):
                        u = npre % 2
                        npre += 1
                        koff = (O_HFF if d == 0 else O_HFB) if which == 0 else O_RK
                        r0 = qoff + hd * 64
                        P.dma("sync", qf[hd][:, :n], PJ[r0:r0 + 64, t0:t0 + n], writes=[("qf", hd)])
                        r1 = koff + hd * 64
                        P.dma("sync", kf[hd][:, :n], PJ[r1:r1 + 64, t0:t0 + n], writes=[("kf", hd)])
                        P.dma("sync", vv[hd][0:CS, :n // CS, :],
                              self.VT[which, t0:t0 + n, hd * 64:(hd + 1) * 64].rearrange("(a p) c -> p a c", p=CS), writes=[("vv", hd)])
                        if d == 1:
                            P.dma("sync", oa[hd][:, :n], self.OA[which, hd * 64:(hd + 1) * 64, t0:t0 + n], writes=[("oak", hd)])
                        if which == 0:
                            P.dma("sync", lf[u][:, :n], self.LF[d, hd * 64:(hd + 1) * 64, t0:t0 + n], writes=[("lf", u)])
                            rr, rb, rl = reset[:, :n], R(bb[u][:, :n]), R(lf[u][:, :n])
                            P.op("vector", (lambda e, rr=rr, rb=rb, rl=rl: e.tensor_tensor_scan(rb, rr, rl, 0.0, ALU.mult, ALU.add)),
                                 [("lf", u), "cst"], [("bb", u)])
                            b3 = bb[u][:, :n].rearrange("p (c i) -> p c i", i=CS)
                            d3 = d1[u][:, :n].rearrange("p (c i) -> p c i", i=CS)
                            kb = ("bb", u)
                            d3b = d1b[u][:, :n].rearrange("p (c i) -> p c i", i=CS)
                            P.tt(d3, b3, b3[:, :, pm:pm + 1].to_broadcast([64, nch, CS]), ALU.subtract, [kb], [("d1", u)])
                            P.tt(d3b, b3, b3[:, :, pe:pe + 1].to_broadcast([64, nch, CS]), ALU.subtract, [kb], [("d1b", u)])
                            P.act(ex[u][:, :n], d1[u][:, :n], AF.Exp, [("d1", u)], [("ex", u)])
                            P.act(exk[u][:, :n], d1[u][:, :n], AF.Exp, [("d1", u)], [("exk", u)], scale=-1.0)
                            P.act(exh[u][:, :n], bb[u][:, :n], AF.Exp, [kb], [("exh", u)])
                            P.act(exd[u][:, :n], d1b[u][:, :n], AF.Exp, [("d1b", u)], [("exd", u)], scale=-1.0)
                            P.tt(qt[hd][:, :n], qf[hd][:, :n], ex[u][:, :n], ALU.mult, [("qf", hd), ("ex", u)], [("qt", hd)])
                            P.tt(ktl[hd][:, :n], kf[hd][:, :n], exk[u][:, :n], ALU.mult, [("kf", hd), ("exk", u)], [("ktl", hd)])
                            P.tt(qh[hd][:, :n], qf[hd][:, :n], exh[u][:, :n], ALU.mult, [("qf", hd), ("exh", u)], [("qh", hd)])
                            P.tt(kd[hd][:, :n], kf[hd][:, :n], exd[u][:, :n], ALU.mult, [("kf", hd), ("exd", u)], [("kd", hd)])
                            P.act(ebend[hd][:, :nch], b3[:, :, pe], AF.Exp, [kb], [("ebend", hd)])
                        else:
                            q3 = qf[hd][:, :n].rearrange("p (c i) -> p c i", i=CS)
                            k3 = kf[hd][:, :n].rearrange("p (c i) -> p c i", i=CS)

                            def tbc(nm, R=R, nch=nch, hd=hd):
                                return R(tb[hd][nm][:]).unsqueeze(1).to_broadcast([64, nch, CS])
                            P.tt(qt[hd][:, :n].rearrange("p (c i) -> p c i", i=CS), q3, tbc("q"), ALU.mult, [("qf", hd), ("rtq", hd)], [("qt", hd)])
                            P.tt(ktl[hd][:, :n].rearrange("p (c i) -> p c i", i=CS), k3, tbc("k"), ALU.mult, [("kf", hd), ("rtk", hd)], [("ktl", hd)])
                            P.tt(qh[hd][:, :n].rearrange("p (c i) -> p c i", i=CS), q3, tbc("e"), ALU.mult, [("qf", hd), ("rte", hd)], [("qh", hd)])
                            P.tt(kd[hd][:, :n].rearrange("p (c i) -> p c i", i=CS), k3, tbc("d"), ALU.mult, [("kf", hd), ("rtd", hd)], [("kd", hd)])
                        for a in range(n // CS):
                            pc = (a % 4) * 64
                            P.mm(PT[0:64, pc:pc + 64], kd[hd][:, a * CS:a * CS + 64], self.identb[0:64, 0:64], True, True,
                                 [("kd", hd), "identb"], ["PT"])
                            if a % 4 == 3 or a == n // CS - 1:
                                a0 = a - (a % 4)
                                na = a - a0 + 1
                                P.copy(kdT[hd][0:CS, a0:a0 + na, :], PT[0:CS, 0:na * 64].rearrange("p (a k) -> p a k", k=64), ["PT"],
                                       [("kdT", hd)], eng="scalar")
                    corder = list(range(nch)) if d == 0 else list(range(nch - 1, -1, -1))
                    for c in corder:
                        sl = nstep % 2
                        nstep += 1
                        cs = slice(c * CS, (c + 1) * CS)
                        def regs(hd):
                            return (LB[hd][0:64, sl * 192:sl * 192 + CS], LB[hd][0:64, sl * 192 + 64:sl * 192 + 64 + CS],
                                    LB[hd][0:64, 384:448], Asb[hd][d][sl], ("Asb", hd, d, sl), ("LB", hd))
                        for hd in range(NH):
                            PAr, POr, PSr, A, ak, lk = regs(hd)
                            P.mm(PAr, ktl[hd][:, c * CS:c * CS + 64], qt[hd][:, cs], True, True, [("ktl", hd), ("qt", hd)], [lk])
                        for hd in range(NH):
                            PAr, POr, PSr, A, ak, lk = regs(hd)
                            P.op("vector", (lambda e, A=A, PAr=PAr, mask=mask: e.copy_predicated(A[0:CS, :], mask, PAr[0:CS, :])),
                                 [lk, "cmask", ak], [ak])
                        for hd in range(NH):
                            PAr, POr, PSr, A, ak, lk = regs(hd)
                            P.mm(POr, vv[hd][0:CS, c, :], A[0:CS, :], True, False, [("vv", hd), ak], [lk])
                            P.mm(POr, Sb[hd][:, :], qh[hd][:, cs], False, True, [("Sb", hd), ("qh", hd)], [lk])
                        for hd in range(NH):
                            PAr, POr, PSr, A, ak, lk = regs(hd)
                            if d == 0:
                                P.copy(ob[hd][:, cs], POr, [lk], [("obk", hd)], eng="scalar")
                            else:
                                P.tt(ob[hd][:, cs], POr, oa[hd][:, cs], ALU.add, [lk, ("oak", hd)], [("obk", hd)])
                        for hd in range(NH):
                            PAr, POr, PSr, A, ak, lk = regs(hd)
                            P.mm(PSr, kdT[hd][0:CS, c, :], vv[hd][0:CS, c, :], True, True, [("kdT", hd), ("vv", hd)], [lk])
                        for hd in range(NH):
                            PAr, POr, PSr, A, ak, lk = regs(hd)
                            esc = ebend[hd][:, c:c + 1] if which == 0 else ebr[hd][:, 0:1]
                            P.stt(S[hd][:], S[hd][:], esc, PSr, ALU.mult, ALU.add,
                                  [("S", hd), ("ebend", hd) if which == 0 else ("ebr", hd), lk], [("S", hd)])
                        for hd in range(NH):
                            P.copy(Sb[hd][:], S[hd][:], [("S", hd)], [("Sb", hd)], eng="scalar")
                    for hd in range(NH):
                        u = hd % 2
                        if d == 0:
                            P.dma("sync", self.OA[which, hd * 64:(hd + 1) * 64, t0:t0 + n], ob[hd][:, :n], reads=[("obk", hd)],
                                  writes=[("OA", blk, hd)])
                        else:
                            gr = goff + hd * 64
                            P.dma("sync", gsb[u][:, :n], PJ[gr:gr + 64, t0:t0 + n], writes=[("gsb", u)])
                            P.act(osq[u][:, :n], ob[hd][:, :n], AF.Square, [("obk", hd)], [("osq", u)])
                            P.mm(PN[0:64, :n], self.onesb[0:64, 0:64], osq[u][:, :n], True, True, [("osq", u), "onesb"], ["PN"])
                            P.act(rs_[u][:, :n], PN[0:64, :n], AF.Sqrt, ["PN"], [("rs_", u)], bias=self.epsc[0:64, 0:1], scale=1.0 / 64)
                            P.recip(rs_[u][:, :n], rs_[u][:, :n], [("rs_", u)], [("rs_", u)])
                            P.tt(rs_[u][:, :n], rs_[u][:, :n], ob[hd][:, :n], ALU.mult, [("rs_", u), ("obk", hd)], [("rs_", u)])
                            P.tt(yo[u][:, :n], rs_[u][:, :n], gsb[u][:, :n], ALU.mult, [("rs_", u), ("gsb", u)], [("yo", u)])
                            yr = yrow0 + hd * 64
                            P.dma("sync", self.YB[yr:yr + 64, t0:t0 + n], yo[u][:, :n], reads=[("yo", u)], writes=[("YBg", blk, hd)])

    def phase_merge(self, l):
        P, nc = self.P, self.nc
        T, L = self.T, self.L
        XTv = self.XT.rearrange("(ft p) t -> p ft t", p=128)
        YBv = self.YB.rearrange("(ft p) t -> p ft t", p=128)
        hTv = self.hT_scr.rearrange("(ft p) t -> p ft t", p=128)
        self.h2T_scr = self.scr.get("h2T") or self.scratch("h2T", [D, T], BF16)
        self.GTT = self.scr.get("GTT") or self.scratch("GTT", [NE, T], F32)
        h2v = self.h2T_scr.rearrange("(ft p) t -> p ft t", p=128)
        w_in = self.din["w_in"][l].rearrange("(kt p) f -> p kt f", p=128)
        with ExitStack() as st:
            nb = self.alloc_norm_bufs(st)
            gz = P.sb(st, [128, 8, 3072], BF16, "gz")
            for j in range(3):
                P.dma("gpsimd", gz[:, :, j * 1024:(j + 1) * 1024], w_in[:, :, O_GZ + j * 1024:O_GZ + (j + 1) * 1024], writes=[("gz", j)])
            wbr = P.sb(st, [128, 8, 1024], BF16, "wbr")
            P.dma("gpsimd", wbr[:, 0:2, :], self.din["w_branch_s5"][l].rearrange("(kt p) f -> p kt f", p=128), writes=[("wbr", 0)])
            P.dma("gpsimd", wbr[:, 2:5, :], self.din["w_branch_hgrn"][l].rearrange("(kt p) f -> p kt f", p=128), writes=[("wbr", 1)])
            P.dma("gpsimd", wbr[:, 5:8, :], self.din["w_branch_ret"][l].rearrange("(kt p) f -> p kt f", p=128), writes=[("wbr", 2)])
            wout = P.sb(st, [128, 8, 1024], BF16, "wout")
            P.dma("gpsimd", wout[:], self.din["w_out"][l].rearrange("(kt p) f -> p kt f", p=128), writes=["wout"])
            wr = P.sb(st, [128, 8, 36], F32, "wr")
            P.dma("sync", wr[:, :, 0:4], self.din["moe_w_group"][l].rearrange("(kt p) e -> p kt e", p=128), writes=[("wr", 0)])
            P.dma("sync", wr[:, :, 4:36], self.din["moe_w_expert"][l].rearrange("(kt p) e -> p kt e", p=128), writes=[("wr", 1)])
            brow = P.sb(st, [128, 36], F32, "brow")
            P.dma("sync", brow[:, 0:4], self.din["moe_b_group"][l:l + 1, :].partition_broadcast(128), writes=[("brow", 0)])
            P.dma("sync", brow[:, 4:36], self.din["moe_b_expert"][l:l + 1, :].partition_broadcast(128), writes=[("brow", 1)])
            hT = P.sb(st, [128, 8, 512], BF16, "mhT")
            yb = P.sb(st, [128, 8, 512], BF16, "myb")
            xt = P.sb(st, [128, 8, 512], F32, "mxt")
            sig = P.sb(st, [128, 3, 512], F32, "msig")
            mt = P.sb(st, [128, 512], F32, "mmt")
            mt2 = P.sb(st, [128, 512], F32, "mmt2")
            mg = P.sb(st, [128, 8, 512], BF16, "mmg")
            h2f = P.sb(st, [128, 8, 512], F32, "h2f")
            h2b = P.sb(st, [128, 8, 512], BF16, "h2b")
            pgt = [P.ps(st, [128, 512], F32, "pgt") for _ in range(3)]
            pbt = [P.ps(st, [128, 512], F32, "pbt") for _ in range(2)]
            px = P.ps(st, [128, 512], F32, "px")
            prt = P.ps(st, [128, 512], F32, "prt")
            rt = {n: P.sb(st, [128, w], F32, "r_" + n) for n, w in
                  (("l36", 36), ("gmax", 1), ("eg", 4), ("gsum", 1), ("og", 4), ("pen", 4), ("lem", 32), ("m1", 1), ("oh1", 32),
                   ("lem2", 32), ("m2", 1), ("oh2", 32), ("r", 1), ("w1", 1), ("w2", 1), ("G", 64))}
            P.memset(rt["G"][:], 0.0, ["G"])
            gts = P.sb(st, [32, 128], F32, "gts")
            ident = self.cst[:, C_IDENT:C_IDENT + 128]
            ktr = ((0, 2), (2, 5), (5, 8))
            nbr = 0
            for bi, (t0, n) in enumerate(self.blocks):
                who = 1 if bi == 0 else 0
                P.dma("sync", hT[:, :, :n], hTv[:, :, t0:t0 + n], writes=["mhT"])
                P.dma("sync", yb[:, :, :n], YBv[:, :, t0:t0 + n], writes=["myb"])
                P.dma("sync", xt[:, :, :n], XTv[:, :, t0:t0 + n], writes=["mxt"])
                for ft in range(8):
                    fs = slice(ft * 128, (ft + 1) * 128)
                    for j in range(3):
                        for kt in range(8):
                            P.mm(pgt[j][:, :n], gz[:, kt, j * 1024 + ft * 128:j * 1024 + (ft + 1) * 128], hT[:, kt, :n], kt == 0, kt == 7,
                                 [("gz", j), "mhT"], [("pgt", j)])
                        P.act(sig[:, j, :n], pgt[j][:, :n], AF.Sigmoid, [("pgt", j)], [("msig", j)])
                    for j in range(3):
                        pb = nbr % 2
                        nbr += 1
                        k0, k1 = ktr[j]
                        for kt in range(k0, k1):
                            P.mm(pbt[pb][:, :n], wbr[:, kt, fs], yb[:, kt, :n], kt == k0, kt == k1 - 1, [("wbr", j), "myb"], [("pbt", pb)])
                        if j == 0:
                            P.tt(mt[:, :n], pbt[pb][:, :n], sig[:, j, :n], ALU.mult, [("pbt", pb), ("msig", j)], ["mmt"])
                        else:
                            P.tt(mt2[:, :n], pbt[pb][:, :n], sig[:, j, :n], ALU.mult, [("pbt", pb), ("msig", j)], ["mmt2"])
                            if j == 1:
                                P.tt(mt[:, :n], mt[:, :n], mt2[:, :n], ALU.add, ["mmt", "mmt2"], ["mmt"])
                            else:
                                P.tt(mg[:, ft, :n], mt[:, :n], mt2[:, :n], ALU.add, ["mmt", "mmt2"], [("mmg", ft)])
                mk = [("mmg", ft) for ft in range(8)]
                for ft in range(8):
                    for kt in range(8):
                        P.mm(px[:, :n], wout[:, kt, ft * 128:(ft + 1) * 128], mg[:, kt, :n], kt == 0, kt == 7, ["wout"] + mk, ["px"])
                    P.stt(xt[:, ft, :n], px[:, :n], self.mod[:, 2, ft, who:who + 1], xt[:, ft, :n], ALU.mult, ALU.add,
                          ["px", ("mod", 2, who), "mxt"], ["mxt"])
                P.dma("sync", XTv[:, :, t0:t0 + n], xt[:, :, :n], reads=["mxt"], writes=[("XTw", bi)])
                self.norm_block(nb, xt, "norm2_gs", 3, who, h2f, ["mxt"], "h2f", n)
                P.copy(h2b[:, :, :n], h2f[:, :, :n], ["h2f"], ["h2b"], eng="gpsimd")
                P.dma("sync", h2v[:, :, t0:t0 + n], h2b[:, :, :n], reads=["h2b"], writes=[("h2T", bi)])
                for sblk in range(n // 128):
                    ss = slice(sblk * 128, (sblk + 1) * 128)
                    for kt in range(8):
                        P.mm(prt[:, 0:36], h2f[:, kt, ss], wr[:, kt, :], kt == 0, kt == 7, ["h2f", ("wr", 0), ("wr", 1)], ["prt"])
                    r_ = rt
                    P.tt(r_["l36"][:], prt[:, 0:36], brow[:], ALU.add, ["prt", ("brow", 0), ("brow", 1)], ["l36"])
                    lg4 = r_["l36"][:, 0:4]
                    le = r_["l36"][:, 4:36]
                    P.op("vector", (lambda e, o=r_["gmax"][:], i=lg4: e.tensor_reduce(o, i, AX.X, ALU.max)), ["l36"], ["gmax"])
                    P.ts(r_["eg"][:], lg4, r_["gmax"][:, 0:1], None, ALU.subtract, None, ["l36", "gmax"], ["eg"])
                    P.act(r_["eg"][:], r_["eg"][:], AF.Exp, ["eg"], ["eg"])
                    P.op("vector", (lambda e, o=r_["gsum"][:], i=r_["eg"][:]: e.tensor_reduce(o, i, AX.X, ALU.add)), ["eg"], ["gsum"])
                    P.recip(r_["gsum"][:], r_["gsum"][:], ["gsum"], ["gsum"])
                    P.ts(r_["og"][:], lg4, r_["gmax"][:, 0:1], None, ALU.is_ge, None, ["l36", "gmax"], ["og"])
                    P.ts(r_["pen"][:], r_["og"][:], -1.0, 1.0e4, ALU.add, ALU.mult, ["og"], ["pen"])
                    P.tt(r_["lem"][:].rearrange("p (g j) -> p g j", g=4), le.rearrange("p (g j) -> p g j", g=4),
                         r_["pen"][:].unsqueeze(2).to_broadcast([128, 4, 8]), ALU.add, ["l36", "pen"], ["lem"])
                    P.op("vector", (lambda e, o=r_["m1"][:], i=r_["lem"][:]: e.tensor_reduce(o, i, AX.X, ALU.max)), ["lem"], ["m1"])
                    P.ts(r_["oh1"][:], r_["lem"][:], r_["m1"][:, 0:1], None, ALU.is_ge, None, ["lem", "m1"], ["oh1"])
                    P.stt(r_["lem2"][:], r_["oh1"][:], -1.0e4, r_["lem"][:], ALU.mult, ALU.add, ["oh1", "lem"], ["lem2"])
                    P.op("vector", (lambda e, o=r_["m2"][:], i=r_["lem2"][:]: e.tensor_reduce(o, i, AX.X, ALU.max)), ["lem2"], ["m2"])
                    P.ts(r_["oh2"][:], r_["lem2"][:], r_["m2"][:, 0:1], None, ALU.is_ge, None, ["lem2", "m2"], ["oh2"])
                    P.tt(r_["r"][:], r_["m2"][:], r_["m1"][:], ALU.subtract, ["m1", "m2"], ["r"])
                    P.act(r_["r"][:], r_["r"][:], AF.Exp, ["r"], ["r"])
                    P.ts(r_["w1"][:], r_["r"][:], 1.0, None, ALU.add, None, ["r"], ["w1"])
                    P.recip(r_["w1"][:], r_["w1"][:], ["w1"], ["w1"])
                    P.tt(r_["w1"][:], r_["w1"][:], r_["gsum"][:], ALU.mult, ["w1", "gsum"], ["w1"])
                    P.tt(r_["w2"][:], r_["w1"][:], r_["r"][:], ALU.mult, ["w1", "r"], ["w2"])
                    P.ts(r_["G"][:, 0:32], r_["oh1"][:], r_["w1"][:, 0:1], None, ALU.mult, None, ["oh1", "w1"], ["G"])
                    P.stt(r_["G"][:, 0:32], r_["oh2"][:], r_["w2"][:, 0:1], r_["G"][:, 0:32], ALU.mult, ALU.add, ["oh2", "w2", "G"], ["G"])
                    P.mm(prt[0:64, 128:256], r_["G"][:], ident, True, True, ["G", "cst"], ["prt"])
                    P.copy(gts[:], prt[0:32, 128:256], ["prt"], ["gts"], eng="scalar")
                    c0 = t0 + sblk * 128
                    P.dma("sync", self.GTT[:, c0:c0 + 128], gts[:], reads=["gts"], writes=[("GTT", c0)])

    def phase_moe(self, l):
        P, nc = self.P, self.nc
        T, L = self.T, self.L
        XTv = self.XT.rearrange("(ft p) t -> p ft t", p=128)
        h2v = self.h2T_scr.rearrange("(ft p) t -> p ft t", p=128)
        groups, cur, tot = [], [], 0
        for b in self.blocks:
            if tot + b[1] > 1536:
                groups.append(cur)
                cur, tot = [], 0
            cur.append(b)
            tot += b[1]
        groups.append(cur)
        with ExitStack() as st:
            h2 = P.sb(st, [128, 8, 1536], BF16, "eh2")
            acc = P.sb(st, [128, 8, 1536], F32, "eacc")
            grep = [P.sb(st, [128, 1536], F32, "egrep") for _ in range(2)]
            wg = [P.sb(st, [128, 8, 512], BF16, "ewg") for _ in range(2)]
            wu = [P.sb(st, [128, 8, 512], BF16, "ewu") for _ in range(2)]
            wd = [P.sb(st, [128, 4, 1024], BF16, "ewd") for _ in range(2)]
            sg = [P.sb(st, [128, 512], F32, "esg") for _ in range(2)]
            hu = [P.sb(st, [128, 512], F32, "ehu") for _ in range(2)]
            hid = [P.sb(st, [128, 4, 512], BF16, "ehid") for _ in range(2)]
            xt = P.sb(st, [128, 8, 512], F32, "ext")
            pg = [P.ps(st, [128, 512], F32, "epg") for _ in range(2)]
            pu = [P.ps(st, [128, 512], F32, "epu") for _ in range(2)]
            pd = [P.ps(st, [128, 2, 512], F32, "epd") for _ in range(2)]
            nj = 0
            nf = 0
            for grp in groups:
                g0 = grp[0][0]
                ng = sum(b[1] for b in grp)
                P.dma("sync", h2[:, :, :ng], h2v[:, :, g0:g0 + ng], writes=["eh2"])
                P.memset(acc[:], 0.0, ["eacc"])
                units = [(e, t0, n) for e in range(NE) for (t0, n) in grp]

                def emit_gu(ui):
                    nonlocal nj
                    e, t0, n = units[ui]
                    s_ = e % 2
                    hs_ = ui % 2
                    o = t0 - g0
                    if t0 == grp[0][0]:
                        P.dma("gpsimd", wg[s_][:], self.din["moe_w_gate"][l, e].rearrange("(kt p) f -> p kt f", p=128), writes=[("ewg", s_)])
                        P.dma("gpsimd", wu[s_][:], self.din["moe_w_up"][l, e].rearrange("(kt p) f -> p kt f", p=128), writes=[("ewu", s_)])
                        P.dma("gpsimd", wd[s_][:], self.din["moe_w_down"][l, e].rearrange("(jt p) f -> p jt f", p=128), writes=[("ewd", s_)])
                        P.dma("sync", grep[s_][:, :ng], self.GTT[e:e + 1, g0:g0 + ng].partition_broadcast(128), writes=[("egrep", s_)])
                    for jt in range(4):
                        u = nj % 2
                        nj += 1
                        for kt in range(8):
                            P.mm(pg[u][:, :n], wg[s_][:, kt, jt * 128:(jt + 1) * 128], h2[:, kt, o:o + n], kt == 0, kt == 7,
                                 [("ewg", s_), "eh2"], [("epg", u)])
                        for kt in range(8):
                            P.mm(pu[u][:, :n], wu[s_][:, kt, jt * 128:(jt + 1) * 128], h2[:, kt, o:o + n], kt == 0, kt == 7,
                                 [("ewu", s_), "eh2"], [("epu", u)])
                        P.act(sg[u][:, :n], pg[u][:, :n], AF.Silu, [("epg", u)], [("esg", u)])
                        P.tt(hu[u][:, :n], pu[u][:, :n], sg[u][:, :n], ALU.mult, [("epu", u), ("esg", u)], [("ehu", u)])
                        P.tt(hid[hs_][:, jt, :n], hu[u][:, :n], grep[s_][:, o:o + n], ALU.mult, [("ehu", u), ("egrep", s_)], [("ehid", hs_, jt)],
                             eng="gpsimd")

                def emit_d(ui):
                    nonlocal nf
                    e, t0, n = units[ui]
                    s_ = e % 2
                    hs_ = ui % 2
                    o = t0 - g0
                    hk = [("ehid", hs_, jt) for jt in range(4)]
                    for fp in range(4):
                        u = nf % 2
                        nf += 1
                        for fq in range(2):
                            ft = fp * 2 + fq
                            for jt in range(4):
                                P.mm(pd[u][:, fq, :n], wd[s_][:, jt, ft * 128:(ft + 1) * 128], hid[hs_][:, jt, :n], jt == 0, jt == 3,
                                     [("ewd", s_)] + hk, [("epd", u, fq)])
                        P.tt(acc[:, fp * 2:(fp + 1) * 2, o:o + n], acc[:, fp * 2:(fp + 1) * 2, o:o + n], pd[u][:, :, :n], ALU.add,
                             ["eacc", ("epd", u, 0), ("epd", u, 1)], ["eacc"])

                for ui in range(len(units)):
                    emit_gu(ui)
                    if ui > 0:
                        emit_d(ui - 1)
                emit_d(len(units) - 1)
                for (t0, n) in grp:
                    o = t0 - g0
                    who = 1 if t0 == 0 else 0
                    P.dma("sync", xt[:, :, :n], XTv[:, :, t0:t0 + n], writes=["ext"])
                    for ft in range(8):
                        P.stt(xt[:, ft, :n], acc[:, ft, o:o + n], self.mod[:, 5, ft, who:who + 1], xt[:, ft, :n], ALU.mult, ALU.add,
                              ["eacc", ("mod", 5, who), "ext"], ["ext"])
                    P.dma("sync", XTv[:, :, t0:t0 + n], xt[:, :, :n], reads=["ext"], writes=[("XTe", t0)])


_CACHE = {}


def kernel(**inputs):
    L = inputs["x"].shape[1]
    B = inputs["x"].shape[0]
    depth = inputs["w_in"].shape[0]
    shapes = {n: tuple(inputs[n].shape) for n in PARAM_NAMES}
    shapes["c"] = (D,)
    key = (L, depth)
    if key not in _CACHE:
        _CACHE[key] = Builder(L, depth, shapes).build()
    nc = _CACHE[key]
    cst, cmask, pos = host_consts(L)
    shared = {n: np.ascontiguousarray(np.asarray(inputs[n], dtype=np.float32)) for n in PARAM_NAMES if n != "c"}
    shared["cst"] = cst
    shared["cmask"] = cmask
    shared["pos"] = pos
    in_maps = []
    for b in range(B):
        m = dict(shared)
        m["x"] = np.ascontiguousarray(np.asarray(inputs["x"][b], dtype=np.float32))
        m["ctx"] = np.ascontiguousarray(np.asarray(inputs["ctx"][b], dtype=np.float32))
        m["c"] = np.ascontiguousarray(np.asarray(inputs["c"][b], dtype=np.float32))
        in_maps.append(m)
    res = run_bass_kernel_spmd(nc, in_maps, core_ids=list(range(B)))
    out = np.stack([np.asarray(r["out"], dtype=np.float32) for r in res.results], axis=0)
    return out
```
